# Optimizing a Trainium2 kernel written in Bass

```python
import math
import jax, jax.numpy as jnp
from jax import lax
import numpy as np

D_MODEL = 1024
BATCH = 8
SEQ = 4096
DEPTH = 2

N_MIXERS = 4
GROUP_WIDTH = D_MODEL // N_MIXERS
HEAD_DIM = 64
BLOCK = 128
N_META = 16
SWA_HEADS = GROUP_WIDTH // HEAD_DIM
SWA_KV_HEADS = SWA_HEADS // 2
SWA_GROUP = SWA_HEADS // SWA_KV_HEADS
SWA_WINDOW = 128
DIFF_HEADS = GROUP_WIDTH // HEAD_DIM
DIFF_V_DIM = HEAD_DIM
DIFF_QK_DIM = HEAD_DIM // 2
RWKV_HEADS = GROUP_WIDTH // HEAD_DIM
RWKV_HEAD_SIZE = HEAD_DIM
DECAY_LORA = 64
AAA_LORA = 64
GATE_LORA = 128
CONV_CH = GROUP_WIDTH
CONV_WIDTH = 31
CONV_NORM_GROUPS = 4
REL_BUCKETS = 32
REL_MAX_DIST = 128
N_EXPERTS = 16
N_EXPERT_GROUPS = 4
EXPERTS_PER_GROUP = N_EXPERTS // N_EXPERT_GROUPS
TOP_K = 2
EXPERT_FF = 512
ALPHA = (2 * DEPTH) ** 0.25
BETA = (8 * DEPTH) ** -0.25
SWA_IN = (SWA_HEADS + 2 * SWA_KV_HEADS) * HEAD_DIM
DIFF_IN = 2 * DIFF_HEADS * 2 * DIFF_QK_DIM + DIFF_HEADS * DIFF_V_DIM
RWKV_IN = 3 * GROUP_WIDTH + DECAY_LORA + AAA_LORA + GATE_LORA
CONV_IN = 2 * CONV_CH
IN_WIDTH = SWA_IN + DIFF_IN + RWKV_IN + CONV_IN
NEG = -1e30

kernel_name = "hybrid_swa_diff_rwkv7_conformer_moe"


def _split(u, widths):
    cuts = [int(c) for c in np.cumsum(widths)[:-1]]
    return jnp.split(u, cuts, axis=-1)


def _layernorm(x, g, b, eps=1e-5):
    xf = x.astype(jnp.float32)
    mu = jnp.mean(xf, -1, keepdims=True)
    var = jnp.mean(jnp.square(xf - mu), -1, keepdims=True)
    return ((xf - mu) * lax.rsqrt(var + eps) * g + b).astype(x.dtype)


def _groupnorm(x, n_groups, g, b, eps):
    shp = x.shape
    xf = x.astype(jnp.float32).reshape(shp[:-1] + (n_groups, shp[-1] // n_groups))
    mu = jnp.mean(xf, -1, keepdims=True)
    var = jnp.mean(jnp.square(xf - mu), -1, keepdims=True)
    y = ((xf - mu) * lax.rsqrt(var + eps)).reshape(shp)
    return (y * g + b).astype(x.dtype)


def _t5_bucket(dist):
    n = jnp.maximum(dist, 0)
    max_exact = REL_BUCKETS // 2
    log_ratio = jnp.log(jnp.maximum(n, 1).astype(jnp.float32) / max_exact) / math.log(REL_MAX_DIST / max_exact)
    large = jnp.minimum(max_exact + (log_ratio * (REL_BUCKETS - max_exact)).astype(jnp.int32), REL_BUCKETS - 1)
    return jnp.where(n < max_exact, n, large)


def _swa_attention(q, k, v, sinks, rel_bias_a):
    b, l = q.shape[:2]
    pad = BLOCK - N_META
    nb = (l + pad) // BLOCK

    def blocks(t):
        t = jnp.pad(t, ((0, 0), (pad, 0)) + ((0, 0),) * (t.ndim - 2))
        return t.reshape((b, nb, BLOCK) + t.shape[2:])

    qb = blocks(q).reshape(b, nb, BLOCK, SWA_KV_HEADS, SWA_GROUP, HEAD_DIM)

    def keys_of(t):
        tb = blocks(t)
        prev = jnp.pad(tb, ((0, 0), (1, 0), (0, 0), (0, 0), (0, 0)))[:, :-1]
        meta = jnp.broadcast_to(t[:, None, :N_META], (b, nb, N_META) + t.shape[2:])
        return jnp.concatenate([meta, prev, tb], axis=2)

    kw, vw = keys_of(k), keys_of(v)
    n_keys = N_META + 2 * BLOCK
    qpos = jnp.arange(nb * BLOCK).reshape(nb, BLOCK) - pad
    kpos = jnp.concatenate([jnp.broadcast_to(jnp.arange(N_META), (nb, N_META)), qpos - BLOCK, qpos], axis=1)
    dq = qpos[:, :, None] - kpos[:, None, :]
    is_meta_slot = (jnp.arange(n_keys) < N_META)[None, None, :]
    band_ok = (kpos[:, None, :] >= N_META) & (dq < SWA_WINDOW)
    mask = (dq >= 0) & (is_meta_slot | band_ok)
    bias = jnp.moveaxis(rel_bias_a[_t5_bucket(dq)], -1, 1)
    bias = bias.reshape(nb, SWA_KV_HEADS, SWA_GROUP, BLOCK, n_keys).astype(jnp.float32)
    s = jnp.einsum('bnqhgd,bnkhd->bnhgqk', qb, kw).astype(jnp.float32) * (HEAD_DIM ** -0.5) + bias[None]
    s = jnp.where(mask[None, :, None, None], s, NEG)
    sink = jnp.broadcast_to(sinks.astype(jnp.float32).reshape(1, 1, SWA_KV_HEADS, SWA_GROUP, 1, 1), s.shape[:-1] + (1,))
    p = jax.nn.softmax(jnp.concatenate([s, sink], axis=-1), axis=-1)[..., :-1]
    o = jnp.einsum('bnhgqk,bnkhd->bnqhgd', p.astype(v.dtype), vw)
    return o.reshape(b, nb * BLOCK, SWA_HEADS * HEAD_DIM)[:, pad:]


def _diff_attention(q, k, v, lam, lam_init, subln_g, rel_bias_b):
    b, l = q.shape[:2]
    pad = BLOCK - N_META
    lp = l + pad
    nb = lp // BLOCK
    padt = lambda t: jnp.pad(t, ((0, 0), (pad, 0)) + ((0, 0),) * (t.ndim - 2))
    qp, kp, vp = padt(q), padt(k), padt(v)
    qblocks = jnp.moveaxis(qp.reshape((b, nb, BLOCK) + q.shape[2:]), 1, 0)
    pos = jnp.arange(lp) - pad
    qpos_blocks = pos.reshape(nb, BLOCK)
    scale = DIFF_QK_DIM ** -0.5

    def one_block(args):
        qblk, qpos = args
        s = jnp.einsum('bqhcd,bkhcd->bhcqk', qblk, kp).astype(jnp.float32) * scale
        dq = qpos[:, None] - pos[None, :]
        bias = jnp.moveaxis(rel_bias_b[_t5_bucket(dq)], -1, 0).astype(jnp.float32)
        s = s + bias[None, :, None]
        mask = (dq >= 0) & (pos >= 0)[None, :]
        p = jax.nn.softmax(jnp.where(mask, s, NEG), axis=-1)
        attn = p[:, :, 0] - lam * p[:, :, 1]
        return jnp.einsum('bhqk,bkhd->bqhd', attn.astype(v.dtype), vp)

    o = lax.map(one_block, (qblocks, qpos_blocks))
    o = jnp.moveaxis(o, 0, 1).reshape(b, lp, DIFF_HEADS, DIFF_V_DIM)[:, pad:]
    of = o.astype(jnp.float32)
    of = of * lax.rsqrt(jnp.mean(jnp.square(of), -1, keepdims=True) + 1e-5) * subln_g * (1.0 - lam_init)
    return of.reshape(b, l, DIFF_HEADS * DIFF_V_DIM).astype(v.dtype)


def _rwkv7_mix(uc, mu, w0, w2, a0, a2, g2, k_k, k_a, r_k, lnx_g, lnx_b):
    b, l, _ = uc.shape
    f32 = jnp.float32
    prev = jnp.pad(uc, ((0, 0), (1, 0), (0, 0)))[:, :-1]
    uc = uc + mu * (prev - uc)
    r, k, v, xw, xa, xg = _split(uc, (GROUP_WIDTH, GROUP_WIDTH, GROUP_WIDTH, DECAY_LORA, AAA_LORA, GATE_LORA))
    w = jnp.exp(-math.exp(-0.5) * jax.nn.sigmoid((w0 + jnp.tanh(xw) @ w2).astype(f32)))
    a = jax.nn.sigmoid((a0 + xa @ a2).astype(f32))
    g = jax.nn.sigmoid(xg) @ g2
    k = k.astype(f32)
    heads = lambda t: t.reshape(b, l, RWKV_HEADS, RWKV_HEAD_SIZE)
    kk = heads(k * k_k)
    kk = kk / jnp.maximum(jnp.sqrt(jnp.sum(jnp.square(kk), -1, keepdims=True)), 1e-12)
    k = k * (1.0 + (a - 1.0) * k_a)
    r_h, w_h, k_h, v_h, a_h = heads(r.astype(f32)), heads(w), heads(k), heads(v.astype(f32)), heads(a)

    def step(S, inp):
        r_t, w_t, k_t, v_t, kk_t, a_t = inp
        sa = jnp.einsum('bhij,bhj->bhi', S, -kk_t)
        S = S * w_t[:, :, None, :] + sa[..., None] * (kk_t * a_t)[:, :, None, :] + v_t[..., None] * k_t[:, :, None, :]
        return S, jnp.einsum('bhij,bhj->bhi', S, r_t)

    S0 = jnp.zeros((b, RWKV_HEADS, RWKV_HEAD_SIZE, RWKV_HEAD_SIZE), f32)
    xs = tuple(jnp.moveaxis(t, 1, 0) for t in (r_h, w_h, k_h, v_h, kk, a_h))
    _, ys = lax.scan(step, S0, xs)
    y = jnp.moveaxis(ys, 0, 1).reshape(b, l, GROUP_WIDTH)
    y = _groupnorm(y, RWKV_HEADS, lnx_g, lnx_b, 64e-5)
    bonus = (jnp.sum(r_h * k_h * r_k, -1, keepdims=True) * v_h).reshape(b, l, GROUP_WIDTH)
    return ((y + bonus) * g).astype(uc.dtype)


def _conv_module(ud, conv_w, conv_b, gn_g, gn_b):
    a, gate = jnp.split(ud, 2, axis=-1)
    h = a * jax.nn.sigmoid(gate)
    h = lax.conv_general_dilated(h, conv_w[:, None, :], window_strides=(1,),
                                 padding=[(CONV_WIDTH - 1, 0)],
                                 dimension_numbers=('NWC', 'WIO', 'NWC'),
                                 feature_group_count=CONV_CH) + conv_b
    h = _groupnorm(h, CONV_NORM_GROUPS, gn_g, gn_b, 1e-5)
    return jax.nn.silu(h)


def _moe(h, router_w, router_b, w1, w3, w2):
    b, l, d = h.shape
    n_tok = b * l
    t = h.reshape(n_tok, d)
    scores = jax.nn.sigmoid((t @ router_w).astype(jnp.float32))
    biased = scores + router_b.astype(jnp.float32)
    grp_top = lax.top_k(biased.reshape(n_tok, N_EXPERT_GROUPS, EXPERTS_PER_GROUP), TOP_K)[0]
    best_group = jnp.argmax(jnp.sum(grp_top, -1), axis=-1)
    in_group = jnp.repeat(jnp.arange(N_EXPERT_GROUPS)[None, :] == best_group[:, None], EXPERTS_PER_GROUP, axis=1)
    _, top_idx = lax.top_k(jnp.where(in_group, biased, NEG), TOP_K)
    top_s = jnp.take_along_axis(scores, top_idx, axis=-1)
    gates = top_s / jnp.sum(top_s, -1, keepdims=True)
    combine = jnp.sum(jax.nn.one_hot(top_idx, N_EXPERTS, dtype=jnp.float32) * gates[..., None], axis=1)
    out = jnp.zeros((n_tok, d), jnp.float32)
    for e in range(N_EXPERTS):
        act = jax.nn.silu(t @ w1[e]) * (t @ w3[e])
        out = out + combine[:, e:e + 1] * (act @ w2[e])
    return out.astype(h.dtype).reshape(b, l, d)


def setup_inputs(seed: int = 0) -> dict:
    key = jax.random.key(seed)
    ks = jax.random.split(key, 40)
    n = lambda i, shp, s=1.0: jax.random.normal(ks[i], shp, jnp.float32) * s
    L = DEPTH
    gw = GROUP_WIDTH
    return {
        "x": n(0, (BATCH, SEQ, D_MODEL)),
        "meta": n(1, (N_META, D_MODEL)),
        "ln0_g": 1.0 + n(2, (D_MODEL,), 0.02),
        "ln0_b": n(3, (D_MODEL,), 0.02),
        "w_in": n(4, (L, D_MODEL, IN_WIDTH), D_MODEL ** -0.5),
        "swa_sinks": n(5, (L, SWA_HEADS), 0.5),
        "rel_bias": n(6, (REL_BUCKETS, SWA_HEADS + DIFF_HEADS), 0.5),
        "diff_lq1": n(7, (L, DIFF_QK_DIM), 0.1),
        "diff_lk1": n(8, (L, DIFF_QK_DIM), 0.1),
        "diff_lq2": n(9, (L, DIFF_QK_DIM), 0.1),
        "diff_lk2": n(10, (L, DIFF_QK_DIM), 0.1),
        "diff_subln_g": 1.0 + n(11, (L, DIFF_V_DIM), 0.02),
        "rwkv_mu": jax.random.uniform(ks[12], (L, RWKV_IN), jnp.float32),
        "rwkv_w0": n(13, (L, gw)),
        "rwkv_w2": n(14, (L, DECAY_LORA, gw), DECAY_LORA ** -0.5),
        "rwkv_a0": n(15, (L, gw), 0.5),
        "rwkv_a2": n(16, (L, AAA_LORA, gw), AAA_LORA ** -0.5),
        "rwkv_g2": n(17, (L, GATE_LORA, gw), GATE_LORA ** -0.5),
        "rwkv_kk": 0.85 + n(18, (L, gw), 0.05),
        "rwkv_ka": 1.0 + n(19, (L, gw), 0.05),
        "rwkv_rk": n(20, (L, RWKV_HEADS, RWKV_HEAD_SIZE), 0.1),
        "rwkv_lnx_g": 1.0 + n(21, (L, gw), 0.02),
        "rwkv_lnx_b": n(22, (L, gw), 0.02),
        "conv_w": n(23, (L, CONV_WIDTH, CONV_CH), CONV_WIDTH ** -0.5),
        "conv_b": n(24, (L, CONV_CH), 0.02),
        "conv_gn_g": 1.0 + n(25, (L, CONV_CH), 0.02),
        "conv_gn_b": n(26, (L, CONV_CH), 0.02),
        "w_out": n(27, (L, D_MODEL, D_MODEL), BETA * D_MODEL ** -0.5),
        "ln1_g": 1.0 + n(28, (L, D_MODEL), 0.02),
        "ln1_b": n(29, (L, D_MODEL), 0.02),
        "router_w": n(30, (D_MODEL, N_EXPERTS), D_MODEL ** -0.5),
        "router_b": n(31, (N_EXPERTS,), 0.01),
        "exp_w1": n(32, (L, N_EXPERTS, D_MODEL, EXPERT_FF), D_MODEL ** -0.5),
        "exp_w3": n(33, (L, N_EXPERTS, D_MODEL, EXPERT_FF), D_MODEL ** -0.5),
        "exp_w2": n(34, (L, N_EXPERTS, EXPERT_FF, D_MODEL), BETA * EXPERT_FF ** -0.5),
        "ln2_g": 1.0 + n(35, (L, D_MODEL), 0.02),
        "ln2_b": n(36, (L, D_MODEL), 0.02),
    }


def reference(x, meta, ln0_g, ln0_b, w_in, swa_sinks, rel_bias, diff_lq1, diff_lk1, diff_lq2, diff_lk2,
              diff_subln_g, rwkv_mu, rwkv_w0, rwkv_w2, rwkv_a0, rwkv_a2, rwkv_g2, rwkv_kk, rwkv_ka, rwkv_rk,
              rwkv_lnx_g, rwkv_lnx_b, conv_w, conv_b, conv_gn_g, conv_gn_b, w_out, ln1_g, ln1_b,
              router_w, router_b, exp_w1, exp_w3, exp_w2, ln2_g, ln2_b):
    b = x.shape[0]
    f32 = jnp.float32
    h = jnp.concatenate([jnp.broadcast_to(meta[None].astype(x.dtype), (b, N_META, D_MODEL)), x], axis=1)
    h = _layernorm(h, ln0_g, ln0_b)
    lt = h.shape[1]
    rel_a = rel_bias[:, :SWA_HEADS]
    rel_b = rel_bias[:, SWA_HEADS:]
    for l in range(DEPTH):
        u = h @ w_in[l]
        ua, ub, uc, ud = _split(u, (SWA_IN, DIFF_IN, RWKV_IN, CONV_IN))
        qa, ka, va = _split(ua, (SWA_HEADS * HEAD_DIM, SWA_KV_HEADS * HEAD_DIM, SWA_KV_HEADS * HEAD_DIM))
        ya = _swa_attention(qa.reshape(b, lt, SWA_HEADS, HEAD_DIM),
                            ka.reshape(b, lt, SWA_KV_HEADS, HEAD_DIM),
                            va.reshape(b, lt, SWA_KV_HEADS, HEAD_DIM), swa_sinks[l], rel_a)
        qd, kd, vd = _split(ub, (DIFF_HEADS * 2 * DIFF_QK_DIM, DIFF_HEADS * 2 * DIFF_QK_DIM, DIFF_HEADS * DIFF_V_DIM))
        lam_init = 0.8 - 0.6 * math.exp(-0.3 * l)
        lam = (jnp.exp(jnp.sum(diff_lq1[l].astype(f32) * diff_lk1[l].astype(f32)))
               - jnp.exp(jnp.sum(diff_lq2[l].astype(f32) * diff_lk2[l].astype(f32))) + lam_init)
        yb = _diff_attention(qd.reshape(b, lt, DIFF_HEADS, 2, DIFF_QK_DIM),
                             kd.reshape(b, lt, DIFF_HEADS, 2, DIFF_QK_DIM),
                             vd.reshape(b, lt, DIFF_HEADS, DIFF_V_DIM), lam, lam_init, diff_subln_g[l], rel_b)
        yc = _rwkv7_mix(uc, rwkv_mu[l], rwkv_w0[l], rwkv_w2[l], rwkv_a0[l], rwkv_a2[l], rwkv_g2[l],
                        rwkv_kk[l], rwkv_ka[l], rwkv_rk[l], rwkv_lnx_g[l], rwkv_lnx_b[l])
        yd = _conv_module(ud, conv_w[l], conv_b[l], conv_gn_g[l], conv_gn_b[l])
        mix = jnp.concatenate([ya, yb.astype(ya.dtype), yc.astype(ya.dtype), yd.astype(ya.dtype)], axis=-1) @ w_out[l]
        h = _layernorm(ALPHA * h + mix, ln1_g[l], ln1_b[l])
        h = _layernorm(ALPHA * h + _moe(h, router_w, router_b, exp_w1[l], exp_w3[l], exp_w2[l]), ln2_g[l], ln2_b[l])
    return h[:, N_META:]
```

```python
import numpy as np
import concourse.bass as bass
import concourse.mybir as mybir

F32 = mybir.dt.float32
BF16 = mybir.dt.bfloat16
AF = mybir.ActivationFunctionType
ALU = mybir.AluOpType
AX = mybir.AxisListType

ENGS = ("pe", "act", "dve", "pool", "sp")
DQS = ("sp", "act", "pool")


class Sched:
    def __init__(self, nc, stack, ndma=6):
        self.nc = nc
        self.stack = stack
        self.ndma = ndma
        self.gen = 0
        self.dgen = 0
        self.objs = {}
        self.cnt = {e: 0 for e in ENGS}
        self.dcnt = {q: [0] * ndma for q in DQS}
        self.dnext = {q: 0 for q in DQS}
        self.streams = {e: [] for e in ENGS}
        self.waited = {e: {} for e in ENGS}
        self.lastw = {}
        self.readers = {}
        self.nops = 0
        self._new_csems()
        self._new_dsems()

    def _new_csems(self):
        self.gen += 1
        for e in ENGS:
            self.objs[("c", e, self.gen)] = self.stack.enter_context(self.nc.semaphore(f"c_{e}_{self.gen}"))
            self.cnt[e] = 0

    def _new_dsems(self):
        self.dgen += 1
        for q in DQS:
            for i in range(self.ndma):
                self.objs[("d", q, i, self.dgen)] = self.stack.enter_context(self.nc.semaphore(f"d_{q}{i}_{self.dgen}"))
            self.dcnt[q] = [0] * self.ndma

    def ckey(self, e):
        return ("c", e, self.gen)

    def dkey(self, q, i):
        return ("d", q, i, self.dgen)

    def _semobj(self, key):
        return self.objs[key]

    def _wait(self, eng, tok):
        key, val, src = tok
        if self.waited[eng].get(key, 0) >= val:
            return
        self.waited[eng][key] = val
        s = self._semobj(key)
        self.streams[eng].append(lambda E, s=s, val=val: E.wait_ge(s, val))

    def op(self, eng, fn, kw=None, r=(), w=(), dma=False):
        if isinstance(fn, str):
            name = fn
            kw = dict(kw)
            fn = lambda E, name=name, kw=kw: getattr(E, name)(**kw)
        deps = []
        for res in r:
            t = self.lastw.get(res)
            if t is not None:
                deps.append((t, "raw"))
        for res in w:
            t = self.lastw.get(res)
            if t is not None:
                deps.append((t, "waw"))
            for t in self.readers.get(res, ()):
                deps.append((t, "war"))
        for t, kind in deps:
            src = t[2]
            if src == eng and t[0][0] == "c":
                if eng == "pe":
                    continue
                if kind == "war":
                    continue
            self._wait(eng, t)
        if dma:
            q = eng
            i = self.dnext[q] % self.ndma
            self.dnext[q] += 1
            if self.dcnt[q][i] > 0:
                self._wait(eng, (self.dkey(q, i), self.dcnt[q][i], None))
            self.dcnt[q][i] += 16
            tok = (self.dkey(q, i), self.dcnt[q][i], None)
            s = self.objs[self.dkey(q, i)]
            self.streams[eng].append(lambda E, fn=fn, s=s: fn(E).then_inc(s, 16))
        else:
            self.cnt[eng] += 1
            tok = (self.ckey(eng), self.cnt[eng], eng)
            s = self.objs[self.ckey(eng)]
            self.streams[eng].append(lambda E, fn=fn, s=s: fn(E).then_inc(s, 1))
        for res in w:
            self.lastw[res] = tok
            self.readers[res] = []
        for res in r:
            self.readers.setdefault(res, []).append(tok)
        self.nops += 1
        return tok

    def barrier(self, rotate_dma=False):
        for e in ENGS:
            for s in ENGS:
                if s != e and self.cnt[s] > 0:
                    self._wait(e, (self.ckey(s), self.cnt[s], s))
            for q in DQS:
                for i in range(self.ndma):
                    if self.dcnt[q][i] > 0:
                        self._wait(e, (self.dkey(q, i), self.dcnt[q][i], None))
        self.lastw = {}
        self.readers = {}
        for e in ENGS:
            self.streams[e].append(None)
        if max(self.cnt.values()) > 4000:
            self._new_csems()
        if rotate_dma:
            self._new_dsems()

    def final_wait(self, eng="sp"):
        for q in DQS:
            for i in range(self.ndma):
                if self.dcnt[q][i] > 0:
                    self._wait(eng, (self.dkey(q, i), self.dcnt[q][i], None))
        for s in ENGS:
            if s != eng and self.cnt[s] > 0:
                self._wait(eng, (self.ckey(s), self.cnt[s], s))

    def emit(self):
        nc = self.nc
        segs = {e: [[]] for e in ENGS}
        for e in ENGS:
            for f in self.streams[e]:
                if f is None:
                    segs[e].append([])
                else:
                    segs[e][-1].append(f)
        nseg = max(len(v) for v in segs.values())
        for i in range(nseg):
            cur = {e: (segs[e][i] if i < len(segs[e]) else []) for e in ENGS}
            if not any(cur.values()):
                continue
            with nc.Block() as block:
                @block.tensor
                def _(E, fs=cur["pe"]):
                    for f in fs:
                        f(E)

                @block.scalar
                def _(E, fs=cur["act"]):
                    for f in fs:
                        f(E)

                @block.vector
                def _(E, fs=cur["dve"]):
                    for f in fs:
                        f(E)

                @block.gpsimd
                def _(E, fs=cur["pool"]):
                    for f in fs:
                        f(E)

                @block.sync
                def _(E, fs=cur["sp"]):
                    for f in fs:
                        f(E)


class SbufAlloc:
    def __init__(self, nc, base=16640, limit=224 * 1024 - 256):
        self.nc = nc
        self.off = base
        self.limit = limit
        self.n = 0
        self.marks = []

    def mark(self):
        self.marks.append(self.off)

    def release(self):
        self.off = self.marks.pop()

    def t(self, shape, dtype, name=None):
        esz = 4 if dtype == F32 else 2
        if dtype in (mybir.dt.int32, mybir.dt.uint32):
            esz = 4
        nbytes = int(np.prod(shape[1:])) * esz
        nbytes = (nbytes + 63) // 64 * 64
        assert self.off + nbytes <= self.limit, f"SBUF overflow {self.off}+{nbytes} > {self.limit} ({name})"
        self.n += 1
        h = self.nc.alloc_sbuf_tensor_at(f"{name or 't'}_{self.n}", list(shape), dtype, offset=self.off)
        self.off += nbytes
        return h


import math
import numpy as np
import ml_dtypes
from contextlib import ExitStack
import concourse.bass as bass
import concourse.mybir as mybir
from concourse.bass_utils import run_bass_kernel_spmd

NPOS = 4112
NT = 33
DEPTH = 2
ALPHA = (2 * DEPTH) ** 0.25
NEG = -1e30


def tile_rows(t):
    return (0, 16) if t == 0 else (16 + 128 * (t - 1), 128)


GROUPS = [[0]] + [[4 * g + 1 + i for i in range(4)] for g in range(8)]


def t5_bucket(dist):
    n = np.maximum(dist, 0)
    lr = np.log(np.maximum(n, 1).astype(np.float32) / np.float32(16)) / np.float32(math.log(128 / 16))
    large = np.minimum(16 + (lr * np.float32(16)).astype(np.int32), 31)
    return np.where(n < 16, n, large)


class K:
    def __init__(self, ext_in=(), ext_out=(), layers=(0, 1)):
        self.nc = bass.Bass("TRN2", target_bir_lowering=False)
        self.st = ExitStack()
        self.S = Sched(self.nc, self.st)
        self.A = SbufAlloc(self.nc)
        self.ext_in = set(ext_in)
        self.ext_out = set(ext_out)
        self.dr = {}
        nc = self.nc
        self.psum = [self.st.enter_context(nc.psum_tensor(f"ps{i}", [128, 512], F32)) for i in range(8)]

    def D(self, name, shape, dtype, kind=None):
        if kind is None:
            kind = "ExternalInput" if name in self.ext_in else ("ExternalOutput" if name in self.ext_out else "Internal")
        self.dr[name] = self.nc.dram_tensor(name, list(shape), dtype, kind=kind).ap()
        return self.dr[name]

    def I(self, name, shape, dtype=F32):
        return self.D(name, shape, dtype, kind="ExternalInput")

    def ps(self, i):
        return self.psum[i]

    def op(self, eng, name, r=(), w=(), **kw):
        return self.S.op(eng, name, kw, r=r, w=w)

    def dma(self, q, out, in_, r=(), w=()):
        return self.S.op(q, "dma_start", dict(out=out, in_=in_), r=r, w=w, dma=True)

    def mm(self, out, lhsT, rhs, start, stop, r=(), w=(), skip=False):
        kw = dict(out=out, lhsT=lhsT, rhs=rhs, start=start, stop=stop)
        if skip:
            kw["skip_group_check"] = True
        return self.S.op("pe", "matmul", kw, r=r, w=w)

    def tr(self, out, in_, identity, r=(), w=()):
        return self.S.op("pe", "transpose", dict(out=out, in_=in_, identity=identity), r=r, w=w)

    def ps2(self, i, dtype=F32):
        raise NotImplementedError


class Vw:
    def __init__(self, ap, res):
        self.ap = ap
        self.res = res

    def __getitem__(self, idx):
        return Vw(self.ap[idx], self.res)

    def re(self, pat, **kw):
        return Vw(self.ap.rearrange(pat, **kw), self.res)

    def bc(self, shape):
        return Vw(self.ap.to_broadcast(list(shape)), self.res)

    def un(self, ax):
        return Vw(self.ap.unsqueeze(ax), self.res)


class TB:
    def __init__(self, h, res):
        self.h = h
        self.res = res if isinstance(res, list) else [res]

    def __getitem__(self, idx):
        return Vw(self.h[idx], self.res)


def _do(self, eng, name, out, extra_r=(), extra_w=(), **kw):
    r = list(extra_r)
    w = list(extra_w)
    args = {}
    for kk_, v in kw.items():
        if isinstance(v, Vw):
            r += v.res
            args[kk_] = v.ap
        else:
            args[kk_] = v
    okey = "ap" if name == "memset" else "out"
    if isinstance(out, Vw):
        w += out.res
        args[okey] = out.ap
    else:
        args[okey] = out
    if name == "dma_start":
        return self.S.op(eng, name, args, r=r, w=w, dma=True)
    return self.S.op(eng, name, args, r=r, w=w)


K.do = _do


def _mmv(self, out, lhsT, rhs, start, stop, skip=False):
    kw = dict(out=out.ap, lhsT=lhsT.ap, rhs=rhs.ap, start=start, stop=stop)
    if skip:
        kw["skip_group_check"] = True
    return self.S.op("pe", "matmul", kw, r=lhsT.res + rhs.res, w=out.res)


def _trv(self, out, in_, ident):
    return self.S.op("pe", "transpose", dict(out=out.ap, in_=in_.ap, identity=ident.ap), r=in_.res + ident.res, w=out.res)


K.mmv = _mmv
K.trv = _trv


def declare_io(k, final_out=True):
    L = DEPTH
    k.I("x", [4096, 1024]); k.I("meta", [16, 1024]); k.I("ln0_g", [1, 1024]); k.I("ln0_b", [1, 1024])
    k.I("w_in", [L, 1024, 2816]); k.I("swa_sinks", [L, 4])
    k.I("diff_l", [L, 4, 32]); k.I("diff_subln_g", [L, 64])
    k.I("rwkv_mu", [L, 1024]); k.I("rwkv_w0", [L, 256]); k.I("rwkv_w2", [L, 64, 256]); k.I("rwkv_a0", [L, 256])
    k.I("rwkv_a2", [L, 64, 256]); k.I("rwkv_g2", [L, 128, 256]); k.I("rwkv_kk", [L, 256]); k.I("rwkv_ka", [L, 256])
    k.I("rwkv_rk", [L, 256]); k.I("rwkv_lnx_g", [L, 256]); k.I("rwkv_lnx_b", [L, 256])
    k.I("conv_wT", [L, 256, 31]); k.I("conv_b", [L, 256, 1]); k.I("conv_gn_g", [L, 256, 1]); k.I("conv_gn_b", [L, 256, 1])
    k.I("w_out", [L, 1024, 1024]); k.I("ln1_g", [L, 1024]); k.I("ln1_b", [L, 1024])
    k.I("router_w", [1024, 16]); k.I("router_b", [1, 16])
    k.I("exp_w1", [L, 16, 1024, 512]); k.I("exp_w3", [L, 16, 1024, 512]); k.I("exp_w2", [L, 16, 512, 1024])
    k.I("ln2_g", [L, 1024]); k.I("ln2_b", [L, 1024])
    k.I("ident_bf", [128, 128], BF16); k.I("ident_f", [128, 128], F32)
    k.I("ba_meta", [3, 4, 16, 128], BF16); k.I("ba_pc", [4, 128, 256], BF16)
    k.I("bb_pc", [4, 128, 256], F32); k.I("b31", [1, 8], F32)
    k.I("gmat", [128, 128], F32)
    k.I("tri", [3, 64, 64], F32)
    k.I("ctri", [128, 128], F32); k.I("cblk", [128, 128], F32); k.I("cones", [128, 2, 64], F32); k.I("identrep", [64, 256], F32)
    k.I("rmask", [64, 5, 64], F32)
    k.D("H", [NPOS, 1024], F32); k.D("HM", [NPOS, 1024], F32)
    k.D("QKA", [384, NPOS], BF16); k.D("QKB", [512, NPOS], BF16); k.D("CV", [512, NPOS], F32)
    k.D("VA", [NPOS, 128], BF16); k.D("VB", [NPOS, 256], BF16); k.D("UC", [NPOS + 1, 1024], F32)
    k.D("MIX", [NPOS, 768], BF16); k.D("MIXD", [256, NPOS], BF16)
    k.D("HT", [1024, NPOS], BF16)
    k.D("RWT", [NPOS, 7, 256], BF16); k.D("BG", [NPOS, 2, 256], F32); k.D("DW", [65, 64, 256], BF16)
    if final_out:
        k.D("out", [4096, 1024], F32, kind="ExternalOutput")


def host_consts(inp):
    c = {}
    c["ident_bf"] = np.eye(128, dtype=ml_dtypes.bfloat16)
    c["ident_f"] = np.eye(128, dtype=np.float32)
    rel = np.asarray(inp["rel_bias"], np.float32)
    rel_a, rel_b = rel[:, :4], rel[:, 4:]
    ki = np.arange(128)[:, None]
    qi = np.arange(128)[None, :]
    bam = np.full((3, 4, 16, 128), NEG, np.float32)
    m = np.arange(16)[:, None]
    dq = np.arange(128)[None, :] - m
    vis = (dq >= 0) & (np.arange(128)[None, :] < 16)
    g = rel_a[t5_bucket(dq)]
    bam[0] = np.where(vis[None], np.moveaxis(g, -1, 0), NEG)
    dq = (16 + np.arange(128))[None, :] - m
    bam[1] = np.moveaxis(rel_a[t5_bucket(dq)], -1, 0)
    bam[2] = np.broadcast_to(rel_a[31][:, None, None], (4, 16, 128))
    c["ba_meta"] = bam.astype(ml_dtypes.bfloat16)
    dq_prev = qi - ki + 128
    dq_cur = qi - ki
    bp = np.where((ki > qi)[None], np.moveaxis(rel_a[t5_bucket(dq_prev)], -1, 0), NEG)
    bc = np.where((ki <= qi)[None], np.moveaxis(rel_a[t5_bucket(dq_cur)], -1, 0), NEG)
    c["ba_pc"] = np.concatenate([bp, bc], axis=2).astype(ml_dtypes.bfloat16)
    bcd = np.where((ki <= qi)[None], np.moveaxis(rel_b[t5_bucket(dq_cur)], -1, 0), NEG)
    bpd = np.moveaxis(rel_b[t5_bucket(dq_prev)], -1, 0)
    c["bb_pc"] = np.ascontiguousarray(np.concatenate([bcd, bpd], axis=2).astype(np.float32))
    c["b31"] = np.ascontiguousarray(rel[31][None, :])
    gm = np.zeros((128, 128), np.float32)
    gm[:64, :64] = 1.0 / 64
    gm[64:, 64:] = 1.0 / 64
    c["gmat"] = gm
    s = np.arange(64)[:, None]
    t = np.arange(64)[None, :]
    c["tri"] = np.stack([(s <= t), (s < t), (s >= t)]).astype(np.float32)
    s2 = np.arange(128)[:, None]; t2 = np.arange(128)[None, :]
    same = (s2 // 64) == (t2 // 64)
    c["ctri"] = (same & (s2 <= t2)).astype(np.float32)
    c["cblk"] = same.astype(np.float32)
    c["cones"] = np.ascontiguousarray(np.stack([(np.arange(128) // 64 == cc)[:, None] * np.ones((1, 64)) for cc in range(2)], axis=1).astype(np.float32))
    c["identrep"] = np.tile(np.eye(64, dtype=np.float32), (1, 4))
    rm = np.stack([(s < t), (s <= t), (s < t), -1.0 * (s <= t), (s > t)], axis=1).astype(np.float32)
    c["rmask"] = np.ascontiguousarray(rm)
    return c


def host_inputs(inp, b):
    f = lambda a: np.ascontiguousarray(np.asarray(a, np.float32))
    L = DEPTH
    m = {
        "x": f(inp["x"][b]), "meta": f(inp["meta"]), "ln0_g": f(inp["ln0_g"])[None], "ln0_b": f(inp["ln0_b"])[None],
        "w_in": f(inp["w_in"]), "swa_sinks": f(inp["swa_sinks"]),
        "diff_l": f(np.stack([inp["diff_lq1"], inp["diff_lk1"], inp["diff_lq2"], inp["diff_lk2"]], axis=1)),
        "diff_subln_g": f(inp["diff_subln_g"]),
        "rwkv_mu": f(inp["rwkv_mu"]), "rwkv_w0": f(inp["rwkv_w0"]), "rwkv_w2": f(inp["rwkv_w2"]), "rwkv_a0": f(inp["rwkv_a0"]),
        "rwkv_a2": f(inp["rwkv_a2"]), "rwkv_g2": f(inp["rwkv_g2"]), "rwkv_kk": f(inp["rwkv_kk"]), "rwkv_ka": f(inp["rwkv_ka"]),
        "rwkv_rk": f(np.asarray(inp["rwkv_rk"]).reshape(L, 256)), "rwkv_lnx_g": f(inp["rwkv_lnx_g"]), "rwkv_lnx_b": f(inp["rwkv_lnx_b"]),
        "conv_wT": f(np.transpose(np.asarray(inp["conv_w"]), (0, 2, 1))), "conv_b": f(inp["conv_b"])[..., None],
        "conv_gn_g": f(inp["conv_gn_g"])[..., None], "conv_gn_b": f(inp["conv_gn_b"])[..., None],
        "w_out": f(inp["w_out"]), "ln1_g": f(inp["ln1_g"]), "ln1_b": f(inp["ln1_b"]),
        "router_w": f(inp["router_w"]), "router_b": f(inp["router_b"])[None],
        "exp_w1": f(inp["exp_w1"]), "exp_w3": f(inp["exp_w3"]), "exp_w2": f(inp["exp_w2"]),
        "ln2_g": f(inp["ln2_g"]), "ln2_b": f(inp["ln2_b"]),
    }
    return m


def layernorm_tile(k, z, y, n, key, gb, bb, eps=1e-5, tmp=None, gbres=("gb", "bb")):
    stt, mv, rs = tmp
    for c in range(2):
        k.op("dve", "bn_stats", out=stt[0:n, c, :], in_=z[0:n, c * 512:(c + 1) * 512], r=[("z", key)], w=[("st", key, c)])
    k.op("dve", "bn_aggr", out=mv[0:n, :], in_=stt[0:n].rearrange("p a b -> p (a b)"), r=[("st", key, 0), ("st", key, 1)], w=[("mv", key)])
    k.op("act", "activation", out=rs[0:n, 0:1], in_=mv[0:n, 1:2], func=AF.Sqrt, bias=eps, scale=1.0, r=[("mv", key)], w=[("rs", key, 0)])
    k.op("dve", "reciprocal", out=rs[0:n, 0:1], in_=rs[0:n, 0:1], r=[("rs", key, 0)], w=[("rs", key, 0)])
    k.op("dve", "scalar_tensor_tensor", out=rs[0:n, 1:2], in0=mv[0:n, 0:1], scalar=-1.0, in1=rs[0:n, 0:1], op0=ALU.mult, op1=ALU.mult, r=[("mv", key), ("rs", key, 0)], w=[("rs", key, 1)])
    k.op("act", "activation", out=y[0:n, :], in_=z[0:n, :], func=AF.Identity, bias=rs[0:n, 1:2], scale=rs[0:n, 0:1], r=[("z", key), ("rs", key, 0), ("rs", key, 1)], w=[("y", key)])
    k.op("pool", "tensor_tensor", out=y[0:n, :], in0=y[0:n, :], in1=gb[0:n, :], op=ALU.mult, r=[("y", key), gbres[0]], w=[("y", key)])
    k.op("pool", "tensor_tensor", out=y[0:n, :], in0=y[0:n, :], in1=bb[0:n, :], op=ALU.add, r=[("y", key), gbres[1]], w=[("y", key)])


def ln_tmp(k, nm):
    A = k.A
    return (A.t([128, 2, 6], F32, "st" + nm), A.t([128, 2], F32, "mv" + nm), A.t([128, 2], F32, "rs" + nm))


def load_bcast(k, dst, src_row, res, q="sp"):
    k.dma(q, dst[:], src_row.partition_broadcast(128), w=[res])


def stage_ln0(k):
    S, A, d = k.S, k.A, k.dr
    A.mark()
    gb = A.t([128, 1024], F32, "gb"); bb = A.t([128, 1024], F32, "bb")
    load_bcast(k, gb, d["ln0_g"], "gb"); load_bcast(k, bb, d["ln0_b"], "bb")
    zs = [A.t([128, 1024], F32, f"z{i}") for i in range(3)]
    ys = [A.t([128, 1024], F32, f"y{i}") for i in range(3)]
    tmps = [ln_tmp(k, str(i)) for i in range(3)]
    for t in range(NT):
        p0, n = tile_rows(t)
        b = t % 3
        z, y = zs[b], ys[b]
        src = d["meta"] if t == 0 else d["x"][128 * (t - 1):128 * t, :]
        k.dma("sp", z[0:n, :], src, w=[("z", b)])
        layernorm_tile(k, z, y, n, b, gb, bb, tmp=tmps[b])
        k.dma("act", d["H"][p0:p0 + n, :], y[0:n, :], r=[("y", b)], w=[("H", t)])
    S.barrier()
    A.release()


FM_TILES = [
    ("QKA", 0, 0, 0.125), ("QKA", 128, 128, 0.125), ("QKA", 256, 256, 1.0),
    ("QKB", 0, 512, 32 ** -0.5), ("QKB", 128, 640, 32 ** -0.5), ("QKB", 256, 768, 1.0), ("QKB", 384, 896, 1.0),
    ("CV", 0, 2304, 1.0), ("CV", 128, 2432, 1.0), ("CV", 256, 2560, 1.0), ("CV", 384, 2688, 1.0),
]


def stage_in(k, l):
    S, A, d = k.S, k.A, k.dr
    A.mark()
    idt = A.t([128, 128], BF16, "ident")
    k.dma("sp", idt[:], d["ident_bf"], w=["ident"])
    wsb = A.t([128, 8, 2816], BF16, "w_in")
    for kk in range(8):
        for c in range(2):
            k.dma("pool", wsb[:, kk, c * 1408:(c + 1) * 1408], d["w_in"][l, kk * 128:(kk + 1) * 128, c * 1408:(c + 1) * 1408], w=[("w_in", kk, c)])
    wres = [("w_in", kk, c) for kk in range(8) for c in range(2)]
    zs = [A.t([128, 1024], F32, f"z{i}") for i in range(2)]
    hb = [A.t([128, 1024], BF16, f"hb{i}") for i in range(2)]
    hTg = [A.t([128, 8, 512], BF16, f"hT{i}") for i in range(2)]
    ofm_b = [A.t([128, 512], BF16, f"ofb{i}") for i in range(3)]
    ofm_f = [A.t([128, 512], F32, f"off{i}") for i in range(2)]
    otm_v = [A.t([128, 384], BF16, f"otv{i}") for i in range(2)]
    otm_u = [A.t([128, 1024], F32, f"otu{i}") for i in range(2)]
    tcount = 0
    fmc = 0
    tmc = 0
    for gi, grp in enumerate(GROUPS):
        gb_ = gi % 2
        hT = hTg[gb_]
        ntok = sum(tile_rows(t)[1] for t in grp)
        gp0 = tile_rows(grp[0])[0]
        for ti, t in enumerate(grp):
            p0, n = tile_rows(t)
            b = tcount % 2
            tcount += 1
            z = zs[b]
            k.dma("sp", z[0:n, :], d["H"][p0:p0 + n, :], r=[("H", t)], w=[("z", b)])
            k.op("act", "copy", out=hb[b][0:n, :], in_=z[0:n, :], r=[("z", b)], w=[("hb", b)])
            pt = k.ps(b)[:].bitcast(BF16)
            for kk in range(8):
                k.tr(pt[:, kk * 128:kk * 128 + n], hb[b][0:n, kk * 128:(kk + 1) * 128], idt[0:n, 0:n], r=[("hb", b), "ident"], w=[("ps", b)])
            k.op("dve", "tensor_copy", out=hT[:, :, ti * 128:ti * 128 + n], in_=pt.rearrange("p (k t) -> p k t", k=8)[:, :, 0:n], r=[("ps", b)], w=[("hT", gb_, ti)])
        hres = [("hT", gb_, ti) for ti in range(len(grp))]
        for (dn, r0, c0, sc) in FM_TILES:
            pb = 2 + fmc % 3
            isf = dn == "CV"
            ob = ofm_f[fmc % 2] if isf else ofm_b[fmc % 3]
            ores = ("off", fmc % 2) if isf else ("ofb", fmc % 3)
            for kk in range(8):
                k.mm(k.ps(pb)[:, 0:ntok], wsb[:, kk, c0:c0 + 128], hT[:, kk, 0:ntok], kk == 0, kk == 7, r=hres + wres, w=[("ps", pb)])
            if fmc % 2 == 0:
                k.op("act", "activation", out=ob[:, 0:ntok], in_=k.ps(pb)[:, 0:ntok], func=AF.Copy, scale=sc, r=[("ps", pb)], w=[ores])
            else:
                k.op("dve", "tensor_scalar", out=ob[:, 0:ntok], in0=k.ps(pb)[:, 0:ntok], scalar1=sc, scalar2=None, op0=ALU.mult, r=[("ps", pb)], w=[ores])
            k.dma("sp", d[dn][r0:r0 + 128, gp0:gp0 + ntok], ob[:, 0:ntok], r=[ores], w=[(dn, r0, gi)])
            fmc += 1
        for ti, t in enumerate(grp):
            p0, n = tile_rows(t)
            ov = otm_v[tmc % 2]
            ou = otm_u[tmc % 2]
            tb = tmc % 2
            tmc += 1
            lt = hT[:, :, ti * 128:ti * 128 + n]
            for kk in range(8):
                k.mm(k.ps(5)[0:n, 0:128], lt[:, kk, :], wsb[:, kk, 384:512], kk == 0, kk == 7, r=hres + wres, w=[("ps", 5, 0)])
            for kk in range(8):
                k.mm(k.ps(5)[0:n, 128:384], lt[:, kk, :], wsb[:, kk, 1024:1280], kk == 0, kk == 7, r=hres + wres, w=[("ps", 5, 1)])
            k.op("act", "copy", out=ov[0:n, :], in_=k.ps(5)[0:n, 0:384], r=[("ps", 5, 0), ("ps", 5, 1)], w=[("otv", tb)])
            k.dma("act", d["VA"][p0:p0 + n, :], ov[0:n, 0:128], r=[("otv", tb)], w=[("VA", t)])
            k.dma("act", d["VB"][p0:p0 + n, :], ov[0:n, 128:384], r=[("otv", tb)], w=[("VB", t)])
            for c in range(2):
                pb = 6 + c
                for kk in range(8):
                    k.mm(k.ps(pb)[0:n, :], lt[:, kk, :], wsb[:, kk, 1280 + c * 512:1280 + (c + 1) * 512], kk == 0, kk == 7, r=hres + wres, w=[("ps", pb)])
                k.op("dve", "tensor_copy", out=ou[0:n, c * 512:(c + 1) * 512], in_=k.ps(pb)[0:n, :], r=[("ps", pb)], w=[("otu", tb, c)])
            k.dma("sp", d["UC"][1 + p0:1 + p0 + n, :], ou[0:n, :], r=[("otu", tb, 0), ("otu", tb, 1)], w=[("UC", t)])
    S.barrier()
    A.release()


def stage_swa(k, l):
    S, A, d = k.S, k.A, k.dr
    A.mark()
    idt = A.t([128, 128], BF16, "ident")
    k.dma("sp", idt[:], d["ident_bf"], w=["ident"])
    qT = A.t([64, 4, NPOS], BF16, "qTa")
    kT = A.t([64, 2, NPOS], BF16, "kTa")
    for h in range(4):
        k.dma("sp", qT[:, h, :], d["QKA"][64 * h:64 * h + 64, :], w=[("qT", h)])
    for kv in range(2):
        k.dma("sp", kT[:, kv, :], d["QKA"][256 + 64 * kv:256 + 64 * kv + 64, :], w=[("kT", kv)])
    va = A.t([128, NT, 2, 65], BF16, "va")
    k.op("pool", "memset", ap=va[:, :, :, 64:65], constant=1.0, w=["va_ones"])
    for t in range(NT):
        p0, n = tile_rows(t)
        k.dma("act", va[0:n, t, :, 0:64], d["VA"][p0:p0 + n, :].rearrange("p (h e) -> p h e", h=2), w=[("va", t)])
    bam = A.t([16, 3, 4, 128], BF16, "bam")
    k.dma("sp", bam[:], d["ba_meta"].rearrange("c h m q -> m c h q"), w=["bam"])
    bapc = A.t([128, 4, 256], BF16, "bapc")
    k.dma("sp", bapc[:], d["ba_pc"].rearrange("h k q -> k h q"), w=["bapc"])
    sk = A.t([128, 4, 1], F32, "sk")
    esk = A.t([128, 4, 1], F32, "esk")
    k.dma("sp", sk[:].rearrange("p h o -> p (h o)"), d["swa_sinks"][l:l + 1, :].partition_broadcast(128), w=["sk"])
    k.op("act", "activation", out=esk[:], in_=sk[:], func=AF.Exp, r=["sk"], w=["esk"])
    pms = [A.t([16, 4, 128], BF16, f"pm{i}") for i in range(2)]
    pps = [A.t([128, 2, 512], BF16, f"pp{i}") for i in range(2)]
    dens = [A.t([128, 4, 1], F32, f"den{i}") for i in range(2)]
    yos = [A.t([128, 4, 64], BF16, f"yo{i}") for i in range(2)]
    for t in range(NT):
        p0, n = tile_rows(t)
        s = t % 2
        psA, psB, psD = k.ps(4 * s), [k.ps(4 * s + 1), k.ps(4 * s + 2)], k.ps(4 * s + 3)
        pm, pp, den, yo = pms[s], pps[s], dens[s], yos[s]
        case = min(t, 2)
        psAv = psA[0:16, :].rearrange("p (h q) -> p h q", h=4)
        for h in range(4):
            kv = h // 2
            hp, cb = h // 2, (h % 2) * 256
            k.mm(psAv[:, h, 0:n], kT[:, kv, 0:16], qT[:, h, p0:p0 + n], True, False, r=[("kT", kv), ("qT", h)], w=[("psA", s)])
            k.mm(psAv[:, h, 0:n], idt[0:16, 0:16], bam[:, case, h, 0:n], False, True, r=["ident", "bam"], w=[("psA", s)])
            if t >= 2:
                pp0 = p0 - 128
                k.mm(psB[hp][:, cb:cb + n], kT[:, kv, pp0:pp0 + 128], qT[:, h, p0:p0 + n], True, False, r=[("kT", kv), ("qT", h)], w=[("psB", s, hp)])
                k.mm(psB[hp][:, cb:cb + n], idt[:, :], bapc[:, h, 0:n], False, True, r=["ident", "bapc"], w=[("psB", s, hp)])
            if t >= 1:
                k.mm(psB[hp][:, cb + 128:cb + 128 + n], kT[:, kv, p0:p0 + 128], qT[:, h, p0:p0 + n], True, False, r=[("kT", kv), ("qT", h)], w=[("psB", s, hp)])
                k.mm(psB[hp][:, cb + 128:cb + 128 + n], idt[:, :], bapc[:, h, 128:128 + n], False, True, r=["ident", "bapc"], w=[("psB", s, hp)])
        k.op("act", "activation", out=pm[:, :, 0:n], in_=psAv[:, :, 0:n], func=AF.Exp, r=[("psA", s)], w=[("pm", s)])
        for hp in range(2):
            if t >= 2:
                k.op("act", "activation", out=pp[:, hp, :], in_=psB[hp][:, :], func=AF.Exp, r=[("psB", s, hp)], w=[("pp", s, hp)])
            elif t == 1:
                k.op("act", "activation", out=pp[:, hp, :].rearrange("p (h x) -> p h x", h=2)[:, :, 128:256],
                     in_=psB[hp][:, :].rearrange("p (h x) -> p h x", h=2)[:, :, 128:256], func=AF.Exp, r=[("psB", s, hp)], w=[("pp", s, hp)])
        psDv = psD[:, 0:260].rearrange("p (h e) -> p h e", h=4)
        for h in range(4):
            kv = h // 2
            hp, cb = h // 2, (h % 2) * 256
            k.mm(psDv[0:n, h, :], pm[0:16, h, 0:n], va[0:16, 0, kv, :], True, t == 0, r=[("pm", s), ("va", 0), "va_ones"], w=[("psD", s)])
            if t >= 2:
                k.mm(psDv[0:n, h, :], pp[:, hp, cb:cb + n], va[:, t - 1, kv, :], False, False, r=[("pp", s, hp), ("va", t - 1), "va_ones"], w=[("psD", s)])
            if t >= 1:
                k.mm(psDv[0:n, h, :], pp[:, hp, cb + 128:cb + 128 + n], va[:, t, kv, :], False, True, r=[("pp", s, hp), ("va", t), "va_ones"], w=[("psD", s)])
        k.op("dve", "tensor_tensor", out=den[0:n], in0=psDv[0:n, :, 64:65], in1=esk[0:n], op=ALU.add, r=[("psD", s), "esk"], w=[("den", s)])
        k.op("dve", "reciprocal", out=den[0:n], in_=den[0:n], r=[("den", s)], w=[("den", s)])
        k.op("dve", "tensor_tensor", out=yo[0:n], in0=psDv[0:n, :, 0:64], in1=den[0:n].to_broadcast([n, 4, 64]), op=ALU.mult, r=[("psD", s), ("den", s)], w=[("yo", s)])
        k.dma("sp", d["MIX"][p0:p0 + n, 0:256], yo[0:n].rearrange("p h e -> p (h e)"), r=[("yo", s)], w=[("MIXa", t)])
    S.barrier()
    A.release()


def stage_diff(k, l):
    S, A, d = k.S, k.A, k.dr
    A.mark()
    lam_init = 0.8 - 0.6 * math.exp(-0.3 * l)
    idt = A.t([128, 128], BF16, "ident")
    k.dma("sp", idt[:], d["ident_bf"], w=["ident"])
    qT = A.t([32, 8, NPOS], BF16, "qTb")
    kT = A.t([32, 8, NPOS], BF16, "kTb")
    for sl in range(8):
        k.dma("sp", qT[:, sl, :], d["QKB"][32 * sl:32 * sl + 32, :], w=[("qT", sl)])
        k.dma("sp", kT[:, sl, :], d["QKB"][256 + 32 * sl:256 + 32 * sl + 32, :], w=[("kT", sl)])
    vb = A.t([128, NT, 4, 65], BF16, "vb")
    k.op("pool", "memset", ap=vb[:, :, :, 64:65], constant=1.0, w=["vb_ones"])
    for t in range(NT):
        p0, n = tile_rows(t)
        k.dma("act", vb[0:n, t, :, 0:64], d["VB"][p0:p0 + n, :].rearrange("p (h e) -> p h e", h=4), w=[("vb", t)])
    bbf = A.t([128, 4, 256], F32, "bbf")
    k.dma("sp", bbf[:], d["bb_pc"].rearrange("h k q -> k h q"), w=["bbf"])
    b31b = A.t([128, 8], F32, "b31b")
    k.dma("sp", b31b[:], d["b31"].partition_broadcast(128), w=["b31b"])
    bbt = A.t([128, 4, 256], BF16, "bbt")
    for h in range(4):
        k.op("dve", "tensor_scalar", out=bbt[:, h, :], in0=bbf[:, h, :], scalar1=b31b[:, 4 + h:5 + h], scalar2=None, op0=ALU.subtract, r=["bbf", "b31b"], w=["bbt"])
    dl = A.t([128, 4, 32], F32, "dl")
    k.dma("sp", dl[:].rearrange("p a b -> p (a b)"), d["diff_l"][l:l + 1].rearrange("o a b -> o (a b)").partition_broadcast(128), w=["dl"])
    pr = A.t([128, 2, 32], F32, "pr")
    ss = A.t([128, 2], F32, "ss")
    nlam = A.t([128, 1], F32, "nlam")
    k.op("dve", "tensor_tensor", out=pr[:, 0, :], in0=dl[:, 0, :], in1=dl[:, 1, :], op=ALU.mult, r=["dl"], w=["pr0"])
    k.op("dve", "tensor_tensor", out=pr[:, 1, :], in0=dl[:, 2, :], in1=dl[:, 3, :], op=ALU.mult, r=["dl"], w=["pr1"])
    k.op("dve", "tensor_reduce", out=ss[:], in_=pr[:], axis=AX.X, op=ALU.add, r=["pr0", "pr1"], w=["ss"])
    k.op("act", "activation", out=ss[:], in_=ss[:], func=AF.Exp, r=["ss"], w=["ss"])
    k.op("dve", "tensor_tensor", out=nlam[:], in0=ss[:, 1:2], in1=ss[:, 0:1], op=ALU.subtract, r=["ss"], w=["nlam"])
    k.op("dve", "tensor_scalar", out=nlam[:], in0=nlam[:], scalar1=-lam_init, scalar2=None, op0=ALU.add, r=["nlam"], w=["nlam"])
    gvec = A.t([128, 1, 64], F32, "gvec")
    k.dma("sp", gvec[:].rearrange("p o e -> p (o e)"), d["diff_subln_g"][l:l + 1, :].partition_broadcast(128), w=["gvec"])
    k.op("act", "mul", out=gvec[:], in_=gvec[:], mul=(1.0 - lam_init), r=["gvec"], w=["gvec"])
    PTs = [A.t([128, 512], BF16, f"PT{i}") for i in range(3)]
    rr = [A.t([128, 2, 4, 1], F32, f"rr{i}") for i in range(2)]
    t1 = [A.t([128, 4, 64], F32, f"t1{i}") for i in range(2)]
    t2 = [A.t([128, 4, 64], F32, f"t2{i}") for i in range(2)]
    ms = [A.t([128, 4, 1], F32, f"ms{i}") for i in range(2)]
    ybo = [A.t([128, 4, 4, 64], BF16, f"ybo{i}") for i in range(2)]
    sc = 0
    for qg, tiles in enumerate(GROUPS):
        ntok = sum(tile_rows(t)[1] for t in tiles)
        gp0 = tile_rows(tiles[0])[0]
        nt = len(tiles)
        nq = tile_rows(tiles[0])[1]
        yb_ = ybo[qg % 2]
        for h in range(4):
            hb = h % 2
            Ob = [k.ps(4 + 2 * hb), k.ps(5 + 2 * hb)]
            Ov = [Ob[c][:, 0:65 * nt].rearrange("p (t e) -> p t e", e=65) for c in range(2)]
            for c in range(2):
                sl = 2 * h + c
                for j in range(0, tiles[-1] + 1):
                    kp0, nk = tile_rows(j)
                    fi = max(0, j - tiles[0])
                    col0 = fi * 128
                    sb = sc % 4
                    pt = PTs[sc % 3]
                    ptk = sc % 3
                    sc += 1
                    psb = k.ps(sb)
                    bl = [i for i in (j, j + 1) if i in tiles]
                    c1 = col0 + 128 * len(bl) if tiles[0] != 0 else (nq if bl else 0)
                    c1 = min(c1, ntok)
                    if bl:
                        k.mm(psb[0:nk, col0:c1], kT[:, sl, kp0:kp0 + nk], qT[:, sl, gp0 + col0:gp0 + c1], True, False, r=[("kT", sl), ("qT", sl)], w=[("psS", sb)])
                        if j == 0:
                            if tiles[0] == 0:
                                k.mm(psb[0:16, 0:16], idt[0:16, 0:16], bbt[0:16, h, 0:16], False, True, r=["ident", "bbt"], w=[("psS", sb)])
                            else:
                                k.mm(psb[0:16, 0:128], idt[:, 112:128], bbt[:, h, 128:256], False, True, r=["ident", "bbt"], w=[("psS", sb)])
                        else:
                            b0 = 0 if bl[0] == j else 128
                            k.mm(psb[:, col0:c1], idt[:, :], bbt[:, h, b0:b0 + (c1 - col0)], False, True, r=["ident", "bbt"], w=[("psS", sb)])
                    else:
                        c1 = col0
                    if c1 < ntok:
                        k.mm(psb[0:nk, c1:ntok], kT[:, sl, kp0:kp0 + nk], qT[:, sl, gp0 + c1:gp0 + ntok], True, True, r=[("kT", sl), ("qT", sl)], w=[("psS", sb)])
                    k.op("act", "activation", out=pt[0:nk, col0:ntok], in_=psb[0:nk, col0:ntok], func=AF.Exp, bias=b31b[0:nk, 4 + h:5 + h], scale=1.0,
                         r=[("psS", sb), "b31b"], w=[("PT", ptk)])
                    for il, i in enumerate(tiles):
                        if i < j:
                            continue
                        ni = tile_rows(i)[1]
                        k.mm(Ov[c][0:ni, il, :], pt[0:nk, il * 128:il * 128 + ni], vb[0:nk, j, h, :], (j == 0 and il == 0), False, r=[("PT", ptk), ("vb", j), "vb_ones"], w=[("psO", hb, c)], skip=True)
            n = nq
            e = hb
            for c in range(2):
                k.op("dve", "reciprocal", out=rr[e][0:n, c, 0:nt, :], in_=Ov[c][0:n, :, 64:65], r=[("psO", hb, c)], w=[("rr", e, c)])
            k.op("dve", "tensor_tensor", out=t1[e][0:n, 0:nt, :], in0=Ov[0][0:n, :, 0:64], in1=rr[e][0:n, 0, 0:nt, :].to_broadcast([n, nt, 64]), op=ALU.mult, r=[("psO", hb, 0), ("rr", e, 0)], w=[("t1", e)])
            k.op("dve", "tensor_tensor", out=t2[e][0:n, 0:nt, :], in0=Ov[1][0:n, :, 0:64], in1=rr[e][0:n, 1, 0:nt, :].to_broadcast([n, nt, 64]), op=ALU.mult, r=[("psO", hb, 1), ("rr", e, 1)], w=[("t2", e)])
            k.op("dve", "scalar_tensor_tensor", out=t1[e][0:n, 0:nt, :], in0=t2[e][0:n, 0:nt, :], scalar=nlam[0:n, 0:1], in1=t1[e][0:n, 0:nt, :], op0=ALU.mult, op1=ALU.add, r=[("t1", e), ("t2", e), "nlam"], w=[("t1", e)])
            k.op("pool", "tensor_tensor", out=t2[e][0:n, 0:nt, :], in0=t1[e][0:n, 0:nt, :], in1=t1[e][0:n, 0:nt, :], op=ALU.mult, r=[("t1", e)], w=[("t2", e)])
            k.op("dve", "tensor_reduce", out=ms[e][0:n, 0:nt, :], in_=t2[e][0:n, 0:nt, :], axis=AX.X, op=ALU.add, r=[("t2", e)], w=[("ms", e)])
            k.op("act", "activation", out=ms[e][0:n, 0:nt, :], in_=ms[e][0:n, 0:nt, :], func=AF.Sqrt, bias=1e-5, scale=1.0 / 64, r=[("ms", e)], w=[("ms", e)])
            k.op("dve", "reciprocal", out=ms[e][0:n, 0:nt, :], in_=ms[e][0:n, 0:nt, :], r=[("ms", e)], w=[("ms", e)])
            k.op("dve", "tensor_tensor", out=t1[e][0:n, 0:nt, :], in0=t1[e][0:n, 0:nt, :], in1=ms[e][0:n, 0:nt, :].to_broadcast([n, nt, 64]), op=ALU.mult, r=[("t1", e), ("ms", e)], w=[("t1", e)])
            k.op("pool", "tensor_tensor", out=yb_[0:n, 0:nt, h, :], in0=t1[e][0:n, 0:nt, :], in1=gvec[0:n].to_broadcast([n, nt, 64]), op=ALU.mult, r=[("t1", e), "gvec"], w=[("ybo", qg % 2, h)])
        dst = d["MIX"][gp0:gp0 + ntok, 256:512]
        if nt > 1:
            dst = dst.rearrange("(t p) c -> p t c", p=128)
            src = yb_[:, 0:nt].rearrange("p t h e -> p t (h e)")
        else:
            src = yb_[0:nq, 0].rearrange("p h e -> p (h e)")
        k.dma("sp", dst, src, r=[("ybo", qg % 2, h) for h in range(4)], w=[("MIXb", qg)])
    S.barrier()
    A.release()


def stage_conv(k, l, lvl=9):
    S, A, d = k.S, k.A, k.dr
    A.mark()
    gmat = A.t([128, 128], F32, "gmat")
    k.dma("sp", gmat[:], d["gmat"], w=["gmat"])
    a = A.t([128, NPOS], F32, "cva")
    gate = A.t([128, NPOS], F32, "cvg")
    hg = A.t([128, 30 + NPOS], F32, "hg")
    acc1 = A.t([128, NPOS], F32, "acc1")
    acc2 = A.t([128, NPOS], F32, "acc2")
    cw = A.t([128, 31], F32, "cw")
    ctmp = [A.t([128, NPOS], F32, f"ctmp{i}") for i in range(2)]
    cp = A.t([128, 3], F32, "cp")
    sqb = [A.t([128, 512], F32, f"sqb{i}") for i in range(2)]
    ddb = [A.t([128, 512], F32, f"ddb{i}") for i in range(2)]
    m2b = [A.t([128, 512], F32, f"m2b{i}") for i in range(2)]
    ob = [A.t([128, 512], BF16, f"cob{i}") for i in range(2)]
    k.op("pool", "memset", ap=hg[:, 0:30], constant=0.0, w=["hgz"])
    cc = 0
    for ct in range(2):
        r0 = ct * 128
        k.dma("sp", a[:], d["CV"][r0:r0 + 128, :], w=["cva"])
        k.dma("sp", gate[:], d["CV"][256 + r0:256 + r0 + 128, :], w=["cvg"])
        k.dma("act", cw[:], d["conv_wT"][l, r0:r0 + 128, :], w=["cw"])
        k.dma("act", cp[:, 0:1], d["conv_b"][l, r0:r0 + 128, :], w=["cp0"])
        k.dma("act", cp[:, 1:2], d["conv_gn_g"][l, r0:r0 + 128, :], w=["cp1"])
        k.dma("act", cp[:, 2:3], d["conv_gn_b"][l, r0:r0 + 128, :], w=["cp2"])
        H2 = NPOS // 2
        for hh in range(2):
            k.op("act", "activation", out=gate[:, hh * H2:(hh + 1) * H2], in_=gate[:, hh * H2:(hh + 1) * H2], func=AF.Sigmoid, r=["cvg"], w=[("sig", hh)])
            k.op("pool", "tensor_tensor", out=hg[:, 30 + hh * H2:30 + (hh + 1) * H2], in0=a[:, hh * H2:(hh + 1) * H2], in1=gate[:, hh * H2:(hh + 1) * H2], op=ALU.mult,
                 r=["cva", ("sig", hh)], w=[("hg", hh)])
        hres = ["hgz", ("hg", 0), ("hg", 1)]
        if lvl < 2:
            k.dma("sp", d["CV"][r0:r0 + 128, :], hg[:, 30:], r=hres, w=["dbg"])
            continue
        k.op("dve", "tensor_scalar", out=acc1[:], in0=hg[:, 0:NPOS], scalar1=cw[:, 0:1], scalar2=cp[:, 0:1], op0=ALU.mult, op1=ALU.add, r=hres + ["cw", "cp0"], w=["acc1"])
        for j in range(1, 20):
            k.op("dve", "scalar_tensor_tensor", out=acc1[:], in0=hg[:, j:j + NPOS], scalar=cw[:, j:j + 1], in1=acc1[:], op0=ALU.mult, op1=ALU.add, r=hres + ["cw", "acc1"], w=["acc1"])
        if lvl < 3:
            k.dma("sp", d["CV"][r0:r0 + 128, :], acc1[:], r=["acc1"], w=["dbg"])
            continue
        k.op("pool", "tensor_scalar", out=acc2[:], in0=hg[:, 20:20 + NPOS], scalar1=cw[:, 20:21], scalar2=None, op0=ALU.mult, r=hres + ["cw"], w=["acc2"])
        for j in range(21, 31):
            tb = j % 2
            k.op("act", "activation", out=ctmp[tb][:], in_=hg[:, j:j + NPOS], func=AF.Identity, scale=cw[:, j:j + 1], r=hres + ["cw"], w=[("ctmp", tb)])
            k.op("pool", "tensor_tensor", out=acc2[:], in0=acc2[:], in1=ctmp[tb][:], op=ALU.add, r=[("ctmp", tb), "acc2"], w=["acc2"])
        k.op("dve", "tensor_tensor", out=acc1[:], in0=acc1[:], in1=acc2[:], op=ALU.add, r=["acc1", "acc2"], w=["acc1"])
        if lvl < 4:
            k.dma("sp", d["CV"][r0:r0 + 128, :], acc1[:], r=["acc1"], w=["dbg"])
            continue
        for c0 in range(0, NPOS, 512):
            n = min(512, NPOS - c0)
            b = cc % 2
            cc += 1
            psM, psE = k.ps(2 * b), k.ps(2 * b + 1)
            k.mm(psM[:, 0:n], gmat[:], acc1[:, c0:c0 + n], True, True, r=["gmat", "acc1"], w=[("psM", b)])
            k.op("dve", "tensor_tensor", out=ddb[b][:, 0:n], in0=acc1[:, c0:c0 + n], in1=psM[:, 0:n], op=ALU.subtract, r=["acc1", ("psM", b)], w=[("ddb", b)])
            k.op("pool", "tensor_tensor", out=sqb[b][:, 0:n], in0=ddb[b][:, 0:n], in1=ddb[b][:, 0:n], op=ALU.mult, r=[("ddb", b)], w=[("sqb", b)])
            k.mm(psE[:, 0:n], gmat[:], sqb[b][:, 0:n], True, True, r=["gmat", ("sqb", b)], w=[("psE", b)])
            if lvl == 4:
                continue
            k.op("act", "activation", out=m2b[b][:, 0:n], in_=psE[:, 0:n], func=AF.Sqrt, bias=1e-5, scale=1.0, r=[("psE", b)], w=[("m2b", b)])
            k.op("dve", "reciprocal", out=m2b[b][:, 0:n], in_=m2b[b][:, 0:n], r=[("m2b", b)], w=[("m2b", b)])
            k.op("pool", "tensor_tensor", out=ddb[b][:, 0:n], in0=ddb[b][:, 0:n], in1=m2b[b][:, 0:n], op=ALU.mult, r=[("ddb", b), ("m2b", b)], w=[("ddb", b)])
            if lvl == 5:
                continue
            k.op("act", "activation", out=ob[b][:, 0:n], in_=ddb[b][:, 0:n], func=AF.Silu, bias=cp[:, 2:3], scale=cp[:, 1:2], r=[("ddb", b), "cp1", "cp2"], w=[("cob", b)])
            k.dma("sp", d["MIXD"][r0:r0 + 128, c0:c0 + n], ob[b][:, 0:n], r=[("cob", b)], w=[("MIXD", ct, c0)])
    S.barrier()
    A.release()


def stage_out(k, l):
    S, A, d = k.S, k.A, k.dr
    A.mark()
    idt = A.t([128, 128], BF16, "ident")
    k.dma("sp", idt[:], d["ident_bf"], w=["ident"])
    wo = A.t([128, 8, 1024], BF16, "wo")
    for kk in range(8):
        k.dma("pool", wo[:, kk, :], d["w_out"][l, kk * 128:(kk + 1) * 128, :], w=[("wo", kk)])
    wres = [("wo", kk) for kk in range(8)]
    gb = A.t([128, 1024], F32, "gb"); bb = A.t([128, 1024], F32, "bb")
    load_bcast(k, gb, d["ln1_g"][l:l + 1, :], "gb"); load_bcast(k, bb, d["ln1_b"][l:l + 1, :], "bb")
    mxs = [A.t([128, 768], BF16, f"mx{i}") for i in range(2)]
    mxds = [A.t([128, 2, 128], BF16, f"mxd{i}") for i in range(2)]
    mTs = [A.t([128, 6, 128], BF16, f"mT{i}") for i in range(2)]
    hs = [A.t([128, 1024], F32, f"h{i}") for i in range(2)]
    zs = [A.t([128, 1024], F32, f"z{i}") for i in range(2)]
    ys = [A.t([128, 1024], F32, f"y{i}") for i in range(2)]
    tmps = [ln_tmp(k, str(i)) for i in range(2)]
    for t in range(NT):
        p0, n = tile_rows(t)
        b = t % 2
        mx, mxd, mT, h, z, y = mxs[b], mxds[b], mTs[b], hs[b], zs[b], ys[b]
        k.dma("sp", mx[0:n, :], d["MIX"][p0:p0 + n, :], w=[("mx", b)])
        for c in range(2):
            k.dma("sp", mxd[:, c, 0:n], d["MIXD"][c * 128:(c + 1) * 128, p0:p0 + n], w=[("mxd", b, c)])
        k.dma("act", h[0:n, :], d["H"][p0:p0 + n, :], w=[("h", b)])
        pt = k.ps(b)[:].bitcast(BF16)
        for kk in range(6):
            k.tr(pt[:, kk * 128:kk * 128 + n], mx[0:n, kk * 128:(kk + 1) * 128], idt[0:n, 0:n], r=[("mx", b), "ident"], w=[("ps", b)])
        k.op("dve", "tensor_copy", out=mT[:, :, 0:n], in_=pt[:, 0:768].rearrange("p (k t) -> p k t", k=6)[:, :, 0:n], r=[("ps", b)], w=[("mT", b)])
        for half in range(2):
            pb = 2 + 2 * b + half
            for kk in range(8):
                lhsT = mT[:, kk, 0:n] if kk < 6 else mxd[:, kk - 6, 0:n]
                k.mm(k.ps(pb)[0:n, :], lhsT, wo[:, kk, half * 512:(half + 1) * 512], kk == 0, kk == 7,
                     r=[("mT", b), ("mxd", b, 0), ("mxd", b, 1)] + wres, w=[("ps", pb)])
            k.op("dve", "scalar_tensor_tensor", out=z[0:n, half * 512:(half + 1) * 512], in0=h[0:n, half * 512:(half + 1) * 512], scalar=ALPHA, in1=k.ps(pb)[0:n, :],
                 op0=ALU.mult, op1=ALU.add, r=[("h", b), ("ps", pb)], w=[("z", b)])
        layernorm_tile(k, z, y, n, b, gb, bb, tmp=tmps[b])
        k.dma("act", d["HM"][p0:p0 + n, :], y[0:n, :], r=[("y", b)], w=[("HM", t)])
    S.barrier()
    A.release()


def stage_moe(k, l, last=False):
    S, A, d = k.S, k.A, k.dr
    A.mark()
    comb = A.t([128, NT, 16], F32, "comb")
    A.mark()
    idf = A.t([128, 128], F32, "identf")
    k.dma("sp", idf[:], d["ident_f"], w=["identf"])
    rw = A.t([128, 8, 16], F32, "rw")
    k.dma("sp", rw[:], d["router_w"].rearrange("(k p) e -> p k e", p=128), w=["rw"])
    rb = A.t([128, 16], F32, "rb")
    load_bcast(k, rb, d["router_b"], "rb")
    hms = [A.t([128, 1024], F32, f"hm{i}") for i in range(2)]
    hT32 = [A.t([128, 8, 128], F32, f"hT32{i}") for i in range(2)]
    hTb = [A.t([128, 8, 128], BF16, f"hTb{i}") for i in range(2)]
    rt = [dict((nm, A.t([128, 16], F32, f"{nm}{i}")) for nm in ("sc", "bi", "eq", "b2", "mk", "s1", "mk2", "s2")) for i in range(2)]
    rs4 = [dict((nm, A.t([128, 4], F32, f"{nm}{i}")) for nm in ("m1", "m2", "gs", "ing")) for i in range(2)]
    rs1 = [dict((nm, A.t([128, 1], F32, f"{nm}{i}")) for nm in ("gm", "t1", "t2", "den")) for i in range(2)]
    for t in range(NT):
        p0, n = tile_rows(t)
        b = t % 2
        hm = hms[b]
        k.dma("sp", hm[0:n, :], d["HM"][p0:p0 + n, :], r=[("HM", t)], w=[("hm", b)])
        for kk in range(8):
            pb = 2 * b + kk // 4
            k.tr(k.ps(pb)[:, (kk % 4) * 128:(kk % 4) * 128 + n], hm[0:n, kk * 128:(kk + 1) * 128], idf[0:n, 0:n], r=[("hm", b), "identf"], w=[("ps", pb)])
        for hf in range(2):
            pb = 2 * b + hf
            src = k.ps(pb)[:].rearrange("p (k t) -> p k t", k=4)[:, :, 0:n]
            k.op("act", "copy", out=hT32[b][:, 4 * hf:4 * hf + 4, 0:n], in_=src, r=[("ps", pb)], w=[("hT32", b, hf)])
            k.op("dve", "tensor_copy", out=hTb[b][:, 4 * hf:4 * hf + 4, 0:n], in_=src, r=[("ps", pb)], w=[("hTb", b, hf)])
        k.dma("act", d["HT"][:, p0:p0 + n].rearrange("(k p) t -> p k t", p=128), hTb[b][:, :, 0:n], r=[("hTb", b, 0), ("hTb", b, 1)], w=[("HT", t)])
        pr = k.ps(4 + b)
        for kk in range(8):
            k.mm(pr[0:n, 0:16], hT32[b][:, kk, 0:n], rw[:, kk, :], kk == 0, kk == 7, r=[("hT32", b, 0), ("hT32", b, 1), "rw"], w=[("psr", b)])
        R_, R4, R1 = rt[b], rs4[b], rs1[b]
        rk = lambda nm: ("rt", nm, b)
        v4 = lambda ap: ap[0:n, :].rearrange("p (g e) -> p g e", g=4)
        k.op("act", "activation", out=R_["sc"][0:n, :], in_=pr[0:n, 0:16], func=AF.Sigmoid, r=[("psr", b)], w=[rk("sc")])
        k.op("dve", "tensor_tensor", out=R_["bi"][0:n, :], in0=R_["sc"][0:n, :], in1=rb[0:n, :], op=ALU.add, r=[rk("sc"), "rb"], w=[rk("bi")])
        k.op("dve", "tensor_reduce", out=R4["m1"][0:n, :], in_=v4(R_["bi"]), axis=AX.X, op=ALU.max, r=[rk("bi")], w=[rk("m1")])
        k.op("dve", "tensor_tensor", out=v4(R_["eq"]), in0=v4(R_["bi"]), in1=R4["m1"][0:n, :].unsqueeze(2).to_broadcast([n, 4, 4]), op=ALU.is_equal, r=[rk("bi"), rk("m1")], w=[rk("eq")])
        k.op("dve", "scalar_tensor_tensor", out=R_["b2"][0:n, :], in0=R_["eq"][0:n, :], scalar=NEG, in1=R_["bi"][0:n, :], op0=ALU.mult, op1=ALU.add, r=[rk("eq"), rk("bi")], w=[rk("b2")])
        k.op("dve", "tensor_reduce", out=R4["m2"][0:n, :], in_=v4(R_["b2"]), axis=AX.X, op=ALU.max, r=[rk("b2")], w=[rk("m2")])
        k.op("dve", "tensor_tensor", out=R4["gs"][0:n, :], in0=R4["m1"][0:n, :], in1=R4["m2"][0:n, :], op=ALU.add, r=[rk("m1"), rk("m2")], w=[rk("gs")])
        k.op("dve", "tensor_reduce", out=R1["gm"][0:n, :], in_=R4["gs"][0:n, :], axis=AX.X, op=ALU.max, r=[rk("gs")], w=[rk("gm")])
        k.op("dve", "tensor_scalar", out=R4["ing"][0:n, :], in0=R4["gs"][0:n, :], scalar1=R1["gm"][0:n, 0:1], scalar2=None, op0=ALU.is_equal, r=[rk("gs"), rk("gm")], w=[rk("ing")])
        k.op("dve", "tensor_scalar", out=R4["ing"][0:n, :], in0=R4["ing"][0:n, :], scalar1=1.0, scalar2=-NEG, op0=ALU.subtract, op1=ALU.mult, r=[rk("ing")], w=[rk("ing")])
        k.op("dve", "tensor_tensor", out=v4(R_["mk"]), in0=v4(R_["bi"]), in1=R4["ing"][0:n, :].unsqueeze(2).to_broadcast([n, 4, 4]), op=ALU.add, r=[rk("bi"), rk("ing")], w=[rk("mk")])
        k.op("dve", "tensor_reduce", out=R1["t1"][0:n, :], in_=R_["mk"][0:n, :], axis=AX.X, op=ALU.max, r=[rk("mk")], w=[rk("t1")])
        k.op("dve", "tensor_scalar", out=R_["s1"][0:n, :], in0=R_["mk"][0:n, :], scalar1=R1["t1"][0:n, 0:1], scalar2=None, op0=ALU.is_equal, r=[rk("mk"), rk("t1")], w=[rk("s1")])
        k.op("dve", "scalar_tensor_tensor", out=R_["mk2"][0:n, :], in0=R_["s1"][0:n, :], scalar=NEG, in1=R_["mk"][0:n, :], op0=ALU.mult, op1=ALU.add, r=[rk("s1"), rk("mk")], w=[rk("mk2")])
        k.op("dve", "tensor_reduce", out=R1["t2"][0:n, :], in_=R_["mk2"][0:n, :], axis=AX.X, op=ALU.max, r=[rk("mk2")], w=[rk("t2")])
        k.op("dve", "tensor_scalar", out=R_["s2"][0:n, :], in0=R_["mk2"][0:n, :], scalar1=R1["t2"][0:n, 0:1], scalar2=None, op0=ALU.is_equal, r=[rk("mk2"), rk("t2")], w=[rk("s2")])
        k.op("dve", "tensor_tensor", out=R_["s1"][0:n, :], in0=R_["s1"][0:n, :], in1=R_["s2"][0:n, :], op=ALU.add, r=[rk("s1"), rk("s2")], w=[rk("s1")])
        k.op("dve", "tensor_tensor", out=R_["s1"][0:n, :], in0=R_["s1"][0:n, :], in1=R_["sc"][0:n, :], op=ALU.mult, r=[rk("s1"), rk("sc")], w=[rk("s1")])
        k.op("dve", "tensor_reduce", out=R1["den"][0:n, :], in_=R_["s1"][0:n, :], axis=AX.X, op=ALU.add, r=[rk("s1")], w=[rk("den")])
        k.op("dve", "reciprocal", out=R1["den"][0:n, :], in_=R1["den"][0:n, :], r=[rk("den")], w=[rk("den")])
        k.op("dve", "tensor_scalar", out=comb[0:n, t, :], in0=R_["s1"][0:n, :], scalar1=R1["den"][0:n, 0:1], scalar2=None, op0=ALU.mult, r=[rk("s1"), rk("den")], w=[("comb", t)])
    S.barrier()
    A.release()
    gb = A.t([128, 1024], F32, "gb"); bb = A.t([128, 1024], F32, "bb")
    load_bcast(k, gb, d["ln2_g"][l:l + 1, :], "gb"); load_bcast(k, bb, d["ln2_b"][l:l + 1, :], "bb")
    hTh = A.t([128, 8, 2064], BF16, "hTh")
    acc = A.t([128, 17, 1024], F32, "acc")
    w1s = [A.t([128, 8, 512], BF16, f"w1s{i}") for i in range(2)]
    w3s = [A.t([128, 8, 512], BF16, f"w3s{i}") for i in range(2)]
    w2s = [A.t([128, 4, 1024], BF16, f"w2s{i}") for i in range(2)]
    actT = [A.t([128, 4, 256], BF16, f"actT{i}") for i in range(2)]
    s1b = [A.t([128, 256], F32, f"s1b{i}") for i in range(2)]
    hm = A.t([128, 1024], F32, "hm2"); z = A.t([128, 1024], F32, "z2"); y = A.t([128, 1024], F32, "y2")
    tmp = ln_tmp(k, "m")
    ecount = 0
    fcount = 0
    ocount = 0
    gcount = 0
    for half, tiles in enumerate([list(range(0, 17)), list(range(17, 33))]):
        hp0 = tile_rows(tiles[0])[0]
        hn = sum(tile_rows(t)[1] for t in tiles)
        for kk in range(8):
            k.dma("sp", hTh[:, kk, 0:hn], d["HT"][kk * 128:(kk + 1) * 128, hp0:hp0 + hn], r=[("HT", t) for t in tiles], w=[("hTh", kk)])
        hres = [("hTh", kk) for kk in range(8)]
        groups = []
        tl = list(tiles)
        if tl[0] == 0:
            groups.append([0]); tl = tl[1:]
        for i in range(0, len(tl), 2):
            groups.append(tl[i:i + 2])
        for e in range(16):
            eb = ecount % 2
            ecount += 1
            k.dma("pool", w1s[eb][:], d["exp_w1"][l, e].rearrange("(k p) f -> p k f", p=128), w=[("w1s", eb)])
            k.dma("pool", w3s[eb][:], d["exp_w3"][l, e].rearrange("(k p) f -> p k f", p=128), w=[("w3s", eb)])
            k.dma("pool", w2s[eb][:], d["exp_w2"][l, e].rearrange("(k p) f -> p k f", p=128), w=[("w2s", eb)])
            for grp in groups:
                c0 = tile_rows(grp[0])[0] - hp0
                ng = sum(tile_rows(t)[1] for t in grp)
                ab = gcount % 2
                gcount += 1
                for f in range(4):
                    hb_ = fcount % 2
                    fcount += 1
                    ph = k.ps(hb_)
                    for kk in range(8):
                        k.mm(ph[:, 0:ng], w1s[eb][:, kk, f * 128:(f + 1) * 128], hTh[:, kk, c0:c0 + ng], kk == 0, kk == 7, r=hres + [("w1s", eb)], w=[("psh", hb_)])
                    for kk in range(8):
                        k.mm(ph[:, 256:256 + ng], w3s[eb][:, kk, f * 128:(f + 1) * 128], hTh[:, kk, c0:c0 + ng], kk == 0, kk == 7, r=hres + [("w3s", eb)], w=[("psh", hb_)])
                    k.op("act", "activation", out=s1b[hb_][:, 0:ng], in_=ph[:, 0:ng], func=AF.Silu, r=[("psh", hb_)], w=[("s1b", hb_)])
                    k.op("dve", "tensor_tensor", out=actT[ab][:, f, 0:ng], in0=s1b[hb_][:, 0:ng], in1=ph[:, 256:256 + ng], op=ALU.mult, r=[("s1b", hb_), ("psh", hb_)], w=[("actT", ab, f)])
                for ti, t in enumerate(grp):
                    nt_ = tile_rows(t)[1]
                    tloc = t - tiles[0]
                    for h2 in range(2):
                        ob_ = 2 + ocount % 6
                        ocount += 1
                        po = k.ps(ob_)
                        for f in range(4):
                            k.mm(po[0:nt_, :], actT[ab][:, f, ti * 128:ti * 128 + nt_], w2s[eb][:, f, h2 * 512:(h2 + 1) * 512], f == 0, f == 3, r=[("actT", ab, f), ("w2s", eb)], w=[("pso", ob_)])
                        dst = acc[0:nt_, tloc, h2 * 512:(h2 + 1) * 512]
                        if e == 0:
                            k.op("dve", "tensor_scalar", out=dst, in0=po[0:nt_, :], scalar1=comb[0:nt_, t, e:e + 1], scalar2=None, op0=ALU.mult, r=[("pso", ob_), ("comb", t)], w=[("acc", tloc, h2)])
                        else:
                            k.op("dve", "scalar_tensor_tensor", out=dst, in0=po[0:nt_, :], scalar=comb[0:nt_, t, e:e + 1], in1=dst, op0=ALU.mult, op1=ALU.add, r=[("pso", ob_), ("comb", t), ("acc", tloc, h2)], w=[("acc", tloc, h2)])
        for t in tiles:
            p0, n = tile_rows(t)
            tloc = t - tiles[0]
            k.dma("sp", hm[0:n, :], d["HM"][p0:p0 + n, :], r=[("HM", t)], w=[("hm2",)])
            k.op("dve", "scalar_tensor_tensor", out=z[0:n, :], in0=hm[0:n, :], scalar=ALPHA, in1=acc[0:n, tloc, :], op0=ALU.mult, op1=ALU.add, r=[("hm2",), ("acc", tloc, 0), ("acc", tloc, 1)], w=[("z", "m")])
            layernorm_tile(k, z, y, n, "m", gb, bb, tmp=tmp)
            if last:
                if t >= 1:
                    k.dma("act", d["out"][128 * (t - 1):128 * t, :], y[0:n, :], r=[("y", "m")], w=[("outT", t)])
            else:
                k.dma("act", d["H"][p0:p0 + n, :], y[0:n, :], r=[("y", "m")], w=[("H", t)])
        S.barrier()
    A.release()


def stage_rwkv_a(k, l, lvl=9):
    S, A, d = k.S, k.A, k.dr
    A.mark()
    cnt = [0]

    def T(shape, dt, name):
        cnt[0] += 1
        return TB(A.t(shape, dt, name), (name, cnt[0]))

    P = [TB(k.ps(i), ("ps", i)) for i in range(8)]
    DV = lambda ap: Vw(ap, [])
    idt = T([128, 128], BF16, "ident"); k.do("sp", "dma_start", idt[:], in_=DV(d["ident_bf"]))
    mu_b = T([128, 1024], F32, "mu_b"); k.do("sp", "dma_start", mu_b[:], in_=DV(d["rwkv_mu"][l:l + 1, :].partition_broadcast(128)))
    wa0 = T([128, 512], F32, "wa0")
    k.do("sp", "dma_start", wa0[:, 0:256], in_=DV(d["rwkv_w0"][l:l + 1, :].partition_broadcast(128)))
    k.do("sp", "dma_start", wa0[:, 256:512], in_=DV(d["rwkv_a0"][l:l + 1, :].partition_broadcast(128)))
    kk_b = T([128, 256], F32, "kk_b"); k.do("sp", "dma_start", kk_b[:], in_=DV(d["rwkv_kk"][l:l + 1, :].partition_broadcast(128)))
    ka_b = T([128, 256], F32, "ka_b"); k.do("sp", "dma_start", ka_b[:], in_=DV(d["rwkv_ka"][l:l + 1, :].partition_broadcast(128)))
    rk_b = T([128, 256], F32, "rk_b"); k.do("sp", "dma_start", rk_b[:], in_=DV(d["rwkv_rk"][l:l + 1, :].partition_broadcast(128)))
    WA = T([128, 512], BF16, "WA")
    k.do("pool", "memset", WA[:], constant=0.0)
    k.do("pool", "dma_start", WA[0:64, 0:256], in_=DV(d["rwkv_w2"][l]))
    k.do("pool", "dma_start", WA[64:128, 256:512], in_=DV(d["rwkv_a2"][l]))
    G2 = T([128, 256], BF16, "G2"); k.do("pool", "dma_start", G2[:], in_=DV(d["rwkv_g2"][l]))
    ctri = T([128, 128], F32, "ctri"); k.do("sp", "dma_start", ctri[:], in_=DV(d["ctri"]))
    cblk = T([128, 128], F32, "cblk"); k.do("sp", "dma_start", cblk[:], in_=DV(d["cblk"]))
    cones = T([128, 2, 64], F32, "cones"); k.do("sp", "dma_start", cones[:], in_=DV(d["cones"]))
    idrep = T([64, 1, 256], F32, "idrep"); k.do("sp", "dma_start", idrep[:].re("p o f -> p (o f)"), in_=DV(d["identrep"]))
    zrow = T([1, 1024], F32, "zrow")
    k.do("pool", "memset", zrow[:], constant=0.0)
    uc0 = ("UC0",)
    k.do("sp", "dma_start", Vw(d["UC"][0:1, :], [uc0]), in_=zrow[:])
    nb = 2
    mk_ = lambda shape, dt, nm: [T(shape, dt, f"{nm}{i}") for i in range(nb)]
    cur_, prv_, ucs_ = mk_([128, 1024], F32, "cur"), mk_([128, 1024], F32, "prv"), mk_([128, 1024], F32, "ucs")
    LI_, LT_ = mk_([128, 256], BF16, "LI"), mk_([128, 2, 128], BF16, "LT")
    wa_, logw_ = mk_([128, 512], F32, "wa"), mk_([128, 256], F32, "logw")
    W_, Wi_, Wp_, Web_ = mk_([128, 256], F32, "W"), mk_([128, 256], F32, "Wi"), mk_([128, 256], F32, "Wp"), mk_([128, 256], F32, "Web")
    DWe_ = mk_([64, 2, 256], F32, "DWe"); DWt_ = mk_([64, 2, 256], BF16, "DWt")
    kq_, sq_, kkn_, kmod_, bb_, t1_ = (mk_([128, 256], F32, nm) for nm in ("kq", "sq", "kkn", "kmod", "bb", "t1"))
    ss_, rs_ = mk_([128, 4, 1], F32, "ss"), mk_([128, 4, 1], F32, "rs")
    kh_, bh_ = mk_([128, 256], F32, "kh"), mk_([128, 256], F32, "bh")
    OT_ = mk_([128, 7, 256], BF16, "OT"); BG_ = mk_([128, 2, 256], F32, "BGt")
    NE5 = -math.exp(-0.5)
    for t in range(NT):
        p0, n = tile_rows(t)
        b = t % nb
        C = 16 if t == 0 else 64
        ncn = 1 if t == 0 else 2
        ch0 = 0 if t == 0 else 2 * (t - 1) + 1
        cur, prv, ucs, LI, LT, wa, logw = cur_[b], prv_[b], ucs_[b], LI_[b], LT_[b], wa_[b], logw_[b]
        W, Wi, Wp, Web, DWe, DWt = W_[b], Wi_[b], Wp_[b], Web_[b], DWe_[b], DWt_[b]
        kq, sq, kkn, kmod, bb, t1, ss, rs, kh, bh, OT, BGt = kq_[b], sq_[b], kkn_[b], kmod_[b], bb_[b], t1_[b], ss_[b], rs_[b], kh_[b], bh_[b], OT_[b], BG_[b]
        k.do("sp", "dma_start", cur[0:n, :], in_=DV(d["UC"][1 + p0:1 + p0 + n, :]))
        k.do("sp", "dma_start", prv[0:n, :], in_=Vw(d["UC"][p0:p0 + n, :], [uc0] if t == 0 else []))
        k.do("pool", "tensor_tensor", prv[0:n, :], in0=prv[0:n, :], in1=cur[0:n, :], op=ALU.subtract)
        k.do("pool", "tensor_tensor", prv[0:n, :], in0=prv[0:n, :], in1=mu_b[0:n, :], op=ALU.mult)
        k.do("dve", "tensor_tensor", ucs[0:n, :], in0=cur[0:n, :], in1=prv[0:n, :], op=ALU.add)
        r_, kraw, v_ = ucs[0:n, 0:256], ucs[0:n, 256:512], ucs[0:n, 512:768]
        k.do("act", "activation", LI[0:n, 0:64], in_=ucs[0:n, 768:832], func=AF.Tanh)
        k.do("act", "copy", LI[0:n, 64:128], in_=ucs[0:n, 832:896])
        k.do("act", "activation", LI[0:n, 128:256], in_=ucs[0:n, 896:1024], func=AF.Sigmoid)
        if lvl < 2:
            continue
        ptb = Vw(k.ps(0)[:].bitcast(BF16), P[0].res)
        k.trv(ptb[:, 0:n], LI[0:n, 0:128], idt[0:n, 0:n])
        k.trv(ptb[:, 128:128 + n], LI[0:n, 128:256], idt[0:n, 0:n])
        k.do("dve", "tensor_copy", LT[:, :, 0:n], in_=ptb[:, 0:256].re("p (a t) -> p a t", a=2)[:, :, 0:n])
        k.mmv(P[1][0:n, :], LT[:, 0, 0:n], WA[:], True, True)
        k.mmv(P[2][0:n, 0:256], LT[:, 1, 0:n], G2[:], True, True)
        k.do("dve", "tensor_tensor", wa[0:n, :], in0=P[1][0:n, :], in1=wa0[0:n, :], op=ALU.add)
        k.do("act", "activation", wa[0:n, :], in_=wa[0:n, :], func=AF.Sigmoid)
        a_ = wa[0:n, 256:512]
        k.do("pool", "tensor_scalar", logw[0:n, :], in0=wa[0:n, 0:256], scalar1=NE5, scalar2=None, op0=ALU.mult)
        k.do("act", "copy", BGt[0:n, 1, :], in_=P[2][0:n, 0:256])
        if lvl < 3:
            continue
        k.mmv(P[3][0:n, 0:256], ctri[0:n, 0:n], logw[0:n, :], True, True)
        k.mmv(P[4][0:n, 0:256], cblk[0:n, 0:n], logw[0:n, :], True, True)
        for c in range(ncn):
            k.mmv(P[5][0:64, c * 256:(c + 1) * 256], cones[0:n, c, :], logw[0:n, :], True, True)
        k.do("act", "activation", W[0:n, :], in_=P[3][0:n, 0:256], func=AF.Exp)
        k.do("act", "activation", Wi[0:n, :], in_=P[3][0:n, 0:256], func=AF.Exp, scale=-1.0)
        k.do("dve", "tensor_tensor", Wp[0:n, :], in0=P[3][0:n, 0:256], in1=logw[0:n, :], op=ALU.subtract)
        k.do("act", "activation", Wp[0:n, :], in_=Wp[0:n, :], func=AF.Exp)
        k.do("act", "activation", Web[0:n, :], in_=P[4][0:n, 0:256], func=AF.Exp)
        k.do("act", "activation", DWe[:, 0:ncn, :], in_=P[5][0:64, 0:ncn * 256].re("p (c f) -> p c f", c=ncn), func=AF.Exp)
        k.do("pool", "tensor_tensor", DWt[:, 0:ncn, :], in0=DWe[:, 0:ncn, :], in1=idrep[:].bc([64, ncn, 256]), op=ALU.mult)
        k.do("act", "dma_start", DV(d["DW"][ch0:ch0 + ncn].rearrange("c p f -> p c f")), in_=DWt[:, 0:ncn, :])
        if lvl < 4:
            continue
        k.do("pool", "tensor_tensor", kq[0:n, :], in0=kraw, in1=kk_b[0:n, :], op=ALU.mult)
        k.do("pool", "tensor_tensor", sq[0:n, :], in0=kq[0:n, :], in1=kq[0:n, :], op=ALU.mult)
        k.do("dve", "tensor_reduce", ss[0:n], in_=sq[0:n, :].re("p (h e) -> p h e", h=4), axis=AX.X, op=ALU.add)
        k.do("act", "activation", ss[0:n], in_=ss[0:n], func=AF.Sqrt)
        k.do("dve", "tensor_scalar", ss[0:n], in0=ss[0:n], scalar1=1e-12, scalar2=None, op0=ALU.max)
        k.do("dve", "reciprocal", ss[0:n], in_=ss[0:n])
        k.do("dve", "tensor_tensor", kkn[0:n, :].re("p (h e) -> p h e", h=4), in0=kq[0:n, :].re("p (h e) -> p h e", h=4), in1=ss[0:n].bc([n, 4, 64]), op=ALU.mult)
        k.do("dve", "scalar_tensor_tensor", t1[0:n, :], in0=a_, scalar=-1.0, in1=ka_b[0:n, :], op0=ALU.add, op1=ALU.mult)
        k.do("dve", "scalar_tensor_tensor", kmod[0:n, :], in0=t1[0:n, :], scalar=1.0, in1=kraw, op0=ALU.add, op1=ALU.mult)
        k.do("pool", "tensor_tensor", bb[0:n, :], in0=kkn[0:n, :], in1=a_, op=ALU.mult)
        k.do("pool", "tensor_tensor", t1[0:n, :], in0=r_, in1=kmod[0:n, :], op=ALU.mult)
        k.do("pool", "tensor_tensor", t1[0:n, :], in0=t1[0:n, :], in1=rk_b[0:n, :], op=ALU.mult)
        k.do("dve", "tensor_reduce", rs[0:n], in_=t1[0:n, :].re("p (h e) -> p h e", h=4), axis=AX.X, op=ALU.add)
        k.do("dve", "tensor_tensor", BGt[0:n, 0, :].re("p (h e) -> p h e", h=4), in0=v_.re("p (h e) -> p h e", h=4), in1=rs[0:n].bc([n, 4, 64]), op=ALU.mult)
        if lvl < 5:
            continue
        k.do("dve", "tensor_tensor", OT[0:n, 0, :], in0=kkn[0:n, :], in1=Wp[0:n, :], op=ALU.mult)
        k.do("pool", "tensor_tensor", OT[0:n, 1, :], in0=r_, in1=W[0:n, :], op=ALU.mult)
        k.do("dve", "tensor_tensor", kh[0:n, :], in0=kmod[0:n, :], in1=Wi[0:n, :], op=ALU.mult)
        k.do("pool", "tensor_tensor", bh[0:n, :], in0=bb[0:n, :], in1=Wi[0:n, :], op=ALU.mult)
        k.do("act", "copy", OT[0:n, 2, :], in_=kh[0:n, :])
        k.do("act", "copy", OT[0:n, 3, :], in_=bh[0:n, :])
        k.do("dve", "tensor_tensor", OT[0:n, 4, :], in0=kh[0:n, :], in1=Web[0:n, :], op=ALU.mult)
        k.do("dve", "scalar_tensor_tensor", OT[0:n, 5, :], in0=bh[0:n, :], scalar=-1.0, in1=Web[0:n, :], op0=ALU.mult, op1=ALU.mult)
        k.do("act", "copy", OT[0:n, 6, :], in_=v_)
        k.do("sp", "dma_start", DV(d["RWT"][p0:p0 + n]), in_=OT[0:n])
        k.do("sp", "dma_start", DV(d["BG"][p0:p0 + n]), in_=BGt[0:n])
    S.barrier()
    A.release()


def stage_rwkv_b(k, l, tmax=NT):
    S, A, d = k.S, k.A, k.dr
    A.mark()
    cnt = [0]

    def T(shape, dt, name):
        cnt[0] += 1
        return TB(A.t(shape, dt, name), (name, cnt[0]))

    P = [TB(k.ps(i), ("ps", i)) for i in range(8)]
    DV = lambda ap: Vw(ap, [])
    idt = T([128, 128], BF16, "ident"); k.do("sp", "dma_start", idt[:], in_=DV(d["ident_bf"]))
    rmask = T([64, 5, 64], F32, "rmask"); k.do("sp", "dma_start", rmask[:], in_=DV(d["rmask"]))
    lg_b = T([64, 256], F32, "lg_b"); k.do("sp", "dma_start", lg_b[:], in_=DV(d["rwkv_lnx_g"][l:l + 1, :].partition_broadcast(64)))
    lb_b = T([64, 256], F32, "lb_b"); k.do("sp", "dma_start", lb_b[:], in_=DV(d["rwkv_lnx_b"][l:l + 1, :].partition_broadcast(64)))
    Tb = [T([64, 4, 64], BF16, f"Tst{i}") for i in range(2)]
    k.do("pool", "memset", Tb[0][:], constant=0.0)
    nb = 2
    mk_ = lambda shape, dt, nm: [T(shape, dt, f"{nm}{i}") for i in range(nb)]
    tokX_ = mk_([64, 2, 7, 256], BF16, "tokX"); DWc_ = mk_([64, 2, 256], BF16, "DWc"); BGc_ = mk_([64, 2, 2, 256], F32, "BGc")
    XT_ = mk_([64, 2, 4, 4, 64], BF16, "XT")
    AT_ = mk_([64, 8, 2, 64], BF16, "AT"); BT_ = mk_([64, 8, 2, 64], BF16, "BT"); Nn_ = mk_([64, 8, 64], BF16, "Nn")
    NPa = mk_([64, 8, 64], BF16, "NPa"); NPTa = mk_([64, 8, 64], BF16, "NPTa")
    Zf_ = mk_([64, 8, 128], F32, "Zf"); Zb_ = mk_([64, 8, 128], BF16, "Zb")
    Q1T_ = mk_([64, 8, 64], BF16, "Q1T"); Q2s_ = mk_([64, 8, 64], F32, "Q2s"); GT_ = mk_([64, 8, 64], BF16, "GT"); Hs_ = mk_([64, 8, 64], F32, "Hs")
    ys_ = mk_([64, 4, 64], F32, "ys"); dd_ = mk_([64, 4, 64], F32, "dd"); sq_ = mk_([64, 4, 64], F32, "sq")
    st_ = mk_([64, 4, 1], F32, "st"); yo_ = mk_([64, 256], BF16, "yo")
    tcur = 0
    ccount = 0
    for t in range(tmax):
        p0, n = tile_rows(t)
        b = t % nb
        C = 16 if t == 0 else 64
        ncn = 1 if t == 0 else 2
        nq = 4 * ncn
        ch0 = 0 if t == 0 else 2 * (t - 1) + 1
        tokX, DWc, BGc, XT, AT, BT, Nn, Zf, Zb = tokX_[b], DWc_[b], BGc_[b], XT_[b], AT_[b], BT_[b], Nn_[b], Zf_[b], Zb_[b]
        Q1T, Q2s, GT, Hs = Q1T_[b], Q2s_[b], GT_[b], Hs_[b]
        for c in range(ncn):
            r0 = p0 + 64 * c
            k.do("sp", "dma_start", tokX[0:C, c], in_=DV(d["RWT"][r0:r0 + C]))
            k.do("act", "dma_start", BGc[0:C, c], in_=DV(d["BG"][r0:r0 + C]))
        k.do("act", "dma_start", DWc[:, 0:ncn, :], in_=DV(d["DW"][ch0:ch0 + ncn].rearrange("c p f -> p c f")))
        for c in range(ncn):
            ptb = Vw(k.ps(6 + c)[:].bitcast(BF16), P[6 + c].res)
            pv = ptb[0:64, :].re("p (h x t) -> p h x t", h=4, x=4)
            for h in range(4):
                for X in range(4):
                    k.trv(pv[:, h, X, 0:C], tokX[0:C, c, X, h * 64:(h + 1) * 64], idt[0:C, 0:C])
            if c == 0:
                k.do("dve", "tensor_copy", XT[:, c, :, :, 0:C], in_=pv[:, :, :, 0:C])
            else:
                k.do("act", "copy", XT[:, c, :, :, 0:C], in_=pv[:, :, :, 0:C])
        for c in range(ncn):
            for h in range(4):
                q = c * 4 + h
                o1 = P[q // 4][0:C, :].re("p (q a t) -> p q a t", q=4, a=2)[:, q % 4, :, 0:C]
                k.mmv(o1, XT[:, c, h, 2, 0:C], XT[:, c, h, 0:2, 0:C], True, True)
                o2 = P[2 + q // 4][0:C, :].re("p (q a t) -> p q a t", q=4, a=2)[:, q % 4, :, 0:C]
                k.mmv(o2, XT[:, c, h, 3, 0:C], XT[:, c, h, 0:2, 0:C], True, True)
                o3 = P[4][0:C, :].re("p (q t) -> p q t", q=8)[:, q, 0:C]
                k.mmv(o3, XT[:, c, h, 0, 0:C], XT[:, c, h, 3, 0:C], True, True)
        for c in range(ncn):
            pv1 = P[c][0:C, :].re("p (q a t) -> p q a t", q=4, a=2)[:, :, :, 0:C]
            k.do("dve", "tensor_tensor", AT[0:C, 4 * c:4 * c + 4, :, 0:C], in0=pv1, in1=rmask[0:C, 0:2, 0:C].un(1).bc([C, 4, 2, C]), op=ALU.mult)
            pv2 = P[2 + c][0:C, :].re("p (q a t) -> p q a t", q=4, a=2)[:, :, :, 0:C]
            k.do("dve", "tensor_tensor", BT[0:C, 4 * c:4 * c + 4, :, 0:C], in0=pv2, in1=rmask[0:C, 2:4, 0:C].un(1).bc([C, 4, 2, C]), op=ALU.mult)
        pv3 = P[4][0:C, :].re("p (q t) -> p q t", q=8)[:, 0:nq, 0:C]
        k.do("dve", "tensor_tensor", Nn[0:C, 0:nq, 0:C], in0=pv3, in1=rmask[0:C, 4:5, 0:C].bc([C, nq, C]), op=ALU.mult)
        pav = P[5][0:C, :].re("p (q i) -> p q i", q=8)
        for c in range(ncn):
            for h in range(4):
                q = c * 4 + h
                k.mmv(pav[:, q, :], AT[0:C, q, 0, 0:C], tokX[0:C, c, 6, h * 64:(h + 1) * 64], True, True)
        k.do("pool", "tensor_copy", Zf[0:C, 0:nq, 0:64].re("p (c h) e -> p c h e", c=ncn), in_=tokX[0:C, 0:ncn, 0, :].re("p c (h e) -> p c h e", h=4))
        k.do("act", "copy", Zf[0:C, 0:nq, 64:128], in_=pav[:, 0:nq, :])
        k.do("dve", "tensor_copy", Zb[0:C, 0:nq, :], in_=Zf[0:C, 0:nq, :])
        L = 3 if t == 0 else 5
        NPc, NPTc = Nn, None
        for lev in range(L + 1):
            def npt(q):
                return BT[0:C, q, 0, 0:C] if lev == 0 else NPTc[0:C, q, 0:C]
            for q in range(nq):
                oz = P[q // 4][0:C, :].re("p (q e) -> p q e", q=4)[:, q % 4, :]
                k.mmv(oz, npt(q), Zb[0:C, q, :], True, True)
            if lev < L:
                nxtP, nxtPT = NPa[lev % 2], NPTa[lev % 2]
                pa = P[5][0:C, :].re("p (q i) -> p q i", q=8)
                pbk = P[7][0:C, :].re("p (q i) -> p q i", q=8)
                for q in range(nq):
                    if lev + 1 < L:
                        k.mmv(pa[:, q, 0:C], npt(q), NPc[0:C, q, 0:C], True, True)
                    k.mmv(pbk[:, q, 0:C], NPc[0:C, q, 0:C], npt(q), True, True)
            for c in range(ncn):
                zv = P[c][0:C, :].re("p (q e) -> p q e", q=4)
                k.do("dve", "tensor_tensor", Zf[0:C, 4 * c:4 * c + 4, :], in0=Zf[0:C, 4 * c:4 * c + 4, :], in1=zv, op=(ALU.subtract if lev == 0 else ALU.add))
            k.do("act", "copy", Zb[0:C, 0:nq, :], in_=Zf[0:C, 0:nq, :])
            if lev < L:
                if lev + 1 < L:
                    k.do("act", "copy", nxtP[0:C, 0:nq, 0:C], in_=pa[:, 0:nq, 0:C])
                k.do("dve", "tensor_copy", nxtPT[0:C, 0:nq, 0:C], in_=pbk[:, 0:nq, 0:C])
                NPc, NPTc = nxtP, nxtPT
        pq1 = P[2][0:64, :].re("p (q t) -> p q t", q=8)
        pq2 = P[3][0:C, :].re("p (q i) -> p q i", q=8)
        pg = P[4][0:64, :].re("p (q j) -> p q j", q=8)
        ph = P[5][0:64, :].re("p (q i) -> p q i", q=8)
        for c in range(ncn):
            for h in range(4):
                q = c * 4 + h
                hs = slice(h * 64, (h + 1) * 64)
                P1b, P2b = Zb[0:C, q, 0:64], Zb[0:C, q, 64:128]
                V_ = tokX[0:C, c, 6, hs]
                k.mmv(pq1[:, q, 0:C], P1b, BT[0:C, q, 1, 0:C], True, False)
                k.mmv(pq1[:, q, 0:C], idt[0:64, 0:64], XT[:, c, h, 1, 0:C], False, True)
                k.mmv(pq2[:, q, :], AT[0:C, q, 1, 0:C], V_, True, False)
                k.mmv(pq2[:, q, :], BT[0:C, q, 1, 0:C], P2b, False, True)
                k.mmv(pg[:, q, :], idt[0:64, 0:64], DWc[:, c, hs], True, False)
                k.mmv(pg[:, q, :], P1b, tokX[0:C, c, 5, hs], False, True)
                k.mmv(ph[:, q, :], tokX[0:C, c, 4, hs], V_, True, False)
                k.mmv(ph[:, q, :], tokX[0:C, c, 5, hs], P2b, False, True)
        k.do("act", "copy", Q1T[:, 0:nq, 0:C], in_=pq1[:, 0:nq, 0:C])
        k.do("dve", "tensor_copy", Q2s[0:C, 0:nq, :], in_=pq2[:, 0:nq, :])
        k.do("act", "copy", GT[:, 0:nq, :], in_=pg[:, 0:nq, :])
        k.do("dve", "tensor_copy", Hs[:, 0:nq, :], in_=ph[:, 0:nq, :])
        for c in range(ncn):
            cb = ccount % 2
            ccount += 1
            Tc, Tn = Tb[tcur], Tb[1 - tcur]
            tcur = 1 - tcur
            py = P[6][0:C, 0:256].re("p (h i) -> p h i", h=4)
            pt_ = P[6][0:64, 256:512].re("p (h i) -> p h i", h=4)
            for h in range(4):
                q = c * 4 + h
                k.mmv(pt_[:, h, :], GT[:, q, :], Tc[:, h, :], True, True)
            for h in range(4):
                q = c * 4 + h
                k.mmv(py[:, h, :], Q1T[:, q, 0:C], Tc[:, h, :], True, True)
            k.do("dve", "tensor_tensor", Tn[:], in0=pt_, in1=Hs[:, 4 * c:4 * c + 4, :], op=ALU.add)
            ys, dd, sq, st, yo = ys_[cb], dd_[cb], sq_[cb], st_[cb], yo_[cb]
            k.do("dve", "tensor_tensor", ys[0:C], in0=py, in1=Q2s[0:C, 4 * c:4 * c + 4, :], op=ALU.add)
            k.do("dve", "tensor_reduce", st[0:C], in_=ys[0:C], axis=AX.X, op=ALU.add)
            k.do("dve", "tensor_scalar", st[0:C], in0=st[0:C], scalar1=-1.0 / 64, scalar2=None, op0=ALU.mult)
            k.do("dve", "tensor_tensor", dd[0:C], in0=ys[0:C], in1=st[0:C].bc([C, 4, 64]), op=ALU.add)
            k.do("pool", "tensor_tensor", sq[0:C], in0=dd[0:C], in1=dd[0:C], op=ALU.mult)
            k.do("dve", "tensor_reduce", st[0:C], in_=sq[0:C], axis=AX.X, op=ALU.add)
            k.do("act", "activation", st[0:C], in_=st[0:C], func=AF.Sqrt, bias=64e-5, scale=1.0 / 64)
            k.do("dve", "reciprocal", st[0:C], in_=st[0:C])
            k.do("dve", "tensor_tensor", dd[0:C], in0=dd[0:C], in1=st[0:C].bc([C, 4, 64]), op=ALU.mult)
            ddf = dd[0:C].re("p h e -> p (h e)")
            k.do("pool", "tensor_tensor", ddf, in0=ddf, in1=lg_b[0:C, :], op=ALU.mult)
            k.do("pool", "tensor_tensor", ddf, in0=ddf, in1=lb_b[0:C, :], op=ALU.add)
            k.do("pool", "tensor_tensor", ddf, in0=ddf, in1=BGc[0:C, c, 0, :], op=ALU.add)
            k.do("pool", "tensor_tensor", yo[0:C, :], in0=ddf, in1=BGc[0:C, c, 1, :], op=ALU.mult)
            r0 = p0 + 64 * c
            k.do("sp", "dma_start", DV(d["MIX"][r0:r0 + C, 512:768]), in_=yo[0:C, :])
    S.barrier()
    A.release()


def build_full():
    k = K()
    declare_io(k, final_out=True)
    stage_ln0(k)
    for l in range(DEPTH):
        stage_in(k, l)
        stage_swa(k, l)
        stage_diff(k, l)
        stage_rwkv_a(k, l)
        stage_rwkv_b(k, l)
        stage_conv(k, l)
        stage_out(k, l)
        stage_moe(k, l, last=(l == DEPTH - 1))
        k.S.barrier(rotate_dma=True)
    k.S.final_wait("sp")
    k.S.emit()
    k.st.close()
    return k


def kernel(**inputs):
    inp = {kk_: np.asarray(v) for kk_, v in inputs.items()}
    k = build_full()
    consts = host_consts(inp)
    in_maps = []
    for b in range(8):
        m = host_inputs(inp, b)
        m.update(consts)
        in_maps.append(m)
    res = run_bass_kernel_spmd(k.nc, in_maps, core_ids=list(range(8)))
    out = np.stack([np.asarray(res.results[b]["out"], np.float32) for b in range(8)], axis=0)
    return out
```

```python
import numpy as np
import concourse.bass as bass
import concourse.mybir as mybir

F32 = mybir.dt.float32
BF16 = mybir.dt.bfloat16
AF = mybir.ActivationFunctionType
ALU = mybir.AluOpType
AX = mybir.AxisListType

ENGS = ("pe", "act", "dve", "pool", "sp")
DQS = ("sp", "act", "pool")


class Sched:
    def __init__(self, nc, stack, ndma=6):
        self.nc = nc
        self.stack = stack
        self.ndma = ndma
        self.gen = 0
        self.dgen = 0
        self.objs = {}
        self.cnt = {e: 0 for e in ENGS}
        self.dcnt = {q: [0] * ndma for q in DQS}
        self.dnext = {q: 0 for q in DQS}
        self.streams = {e: [] for e in ENGS}
        self.waited = {e: {} for e in ENGS}
        self.lastw = {}
        self.readers = {}
        self.nops = 0
        self._new_csems()
        self._new_dsems()

    def _new_csems(self):
        self.gen += 1
        for e in ENGS:
            self.objs[("c", e, self.gen)] = self.stack.enter_context(self.nc.semaphore(f"c_{e}_{self.gen}"))
            self.cnt[e] = 0

    def _new_dsems(self):
        self.dgen += 1
        for q in DQS:
            for i in range(self.ndma):
                self.objs[("d", q, i, self.dgen)] = self.stack.enter_context(self.nc.semaphore(f"d_{q}{i}_{self.dgen}"))
            self.dcnt[q] = [0] * self.ndma

    def ckey(self, e):
        return ("c", e, self.gen)

    def dkey(self, q, i):
        return ("d", q, i, self.dgen)

    def _semobj(self, key):
        return self.objs[key]

    def _wait(self, eng, tok):
        key, val, src = tok
        if self.waited[eng].get(key, 0) >= val:
            return
        self.waited[eng][key] = val
        self.streams[eng].append(("w", key, val))

    def op(self, eng, fn, kw=None, r=(), w=(), dma=False):
        if isinstance(fn, str):
            name = fn
            kw = dict(kw)
            fn = lambda E, name=name, kw=kw: getattr(E, name)(**kw)
        deps = []
        for res in r:
            t = self.lastw.get(res)
            if t is not None:
                deps.append((t, "raw"))
        for res in w:
            t = self.lastw.get(res)
            if t is not None:
                deps.append((t, "waw"))
            for t in self.readers.get(res, ()):
                deps.append((t, "war"))
        for t, kind in deps:
            src = t[2]
            if src == eng and t[0][0] == "c":
                if eng == "pe":
                    continue
                if kind == "war":
                    continue
            self._wait(eng, t)
        if dma:
            q = eng
            i = self.dnext[q] % self.ndma
            self.dnext[q] += 1
            if self.dcnt[q][i] > 0:
                self._wait(eng, (self.dkey(q, i), self.dcnt[q][i], None))
            self.dcnt[q][i] += 16
            tok = (self.dkey(q, i), self.dcnt[q][i], None)
            self.streams[eng].append(("d", fn, self.dkey(q, i)))
        else:
            self.cnt[eng] += 1
            tok = (self.ckey(eng), self.cnt[eng], eng)
            self.streams[eng].append(("o", fn, self.ckey(eng), self.cnt[eng]))
        for res in w:
            self.lastw[res] = tok
            self.readers[res] = []
        for res in r:
            self.readers.setdefault(res, []).append(tok)
        self.nops += 1
        return tok

    def barrier(self, rotate_dma=False):
        for e in ENGS:
            for s in ENGS:
                if s != e and self.cnt[s] > 0:
                    self._wait(e, (self.ckey(s), self.cnt[s], s))
            for q in DQS:
                for i in range(self.ndma):
                    if self.dcnt[q][i] > 0:
                        self._wait(e, (self.dkey(q, i), self.dcnt[q][i], None))
        self.lastw = {}
        self.readers = {}
        for e in ENGS:
            self.streams[e].append(None)
        if max(self.cnt.values()) > 4000:
            self._new_csems()
        if rotate_dma:
            self._new_dsems()

    def final_wait(self, eng="sp"):
        for q in DQS:
            for i in range(self.ndma):
                if self.dcnt[q][i] > 0:
                    self._wait(eng, (self.dkey(q, i), self.dcnt[q][i], None))
        for s in ENGS:
            if s != eng and self.cnt[s] > 0:
                self._wait(eng, (self.ckey(s), self.cnt[s], s))

    def emit(self):
        nc = self.nc
        targets = {}
        for e in ENGS:
            for it in self.streams[e]:
                if it is not None and it[0] == "w" and it[1][0] == "c":
                    targets.setdefault(it[1], set()).add(it[2])
        rank = {key: {v: i + 1 for i, v in enumerate(sorted(vs))} for key, vs in targets.items()}
        self.n_inc = sum(len(v) for v in rank.values())

        def conv(it):
            if it[0] == "w":
                _, key, val = it
                s = self.objs[key]
                v = rank[key][val] if key[0] == "c" else val
                return lambda E, s=s, v=v: E.wait_ge(s, v)
            if it[0] == "d":
                _, fn, key = it
                s = self.objs[key]
                return lambda E, fn=fn, s=s: fn(E).then_inc(s, 16)
            _, fn, key, idx = it
            if idx in rank.get(key, ()):
                s = self.objs[key]
                return lambda E, fn=fn, s=s: fn(E).then_inc(s, 1)
            return lambda E, fn=fn: fn(E)

        segs = {e: [[]] for e in ENGS}
        for e in ENGS:
            for f in self.streams[e]:
                if f is None:
                    segs[e].append([])
                else:
                    segs[e][-1].append(conv(f))
        nseg = max(len(v) for v in segs.values())
        for i in range(nseg):
            cur = {e: (segs[e][i] if i < len(segs[e]) else []) for e in ENGS}
            if not any(cur.values()):
                continue
            with nc.Block() as block:
                @block.tensor
                def _(E, fs=cur["pe"]):
                    for f in fs:
                        f(E)

                @block.scalar
                def _(E, fs=cur["act"]):
                    for f in fs:
                        f(E)

                @block.vector
                def _(E, fs=cur["dve"]):
                    for f in fs:
                        f(E)

                @block.gpsimd
                def _(E, fs=cur["pool"]):
                    for f in fs:
                        f(E)

                @block.sync
                def _(E, fs=cur["sp"]):
                    for f in fs:
                        f(E)


class SbufAlloc:
    def __init__(self, nc, base=16640, limit=224 * 1024 - 256):
        self.nc = nc
        self.off = base
        self.limit = limit
        self.n = 0
        self.marks = []

    def mark(self):
        self.marks.append(self.off)

    def release(self):
        self.off = self.marks.pop()

    def t(self, shape, dtype, name=None):
        esz = 4 if dtype == F32 else 2
        if dtype in (mybir.dt.int32, mybir.dt.uint32):
            esz = 4
        nbytes = int(np.prod(shape[1:])) * esz
        nbytes = (nbytes + 63) // 64 * 64
        assert self.off + nbytes <= self.limit, f"SBUF overflow {self.off}+{nbytes} > {self.limit} ({name})"
        self.n += 1
        h = self.nc.alloc_sbuf_tensor_at(f"{name or 't'}_{self.n}", list(shape), dtype, offset=self.off)
        self.off += nbytes
        return h


import math
import numpy as np
import ml_dtypes
from contextlib import ExitStack
import concourse.bass as bass
import concourse.mybir as mybir
from concourse.bass_utils import run_bass_kernel_spmd

NPOS = 4112
NT = 33
DEPTH = 2
ALPHA = (2 * DEPTH) ** 0.25
NEG = -1e30


def tile_rows(t):
    return (0, 16) if t == 0 else (16 + 128 * (t - 1), 128)


GROUPS = [[0]] + [[4 * g + 1 + i for i in range(4)] for g in range(8)]


def t5_bucket(dist):
    n = np.maximum(dist, 0)
    lr = np.log(np.maximum(n, 1).astype(np.float32) / np.float32(16)) / np.float32(math.log(128 / 16))
    large = np.minimum(16 + (lr * np.float32(16)).astype(np.int32), 31)
    return np.where(n < 16, n, large)


class K:
    def __init__(self, ext_in=(), ext_out=(), layers=(0, 1)):
        self.nc = bass.Bass("TRN2", target_bir_lowering=False)
        self.st = ExitStack()
        self.S = Sched(self.nc, self.st)
        self.A = SbufAlloc(self.nc)
        self.ext_in = set(ext_in)
        self.ext_out = set(ext_out)
        self.dr = {}
        nc = self.nc
        self.psum = [self.st.enter_context(nc.psum_tensor(f"ps{i}", [128, 512], F32)) for i in range(8)]

    def D(self, name, shape, dtype, kind=None):
        if kind is None:
            kind = "ExternalInput" if name in self.ext_in else ("ExternalOutput" if name in self.ext_out else "Internal")
        self.dr[name] = self.nc.dram_tensor(name, list(shape), dtype, kind=kind).ap()
        return self.dr[name]

    def I(self, name, shape, dtype=F32):
        return self.D(name, shape, dtype, kind="ExternalInput")

    def ps(self, i):
        return self.psum[i]

    def op(self, eng, name, r=(), w=(), **kw):
        return self.S.op(eng, name, kw, r=r, w=w)

    def dma(self, q, out, in_, r=(), w=()):
        return self.S.op(q, "dma_start", dict(out=out, in_=in_), r=r, w=w, dma=True)

    def mm(self, out, lhsT, rhs, start, stop, r=(), w=(), skip=False):
        kw = dict(out=out, lhsT=lhsT, rhs=rhs, start=start, stop=stop)
        if skip:
            kw["skip_group_check"] = True
        return self.S.op("pe", "matmul", kw, r=r, w=w)

    def tr(self, out, in_, identity, r=(), w=()):
        return self.S.op("pe", "transpose", dict(out=out, in_=in_, identity=identity), r=r, w=w)

    def ps2(self, i, dtype=F32):
        raise NotImplementedError


class Vw:
    def __init__(self, ap, res):
        self.ap = ap
        self.res = res

    def __getitem__(self, idx):
        return Vw(self.ap[idx], self.res)

    def re(self, pat, **kw):
        return Vw(self.ap.rearrange(pat, **kw), self.res)

    def bc(self, shape):
        return Vw(self.ap.to_broadcast(list(shape)), self.res)

    def un(self, ax):
        return Vw(self.ap.unsqueeze(ax), self.res)


class TB:
    def __init__(self, h, res):
        self.h = h
        self.res = res if isinstance(res, list) else [res]

    def __getitem__(self, idx):
        return Vw(self.h[idx], self.res)


def _do(self, eng, name, out, extra_r=(), extra_w=(), **kw):
    r = list(extra_r)
    w = list(extra_w)
    args = {}
    for kk_, v in kw.items():
        if isinstance(v, Vw):
            r += v.res
            args[kk_] = v.ap
        else:
            args[kk_] = v
    okey = "ap" if name == "memset" else "out"
    if isinstance(out, Vw):
        w += out.res
        args[okey] = out.ap
    else:
        args[okey] = out
    if name == "dma_start":
        return self.S.op(eng, name, args, r=r, w=w, dma=True)
    return self.S.op(eng, name, args, r=r, w=w)


K.do = _do


def _mmv(self, out, lhsT, rhs, start, stop, skip=False):
    kw = dict(out=out.ap, lhsT=lhsT.ap, rhs=rhs.ap, start=start, stop=stop)
    if skip:
        kw["skip_group_check"] = True
    return self.S.op("pe", "matmul", kw, r=lhsT.res + rhs.res, w=out.res)


def _trv(self, out, in_, ident):
    return self.S.op("pe", "transpose", dict(out=out.ap, in_=in_.ap, identity=ident.ap), r=in_.res + ident.res, w=out.res)


K.mmv = _mmv
K.trv = _trv


def declare_io(k, final_out=True):
    L = DEPTH
    k.I("x", [4096, 1024]); k.I("meta", [16, 1024]); k.I("ln0_g", [1, 1024]); k.I("ln0_b", [1, 1024])
    k.I("w_in", [L, 1024, 2816]); k.I("swa_sinks", [L, 4])
    k.I("diff_l", [L, 4, 32]); k.I("diff_subln_g", [L, 64])
    k.I("rwkv_mu", [L, 1024]); k.I("rwkv_w0", [L, 256]); k.I("rwkv_w2", [L, 64, 256]); k.I("rwkv_a0", [L, 256])
    k.I("rwkv_a2", [L, 64, 256]); k.I("rwkv_g2", [L, 128, 256]); k.I("rwkv_kk", [L, 256]); k.I("rwkv_ka", [L, 256])
    k.I("rwkv_rk", [L, 256]); k.I("rwkv_lnx_g", [L, 256]); k.I("rwkv_lnx_b", [L, 256])
    k.I("conv_wT", [L, 256, 31]); k.I("conv_b", [L, 256, 1]); k.I("conv_gn_g", [L, 256, 1]); k.I("conv_gn_b", [L, 256, 1])
    k.I("w_out", [L, 1024, 1024]); k.I("ln1_g", [L, 1024]); k.I("ln1_b", [L, 1024])
    k.I("router_w", [1024, 16]); k.I("router_b", [1, 16])
    k.I("exp_w1", [L, 16, 1024, 512]); k.I("exp_w3", [L, 16, 1024, 512]); k.I("exp_w2", [L, 16, 512, 1024])
    k.I("ln2_g", [L, 1024]); k.I("ln2_b", [L, 1024])
    k.I("ident_bf", [128, 128], BF16); k.I("ident_f", [128, 128], F32)
    k.I("ba_meta", [3, 4, 16, 128], BF16); k.I("ba_pc", [4, 128, 256], BF16)
    k.I("bb_pc", [4, 128, 256], F32); k.I("b31", [1, 8], F32)
    k.I("gmat", [128, 128], F32)
    k.I("tri", [3, 64, 64], F32)
    k.I("ctri", [128, 128], F32); k.I("cblk", [128, 128], F32); k.I("cones", [128, 2, 64], F32); k.I("identrep", [64, 256], F32)
    k.I("rmask", [64, 5, 64], F32)
    k.D("H", [NPOS, 1024], F32); k.D("HM", [NPOS, 1024], F32)
    k.D("QKA", [384, NPOS], BF16); k.D("QKB", [512, NPOS], BF16); k.D("CV", [512, NPOS], F32)
    k.D("VA", [NPOS, 128], BF16); k.D("VB", [NPOS, 256], BF16); k.D("UC", [NPOS + 1, 1024], F32)
    k.D("MIX", [NPOS, 768], BF16); k.D("MIXD", [256, NPOS], BF16)
    k.D("HT", [1024, NPOS], BF16)
    k.D("RWT", [NPOS, 7, 256], BF16); k.D("BG", [NPOS, 2, 256], F32); k.D("DW", [65, 64, 256], BF16)
    if final_out:
        k.D("out", [4096, 1024], F32, kind="ExternalOutput")


def host_consts(inp):
    c = {}
    c["ident_bf"] = np.eye(128, dtype=ml_dtypes.bfloat16)
    c["ident_f"] = np.eye(128, dtype=np.float32)
    rel = np.asarray(inp["rel_bias"], np.float32)
    rel_a, rel_b = rel[:, :4], rel[:, 4:]
    ki = np.arange(128)[:, None]
    qi = np.arange(128)[None, :]
    bam = np.full((3, 4, 16, 128), NEG, np.float32)
    m = np.arange(16)[:, None]
    dq = np.arange(128)[None, :] - m
    vis = (dq >= 0) & (np.arange(128)[None, :] < 16)
    g = rel_a[t5_bucket(dq)]
    bam[0] = np.where(vis[None], np.moveaxis(g, -1, 0), NEG)
    dq = (16 + np.arange(128))[None, :] - m
    bam[1] = np.moveaxis(rel_a[t5_bucket(dq)], -1, 0)
    bam[2] = np.broadcast_to(rel_a[31][:, None, None], (4, 16, 128))
    c["ba_meta"] = bam.astype(ml_dtypes.bfloat16)
    dq_prev = qi - ki + 128
    dq_cur = qi - ki
    bp = np.where((ki > qi)[None], np.moveaxis(rel_a[t5_bucket(dq_prev)], -1, 0), NEG)
    bc = np.where((ki <= qi)[None], np.moveaxis(rel_a[t5_bucket(dq_cur)], -1, 0), NEG)
    c["ba_pc"] = np.concatenate([bp, bc], axis=2).astype(ml_dtypes.bfloat16)
    bcd = np.where((ki <= qi)[None], np.moveaxis(rel_b[t5_bucket(dq_cur)], -1, 0), NEG)
    bpd = np.moveaxis(rel_b[t5_bucket(dq_prev)], -1, 0)
    c["bb_pc"] = np.ascontiguousarray(np.concatenate([bcd, bpd], axis=2).astype(np.float32))
    c["b31"] = np.ascontiguousarray(rel[31][None, :])
    gm = np.zeros((128, 128), np.float32)
    gm[:64, :64] = 1.0 / 64
    gm[64:, 64:] = 1.0 / 64
    c["gmat"] = gm
    s = np.arange(64)[:, None]
    t = np.arange(64)[None, :]
    c["tri"] = np.stack([(s <= t), (s < t), (s >= t)]).astype(np.float32)
    s2 = np.arange(128)[:, None]; t2 = np.arange(128)[None, :]
    same = (s2 // 64) == (t2 // 64)
    c["ctri"] = (same & (s2 <= t2)).astype(np.float32)
    c["cblk"] = same.astype(np.float32)
    c["cones"] = np.ascontiguousarray(np.stack([(np.arange(128) // 64 == cc)[:, None] * np.ones((1, 64)) for cc in range(2)], axis=1).astype(np.float32))
    c["identrep"] = np.tile(np.eye(64, dtype=np.float32), (1, 4))
    rm = np.stack([(s < t), (s <= t), (s < t), -1.0 * (s <= t), (s > t)], axis=1).astype(np.float32)
    c["rmask"] = np.ascontiguousarray(rm)
    return c


def host_inputs(inp, b):
    f = lambda a: np.ascontiguousarray(np.asarray(a, np.float32))
    L = DEPTH
    m = {
        "x": f(inp["x"][b]), "meta": f(inp["meta"]), "ln0_g": f(inp["ln0_g"])[None], "ln0_b": f(inp["ln0_b"])[None],
        "w_in": f(inp["w_in"]), "swa_sinks": f(inp["swa_sinks"]),
        "diff_l": f(np.stack([inp["diff_lq1"], inp["diff_lk1"], inp["diff_lq2"], inp["diff_lk2"]], axis=1)),
        "diff_subln_g": f(inp["diff_subln_g"]),
        "rwkv_mu": f(inp["rwkv_mu"]), "rwkv_w0": f(inp["rwkv_w0"]), "rwkv_w2": f(inp["rwkv_w2"]), "rwkv_a0": f(inp["rwkv_a0"]),
        "rwkv_a2": f(inp["rwkv_a2"]), "rwkv_g2": f(inp["rwkv_g2"]), "rwkv_kk": f(inp["rwkv_kk"]), "rwkv_ka": f(inp["rwkv_ka"]),
        "rwkv_rk": f(np.asarray(inp["rwkv_rk"]).reshape(L, 256)), "rwkv_lnx_g": f(inp["rwkv_lnx_g"]), "rwkv_lnx_b": f(inp["rwkv_lnx_b"]),
        "conv_wT": f(np.transpose(np.asarray(inp["conv_w"]), (0, 2, 1))), "conv_b": f(inp["conv_b"])[..., None],
        "conv_gn_g": f(inp["conv_gn_g"])[..., None], "conv_gn_b": f(inp["conv_gn_b"])[..., None],
        "w_out": f(inp["w_out"]), "ln1_g": f(inp["ln1_g"]), "ln1_b": f(inp["ln1_b"]),
        "router_w": f(inp["router_w"]), "router_b": f(inp["router_b"])[None],
        "exp_w1": f(inp["exp_w1"]), "exp_w3": f(inp["exp_w3"]), "exp_w2": f(inp["exp_w2"]),
        "ln2_g": f(inp["ln2_g"]), "ln2_b": f(inp["ln2_b"]),
    }
    return m


def layernorm_tile(k, z, y, n, key, gb, bb, eps=1e-5, tmp=None, gbres=("gb", "bb")):
    stt, mv, rs = tmp
    for c in range(2):
        k.op("dve", "bn_stats", out=stt[0:n, c, :], in_=z[0:n, c * 512:(c + 1) * 512], r=[("z", key)], w=[("st", key, c)])
    k.op("dve", "bn_aggr", out=mv[0:n, :], in_=stt[0:n].rearrange("p a b -> p (a b)"), r=[("st", key, 0), ("st", key, 1)], w=[("mv", key)])
    k.op("act", "activation", out=rs[0:n, 0:1], in_=mv[0:n, 1:2], func=AF.Sqrt, bias=eps, scale=1.0, r=[("mv", key)], w=[("rs", key, 0)])
    k.op("dve", "reciprocal", out=rs[0:n, 0:1], in_=rs[0:n, 0:1], r=[("rs", key, 0)], w=[("rs", key, 0)])
    k.op("dve", "scalar_tensor_tensor", out=rs[0:n, 1:2], in0=mv[0:n, 0:1], scalar=-1.0, in1=rs[0:n, 0:1], op0=ALU.mult, op1=ALU.mult, r=[("mv", key), ("rs", key, 0)], w=[("rs", key, 1)])
    k.op("act", "activation", out=y[0:n, :], in_=z[0:n, :], func=AF.Identity, bias=rs[0:n, 1:2], scale=rs[0:n, 0:1], r=[("z", key), ("rs", key, 0), ("rs", key, 1)], w=[("y", key)])
    k.op("pool", "tensor_tensor", out=y[0:n, :], in0=y[0:n, :], in1=gb[0:n, :], op=ALU.mult, r=[("y", key), gbres[0]], w=[("y", key)])
    k.op("pool", "tensor_tensor", out=y[0:n, :], in0=y[0:n, :], in1=bb[0:n, :], op=ALU.add, r=[("y", key), gbres[1]], w=[("y", key)])


def ln_tmp(k, nm):
    A = k.A
    return (A.t([128, 2, 6], F32, "st" + nm), A.t([128, 2], F32, "mv" + nm), A.t([128, 2], F32, "rs" + nm))


def load_bcast(k, dst, src_row, res, q="sp"):
    k.dma(q, dst[:], src_row.partition_broadcast(128), w=[res])


def stage_ln0(k):
    S, A, d = k.S, k.A, k.dr
    A.mark()
    gb = A.t([128, 1024], F32, "gb"); bb = A.t([128, 1024], F32, "bb")
    load_bcast(k, gb, d["ln0_g"], "gb"); load_bcast(k, bb, d["ln0_b"], "bb")
    zs = [A.t([128, 1024], F32, f"z{i}") for i in range(3)]
    ys = [A.t([128, 1024], F32, f"y{i}") for i in range(3)]
    tmps = [ln_tmp(k, str(i)) for i in range(3)]
    for t in range(NT):
        p0, n = tile_rows(t)
        b = t % 3
        z, y = zs[b], ys[b]
        src = d["meta"] if t == 0 else d["x"][128 * (t - 1):128 * t, :]
        k.dma("sp", z[0:n, :], src, w=[("z", b)])
        layernorm_tile(k, z, y, n, b, gb, bb, tmp=tmps[b])
        k.dma("act", d["H"][p0:p0 + n, :], y[0:n, :], r=[("y", b)], w=[("H", t)])
    S.barrier()
    A.release()


FM_TILES = [
    ("QKA", 0, 0, 0.125), ("QKA", 128, 128, 0.125), ("QKA", 256, 256, 1.0),
    ("QKB", 0, 512, 32 ** -0.5), ("QKB", 128, 640, 32 ** -0.5), ("QKB", 256, 768, 1.0), ("QKB", 384, 896, 1.0),
    ("CV", 0, 2304, 1.0), ("CV", 128, 2432, 1.0), ("CV", 256, 2560, 1.0), ("CV", 384, 2688, 1.0),
]


def stage_in(k, l):
    S, A, d = k.S, k.A, k.dr
    A.mark()
    idt = A.t([128, 128], BF16, "ident")
    k.dma("sp", idt[:], d["ident_bf"], w=["ident"])
    wsb = A.t([128, 8, 2816], BF16, "w_in")
    for kk in range(8):
        for c in range(2):
            k.dma("pool", wsb[:, kk, c * 1408:(c + 1) * 1408], d["w_in"][l, kk * 128:(kk + 1) * 128, c * 1408:(c + 1) * 1408], w=[("w_in", kk, c)])
    wres = [("w_in", kk, c) for kk in range(8) for c in range(2)]
    zs = [A.t([128, 1024], F32, f"z{i}") for i in range(2)]
    hb = [A.t([128, 1024], BF16, f"hb{i}") for i in range(2)]
    hTg = [A.t([128, 8, 512], BF16, f"hT{i}") for i in range(2)]
    ofm_b = [A.t([128, 512], BF16, f"ofb{i}") for i in range(3)]
    ofm_f = [A.t([128, 512], F32, f"off{i}") for i in range(2)]
    otm_v = [A.t([128, 384], BF16, f"otv{i}") for i in range(2)]
    otm_u = [A.t([128, 1024], F32, f"otu{i}") for i in range(2)]
    tcount = 0
    fmc = 0
    tmc = 0
    for gi, grp in enumerate(GROUPS):
        gb_ = gi % 2
        hT = hTg[gb_]
        ntok = sum(tile_rows(t)[1] for t in grp)
        gp0 = tile_rows(grp[0])[0]
        for ti, t in enumerate(grp):
            p0, n = tile_rows(t)
            b = tcount % 2
            tcount += 1
            z = zs[b]
            k.dma("sp", z[0:n, :], d["H"][p0:p0 + n, :], r=[("H", t)], w=[("z", b)])
            k.op("act", "copy", out=hb[b][0:n, :], in_=z[0:n, :], r=[("z", b)], w=[("hb", b)])
            pt = k.ps(b)[:].bitcast(BF16)
            for kk in range(8):
                k.tr(pt[:, kk * 128:kk * 128 + n], hb[b][0:n, kk * 128:(kk + 1) * 128], idt[0:n, 0:n], r=[("hb", b), "ident"], w=[("ps", b)])
            k.op("dve", "tensor_copy", out=hT[:, :, ti * 128:ti * 128 + n], in_=pt.rearrange("p (k t) -> p k t", k=8)[:, :, 0:n], r=[("ps", b)], w=[("hT", gb_, ti)])
        hres = [("hT", gb_, ti) for ti in range(len(grp))]
        for (dn, r0, c0, sc) in FM_TILES:
            pb = 2 + fmc % 3
            isf = dn == "CV"
            ob = ofm_f[fmc % 2] if isf else ofm_b[fmc % 3]
            ores = ("off", fmc % 2) if isf else ("ofb", fmc % 3)
            for kk in range(8):
                k.mm(k.ps(pb)[:, 0:ntok], wsb[:, kk, c0:c0 + 128], hT[:, kk, 0:ntok], kk == 0, kk == 7, r=hres + wres, w=[("ps", pb)])
            if fmc % 2 == 0:
                k.op("act", "activation", out=ob[:, 0:ntok], in_=k.ps(pb)[:, 0:ntok], func=AF.Copy, scale=sc, r=[("ps", pb)], w=[ores])
            else:
                k.op("dve", "tensor_scalar", out=ob[:, 0:ntok], in0=k.ps(pb)[:, 0:ntok], scalar1=sc, scalar2=None, op0=ALU.mult, r=[("ps", pb)], w=[ores])
            k.dma("sp", d[dn][r0:r0 + 128, gp0:gp0 + ntok], ob[:, 0:ntok], r=[ores], w=[(dn, r0, gi)])
            fmc += 1
        for ti, t in enumerate(grp):
            p0, n = tile_rows(t)
            ov = otm_v[tmc % 2]
            ou = otm_u[tmc % 2]
            tb = tmc % 2
            tmc += 1
            lt = hT[:, :, ti * 128:ti * 128 + n]
            for kk in range(8):
                k.mm(k.ps(5)[0:n, 0:128], lt[:, kk, :], wsb[:, kk, 384:512], kk == 0, kk == 7, r=hres + wres, w=[("ps", 5, 0)])
            for kk in range(8):
                k.mm(k.ps(5)[0:n, 128:384], lt[:, kk, :], wsb[:, kk, 1024:1280], kk == 0, kk == 7, r=hres + wres, w=[("ps", 5, 1)])
            k.op("act", "copy", out=ov[0:n, :], in_=k.ps(5)[0:n, 0:384], r=[("ps", 5, 0), ("ps", 5, 1)], w=[("otv", tb)])
            k.dma("act", d["VA"][p0:p0 + n, :], ov[0:n, 0:128], r=[("otv", tb)], w=[("VA", t)])
            k.dma("act", d["VB"][p0:p0 + n, :], ov[0:n, 128:384], r=[("otv", tb)], w=[("VB", t)])
            for c in range(2):
                pb = 6 + c
                for kk in range(8):
                    k.mm(k.ps(pb)[0:n, :], lt[:, kk, :], wsb[:, kk, 1280 + c * 512:1280 + (c + 1) * 512], kk == 0, kk == 7, r=hres + wres, w=[("ps", pb)])
                k.op("dve", "tensor_copy", out=ou[0:n, c * 512:(c + 1) * 512], in_=k.ps(pb)[0:n, :], r=[("ps", pb)], w=[("otu", tb, c)])
            k.dma("sp", d["UC"][1 + p0:1 + p0 + n, :], ou[0:n, :], r=[("otu", tb, 0), ("otu", tb, 1)], w=[("UC", t)])
    S.barrier()
    A.release()


def stage_swa(k, l):
    S, A, d = k.S, k.A, k.dr
    A.mark()
    idt = A.t([128, 128], BF16, "ident")
    k.dma("sp", idt[:], d["ident_bf"], w=["ident"])
    qT = A.t([64, 4, NPOS], BF16, "qTa")
    kT = A.t([64, 2, NPOS], BF16, "kTa")
    for h in range(4):
        k.dma("sp", qT[:, h, :], d["QKA"][64 * h:64 * h + 64, :], w=[("qT", h)])
    for kv in range(2):
        k.dma("sp", kT[:, kv, :], d["QKA"][256 + 64 * kv:256 + 64 * kv + 64, :], w=[("kT", kv)])
    va = A.t([128, NT, 2, 65], BF16, "va")
    k.op("pool", "memset", ap=va[:, :, :, 64:65], constant=1.0, w=["va_ones"])
    for t in range(NT):
        p0, n = tile_rows(t)
        k.dma("act", va[0:n, t, :, 0:64], d["VA"][p0:p0 + n, :].rearrange("p (h e) -> p h e", h=2), w=[("va", t)])
    bam = A.t([16, 3, 4, 128], BF16, "bam")
    k.dma("sp", bam[:], d["ba_meta"].rearrange("c h m q -> m c h q"), w=["bam"])
    bapc = A.t([128, 4, 256], BF16, "bapc")
    k.dma("sp", bapc[:], d["ba_pc"].rearrange("h k q -> k h q"), w=["bapc"])
    sk = A.t([128, 4, 1], F32, "sk")
    esk = A.t([128, 4, 1], F32, "esk")
    k.dma("sp", sk[:].rearrange("p h o -> p (h o)"), d["swa_sinks"][l:l + 1, :].partition_broadcast(128), w=["sk"])
    k.op("act", "activation", out=esk[:], in_=sk[:], func=AF.Exp, r=["sk"], w=["esk"])
    pms = [A.t([16, 4, 128], BF16, f"pm{i}") for i in range(2)]
    pps = [A.t([128, 2, 512], BF16, f"pp{i}") for i in range(2)]
    dens = [A.t([128, 4, 1], F32, f"den{i}") for i in range(2)]
    yos = [A.t([128, 4, 64], BF16, f"yo{i}") for i in range(2)]
    for t in range(NT):
        p0, n = tile_rows(t)
        s = t % 2
        psA, psB, psD = k.ps(4 * s), [k.ps(4 * s + 1), k.ps(4 * s + 2)], k.ps(4 * s + 3)
        pm, pp, den, yo = pms[s], pps[s], dens[s], yos[s]
        case = min(t, 2)
        psAv = psA[0:16, :].rearrange("p (h q) -> p h q", h=4)
        for h in range(4):
            kv = h // 2
            hp, cb = h // 2, (h % 2) * 256
            k.mm(psAv[:, h, 0:n], kT[:, kv, 0:16], qT[:, h, p0:p0 + n], True, False, r=[("kT", kv), ("qT", h)], w=[("psA", s)])
            k.mm(psAv[:, h, 0:n], idt[0:16, 0:16], bam[:, case, h, 0:n], False, True, r=["ident", "bam"], w=[("psA", s)])
            if t >= 2:
                pp0 = p0 - 128
                k.mm(psB[hp][:, cb:cb + n], kT[:, kv, pp0:pp0 + 128], qT[:, h, p0:p0 + n], True, False, r=[("kT", kv), ("qT", h)], w=[("psB", s, hp)])
                k.mm(psB[hp][:, cb:cb + n], idt[:, :], bapc[:, h, 0:n], False, True, r=["ident", "bapc"], w=[("psB", s, hp)])
            if t >= 1:
                k.mm(psB[hp][:, cb + 128:cb + 128 + n], kT[:, kv, p0:p0 + 128], qT[:, h, p0:p0 + n], True, False, r=[("kT", kv), ("qT", h)], w=[("psB", s, hp)])
                k.mm(psB[hp][:, cb + 128:cb + 128 + n], idt[:, :], bapc[:, h, 128:128 + n], False, True, r=["ident", "bapc"], w=[("psB", s, hp)])
        k.op("act", "activation", out=pm[:, :, 0:n], in_=psAv[:, :, 0:n], func=AF.Exp, r=[("psA", s)], w=[("pm", s)])
        for hp in range(2):
            if t >= 2:
                k.op("act", "activation", out=pp[:, hp, :], in_=psB[hp][:, :], func=AF.Exp, r=[("psB", s, hp)], w=[("pp", s, hp)])
            elif t == 1:
                k.op("act", "activation", out=pp[:, hp, :].rearrange("p (h x) -> p h x", h=2)[:, :, 128:256],
                     in_=psB[hp][:, :].rearrange("p (h x) -> p h x", h=2)[:, :, 128:256], func=AF.Exp, r=[("psB", s, hp)], w=[("pp", s, hp)])
        psDv = psD[:, 0:260].rearrange("p (h e) -> p h e", h=4)
        for h in range(4):
            kv = h // 2
            hp, cb = h // 2, (h % 2) * 256
            k.mm(psDv[0:n, h, :], pm[0:16, h, 0:n], va[0:16, 0, kv, :], True, t == 0, r=[("pm", s), ("va", 0), "va_ones"], w=[("psD", s)])
            if t >= 2:
                k.mm(psDv[0:n, h, :], pp[:, hp, cb:cb + n], va[:, t - 1, kv, :], False, False, r=[("pp", s, hp), ("va", t - 1), "va_ones"], w=[("psD", s)])
            if t >= 1:
                k.mm(psDv[0:n, h, :], pp[:, hp, cb + 128:cb + 128 + n], va[:, t, kv, :], False, True, r=[("pp", s, hp), ("va", t), "va_ones"], w=[("psD", s)])
        k.op("dve", "tensor_tensor", out=den[0:n], in0=psDv[0:n, :, 64:65], in1=esk[0:n], op=ALU.add, r=[("psD", s), "esk"], w=[("den", s)])
        k.op("dve", "reciprocal", out=den[0:n], in_=den[0:n], r=[("den", s)], w=[("den", s)])
        k.op("dve", "tensor_tensor", out=yo[0:n], in0=psDv[0:n, :, 0:64], in1=den[0:n].to_broadcast([n, 4, 64]), op=ALU.mult, r=[("psD", s), ("den", s)], w=[("yo", s)])
        k.dma("sp", d["MIX"][p0:p0 + n, 0:256], yo[0:n].rearrange("p h e -> p (h e)"), r=[("yo", s)], w=[("MIXa", t)])
    S.barrier()
    A.release()


def stage_diff(k, l):
    S, A, d = k.S, k.A, k.dr
    A.mark()
    lam_init = 0.8 - 0.6 * math.exp(-0.3 * l)
    idt = A.t([128, 128], BF16, "ident")
    k.dma("sp", idt[:], d["ident_bf"], w=["ident"])
    qT = A.t([32, 8, NPOS], BF16, "qTb")
    kT = A.t([32, 8, NPOS], BF16, "kTb")
    for sl in range(8):
        k.dma("sp", qT[:, sl, :], d["QKB"][32 * sl:32 * sl + 32, :], w=[("qT", sl)])
        k.dma("sp", kT[:, sl, :], d["QKB"][256 + 32 * sl:256 + 32 * sl + 32, :], w=[("kT", sl)])
    vb = A.t([128, NT, 4, 65], BF16, "vb")
    k.op("pool", "memset", ap=vb[:, :, :, 64:65], constant=1.0, w=["vb_ones"])
    for t in range(NT):
        p0, n = tile_rows(t)
        k.dma("act", vb[0:n, t, :, 0:64], d["VB"][p0:p0 + n, :].rearrange("p (h e) -> p h e", h=4), w=[("vb", t)])
    bbf = A.t([128, 4, 256], F32, "bbf")
    k.dma("sp", bbf[:], d["bb_pc"].rearrange("h k q -> k h q"), w=["bbf"])
    b31b = A.t([128, 8], F32, "b31b")
    k.dma("sp", b31b[:], d["b31"].partition_broadcast(128), w=["b31b"])
    bbt = A.t([128, 4, 256], BF16, "bbt")
    for h in range(4):
        k.op("dve", "tensor_scalar", out=bbt[:, h, :], in0=bbf[:, h, :], scalar1=b31b[:, 4 + h:5 + h], scalar2=None, op0=ALU.subtract, r=["bbf", "b31b"], w=["bbt"])
    dl = A.t([128, 4, 32], F32, "dl")
    k.dma("sp", dl[:].rearrange("p a b -> p (a b)"), d["diff_l"][l:l + 1].rearrange("o a b -> o (a b)").partition_broadcast(128), w=["dl"])
    pr = A.t([128, 2, 32], F32, "pr")
    ss = A.t([128, 2], F32, "ss")
    nlam = A.t([128, 1], F32, "nlam")
    k.op("dve", "tensor_tensor", out=pr[:, 0, :], in0=dl[:, 0, :], in1=dl[:, 1, :], op=ALU.mult, r=["dl"], w=["pr0"])
    k.op("dve", "tensor_tensor", out=pr[:, 1, :], in0=dl[:, 2, :], in1=dl[:, 3, :], op=ALU.mult, r=["dl"], w=["pr1"])
    k.op("dve", "tensor_reduce", out=ss[:], in_=pr[:], axis=AX.X, op=ALU.add, r=["pr0", "pr1"], w=["ss"])
    k.op("act", "activation", out=ss[:], in_=ss[:], func=AF.Exp, r=["ss"], w=["ss"])
    k.op("dve", "tensor_tensor", out=nlam[:], in0=ss[:, 1:2], in1=ss[:, 0:1], op=ALU.subtract, r=["ss"], w=["nlam"])
    k.op("dve", "tensor_scalar", out=nlam[:], in0=nlam[:], scalar1=-lam_init, scalar2=None, op0=ALU.add, r=["nlam"], w=["nlam"])
    gvec = A.t([128, 1, 64], F32, "gvec")
    k.dma("sp", gvec[:].rearrange("p o e -> p (o e)"), d["diff_subln_g"][l:l + 1, :].partition_broadcast(128), w=["gvec"])
    k.op("act", "mul", out=gvec[:], in_=gvec[:], mul=(1.0 - lam_init), r=["gvec"], w=["gvec"])
    PTs = [A.t([128, 512], BF16, f"PT{i}") for i in range(3)]
    rr = [A.t([128, 2, 4, 1], F32, f"rr{i}") for i in range(2)]
    t1 = [A.t([128, 4, 64], F32, f"t1{i}") for i in range(2)]
    t2 = [A.t([128, 4, 64], F32, f"t2{i}") for i in range(2)]
    ms = [A.t([128, 4, 1], F32, f"ms{i}") for i in range(2)]
    ybo = [A.t([128, 4, 4, 64], BF16, f"ybo{i}") for i in range(2)]
    sc = 0
    pending = []

    def flush():
        while pending:
            pending.pop(0)()

    for qg, tiles in enumerate(GROUPS):
        ntok = sum(tile_rows(t)[1] for t in tiles)
        gp0 = tile_rows(tiles[0])[0]
        nt = len(tiles)
        nq = tile_rows(tiles[0])[1]
        yb_ = ybo[qg % 2]
        for h in range(4):
            hb = h % 2
            Ob = [k.ps(4 + 2 * hb), k.ps(5 + 2 * hb)]
            Ov = [Ob[c][:, 0:65 * nt].rearrange("p (t e) -> p t e", e=65) for c in range(2)]
            for c in range(2):
                sl = 2 * h + c
                for j in range(0, tiles[-1] + 1):
                    kp0, nk = tile_rows(j)
                    fi = max(0, j - tiles[0])
                    col0 = fi * 128
                    sb = sc % 4
                    pt = PTs[sc % 3]
                    ptk = sc % 3
                    sc += 1
                    psb = k.ps(sb)
                    bl = [i for i in (j, j + 1) if i in tiles]
                    c1 = col0 + 128 * len(bl) if tiles[0] != 0 else (nq if bl else 0)
                    c1 = min(c1, ntok)
                    if bl:
                        k.mm(psb[0:nk, col0:c1], kT[:, sl, kp0:kp0 + nk], qT[:, sl, gp0 + col0:gp0 + c1], True, False, r=[("kT", sl), ("qT", sl)], w=[("psS", sb)])
                        if j == 0:
                            if tiles[0] == 0:
                                k.mm(psb[0:16, 0:16], idt[0:16, 0:16], bbt[0:16, h, 0:16], False, True, r=["ident", "bbt"], w=[("psS", sb)])
                            else:
                                k.mm(psb[0:16, 0:128], idt[:, 112:128], bbt[:, h, 128:256], False, True, r=["ident", "bbt"], w=[("psS", sb)])
                        else:
                            b0 = 0 if bl[0] == j else 128
                            k.mm(psb[:, col0:c1], idt[:, :], bbt[:, h, b0:b0 + (c1 - col0)], False, True, r=["ident", "bbt"], w=[("psS", sb)])
                    else:
                        c1 = col0
                    if c1 < ntok:
                        k.mm(psb[0:nk, c1:ntok], kT[:, sl, kp0:kp0 + nk], qT[:, sl, gp0 + c1:gp0 + ntok], True, True, r=[("kT", sl), ("qT", sl)], w=[("psS", sb)])
                    k.op("act", "activation", out=pt[0:nk, col0:ntok], in_=psb[0:nk, col0:ntok], func=AF.Exp, bias=b31b[0:nk, 4 + h:5 + h], scale=1.0,
                         r=[("psS", sb), "b31b"], w=[("PT", ptk)])

                    def pv(tiles=tiles, j=j, c=c, h=h, hb=hb, nk=nk, pt=pt, ptk=ptk, Ov=Ov):
                        for il, i in enumerate(tiles):
                            if i < j:
                                continue
                            ni = tile_rows(i)[1]
                            k.mm(Ov[c][0:ni, il, :], pt[0:nk, il * 128:il * 128 + ni], vb[0:nk, j, h, :], (j == 0 and il == 0), False, r=[("PT", ptk), ("vb", j), "vb_ones"], w=[("psO", hb, c)], skip=True)
                    flush()
                    pending.append(pv)

            def epilogue(qg=qg, h=h, hb=hb, nt=nt, nq=nq, Ov=Ov, yb_=yb_):
                n = nq
                e = hb
                for c in range(2):
                    k.op("dve", "reciprocal", out=rr[e][0:n, c, 0:nt, :], in_=Ov[c][0:n, :, 64:65], r=[("psO", hb, c)], w=[("rr", e, c)])
                k.op("dve", "tensor_tensor", out=t1[e][0:n, 0:nt, :], in0=Ov[0][0:n, :, 0:64], in1=rr[e][0:n, 0, 0:nt, :].to_broadcast([n, nt, 64]), op=ALU.mult, r=[("psO", hb, 0), ("rr", e, 0)], w=[("t1", e)])
                k.op("dve", "tensor_tensor", out=t2[e][0:n, 0:nt, :], in0=Ov[1][0:n, :, 0:64], in1=rr[e][0:n, 1, 0:nt, :].to_broadcast([n, nt, 64]), op=ALU.mult, r=[("psO", hb, 1), ("rr", e, 1)], w=[("t2", e)])
                k.op("dve", "scalar_tensor_tensor", out=t1[e][0:n, 0:nt, :], in0=t2[e][0:n, 0:nt, :], scalar=nlam[0:n, 0:1], in1=t1[e][0:n, 0:nt, :], op0=ALU.mult, op1=ALU.add, r=[("t1", e), ("t2", e), "nlam"], w=[("t1", e)])
                k.op("pool", "tensor_tensor", out=t2[e][0:n, 0:nt, :], in0=t1[e][0:n, 0:nt, :], in1=t1[e][0:n, 0:nt, :], op=ALU.mult, r=[("t1", e)], w=[("t2", e)])
                k.op("dve", "tensor_reduce", out=ms[e][0:n, 0:nt, :], in_=t2[e][0:n, 0:nt, :], axis=AX.X, op=ALU.add, r=[("t2", e)], w=[("ms", e)])
                k.op("act", "activation", out=ms[e][0:n, 0:nt, :], in_=ms[e][0:n, 0:nt, :], func=AF.Sqrt, bias=1e-5, scale=1.0 / 64, r=[("ms", e)], w=[("ms", e)])
                k.op("dve", "reciprocal", out=ms[e][0:n, 0:nt, :], in_=ms[e][0:n, 0:nt, :], r=[("ms", e)], w=[("ms", e)])
                k.op("dve", "tensor_tensor", out=t1[e][0:n, 0:nt, :], in0=t1[e][0:n, 0:nt, :], in1=ms[e][0:n, 0:nt, :].to_broadcast([n, nt, 64]), op=ALU.mult, r=[("t1", e), ("ms", e)], w=[("t1", e)])
                k.op("pool", "tensor_tensor", out=yb_[0:n, 0:nt, h, :], in0=t1[e][0:n, 0:nt, :], in1=gvec[0:n].to_broadcast([n, nt, 64]), op=ALU.mult, r=[("t1", e), "gvec"], w=[("ybo", qg % 2, h)])
            pending.append(epilogue)

        def store(qg=qg, gp0=gp0, ntok=ntok, nt=nt, nq=nq, yb_=yb_):
            dst = d["MIX"][gp0:gp0 + ntok, 256:512]
            if nt > 1:
                dst = dst.rearrange("(t p) c -> p t c", p=128)
                src = yb_[:, 0:nt].rearrange("p t h e -> p t (h e)")
            else:
                src = yb_[0:nq, 0].rearrange("p h e -> p (h e)")
            k.dma("sp", dst, src, r=[("ybo", qg % 2, h) for h in range(4)], w=[("MIXb", qg)])
        pending.append(store)
    flush()
    S.barrier()
    A.release()


def stage_conv(k, l, lvl=9):
    S, A, d = k.S, k.A, k.dr
    A.mark()
    gmat = A.t([128, 128], F32, "gmat")
    k.dma("sp", gmat[:], d["gmat"], w=["gmat"])
    a = A.t([128, NPOS], F32, "cva")
    gate = A.t([128, NPOS], F32, "cvg")
    hg = A.t([128, 30 + NPOS], F32, "hg")
    acc1 = A.t([128, NPOS], F32, "acc1")
    acc2 = A.t([128, NPOS], F32, "acc2")
    cw = A.t([128, 31], F32, "cw")
    ctmp = [A.t([128, NPOS], F32, f"ctmp{i}") for i in range(2)]
    cp = A.t([128, 3], F32, "cp")
    sqb = [A.t([128, 512], F32, f"sqb{i}") for i in range(2)]
    ddb = [A.t([128, 512], F32, f"ddb{i}") for i in range(2)]
    m2b = [A.t([128, 512], F32, f"m2b{i}") for i in range(2)]
    ob = [A.t([128, 512], BF16, f"cob{i}") for i in range(2)]
    k.op("pool", "memset", ap=hg[:, 0:30], constant=0.0, w=["hgz"])
    cc = 0
    for ct in range(2):
        r0 = ct * 128
        k.dma("sp", a[:], d["CV"][r0:r0 + 128, :], w=["cva"])
        k.dma("sp", gate[:], d["CV"][256 + r0:256 + r0 + 128, :], w=["cvg"])
        k.dma("act", cw[:], d["conv_wT"][l, r0:r0 + 128, :], w=["cw"])
        k.dma("act", cp[:, 0:1], d["conv_b"][l, r0:r0 + 128, :], w=["cp0"])
        k.dma("act", cp[:, 1:2], d["conv_gn_g"][l, r0:r0 + 128, :], w=["cp1"])
        k.dma("act", cp[:, 2:3], d["conv_gn_b"][l, r0:r0 + 128, :], w=["cp2"])
        H2 = NPOS // 2
        for hh in range(2):
            k.op("act", "activation", out=gate[:, hh * H2:(hh + 1) * H2], in_=gate[:, hh * H2:(hh + 1) * H2], func=AF.Sigmoid, r=["cvg"], w=[("sig", hh)])
            k.op("pool", "tensor_tensor", out=hg[:, 30 + hh * H2:30 + (hh + 1) * H2], in0=a[:, hh * H2:(hh + 1) * H2], in1=gate[:, hh * H2:(hh + 1) * H2], op=ALU.mult,
                 r=["cva", ("sig", hh)], w=[("hg", hh)])
        hres = ["hgz", ("hg", 0), ("hg", 1)]
        if lvl < 2:
            k.dma("sp", d["CV"][r0:r0 + 128, :], hg[:, 30:], r=hres, w=["dbg"])
            continue
        k.op("dve", "tensor_scalar", out=acc1[:], in0=hg[:, 0:NPOS], scalar1=cw[:, 0:1], scalar2=cp[:, 0:1], op0=ALU.mult, op1=ALU.add, r=hres + ["cw", "cp0"], w=["acc1"])
        for j in range(1, 20):
            k.op("dve", "scalar_tensor_tensor", out=acc1[:], in0=hg[:, j:j + NPOS], scalar=cw[:, j:j + 1], in1=acc1[:], op0=ALU.mult, op1=ALU.add, r=hres + ["cw", "acc1"], w=["acc1"])
        if lvl < 3:
            k.dma("sp", d["CV"][r0:r0 + 128, :], acc1[:], r=["acc1"], w=["dbg"])
            continue
        k.op("pool", "tensor_scalar", out=acc2[:], in0=hg[:, 20:20 + NPOS], scalar1=cw[:, 20:21], scalar2=None, op0=ALU.mult, r=hres + ["cw"], w=["acc2"])
        for j in range(21, 31):
            tb = j % 2
            k.op("act", "activation", out=ctmp[tb][:], in_=hg[:, j:j + NPOS], func=AF.Identity, scale=cw[:, j:j + 1], r=hres + ["cw"], w=[("ctmp", tb)])
            k.op("pool", "tensor_tensor", out=acc2[:], in0=acc2[:], in1=ctmp[tb][:], op=ALU.add, r=[("ctmp", tb), "acc2"], w=["acc2"])
        k.op("dve", "tensor_tensor", out=acc1[:], in0=acc1[:], in1=acc2[:], op=ALU.add, r=["acc1", "acc2"], w=["acc1"])
        if lvl < 4:
            k.dma("sp", d["CV"][r0:r0 + 128, :], acc1[:], r=["acc1"], w=["dbg"])
            continue
        for c0 in range(0, NPOS, 512):
            n = min(512, NPOS - c0)
            b = cc % 2
            cc += 1
            psM, psE = k.ps(2 * b), k.ps(2 * b + 1)
            k.mm(psM[:, 0:n], gmat[:], acc1[:, c0:c0 + n], True, True, r=["gmat", "acc1"], w=[("psM", b)])
            k.op("dve", "tensor_tensor", out=ddb[b][:, 0:n], in0=acc1[:, c0:c0 + n], in1=psM[:, 0:n], op=ALU.subtract, r=["acc1", ("psM", b)], w=[("ddb", b)])
            k.op("pool", "tensor_tensor", out=sqb[b][:, 0:n], in0=ddb[b][:, 0:n], in1=ddb[b][:, 0:n], op=ALU.mult, r=[("ddb", b)], w=[("sqb", b)])
            k.mm(psE[:, 0:n], gmat[:], sqb[b][:, 0:n], True, True, r=["gmat", ("sqb", b)], w=[("psE", b)])
            if lvl == 4:
                continue
            k.op("act", "activation", out=m2b[b][:, 0:n], in_=psE[:, 0:n], func=AF.Sqrt, bias=1e-5, scale=1.0, r=[("psE", b)], w=[("m2b", b)])
            k.op("dve", "reciprocal", out=m2b[b][:, 0:n], in_=m2b[b][:, 0:n], r=[("m2b", b)], w=[("m2b", b)])
            k.op("pool", "tensor_tensor", out=ddb[b][:, 0:n], in0=ddb[b][:, 0:n], in1=m2b[b][:, 0:n], op=ALU.mult, r=[("ddb", b), ("m2b", b)], w=[("ddb", b)])
            if lvl == 5:
                continue
            k.op("act", "activation", out=ob[b][:, 0:n], in_=ddb[b][:, 0:n], func=AF.Silu, bias=cp[:, 2:3], scale=cp[:, 1:2], r=[("ddb", b), "cp1", "cp2"], w=[("cob", b)])
            k.dma("sp", d["MIXD"][r0:r0 + 128, c0:c0 + n], ob[b][:, 0:n], r=[("cob", b)], w=[("MIXD", ct, c0)])
    S.barrier()
    A.release()


def stage_out(k, l):
    S, A, d = k.S, k.A, k.dr
    A.mark()
    idt = A.t([128, 128], BF16, "ident")
    k.dma("sp", idt[:], d["ident_bf"], w=["ident"])
    wo = A.t([128, 8, 1024], BF16, "wo")
    for kk in range(8):
        k.dma("pool", wo[:, kk, :], d["w_out"][l, kk * 128:(kk + 1) * 128, :], w=[("wo", kk)])
    wres = [("wo", kk) for kk in range(8)]
    gb = A.t([128, 1024], F32, "gb"); bb = A.t([128, 1024], F32, "bb")
    load_bcast(k, gb, d["ln1_g"][l:l + 1, :], "gb"); load_bcast(k, bb, d["ln1_b"][l:l + 1, :], "bb")
    mxs = [A.t([128, 768], BF16, f"mx{i}") for i in range(2)]
    mxds = [A.t([128, 2, 128], BF16, f"mxd{i}") for i in range(2)]
    mTs = [A.t([128, 6, 128], BF16, f"mT{i}") for i in range(2)]
    hs = [A.t([128, 1024], F32, f"h{i}") for i in range(2)]
    zs = [A.t([128, 1024], F32, f"z{i}") for i in range(2)]
    ys = [A.t([128, 1024], F32, f"y{i}") for i in range(2)]
    tmps = [ln_tmp(k, str(i)) for i in range(2)]
    for t in range(NT):
        p0, n = tile_rows(t)
        b = t % 2
        mx, mxd, mT, h, z, y = mxs[b], mxds[b], mTs[b], hs[b], zs[b], ys[b]
        k.dma("sp", mx[0:n, :], d["MIX"][p0:p0 + n, :], w=[("mx", b)])
        for c in range(2):
            k.dma("sp", mxd[:, c, 0:n], d["MIXD"][c * 128:(c + 1) * 128, p0:p0 + n], w=[("mxd", b, c)])
        k.dma("act", h[0:n, :], d["H"][p0:p0 + n, :], w=[("h", b)])
        pt = k.ps(b)[:].bitcast(BF16)
        for kk in range(6):
            k.tr(pt[:, kk * 128:kk * 128 + n], mx[0:n, kk * 128:(kk + 1) * 128], idt[0:n, 0:n], r=[("mx", b), "ident"], w=[("ps", b)])
        k.op("dve", "tensor_copy", out=mT[:, :, 0:n], in_=pt[:, 0:768].rearrange("p (k t) -> p k t", k=6)[:, :, 0:n], r=[("ps", b)], w=[("mT", b)])
        for half in range(2):
            pb = 2 + 2 * b + half
            for kk in range(8):
                lhsT = mT[:, kk, 0:n] if kk < 6 else mxd[:, kk - 6, 0:n]
                k.mm(k.ps(pb)[0:n, :], lhsT, wo[:, kk, half * 512:(half + 1) * 512], kk == 0, kk == 7,
                     r=[("mT", b), ("mxd", b, 0), ("mxd", b, 1)] + wres, w=[("ps", pb)])
            k.op("dve", "scalar_tensor_tensor", out=z[0:n, half * 512:(half + 1) * 512], in0=h[0:n, half * 512:(half + 1) * 512], scalar=ALPHA, in1=k.ps(pb)[0:n, :],
                 op0=ALU.mult, op1=ALU.add, r=[("h", b), ("ps", pb)], w=[("z", b)])
        layernorm_tile(k, z, y, n, b, gb, bb, tmp=tmps[b])
        k.dma("act", d["HM"][p0:p0 + n, :], y[0:n, :], r=[("y", b)], w=[("HM", t)])
    S.barrier()
    A.release()


def stage_moe(k, l, last=False):
    S, A, d = k.S, k.A, k.dr
    A.mark()
    comb = A.t([128, NT, 16], F32, "comb")
    A.mark()
    idf = A.t([128, 128], F32, "identf")
    k.dma("sp", idf[:], d["ident_f"], w=["identf"])
    rw = A.t([128, 8, 16], F32, "rw")
    k.dma("sp", rw[:], d["router_w"].rearrange("(k p) e -> p k e", p=128), w=["rw"])
    rb = A.t([128, 16], F32, "rb")
    load_bcast(k, rb, d["router_b"], "rb")
    hms = [A.t([128, 1024], F32, f"hm{i}") for i in range(2)]
    hT32 = [A.t([128, 8, 128], F32, f"hT32{i}") for i in range(2)]
    hTb = [A.t([128, 8, 128], BF16, f"hTb{i}") for i in range(2)]
    rt = [dict((nm, A.t([128, 16], F32, f"{nm}{i}")) for nm in ("sc", "bi", "eq", "b2", "mk", "s1", "mk2", "s2")) for i in range(2)]
    rs4 = [dict((nm, A.t([128, 4], F32, f"{nm}{i}")) for nm in ("m1", "m2", "gs", "ing")) for i in range(2)]
    rs1 = [dict((nm, A.t([128, 1], F32, f"{nm}{i}")) for nm in ("gm", "t1", "t2", "den")) for i in range(2)]
    for t in range(NT):
        p0, n = tile_rows(t)
        b = t % 2
        hm = hms[b]
        k.dma("sp", hm[0:n, :], d["HM"][p0:p0 + n, :], r=[("HM", t)], w=[("hm", b)])
        for kk in range(8):
            pb = 2 * b + kk // 4
            k.tr(k.ps(pb)[:, (kk % 4) * 128:(kk % 4) * 128 + n], hm[0:n, kk * 128:(kk + 1) * 128], idf[0:n, 0:n], r=[("hm", b), "identf"], w=[("ps", pb)])
        for hf in range(2):
            pb = 2 * b + hf
            src = k.ps(pb)[:].rearrange("p (k t) -> p k t", k=4)[:, :, 0:n]
            k.op("act", "copy", out=hT32[b][:, 4 * hf:4 * hf + 4, 0:n], in_=src, r=[("ps", pb)], w=[("hT32", b, hf)])
            k.op("dve", "tensor_copy", out=hTb[b][:, 4 * hf:4 * hf + 4, 0:n], in_=src, r=[("ps", pb)], w=[("hTb", b, hf)])
        k.dma("act", d["HT"][:, p0:p0 + n].rearrange("(k p) t -> p k t", p=128), hTb[b][:, :, 0:n], r=[("hTb", b, 0), ("hTb", b, 1)], w=[("HT", t)])
        pr = k.ps(4 + b)
        for kk in range(8):
            k.mm(pr[0:n, 0:16], hT32[b][:, kk, 0:n], rw[:, kk, :], kk == 0, kk == 7, r=[("hT32", b, 0), ("hT32", b, 1), "rw"], w=[("psr", b)])
        R_, R4, R1 = rt[b], rs4[b], rs1[b]
        rk = lambda nm: ("rt", nm, b)
        v4 = lambda ap: ap[0:n, :].rearrange("p (g e) -> p g e", g=4)
        k.op("act", "activation", out=R_["sc"][0:n, :], in_=pr[0:n, 0:16], func=AF.Sigmoid, r=[("psr", b)], w=[rk("sc")])
        k.op("dve", "tensor_tensor", out=R_["bi"][0:n, :], in0=R_["sc"][0:n, :], in1=rb[0:n, :], op=ALU.add, r=[rk("sc"), "rb"], w=[rk("bi")])
        k.op("dve", "tensor_reduce", out=R4["m1"][0:n, :], in_=v4(R_["bi"]), axis=AX.X, op=ALU.max, r=[rk("bi")], w=[rk("m1")])
        k.op("dve", "tensor_tensor", out=v4(R_["eq"]), in0=v4(R_["bi"]), in1=R4["m1"][0:n, :].unsqueeze(2).to_broadcast([n, 4, 4]), op=ALU.is_equal, r=[rk("bi"), rk("m1")], w=[rk("eq")])
        k.op("dve", "scalar_tensor_tensor", out=R_["b2"][0:n, :], in0=R_["eq"][0:n, :], scalar=NEG, in1=R_["bi"][0:n, :], op0=ALU.mult, op1=ALU.add, r=[rk("eq"), rk("bi")], w=[rk("b2")])
        k.op("dve", "tensor_reduce", out=R4["m2"][0:n, :], in_=v4(R_["b2"]), axis=AX.X, op=ALU.max, r=[rk("b2")], w=[rk("m2")])
        k.op("dve", "tensor_tensor", out=R4["gs"][0:n, :], in0=R4["m1"][0:n, :], in1=R4["m2"][0:n, :], op=ALU.add, r=[rk("m1"), rk("m2")], w=[rk("gs")])
        k.op("dve", "tensor_reduce", out=R1["gm"][0:n, :], in_=R4["gs"][0:n, :], axis=AX.X, op=ALU.max, r=[rk("gs")], w=[rk("gm")])
        k.op("dve", "tensor_scalar", out=R4["ing"][0:n, :], in0=R4["gs"][0:n, :], scalar1=R1["gm"][0:n, 0:1], scalar2=None, op0=ALU.is_equal, r=[rk("gs"), rk("gm")], w=[rk("ing")])
        k.op("dve", "tensor_scalar", out=R4["ing"][0:n, :], in0=R4["ing"][0:n, :], scalar1=1.0, scalar2=-NEG, op0=ALU.subtract, op1=ALU.mult, r=[rk("ing")], w=[rk("ing")])
        k.op("dve", "tensor_tensor", out=v4(R_["mk"]), in0=v4(R_["bi"]), in1=R4["ing"][0:n, :].unsqueeze(2).to_broadcast([n, 4, 4]), op=ALU.add, r=[rk("bi"), rk("ing")], w=[rk("mk")])
        k.op("dve", "tensor_reduce", out=R1["t1"][0:n, :], in_=R_["mk"][0:n, :], axis=AX.X, op=ALU.max, r=[rk("mk")], w=[rk("t1")])
        k.op("dve", "tensor_scalar", out=R_["s1"][0:n, :], in0=R_["mk"][0:n, :], scalar1=R1["t1"][0:n, 0:1], scalar2=None, op0=ALU.is_equal, r=[rk("mk"), rk("t1")], w=[rk("s1")])
        k.op("dve", "scalar_tensor_tensor", out=R_["mk2"][0:n, :], in0=R_["s1"][0:n, :], scalar=NEG, in1=R_["mk"][0:n, :], op0=ALU.mult, op1=ALU.add, r=[rk("s1"), rk("mk")], w=[rk("mk2")])
        k.op("dve", "tensor_reduce", out=R1["t2"][0:n, :], in_=R_["mk2"][0:n, :], axis=AX.X, op=ALU.max, r=[rk("mk2")], w=[rk("t2")])
        k.op("dve", "tensor_scalar", out=R_["s2"][0:n, :], in0=R_["mk2"][0:n, :], scalar1=R1["t2"][0:n, 0:1], scalar2=None, op0=ALU.is_equal, r=[rk("mk2"), rk("t2")], w=[rk("s2")])
        k.op("dve", "tensor_tensor", out=R_["s1"][0:n, :], in0=R_["s1"][0:n, :], in1=R_["s2"][0:n, :], op=ALU.add, r=[rk("s1"), rk("s2")], w=[rk("s1")])
        k.op("dve", "tensor_tensor", out=R_["s1"][0:n, :], in0=R_["s1"][0:n, :], in1=R_["sc"][0:n, :], op=ALU.mult, r=[rk("s1"), rk("sc")], w=[rk("s1")])
        k.op("dve", "tensor_reduce", out=R1["den"][0:n, :], in_=R_["s1"][0:n, :], axis=AX.X, op=ALU.add, r=[rk("s1")], w=[rk("den")])
        k.op("dve", "reciprocal", out=R1["den"][0:n, :], in_=R1["den"][0:n, :], r=[rk("den")], w=[rk("den")])
        k.op("dve", "tensor_scalar", out=comb[0:n, t, :], in0=R_["s1"][0:n, :], scalar1=R1["den"][0:n, 0:1], scalar2=None, op0=ALU.mult, r=[rk("s1"), rk("den")], w=[("comb", t)])
    S.barrier()
    A.release()
    gb = A.t([128, 1024], F32, "gb"); bb = A.t([128, 1024], F32, "bb")
    load_bcast(k, gb, d["ln2_g"][l:l + 1, :], "gb"); load_bcast(k, bb, d["ln2_b"][l:l + 1, :], "bb")
    hTh = A.t([128, 8, 2064], BF16, "hTh")
    acc = A.t([128, 17, 1024], F32, "acc")
    w1s = [A.t([128, 8, 512], BF16, f"w1s{i}") for i in range(2)]
    w3s = [A.t([128, 8, 512], BF16, f"w3s{i}") for i in range(2)]
    w2s = [A.t([128, 4, 1024], BF16, f"w2s{i}") for i in range(2)]
    actT = [A.t([128, 4, 256], BF16, f"actT{i}") for i in range(2)]
    s1b = [A.t([128, 256], F32, f"s1b{i}") for i in range(2)]
    hm = A.t([128, 1024], F32, "hm2"); z = A.t([128, 1024], F32, "z2"); y = A.t([128, 1024], F32, "y2")
    tmp = ln_tmp(k, "m")
    ecount = 0
    fcount = 0
    ocount = 0
    gcount = 0
    pending = []
    for half, tiles in enumerate([list(range(0, 17)), list(range(17, 33))]):
        hp0 = tile_rows(tiles[0])[0]
        hn = sum(tile_rows(t)[1] for t in tiles)
        for kk in range(8):
            k.dma("sp", hTh[:, kk, 0:hn], d["HT"][kk * 128:(kk + 1) * 128, hp0:hp0 + hn], r=[("HT", t) for t in tiles], w=[("hTh", kk)])
        hres = [("hTh", kk) for kk in range(8)]
        groups = []
        tl = list(tiles)
        if tl[0] == 0:
            groups.append([0]); tl = tl[1:]
        for i in range(0, len(tl), 2):
            groups.append(tl[i:i + 2])
        for e in range(16):
            eb = ecount % 2
            ecount += 1
            k.dma("pool", w1s[eb][:], d["exp_w1"][l, e].rearrange("(k p) f -> p k f", p=128), w=[("w1s", eb)])
            k.dma("pool", w3s[eb][:], d["exp_w3"][l, e].rearrange("(k p) f -> p k f", p=128), w=[("w3s", eb)])
            k.dma("pool", w2s[eb][:], d["exp_w2"][l, e].rearrange("(k p) f -> p k f", p=128), w=[("w2s", eb)])
            for grp in groups:
                c0 = tile_rows(grp[0])[0] - hp0
                ng = sum(tile_rows(t)[1] for t in grp)
                ab = gcount % 2
                gcount += 1
                for f in range(4):
                    hb_ = fcount % 2
                    fcount += 1
                    ph = k.ps(hb_)
                    for kk in range(8):
                        k.mm(ph[:, 0:ng], w1s[eb][:, kk, f * 128:(f + 1) * 128], hTh[:, kk, c0:c0 + ng], kk == 0, kk == 7, r=hres + [("w1s", eb)], w=[("psh", hb_)])
                    for kk in range(8):
                        k.mm(ph[:, 256:256 + ng], w3s[eb][:, kk, f * 128:(f + 1) * 128], hTh[:, kk, c0:c0 + ng], kk == 0, kk == 7, r=hres + [("w3s", eb)], w=[("psh", hb_)])
                    k.op("act", "activation", out=s1b[hb_][:, 0:ng], in_=ph[:, 0:ng], func=AF.Silu, r=[("psh", hb_)], w=[("s1b", hb_)])
                    k.op("dve", "tensor_tensor", out=actT[ab][:, f, 0:ng], in0=s1b[hb_][:, 0:ng], in1=ph[:, 256:256 + ng], op=ALU.mult, r=[("s1b", hb_), ("psh", hb_)], w=[("actT", ab, f)])
                def phase2(grp=grp, ab=ab, eb=eb, e=e, tiles=tiles):
                    nonlocal ocount
                    for ti, t in enumerate(grp):
                        nt_ = tile_rows(t)[1]
                        tloc = t - tiles[0]
                        for h2 in range(2):
                            ob_ = 2 + ocount % 6
                            ocount += 1
                            po = k.ps(ob_)
                            for f in range(4):
                                k.mm(po[0:nt_, :], actT[ab][:, f, ti * 128:ti * 128 + nt_], w2s[eb][:, f, h2 * 512:(h2 + 1) * 512], f == 0, f == 3, r=[("actT", ab, f), ("w2s", eb)], w=[("pso", ob_)])
                            dst = acc[0:nt_, tloc, h2 * 512:(h2 + 1) * 512]
                            if e == 0:
                                k.op("dve", "tensor_scalar", out=dst, in0=po[0:nt_, :], scalar1=comb[0:nt_, t, e:e + 1], scalar2=None, op0=ALU.mult, r=[("pso", ob_), ("comb", t)], w=[("acc", tloc, h2)])
                            else:
                                k.op("dve", "scalar_tensor_tensor", out=dst, in0=po[0:nt_, :], scalar=comb[0:nt_, t, e:e + 1], in1=dst, op0=ALU.mult, op1=ALU.add, r=[("pso", ob_), ("comb", t), ("acc", tloc, h2)], w=[("acc", tloc, h2)])
                while pending:
                    pending.pop(0)()
                pending.append(phase2)
        while pending:
            pending.pop(0)()
        for t in tiles:
            p0, n = tile_rows(t)
            tloc = t - tiles[0]
            k.dma("sp", hm[0:n, :], d["HM"][p0:p0 + n, :], r=[("HM", t)], w=[("hm2",)])
            k.op("dve", "scalar_tensor_tensor", out=z[0:n, :], in0=hm[0:n, :], scalar=ALPHA, in1=acc[0:n, tloc, :], op0=ALU.mult, op1=ALU.add, r=[("hm2",), ("acc", tloc, 0), ("acc", tloc, 1)], w=[("z", "m")])
            layernorm_tile(k, z, y, n, "m", gb, bb, tmp=tmp)
            if last:
                if t >= 1:
                    k.dma("act", d["out"][128 * (t - 1):128 * t, :], y[0:n, :], r=[("y", "m")], w=[("outT", t)])
            else:
                k.dma("act", d["H"][p0:p0 + n, :], y[0:n, :], r=[("y", "m")], w=[("H", t)])
        S.barrier()
    A.release()


def run_skewed(gens):
    active = []
    it = iter(gens)
    while True:
        g = next(it, None)
        if g is not None:
            active.append(g)
        if not active:
            break
        for g in list(active):
            try:
                next(g)
            except StopIteration:
                active.remove(g)


def stage_rwkv_a(k, l):
    S, A, d = k.S, k.A, k.dr
    A.mark()
    cnt = [0]

    def T(shape, dt, name):
        cnt[0] += 1
        return TB(A.t(shape, dt, name), (name, cnt[0]))

    P = [TB(k.ps(i), ("ps", i)) for i in range(8)]
    DV = lambda ap: Vw(ap, [])
    idt = T([128, 128], BF16, "ident"); k.do("sp", "dma_start", idt[:], in_=DV(d["ident_bf"]))
    mu_b = T([128, 1024], F32, "mu_b"); k.do("sp", "dma_start", mu_b[:], in_=DV(d["rwkv_mu"][l:l + 1, :].partition_broadcast(128)))
    wa0 = T([128, 512], F32, "wa0")
    k.do("sp", "dma_start", wa0[:, 0:256], in_=DV(d["rwkv_w0"][l:l + 1, :].partition_broadcast(128)))
    k.do("sp", "dma_start", wa0[:, 256:512], in_=DV(d["rwkv_a0"][l:l + 1, :].partition_broadcast(128)))
    kk_b = T([128, 256], F32, "kk_b"); k.do("sp", "dma_start", kk_b[:], in_=DV(d["rwkv_kk"][l:l + 1, :].partition_broadcast(128)))
    ka_b = T([128, 256], F32, "ka_b"); k.do("sp", "dma_start", ka_b[:], in_=DV(d["rwkv_ka"][l:l + 1, :].partition_broadcast(128)))
    rk_b = T([128, 256], F32, "rk_b"); k.do("sp", "dma_start", rk_b[:], in_=DV(d["rwkv_rk"][l:l + 1, :].partition_broadcast(128)))
    WA = T([128, 512], BF16, "WA")
    k.do("pool", "memset", WA[:], constant=0.0)
    k.do("pool", "dma_start", WA[0:64, 0:256], in_=DV(d["rwkv_w2"][l]))
    k.do("pool", "dma_start", WA[64:128, 256:512], in_=DV(d["rwkv_a2"][l]))
    G2 = T([128, 256], BF16, "G2"); k.do("pool", "dma_start", G2[:], in_=DV(d["rwkv_g2"][l]))
    ctri = T([128, 128], F32, "ctri"); k.do("sp", "dma_start", ctri[:], in_=DV(d["ctri"]))
    cblk = T([128, 128], F32, "cblk"); k.do("sp", "dma_start", cblk[:], in_=DV(d["cblk"]))
    cones = T([128, 2, 64], F32, "cones"); k.do("sp", "dma_start", cones[:], in_=DV(d["cones"]))
    idrep = T([64, 1, 256], F32, "idrep"); k.do("sp", "dma_start", idrep[:].re("p o f -> p (o f)"), in_=DV(d["identrep"]))
    zrow = T([1, 1024], F32, "zrow")
    k.do("pool", "memset", zrow[:], constant=0.0)
    uc0 = ("UC0",)
    k.do("sp", "dma_start", Vw(d["UC"][0:1, :], [uc0]), in_=zrow[:])
    nb = 5
    mk_ = lambda shape, dt, nm, n=None: [T(shape, dt, f"{nm}{i}") for i in range(n or nb)]
    cur_, prv_, ucs_ = mk_([128, 1024], F32, "cur", 2), mk_([128, 1024], F32, "prv", 2), mk_([128, 1024], F32, "ucs", 6)
    LI_, LT_ = mk_([128, 256], BF16, "LI"), mk_([128, 2, 128], BF16, "LT")
    wa_, logw_ = mk_([128, 512], F32, "wa"), mk_([128, 256], F32, "logw")
    W_, Wi_, Wp_, Web_ = mk_([128, 256], F32, "W"), mk_([128, 256], F32, "Wi"), mk_([128, 256], F32, "Wp"), mk_([128, 256], F32, "Web")
    DWe_ = mk_([64, 2, 256], F32, "DWe"); DWt_ = mk_([64, 2, 256], BF16, "DWt")
    kq_, sq_, kkn_, kmod_, bb_, t1_ = (mk_([128, 256], F32, nm) for nm in ("kq", "sq", "kkn", "kmod", "bb", "t1"))
    ss_, rs_ = mk_([128, 4, 1], F32, "ss"), mk_([128, 4, 1], F32, "rs")
    kh_, bh_ = mk_([128, 256], F32, "kh"), mk_([128, 256], F32, "bh")
    OT_ = mk_([128, 7, 256], BF16, "OT"); BG_ = mk_([128, 2, 256], F32, "BGt")
    NE5 = -math.exp(-0.5)
    def body(t):
        p0, n = tile_rows(t)
        b = t % nb
        C = 16 if t == 0 else 64
        ncn = 1 if t == 0 else 2
        ch0 = 0 if t == 0 else 2 * (t - 1) + 1
        cur, prv, ucs, LI, LT, wa, logw = cur_[t % 2], prv_[t % 2], ucs_[t % 6], LI_[b], LT_[b], wa_[b], logw_[b]
        W, Wi, Wp, Web, DWe, DWt = W_[b], Wi_[b], Wp_[b], Web_[b], DWe_[b], DWt_[b]
        kq, sq, kkn, kmod, bb, t1, ss, rs, kh, bh, OT, BGt = kq_[b], sq_[b], kkn_[b], kmod_[b], bb_[b], t1_[b], ss_[b], rs_[b], kh_[b], bh_[b], OT_[b], BG_[b]
        k.do("sp", "dma_start", cur[0:n, :], in_=DV(d["UC"][1 + p0:1 + p0 + n, :]))
        k.do("sp", "dma_start", prv[0:n, :], in_=Vw(d["UC"][p0:p0 + n, :], [uc0] if t == 0 else []))
        k.do("pool", "tensor_tensor", prv[0:n, :], in0=prv[0:n, :], in1=cur[0:n, :], op=ALU.subtract)
        k.do("pool", "tensor_tensor", prv[0:n, :], in0=prv[0:n, :], in1=mu_b[0:n, :], op=ALU.mult)
        k.do("dve", "tensor_tensor", ucs[0:n, :], in0=cur[0:n, :], in1=prv[0:n, :], op=ALU.add)
        r_, kraw, v_ = ucs[0:n, 0:256], ucs[0:n, 256:512], ucs[0:n, 512:768]
        yield
        k.do("act", "activation", LI[0:n, 0:64], in_=ucs[0:n, 768:832], func=AF.Tanh)
        k.do("act", "copy", LI[0:n, 64:128], in_=ucs[0:n, 832:896])
        k.do("act", "activation", LI[0:n, 128:256], in_=ucs[0:n, 896:1024], func=AF.Sigmoid)
        ptb = Vw(k.ps(0)[:].bitcast(BF16), P[0].res)
        k.trv(ptb[:, 0:n], LI[0:n, 0:128], idt[0:n, 0:n])
        k.trv(ptb[:, 128:128 + n], LI[0:n, 128:256], idt[0:n, 0:n])
        k.do("dve", "tensor_copy", LT[:, :, 0:n], in_=ptb[:, 0:256].re("p (a t) -> p a t", a=2)[:, :, 0:n])
        yield
        k.mmv(P[1][0:n, :], LT[:, 0, 0:n], WA[:], True, True)
        k.mmv(P[2][0:n, 0:256], LT[:, 1, 0:n], G2[:], True, True)
        k.do("dve", "tensor_tensor", wa[0:n, :], in0=P[1][0:n, :], in1=wa0[0:n, :], op=ALU.add)
        k.do("act", "activation", wa[0:n, :], in_=wa[0:n, :], func=AF.Sigmoid)
        a_ = wa[0:n, 256:512]
        k.do("pool", "tensor_scalar", logw[0:n, :], in0=wa[0:n, 0:256], scalar1=NE5, scalar2=None, op0=ALU.mult)
        k.do("act", "copy", BGt[0:n, 1, :], in_=P[2][0:n, 0:256])
        yield
        k.mmv(P[3][0:n, 0:256], ctri[0:n, 0:n], logw[0:n, :], True, True)
        k.mmv(P[4][0:n, 0:256], cblk[0:n, 0:n], logw[0:n, :], True, True)
        for c in range(ncn):
            k.mmv(P[5][0:64, c * 256:(c + 1) * 256], cones[0:n, c, :], logw[0:n, :], True, True)
        k.do("act", "activation", W[0:n, :], in_=P[3][0:n, 0:256], func=AF.Exp)
        k.do("act", "activation", Wi[0:n, :], in_=P[3][0:n, 0:256], func=AF.Exp, scale=-1.0)
        k.do("dve", "tensor_tensor", Wp[0:n, :], in0=P[3][0:n, 0:256], in1=logw[0:n, :], op=ALU.subtract)
        k.do("act", "activation", Wp[0:n, :], in_=Wp[0:n, :], func=AF.Exp)
        k.do("act", "activation", Web[0:n, :], in_=P[4][0:n, 0:256], func=AF.Exp)
        k.do("act", "activation", DWe[:, 0:ncn, :], in_=P[5][0:64, 0:ncn * 256].re("p (c f) -> p c f", c=ncn), func=AF.Exp)
        k.do("pool", "tensor_tensor", DWt[:, 0:ncn, :], in0=DWe[:, 0:ncn, :], in1=idrep[:].bc([64, ncn, 256]), op=ALU.mult)
        k.do("act", "dma_start", DV(d["DW"][ch0:ch0 + ncn].rearrange("c p f -> p c f")), in_=DWt[:, 0:ncn, :])
        yield
        k.do("pool", "tensor_tensor", kq[0:n, :], in0=kraw, in1=kk_b[0:n, :], op=ALU.mult)
        k.do("pool", "tensor_tensor", sq[0:n, :], in0=kq[0:n, :], in1=kq[0:n, :], op=ALU.mult)
        k.do("dve", "tensor_reduce", ss[0:n], in_=sq[0:n, :].re("p (h e) -> p h e", h=4), axis=AX.X, op=ALU.add)
        k.do("act", "activation", ss[0:n], in_=ss[0:n], func=AF.Sqrt)
        k.do("dve", "tensor_scalar", ss[0:n], in0=ss[0:n], scalar1=1e-12, scalar2=None, op0=ALU.max)
        k.do("dve", "reciprocal", ss[0:n], in_=ss[0:n])
        k.do("dve", "tensor_tensor", kkn[0:n, :].re("p (h e) -> p h e", h=4), in0=kq[0:n, :].re("p (h e) -> p h e", h=4), in1=ss[0:n].bc([n, 4, 64]), op=ALU.mult)
        k.do("dve", "scalar_tensor_tensor", t1[0:n, :], in0=a_, scalar=-1.0, in1=ka_b[0:n, :], op0=ALU.add, op1=ALU.mult)
        k.do("dve", "scalar_tensor_tensor", kmod[0:n, :], in0=t1[0:n, :], scalar=1.0, in1=kraw, op0=ALU.add, op1=ALU.mult)
        k.do("pool", "tensor_tensor", bb[0:n, :], in0=kkn[0:n, :], in1=a_, op=ALU.mult)
        k.do("pool", "tensor_tensor", t1[0:n, :], in0=r_, in1=kmod[0:n, :], op=ALU.mult)
        k.do("pool", "tensor_tensor", t1[0:n, :], in0=t1[0:n, :], in1=rk_b[0:n, :], op=ALU.mult)
        k.do("dve", "tensor_reduce", rs[0:n], in_=t1[0:n, :].re("p (h e) -> p h e", h=4), axis=AX.X, op=ALU.add)
        k.do("dve", "tensor_tensor", BGt[0:n, 0, :].re("p (h e) -> p h e", h=4), in0=v_.re("p (h e) -> p h e", h=4), in1=rs[0:n].bc([n, 4, 64]), op=ALU.mult)
        yield
        k.do("dve", "tensor_tensor", OT[0:n, 0, :], in0=kkn[0:n, :], in1=Wp[0:n, :], op=ALU.mult)
        k.do("pool", "tensor_tensor", OT[0:n, 1, :], in0=r_, in1=W[0:n, :], op=ALU.mult)
        k.do("dve", "tensor_tensor", kh[0:n, :], in0=kmod[0:n, :], in1=Wi[0:n, :], op=ALU.mult)
        k.do("pool", "tensor_tensor", bh[0:n, :], in0=bb[0:n, :], in1=Wi[0:n, :], op=ALU.mult)
        k.do("act", "copy", OT[0:n, 2, :], in_=kh[0:n, :])
        k.do("act", "copy", OT[0:n, 3, :], in_=bh[0:n, :])
        k.do("dve", "tensor_tensor", OT[0:n, 4, :], in0=kh[0:n, :], in1=Web[0:n, :], op=ALU.mult)
        k.do("dve", "scalar_tensor_tensor", OT[0:n, 5, :], in0=bh[0:n, :], scalar=-1.0, in1=Web[0:n, :], op0=ALU.mult, op1=ALU.mult)
        k.do("act", "copy", OT[0:n, 6, :], in_=v_)
        k.do("sp", "dma_start", DV(d["RWT"][p0:p0 + n]), in_=OT[0:n])
        k.do("sp", "dma_start", DV(d["BG"][p0:p0 + n]), in_=BGt[0:n])
    run_skewed([body(t) for t in range(NT)])
    S.barrier()
    A.release()


def stage_rwkv_b(k, l, tmax=NT):
    S, A, d = k.S, k.A, k.dr
    A.mark()
    cnt = [0]

    def T(shape, dt, name):
        cnt[0] += 1
        return TB(A.t(shape, dt, name), (name, cnt[0]))

    P = [TB(k.ps(i), ("ps", i)) for i in range(8)]
    DV = lambda ap: Vw(ap, [])
    idt = T([128, 128], BF16, "ident"); k.do("sp", "dma_start", idt[:], in_=DV(d["ident_bf"]))
    rmask = T([64, 5, 64], F32, "rmask"); k.do("sp", "dma_start", rmask[:], in_=DV(d["rmask"]))
    lg_b = T([64, 256], F32, "lg_b"); k.do("sp", "dma_start", lg_b[:], in_=DV(d["rwkv_lnx_g"][l:l + 1, :].partition_broadcast(64)))
    lb_b = T([64, 256], F32, "lb_b"); k.do("sp", "dma_start", lb_b[:], in_=DV(d["rwkv_lnx_b"][l:l + 1, :].partition_broadcast(64)))
    Tb = [T([64, 4, 64], BF16, f"Tst{i}") for i in range(2)]
    k.do("pool", "memset", Tb[0][:], constant=0.0)
    nb = 2
    mk_ = lambda shape, dt, nm: [T(shape, dt, f"{nm}{i}") for i in range(nb)]
    tokX_ = mk_([64, 2, 7, 256], BF16, "tokX"); DWc_ = mk_([64, 2, 256], BF16, "DWc"); BGc_ = mk_([64, 2, 2, 256], F32, "BGc")
    XT_ = mk_([64, 2, 4, 4, 64], BF16, "XT")
    AT_ = mk_([64, 8, 2, 64], BF16, "AT"); BT_ = mk_([64, 8, 2, 64], BF16, "BT"); Nn_ = mk_([64, 8, 64], BF16, "Nn")
    NPa = mk_([64, 8, 64], BF16, "NPa"); NPTa = mk_([64, 8, 64], BF16, "NPTa")
    Zf_ = mk_([64, 8, 128], F32, "Zf"); Zb_ = mk_([64, 8, 128], BF16, "Zb")
    Q1T_ = mk_([64, 8, 64], BF16, "Q1T"); Q2s_ = mk_([64, 8, 64], F32, "Q2s"); GT_ = mk_([64, 8, 64], BF16, "GT"); Hs_ = mk_([64, 8, 64], F32, "Hs")
    ys_ = mk_([64, 4, 64], F32, "ys"); dd_ = mk_([64, 4, 64], F32, "dd"); sq_ = mk_([64, 4, 64], F32, "sq")
    st_ = mk_([64, 4, 1], F32, "st"); yo_ = mk_([64, 256], BF16, "yo")
    tcur = 0
    ccount = 0
    for t in range(tmax):
        p0, n = tile_rows(t)
        b = t % nb
        C = 16 if t == 0 else 64
        ncn = 1 if t == 0 else 2
        nq = 4 * ncn
        ch0 = 0 if t == 0 else 2 * (t - 1) + 1
        tokX, DWc, BGc, XT, AT, BT, Nn, Zf, Zb = tokX_[b], DWc_[b], BGc_[b], XT_[b], AT_[b], BT_[b], Nn_[b], Zf_[b], Zb_[b]
        Q1T, Q2s, GT, Hs = Q1T_[b], Q2s_[b], GT_[b], Hs_[b]
        for c in range(ncn):
            r0 = p0 + 64 * c
            k.do("sp", "dma_start", tokX[0:C, c], in_=DV(d["RWT"][r0:r0 + C]))
            k.do("act", "dma_start", BGc[0:C, c], in_=DV(d["BG"][r0:r0 + C]))
        k.do("act", "dma_start", DWc[:, 0:ncn, :], in_=DV(d["DW"][ch0:ch0 + ncn].rearrange("c p f -> p c f")))
        for c in range(ncn):
            ptb = Vw(k.ps(6 + c)[:].bitcast(BF16), P[6 + c].res)
            pv = ptb[0:64, :].re("p (h x t) -> p h x t", h=4, x=4)
            for h in range(4):
                for X in range(4):
                    k.trv(pv[:, h, X, 0:C], tokX[0:C, c, X, h * 64:(h + 1) * 64], idt[0:C, 0:C])
            if c == 0:
                k.do("dve", "tensor_copy", XT[:, c, :, :, 0:C], in_=pv[:, :, :, 0:C])
            else:
                k.do("act", "copy", XT[:, c, :, :, 0:C], in_=pv[:, :, :, 0:C])
        for c in range(ncn):
            for h in range(4):
                q = c * 4 + h
                o1 = P[q // 4][0:C, :].re("p (q a t) -> p q a t", q=4, a=2)[:, q % 4, :, 0:C]
                k.mmv(o1, XT[:, c, h, 2, 0:C], XT[:, c, h, 0:2, 0:C], True, True)
                o2 = P[2 + q // 4][0:C, :].re("p (q a t) -> p q a t", q=4, a=2)[:, q % 4, :, 0:C]
                k.mmv(o2, XT[:, c, h, 3, 0:C], XT[:, c, h, 0:2, 0:C], True, True)
                o3 = P[4][0:C, :].re("p (q t) -> p q t", q=8)[:, q, 0:C]
                k.mmv(o3, XT[:, c, h, 0, 0:C], XT[:, c, h, 3, 0:C], True, True)
        for c in range(ncn):
            pv1 = P[c][0:C, :].re("p (q a t) -> p q a t", q=4, a=2)[:, :, :, 0:C]
            k.do("dve", "tensor_tensor", AT[0:C, 4 * c:4 * c + 4, :, 0:C], in0=pv1, in1=rmask[0:C, 0:2, 0:C].un(1).bc([C, 4, 2, C]), op=ALU.mult)
            pv2 = P[2 + c][0:C, :].re("p (q a t) -> p q a t", q=4, a=2)[:, :, :, 0:C]
            k.do("dve", "tensor_tensor", BT[0:C, 4 * c:4 * c + 4, :, 0:C], in0=pv2, in1=rmask[0:C, 2:4, 0:C].un(1).bc([C, 4, 2, C]), op=ALU.mult)
        pv3 = P[4][0:C, :].re("p (q t) -> p q t", q=8)[:, 0:nq, 0:C]
        k.do("dve", "tensor_tensor", Nn[0:C, 0:nq, 0:C], in0=pv3, in1=rmask[0:C, 4:5, 0:C].bc([C, nq, C]), op=ALU.mult)
        pav = P[5][0:C, :].re("p (q i) -> p q i", q=8)
        for c in range(ncn):
            for h in range(4):
                q = c * 4 + h
                k.mmv(pav[:, q, :], AT[0:C, q, 0, 0:C], tokX[0:C, c, 6, h * 64:(h + 1) * 64], True, True)
        k.do("pool", "tensor_copy", Zf[0:C, 0:nq, 0:64].re("p (c h) e -> p c h e", c=ncn), in_=tokX[0:C, 0:ncn, 0, :].re("p c (h e) -> p c h e", h=4))
        k.do("act", "copy", Zf[0:C, 0:nq, 64:128], in_=pav[:, 0:nq, :])
        k.do("dve", "tensor_copy", Zb[0:C, 0:nq, :], in_=Zf[0:C, 0:nq, :])
        L = 3 if t == 0 else 5
        NPc, NPTc = Nn, None
        for lev in range(L + 1):
            def npt(q):
                return BT[0:C, q, 0, 0:C] if lev == 0 else NPTc[0:C, q, 0:C]
            for q in range(nq):
                oz = P[q // 4][0:C, :].re("p (q e) -> p q e", q=4)[:, q % 4, :]
                k.mmv(oz, npt(q), Zb[0:C, q, :], True, True)
            if lev < L:
                nxtP, nxtPT = NPa[lev % 2], NPTa[lev % 2]
                pa = P[5][0:C, :].re("p (q i) -> p q i", q=8)
                pbk = P[7][0:C, :].re("p (q i) -> p q i", q=8)
                for q in range(nq):
                    if lev + 1 < L:
                        k.mmv(pa[:, q, 0:C], npt(q), NPc[0:C, q, 0:C], True, True)
                    k.mmv(pbk[:, q, 0:C], NPc[0:C, q, 0:C], npt(q), True, True)
            for c in range(ncn):
                zv = P[c][0:C, :].re("p (q e) -> p q e", q=4)
                k.do("dve", "tensor_tensor", Zf[0:C, 4 * c:4 * c + 4, :], in0=Zf[0:C, 4 * c:4 * c + 4, :], in1=zv, op=(ALU.subtract if lev == 0 else ALU.add))
            k.do("act", "copy", Zb[0:C, 0:nq, :], in_=Zf[0:C, 0:nq, :])
            if lev < L:
                if lev + 1 < L:
                    k.do("act", "copy", nxtP[0:C, 0:nq, 0:C], in_=pa[:, 0:nq, 0:C])
                k.do("dve", "tensor_copy", nxtPT[0:C, 0:nq, 0:C], in_=pbk[:, 0:nq, 0:C])
                NPc, NPTc = nxtP, nxtPT
        pq1 = P[2][0:64, :].re("p (q t) -> p q t", q=8)
        pq2 = P[3][0:C, :].re("p (q i) -> p q i", q=8)
        pg = P[4][0:64, :].re("p (q j) -> p q j", q=8)
        ph = P[5][0:64, :].re("p (q i) -> p q i", q=8)
        for c in range(ncn):
            for h in range(4):
                q = c * 4 + h
                hs = slice(h * 64, (h + 1) * 64)
                P1b, P2b = Zb[0:C, q, 0:64], Zb[0:C, q, 64:128]
                V_ = tokX[0:C, c, 6, hs]
                k.mmv(pq1[:, q, 0:C], P1b, BT[0:C, q, 1, 0:C], True, False)
                k.mmv(pq1[:, q, 0:C], idt[0:64, 0:64], XT[:, c, h, 1, 0:C], False, True)
                k.mmv(pq2[:, q, :], AT[0:C, q, 1, 0:C], V_, True, False)
                k.mmv(pq2[:, q, :], BT[0:C, q, 1, 0:C], P2b, False, True)
                k.mmv(pg[:, q, :], idt[0:64, 0:64], DWc[:, c, hs], True, False)
                k.mmv(pg[:, q, :], P1b, tokX[0:C, c, 5, hs], False, True)
                k.mmv(ph[:, q, :], tokX[0:C, c, 4, hs], V_, True, False)
                k.mmv(ph[:, q, :], tokX[0:C, c, 5, hs], P2b, False, True)
        k.do("act", "copy", Q1T[:, 0:nq, 0:C], in_=pq1[:, 0:nq, 0:C])
        k.do("dve", "tensor_copy", Q2s[0:C, 0:nq, :], in_=pq2[:, 0:nq, :])
        k.do("act", "copy", GT[:, 0:nq, :], in_=pg[:, 0:nq, :])
        k.do("dve", "tensor_copy", Hs[:, 0:nq, :], in_=ph[:, 0:nq, :])
        for c in range(ncn):
            cb = ccount % 2
            ccount += 1
            Tc, Tn = Tb[tcur], Tb[1 - tcur]
            tcur = 1 - tcur
            py = P[6][0:C, 0:256].re("p (h i) -> p h i", h=4)
            pt_ = P[6][0:64, 256:512].re("p (h i) -> p h i", h=4)
            for h in range(4):
                q = c * 4 + h
                k.mmv(pt_[:, h, :], GT[:, q, :], Tc[:, h, :], True, True)
            for h in range(4):
                q = c * 4 + h
                k.mmv(py[:, h, :], Q1T[:, q, 0:C], Tc[:, h, :], True, True)
            k.do("dve", "tensor_tensor", Tn[:], in0=pt_, in1=Hs[:, 4 * c:4 * c + 4, :], op=ALU.add)
            ys, dd, sq, st, yo = ys_[cb], dd_[cb], sq_[cb], st_[cb], yo_[cb]
            k.do("dve", "tensor_tensor", ys[0:C], in0=py, in1=Q2s[0:C, 4 * c:4 * c + 4, :], op=ALU.add)
            k.do("dve", "tensor_reduce", st[0:C], in_=ys[0:C], axis=AX.X, op=ALU.add)
            k.do("dve", "tensor_scalar", st[0:C], in0=st[0:C], scalar1=-1.0 / 64, scalar2=None, op0=ALU.mult)
            k.do("dve", "tensor_tensor", dd[0:C], in0=ys[0:C], in1=st[0:C].bc([C, 4, 64]), op=ALU.add)
            k.do("pool", "tensor_tensor", sq[0:C], in0=dd[0:C], in1=dd[0:C], op=ALU.mult)
            k.do("dve", "tensor_reduce", st[0:C], in_=sq[0:C], axis=AX.X, op=ALU.add)
            k.do("act", "activation", st[0:C], in_=st[0:C], func=AF.Sqrt, bias=64e-5, scale=1.0 / 64)
            k.do("dve", "reciprocal", st[0:C], in_=st[0:C])
            k.do("dve", "tensor_tensor", dd[0:C], in0=dd[0:C], in1=st[0:C].bc([C, 4, 64]), op=ALU.mult)
            ddf = dd[0:C].re("p h e -> p (h e)")
            k.do("pool", "tensor_tensor", ddf, in0=ddf, in1=lg_b[0:C, :], op=ALU.mult)
            k.do("pool", "tensor_tensor", ddf, in0=ddf, in1=lb_b[0:C, :], op=ALU.add)
            k.do("pool", "tensor_tensor", ddf, in0=ddf, in1=BGc[0:C, c, 0, :], op=ALU.add)
            k.do("pool", "tensor_tensor", yo[0:C, :], in0=ddf, in1=BGc[0:C, c, 1, :], op=ALU.mult)
            r0 = p0 + 64 * c
            k.do("sp", "dma_start", DV(d["MIX"][r0:r0 + C, 512:768]), in_=yo[0:C, :])
    S.barrier()
    A.release()


def build_full():
    k = K()
    declare_io(k, final_out=True)
    stage_ln0(k)
    for l in range(DEPTH):
        stage_in(k, l)
        stage_swa(k, l)
        stage_diff(k, l)
        stage_rwkv_a(k, l)
        stage_rwkv_b(k, l)
        stage_conv(k, l)
        stage_out(k, l)
        stage_moe(k, l, last=(l == DEPTH - 1))
        k.S.barrier(rotate_dma=True)
    k.S.final_wait("sp")
    k.S.emit()
    k.st.close()
    return k


def kernel(**inputs):
    inp = {kk_: np.asarray(v) for kk_, v in inputs.items()}
    k = build_full()
    consts = host_consts(inp)
    in_maps = []
    for b in range(8):
        m = host_inputs(inp, b)
        m.update(consts)
        in_maps.append(m)
    res = run_bass_kernel_spmd(k.nc, in_maps, core_ids=list(range(8)))
    out = np.stack([np.asarray(res.results[b]["out"], np.float32) for b in range(8)], axis=0)
    return out
```

```python
import numpy as np
import concourse.bass as bass
import concourse.mybir as mybir

F32 = mybir.dt.float32
BF16 = mybir.dt.bfloat16
AF = mybir.ActivationFunctionType
ALU = mybir.AluOpType
AX = mybir.AxisListType

ENGS = ("pe", "act", "dve", "pool", "sp")
DQS = ("sp", "act", "pool")


class Sched:
    def __init__(self, nc, stack, ndma=6):
        self.nc = nc
        self.stack = stack
        self.ndma = ndma
        self.gen = 0
        self.dgen = 0
        self.objs = {}
        self.cnt = {e: 0 for e in ENGS}
        self.dcnt = {q: [0] * ndma for q in DQS}
        self.dnext = {q: 0 for q in DQS}
        self.streams = {e: [] for e in ENGS}
        self.waited = {e: {} for e in ENGS}
        self.lastw = {}
        self.readers = {}
        self.nops = 0
        self._new_csems()
        self._new_dsems()

    def _new_csems(self):
        self.gen += 1
        for e in ENGS:
            self.objs[("c", e, self.gen)] = self.stack.enter_context(self.nc.semaphore(f"c_{e}_{self.gen}"))
            self.cnt[e] = 0

    def _new_dsems(self):
        self.dgen += 1
        for q in DQS:
            for i in range(self.ndma):
                self.objs[("d", q, i, self.dgen)] = self.stack.enter_context(self.nc.semaphore(f"d_{q}{i}_{self.dgen}"))
            self.dcnt[q] = [0] * self.ndma

    def ckey(self, e):
        return ("c", e, self.gen)

    def dkey(self, q, i):
        return ("d", q, i, self.dgen)

    def _semobj(self, key):
        return self.objs[key]

    def _wait(self, eng, tok):
        key, val, src = tok
        if self.waited[eng].get(key, 0) >= val:
            return
        self.waited[eng][key] = val
        self.streams[eng].append(("w", key, val))

    def op(self, eng, fn, kw=None, r=(), w=(), dma=False):
        if isinstance(fn, str):
            name = fn
            kw = dict(kw)
            fn = lambda E, name=name, kw=kw: getattr(E, name)(**kw)
        deps = []
        for res in r:
            t = self.lastw.get(res)
            if t is not None:
                deps.append((t, "raw"))
        for res in w:
            t = self.lastw.get(res)
            if t is not None:
                deps.append((t, "waw"))
            for t in self.readers.get(res, ()):
                deps.append((t, "war"))
        for t, kind in deps:
            src = t[2]
            if src == eng and t[0][0] == "c":
                if eng == "pe":
                    continue
                if kind == "war":
                    continue
            self._wait(eng, t)
        if dma:
            q = eng
            i = self.dnext[q] % self.ndma
            self.dnext[q] += 1
            if self.dcnt[q][i] > 0:
                self._wait(eng, (self.dkey(q, i), self.dcnt[q][i], None))
            self.dcnt[q][i] += 16
            tok = (self.dkey(q, i), self.dcnt[q][i], None)
            self.streams[eng].append(("d", fn, self.dkey(q, i)))
        else:
            self.cnt[eng] += 1
            tok = (self.ckey(eng), self.cnt[eng], eng)
            self.streams[eng].append(("o", fn, self.ckey(eng), self.cnt[eng]))
        for res in w:
            self.lastw[res] = tok
            self.readers[res] = []
        for res in r:
            self.readers.setdefault(res, []).append(tok)
        self.nops += 1
        return tok

    def barrier(self, rotate_dma=False):
        for e in ENGS:
            for s in ENGS:
                if s != e and self.cnt[s] > 0:
                    self._wait(e, (self.ckey(s), self.cnt[s], s))
            for q in DQS:
                for i in range(self.ndma):
                    if self.dcnt[q][i] > 0:
                        self._wait(e, (self.dkey(q, i), self.dcnt[q][i], None))
        self.lastw = {}
        self.readers = {}
        for e in ENGS:
            self.streams[e].append(None)
        if max(self.cnt.values()) > 4000:
            self._new_csems()
        if rotate_dma:
            self._new_dsems()

    def final_wait(self, eng="sp"):
        for q in DQS:
            for i in range(self.ndma):
                if self.dcnt[q][i] > 0:
                    self._wait(eng, (self.dkey(q, i), self.dcnt[q][i], None))
        for s in ENGS:
            if s != eng and self.cnt[s] > 0:
                self._wait(eng, (self.ckey(s), self.cnt[s], s))

    def emit(self):
        nc = self.nc
        targets = {}
        for e in ENGS:
            for it in self.streams[e]:
                if it is not None and it[0] == "w" and it[1][0] == "c":
                    targets.setdefault(it[1], set()).add(it[2])
        rank = {key: {v: i + 1 for i, v in enumerate(sorted(vs))} for key, vs in targets.items()}
        self.n_inc = sum(len(v) for v in rank.values())

        def conv(it):
            if it[0] == "w":
                _, key, val = it
                s = self.objs[key]
                v = rank[key][val] if key[0] == "c" else val
                return lambda E, s=s, v=v: E.wait_ge(s, v)
            if it[0] == "d":
                _, fn, key = it
                s = self.objs[key]
                return lambda E, fn=fn, s=s: fn(E).then_inc(s, 16)
            _, fn, key, idx = it
            if idx in rank.get(key, ()):
                s = self.objs[key]
                return lambda E, fn=fn, s=s: fn(E).then_inc(s, 1)
            return lambda E, fn=fn: fn(E)

        segs = {e: [[]] for e in ENGS}
        for e in ENGS:
            for f in self.streams[e]:
                if f is None:
                    segs[e].append([])
                else:
                    segs[e][-1].append(conv(f))
        nseg = max(len(v) for v in segs.values())
        for i in range(nseg):
            cur = {e: (segs[e][i] if i < len(segs[e]) else []) for e in ENGS}
            if not any(cur.values()):
                continue
            with nc.Block() as block:
                @block.tensor
                def _(E, fs=cur["pe"]):
                    for f in fs:
                        f(E)

                @block.scalar
                def _(E, fs=cur["act"]):
                    for f in fs:
                        f(E)

                @block.vector
                def _(E, fs=cur["dve"]):
                    for f in fs:
                        f(E)

                @block.gpsimd
                def _(E, fs=cur["pool"]):
                    for f in fs:
                        f(E)

                @block.sync
                def _(E, fs=cur["sp"]):
                    for f in fs:
                        f(E)


class SbufAlloc:
    def __init__(self, nc, base=16640, limit=208 * 1024):
        self.nc = nc
        self.off = base
        self.limit = limit
        self.n = 0
        self.marks = []

    def mark(self):
        self.marks.append(self.off)

    def release(self):
        self.off = self.marks.pop()

    def t(self, shape, dtype, name=None):
        esz = 4 if dtype == F32 else 2
        if dtype in (mybir.dt.int32, mybir.dt.uint32):
            esz = 4
        nbytes = int(np.prod(shape[1:])) * esz
        nbytes = (nbytes + 63) // 64 * 64
        assert self.off + nbytes <= self.limit, f"SBUF overflow {self.off}+{nbytes} > {self.limit} ({name})"
        self.n += 1
        h = self.nc.alloc_sbuf_tensor_at(f"{name or 't'}_{self.n}", list(shape), dtype, offset=self.off)
        self.off += nbytes
        return h


import math
import numpy as np
import ml_dtypes
from contextlib import ExitStack
import concourse.bass as bass
import concourse.mybir as mybir
from concourse.bass_utils import run_bass_kernel_spmd

NPOS = 4112
NT = 33
DEPTH = 2
ALPHA = (2 * DEPTH) ** 0.25
NEG = -1e30


def tile_rows(t):
    return (0, 16) if t == 0 else (16 + 128 * (t - 1), 128)


GROUPS = [[0]] + [[4 * g + 1 + i for i in range(4)] for g in range(8)]


def t5_bucket(dist):
    n = np.maximum(dist, 0)
    lr = np.log(np.maximum(n, 1).astype(np.float32) / np.float32(16)) / np.float32(math.log(128 / 16))
    large = np.minimum(16 + (lr * np.float32(16)).astype(np.int32), 31)
    return np.where(n < 16, n, large)


class K:
    def __init__(self, ext_in=(), ext_out=(), layers=(0, 1)):
        self.nc = bass.Bass("TRN2", target_bir_lowering=False)
        self.st = ExitStack()
        self.S = Sched(self.nc, self.st)
        self.A = SbufAlloc(self.nc)
        self.ext_in = set(ext_in)
        self.ext_out = set(ext_out)
        self.dr = {}
        nc = self.nc
        self.psum = [self.st.enter_context(nc.psum_tensor(f"ps{i}", [128, 512], F32)) for i in range(8)]

    def D(self, name, shape, dtype, kind=None):
        if kind is None:
            kind = "ExternalInput" if name in self.ext_in else ("ExternalOutput" if name in self.ext_out else "Internal")
        self.dr[name] = self.nc.dram_tensor(name, list(shape), dtype, kind=kind).ap()
        return self.dr[name]

    def I(self, name, shape, dtype=F32):
        return self.D(name, shape, dtype, kind="ExternalInput")

    def ps(self, i):
        return self.psum[i]

    def op(self, eng, name, r=(), w=(), **kw):
        return self.S.op(eng, name, kw, r=r, w=w)

    def dma(self, q, out, in_, r=(), w=()):
        return self.S.op(q, "dma_start", dict(out=out, in_=in_), r=r, w=w, dma=True)

    def mm(self, out, lhsT, rhs, start, stop, r=(), w=(), skip=False):
        kw = dict(out=out, lhsT=lhsT, rhs=rhs, start=start, stop=stop)
        if skip:
            kw["skip_group_check"] = True
        return self.S.op("pe", "matmul", kw, r=r, w=w)

    def tr(self, out, in_, identity, r=(), w=()):
        return self.S.op("pe", "transpose", dict(out=out, in_=in_, identity=identity), r=r, w=w)

    def ps2(self, i, dtype=F32):
        raise NotImplementedError


class Vw:
    def __init__(self, ap, res):
        self.ap = ap
        self.res = res

    def __getitem__(self, idx):
        return Vw(self.ap[idx], self.res)

    def re(self, pat, **kw):
        return Vw(self.ap.rearrange(pat, **kw), self.res)

    def bc(self, shape):
        return Vw(self.ap.to_broadcast(list(shape)), self.res)

    def un(self, ax):
        return Vw(self.ap.unsqueeze(ax), self.res)


class TB:
    def __init__(self, h, res):
        self.h = h
        self.res = res if isinstance(res, list) else [res]

    def __getitem__(self, idx):
        return Vw(self.h[idx], self.res)


def _do(self, eng, name, out, extra_r=(), extra_w=(), **kw):
    r = list(extra_r)
    w = list(extra_w)
    args = {}
    for kk_, v in kw.items():
        if isinstance(v, Vw):
            r += v.res
            args[kk_] = v.ap
        else:
            args[kk_] = v
    okey = "ap" if name == "memset" else "out"
    if isinstance(out, Vw):
        w += out.res
        args[okey] = out.ap
    else:
        args[okey] = out
    if name == "dma_start":
        return self.S.op(eng, name, args, r=r, w=w, dma=True)
    return self.S.op(eng, name, args, r=r, w=w)


K.do = _do


def _mmv(self, out, lhsT, rhs, start, stop, skip=False):
    kw = dict(out=out.ap, lhsT=lhsT.ap, rhs=rhs.ap, start=start, stop=stop)
    if skip:
        kw["skip_group_check"] = True
    return self.S.op("pe", "matmul", kw, r=lhsT.res + rhs.res, w=out.res)


def _trv(self, out, in_, ident):
    return self.S.op("pe", "transpose", dict(out=out.ap, in_=in_.ap, identity=ident.ap), r=in_.res + ident.res, w=out.res)


K.mmv = _mmv
K.trv = _trv


def declare_io(k, final_out=True):
    L = DEPTH
    k.I("x", [4096, 1024]); k.I("meta", [16, 1024]); k.I("ln0_g", [1, 1024]); k.I("ln0_b", [1, 1024])
    k.I("w_in", [L, 1024, 2816]); k.I("swa_sinks", [L, 4])
    k.I("diff_l", [L, 4, 32]); k.I("diff_subln_g", [L, 64])
    k.I("rwkv_mu", [L, 1024]); k.I("rwkv_w0", [L, 256]); k.I("rwkv_w2", [L, 64, 256]); k.I("rwkv_a0", [L, 256])
    k.I("rwkv_a2", [L, 64, 256]); k.I("rwkv_g2", [L, 128, 256]); k.I("rwkv_kk", [L, 256]); k.I("rwkv_ka", [L, 256])
    k.I("rwkv_rk", [L, 256]); k.I("rwkv_lnx_g", [L, 256]); k.I("rwkv_lnx_b", [L, 256])
    k.I("conv_wT", [L, 256, 31]); k.I("conv_b", [L, 256, 1]); k.I("conv_gn_g", [L, 256, 1]); k.I("conv_gn_b", [L, 256, 1])
    k.I("w_out", [L, 1024, 1024]); k.I("ln1_g", [L, 1024]); k.I("ln1_b", [L, 1024])
    k.I("router_w", [1024, 16]); k.I("router_b", [1, 16])
    k.I("exp_w1", [L, 16, 1024, 512]); k.I("exp_w3", [L, 16, 1024, 512]); k.I("exp_w2", [L, 16, 512, 1024])
    k.I("ln2_g", [L, 1024]); k.I("ln2_b", [L, 1024])
    k.I("ident_bf", [128, 128], BF16); k.I("ident_f", [128, 128], F32)
    k.I("ba_meta", [3, 4, 16, 128], BF16); k.I("ba_pc", [4, 128, 256], BF16)
    k.I("bb_pc", [4, 128, 256], F32); k.I("b31", [1, 8], F32)
    k.I("gmat", [128, 128], F32)
    k.I("tri", [3, 64, 64], F32)
    k.I("ctri", [128, 128], F32); k.I("cblk", [128, 128], F32); k.I("cones", [128, 2, 64], F32); k.I("identrep", [64, 256], F32)
    k.I("rmask", [64, 5, 64], F32)
    k.D("H", [NPOS, 1024], F32); k.D("HM", [NPOS, 1024], F32)
    k.D("QKA", [384, NPOS], BF16); k.D("QKB", [512, NPOS], BF16); k.D("CV", [512, NPOS], F32)
    k.D("VA", [NPOS, 128], BF16); k.D("VB", [NPOS, 256], BF16); k.D("UC", [NPOS + 1, 1024], F32)
    k.D("MIX", [NPOS, 768], BF16); k.D("MIXD", [256, NPOS], BF16)
    k.D("HT", [1024, NPOS], BF16)
    k.D("RWT", [NPOS, 7, 256], BF16); k.D("BG", [NPOS, 2, 256], F32); k.D("DW", [65, 64, 256], BF16)
    if final_out:
        k.D("out", [4096, 1024], F32, kind="ExternalOutput")


def host_consts(inp):
    c = {}
    c["ident_bf"] = np.eye(128, dtype=ml_dtypes.bfloat16)
    c["ident_f"] = np.eye(128, dtype=np.float32)
    rel = np.asarray(inp["rel_bias"], np.float32)
    rel_a, rel_b = rel[:, :4], rel[:, 4:]
    ki = np.arange(128)[:, None]
    qi = np.arange(128)[None, :]
    bam = np.full((3, 4, 16, 128), NEG, np.float32)
    m = np.arange(16)[:, None]
    dq = np.arange(128)[None, :] - m
    vis = (dq >= 0) & (np.arange(128)[None, :] < 16)
    g = rel_a[t5_bucket(dq)]
    bam[0] = np.where(vis[None], np.moveaxis(g, -1, 0), NEG)
    dq = (16 + np.arange(128))[None, :] - m
    bam[1] = np.moveaxis(rel_a[t5_bucket(dq)], -1, 0)
    bam[2] = np.broadcast_to(rel_a[31][:, None, None], (4, 16, 128))
    c["ba_meta"] = bam.astype(ml_dtypes.bfloat16)
    dq_prev = qi - ki + 128
    dq_cur = qi - ki
    bp = np.where((ki > qi)[None], np.moveaxis(rel_a[t5_bucket(dq_prev)], -1, 0), NEG)
    bc = np.where((ki <= qi)[None], np.moveaxis(rel_a[t5_bucket(dq_cur)], -1, 0), NEG)
    c["ba_pc"] = np.concatenate([bp, bc], axis=2).astype(ml_dtypes.bfloat16)
    bcd = np.where((ki <= qi)[None], np.moveaxis(rel_b[t5_bucket(dq_cur)], -1, 0), NEG)
    bpd = np.moveaxis(rel_b[t5_bucket(dq_prev)], -1, 0)
    c["bb_pc"] = np.ascontiguousarray(np.concatenate([bcd, bpd], axis=2).astype(np.float32))
    c["b31"] = np.ascontiguousarray(rel[31][None, :])
    gm = np.zeros((128, 128), np.float32)
    gm[:64, :64] = 1.0 / 64
    gm[64:, 64:] = 1.0 / 64
    c["gmat"] = gm
    s = np.arange(64)[:, None]
    t = np.arange(64)[None, :]
    c["tri"] = np.stack([(s <= t), (s < t), (s >= t)]).astype(np.float32)
    s2 = np.arange(128)[:, None]; t2 = np.arange(128)[None, :]
    same = (s2 // 64) == (t2 // 64)
    c["ctri"] = (same & (s2 <= t2)).astype(np.float32)
    c["cblk"] = same.astype(np.float32)
    c["cones"] = np.ascontiguousarray(np.stack([(np.arange(128) // 64 == cc)[:, None] * np.ones((1, 64)) for cc in range(2)], axis=1).astype(np.float32))
    c["identrep"] = np.tile(np.eye(64, dtype=np.float32), (1, 4))
    rm = np.stack([(s < t), (s <= t), (s < t), -1.0 * (s <= t), (s > t)], axis=1).astype(np.float32)
    c["rmask"] = np.ascontiguousarray(rm)
    return c


def host_inputs(inp, b):
    f = lambda a: np.ascontiguousarray(np.asarray(a, np.float32))
    L = DEPTH
    m = {
        "x": f(inp["x"][b]), "meta": f(inp["meta"]), "ln0_g": f(inp["ln0_g"])[None], "ln0_b": f(inp["ln0_b"])[None],
        "w_in": f(inp["w_in"]), "swa_sinks": f(inp["swa_sinks"]),
        "diff_l": f(np.stack([inp["diff_lq1"], inp["diff_lk1"], inp["diff_lq2"], inp["diff_lk2"]], axis=1)),
        "diff_subln_g": f(inp["diff_subln_g"]),
        "rwkv_mu": f(inp["rwkv_mu"]), "rwkv_w0": f(inp["rwkv_w0"]), "rwkv_w2": f(inp["rwkv_w2"]), "rwkv_a0": f(inp["rwkv_a0"]),
        "rwkv_a2": f(inp["rwkv_a2"]), "rwkv_g2": f(inp["rwkv_g2"]), "rwkv_kk": f(inp["rwkv_kk"]), "rwkv_ka": f(inp["rwkv_ka"]),
        "rwkv_rk": f(np.asarray(inp["rwkv_rk"]).reshape(L, 256)), "rwkv_lnx_g": f(inp["rwkv_lnx_g"]), "rwkv_lnx_b": f(inp["rwkv_lnx_b"]),
        "conv_wT": f(np.transpose(np.asarray(inp["conv_w"]), (0, 2, 1))), "conv_b": f(inp["conv_b"])[..., None],
        "conv_gn_g": f(inp["conv_gn_g"])[..., None], "conv_gn_b": f(inp["conv_gn_b"])[..., None],
        "w_out": f(inp["w_out"]), "ln1_g": f(inp["ln1_g"]), "ln1_b": f(inp["ln1_b"]),
        "router_w": f(inp["router_w"]), "router_b": f(inp["router_b"])[None],
        "exp_w1": f(inp["exp_w1"]), "exp_w3": f(inp["exp_w3"]), "exp_w2": f(inp["exp_w2"]),
        "ln2_g": f(inp["ln2_g"]), "ln2_b": f(inp["ln2_b"]),
    }
    return m


def layernorm_tile(k, z, y, n, key, gb, bb, eps=1e-5, tmp=None, gbres=("gb", "bb")):
    stt, mv, rs = tmp
    for c in range(2):
        k.op("dve", "bn_stats", out=stt[0:n, c, :], in_=z[0:n, c * 512:(c + 1) * 512], r=[("z", key)], w=[("st", key, c)])
    k.op("dve", "bn_aggr", out=mv[0:n, :], in_=stt[0:n].rearrange("p a b -> p (a b)"), r=[("st", key, 0), ("st", key, 1)], w=[("mv", key)])
    k.op("act", "activation", out=rs[0:n, 0:1], in_=mv[0:n, 1:2], func=AF.Sqrt, bias=eps, scale=1.0, r=[("mv", key)], w=[("rs", key, 0)])
    k.op("dve", "scalar_tensor_tensor", out=y[0:n, :], in0=z[0:n, :], scalar=mv[0:n, 0:1], in1=gb[0:n, :], op0=ALU.subtract, op1=ALU.mult, r=[("z", key), ("mv", key), gbres[0]], w=[("y", key)])
    k.op("dve", "reciprocal", out=rs[0:n, 0:1], in_=rs[0:n, 0:1], r=[("rs", key, 0)], w=[("rs", key, 0)])
    k.op("dve", "scalar_tensor_tensor", out=y[0:n, :], in0=y[0:n, :], scalar=rs[0:n, 0:1], in1=bb[0:n, :], op0=ALU.mult, op1=ALU.add, r=[("y", key), ("rs", key, 0), gbres[1]], w=[("y", key)])


def layernorm_tile_g(k, z, y, n, key, gb, bb, eps=1e-5, tmp=None, gbres=("gb", "bb")):
    stt, mv, rs = tmp
    for c in range(2):
        k.op("dve", "bn_stats", out=stt[0:n, c, :], in_=z[0:n, c * 512:(c + 1) * 512], r=[("z", key)], w=[("st", key, c)])
    k.op("dve", "bn_aggr", out=mv[0:n, :], in_=stt[0:n].rearrange("p a b -> p (a b)"), r=[("st", key, 0), ("st", key, 1)], w=[("mv", key)])
    k.op("act", "activation", out=rs[0:n, 0:1], in_=mv[0:n, 1:2], func=AF.Sqrt, bias=eps, scale=1.0, r=[("mv", key)], w=[("rs", key, 0)])
    k.op("dve", "scalar_tensor_tensor", out=y[0:n, :], in0=z[0:n, :], scalar=mv[0:n, 0:1], in1=gb[0:n, :], op0=ALU.subtract, op1=ALU.mult, r=[("z", key), ("mv", key), gbres[0]], w=[("y", key)])
    yield
    k.op("dve", "reciprocal", out=rs[0:n, 0:1], in_=rs[0:n, 0:1], r=[("rs", key, 0)], w=[("rs", key, 0)])
    k.op("dve", "scalar_tensor_tensor", out=y[0:n, :], in0=y[0:n, :], scalar=rs[0:n, 0:1], in1=bb[0:n, :], op0=ALU.mult, op1=ALU.add, r=[("y", key), ("rs", key, 0), gbres[1]], w=[("y", key)])


def ln_tmp(k, nm):
    A = k.A
    return (A.t([128, 2, 6], F32, "st" + nm), A.t([128, 2], F32, "mv" + nm), A.t([128, 2], F32, "rs" + nm))


def load_bcast(k, dst, src_row, res, q="sp"):
    k.dma(q, dst[:], src_row.partition_broadcast(128), w=[res])


def stage_ln0(k):
    S, A, d = k.S, k.A, k.dr
    A.mark()
    gb = A.t([128, 1024], F32, "gb"); bb = A.t([128, 1024], F32, "bb")
    load_bcast(k, gb, d["ln0_g"], "gb"); load_bcast(k, bb, d["ln0_b"], "bb")
    zs = [A.t([128, 1024], F32, f"z{i}") for i in range(3)]
    ys = [A.t([128, 1024], F32, f"y{i}") for i in range(3)]
    tmps = [ln_tmp(k, str(i)) for i in range(3)]
    for t in range(NT):
        p0, n = tile_rows(t)
        b = t % 3
        z, y = zs[b], ys[b]
        src = d["meta"] if t == 0 else d["x"][128 * (t - 1):128 * t, :]
        k.dma("sp", z[0:n, :], src, w=[("z", b)])
        layernorm_tile(k, z, y, n, b, gb, bb, tmp=tmps[b])
        k.dma("act", d["H"][p0:p0 + n, :], y[0:n, :], r=[("y", b)], w=[("H", t)])
    S.barrier()
    A.release()


FM_TILES = [
    ("QKA", 0, 0, 0.125), ("QKA", 128, 128, 0.125), ("QKA", 256, 256, 1.0),
    ("QKB", 0, 512, 32 ** -0.5), ("QKB", 128, 640, 32 ** -0.5), ("QKB", 256, 768, 1.0), ("QKB", 384, 896, 1.0),
    ("CV", 0, 2304, 1.0), ("CV", 128, 2432, 1.0), ("CV", 256, 2560, 1.0), ("CV", 384, 2688, 1.0),
]


def stage_in(k, l):
    S, A, d = k.S, k.A, k.dr
    A.mark()
    idt = A.t([128, 128], BF16, "ident")
    k.dma("sp", idt[:], d["ident_bf"], w=["ident"])
    wsb = A.t([128, 8, 2816], BF16, "w_in")
    for kk in range(8):
        for c in range(2):
            k.dma("pool", wsb[:, kk, c * 1408:(c + 1) * 1408], d["w_in"][l, kk * 128:(kk + 1) * 128, c * 1408:(c + 1) * 1408], w=[("w_in", kk, c)])
    wres = [("w_in", kk, c) for kk in range(8) for c in range(2)]
    zs = [A.t([128, 1024], F32, f"z{i}") for i in range(2)]
    hb = [A.t([128, 1024], BF16, f"hb{i}") for i in range(2)]
    hTg = [A.t([128, 8, 512], BF16, f"hT{i}") for i in range(2)]
    ofm_b = [A.t([128, 512], BF16, f"ofb{i}") for i in range(3)]
    ofm_f = [A.t([128, 512], F32, f"off{i}") for i in range(2)]
    otm_v = [A.t([128, 384], BF16, f"otv{i}") for i in range(2)]
    otm_u = [A.t([128, 1024], F32, f"otu{i}") for i in range(2)]
    tcount = 0
    fmc = 0
    tmc = 0
    for gi, grp in enumerate(GROUPS):
        gb_ = gi % 2
        hT = hTg[gb_]
        ntok = sum(tile_rows(t)[1] for t in grp)
        gp0 = tile_rows(grp[0])[0]
        for ti, t in enumerate(grp):
            p0, n = tile_rows(t)
            b = tcount % 2
            tcount += 1
            z = zs[b]
            k.dma("sp", z[0:n, :], d["H"][p0:p0 + n, :], r=[("H", t)], w=[("z", b)])
            k.op("act", "copy", out=hb[b][0:n, :], in_=z[0:n, :], r=[("z", b)], w=[("hb", b)])
            pt = k.ps(b)[:].bitcast(BF16)
            for kk in range(8):
                k.tr(pt[:, kk * 128:kk * 128 + n], hb[b][0:n, kk * 128:(kk + 1) * 128], idt[0:n, 0:n], r=[("hb", b), "ident"], w=[("ps", b)])
            k.op("dve", "tensor_copy", out=hT[:, :, ti * 128:ti * 128 + n], in_=pt.rearrange("p (k t) -> p k t", k=8)[:, :, 0:n], r=[("ps", b)], w=[("hT", gb_, ti)])
        hres = [("hT", gb_, ti) for ti in range(len(grp))]
        for (dn, r0, c0, sc) in FM_TILES:
            pb = 2 + fmc % 3
            isf = dn == "CV"
            ob = ofm_f[fmc % 2] if isf else ofm_b[fmc % 3]
            ores = ("off", fmc % 2) if isf else ("ofb", fmc % 3)
            for kk in range(8):
                k.mm(k.ps(pb)[:, 0:ntok], wsb[:, kk, c0:c0 + 128], hT[:, kk, 0:ntok], kk == 0, kk == 7, r=hres + wres, w=[("ps", pb)])
            if fmc % 2 == 0:
                k.op("act", "activation", out=ob[:, 0:ntok], in_=k.ps(pb)[:, 0:ntok], func=AF.Copy, scale=sc, r=[("ps", pb)], w=[ores])
            else:
                k.op("dve", "tensor_scalar", out=ob[:, 0:ntok], in0=k.ps(pb)[:, 0:ntok], scalar1=sc, scalar2=None, op0=ALU.mult, r=[("ps", pb)], w=[ores])
            k.dma("sp", d[dn][r0:r0 + 128, gp0:gp0 + ntok], ob[:, 0:ntok], r=[ores], w=[(dn, r0, gi)])
            fmc += 1
        for ti, t in enumerate(grp):
            p0, n = tile_rows(t)
            ov = otm_v[tmc % 2]
            ou = otm_u[tmc % 2]
            tb = tmc % 2
            tmc += 1
            lt = hT[:, :, ti * 128:ti * 128 + n]
            for kk in range(8):
                k.mm(k.ps(5)[0:n, 0:128], lt[:, kk, :], wsb[:, kk, 384:512], kk == 0, kk == 7, r=hres + wres, w=[("ps", 5, 0)])
            for kk in range(8):
                k.mm(k.ps(5)[0:n, 128:384], lt[:, kk, :], wsb[:, kk, 1024:1280], kk == 0, kk == 7, r=hres + wres, w=[("ps", 5, 1)])
            k.op("act", "copy", out=ov[0:n, :], in_=k.ps(5)[0:n, 0:384], r=[("ps", 5, 0), ("ps", 5, 1)], w=[("otv", tb)])
            k.dma("act", d["VA"][p0:p0 + n, :], ov[0:n, 0:128], r=[("otv", tb)], w=[("VA", t)])
            k.dma("act", d["VB"][p0:p0 + n, :], ov[0:n, 128:384], r=[("otv", tb)], w=[("VB", t)])
            for c in range(2):
                pb = 6 + c
                for kk in range(8):
                    k.mm(k.ps(pb)[0:n, :], lt[:, kk, :], wsb[:, kk, 1280 + c * 512:1280 + (c + 1) * 512], kk == 0, kk == 7, r=hres + wres, w=[("ps", pb)])
                k.op("dve", "tensor_copy", out=ou[0:n, c * 512:(c + 1) * 512], in_=k.ps(pb)[0:n, :], r=[("ps", pb)], w=[("otu", tb, c)])
            k.dma("sp", d["UC"][1 + p0:1 + p0 + n, :], ou[0:n, :], r=[("otu", tb, 0), ("otu", tb, 1)], w=[("UC", t)])
    S.barrier()
    A.release()


def stage_swa(k, l):
    S, A, d = k.S, k.A, k.dr
    A.mark()
    idt = A.t([128, 128], BF16, "ident")
    k.dma("sp", idt[:], d["ident_bf"], w=["ident"])
    qT = A.t([64, 4, NPOS], BF16, "qTa")
    kT = A.t([64, 2, NPOS], BF16, "kTa")
    for h in range(4):
        k.dma("sp", qT[:, h, :], d["QKA"][64 * h:64 * h + 64, :], w=[("qT", h)])
    for kv in range(2):
        k.dma("sp", kT[:, kv, :], d["QKA"][256 + 64 * kv:256 + 64 * kv + 64, :], w=[("kT", kv)])
    va = A.t([128, NT, 2, 65], BF16, "va")
    k.op("pool", "memset", ap=va[:, :, :, 64:65], constant=1.0, w=["va_ones"])
    for t in range(NT):
        p0, n = tile_rows(t)
        k.dma("act", va[0:n, t, :, 0:64], d["VA"][p0:p0 + n, :].rearrange("p (h e) -> p h e", h=2), w=[("va", t)])
    bam = A.t([16, 3, 4, 128], BF16, "bam")
    k.dma("sp", bam[:], d["ba_meta"].rearrange("c h m q -> m c h q"), w=["bam"])
    bapc = A.t([128, 4, 256], BF16, "bapc")
    k.dma("sp", bapc[:], d["ba_pc"].rearrange("h k q -> k h q"), w=["bapc"])
    sk = A.t([128, 4, 1], F32, "sk")
    esk = A.t([128, 4, 1], F32, "esk")
    k.dma("sp", sk[:].rearrange("p h o -> p (h o)"), d["swa_sinks"][l:l + 1, :].partition_broadcast(128), w=["sk"])
    k.op("act", "activation", out=esk[:], in_=sk[:], func=AF.Exp, r=["sk"], w=["esk"])
    pms = [A.t([16, 4, 128], BF16, f"pm{i}") for i in range(2)]
    pps = [A.t([128, 2, 512], BF16, f"pp{i}") for i in range(2)]
    dens = [A.t([128, 4, 1], F32, f"den{i}") for i in range(2)]
    yos = [A.t([128, 4, 64], BF16, f"yo{i}") for i in range(2)]
    for t in range(NT):
        p0, n = tile_rows(t)
        s = t % 2
        psA, psB, psD = k.ps(4 * s), [k.ps(4 * s + 1), k.ps(4 * s + 2)], k.ps(4 * s + 3)
        pm, pp, den, yo = pms[s], pps[s], dens[s], yos[s]
        case = min(t, 2)
        psAv = psA[0:16, :].rearrange("p (h q) -> p h q", h=4)
        for h in range(4):
            kv = h // 2
            hp, cb = h // 2, (h % 2) * 256
            k.mm(psAv[:, h, 0:n], kT[:, kv, 0:16], qT[:, h, p0:p0 + n], True, False, r=[("kT", kv), ("qT", h)], w=[("psA", s)])
            k.mm(psAv[:, h, 0:n], idt[0:16, 0:16], bam[:, case, h, 0:n], False, True, r=["ident", "bam"], w=[("psA", s)])
            if t >= 2:
                pp0 = p0 - 128
                k.mm(psB[hp][:, cb:cb + n], kT[:, kv, pp0:pp0 + 128], qT[:, h, p0:p0 + n], True, False, r=[("kT", kv), ("qT", h)], w=[("psB", s, hp)])
                k.mm(psB[hp][:, cb:cb + n], idt[:, :], bapc[:, h, 0:n], False, True, r=["ident", "bapc"], w=[("psB", s, hp)])
            if t >= 1:
                k.mm(psB[hp][:, cb + 128:cb + 128 + n], kT[:, kv, p0:p0 + 128], qT[:, h, p0:p0 + n], True, False, r=[("kT", kv), ("qT", h)], w=[("psB", s, hp)])
                k.mm(psB[hp][:, cb + 128:cb + 128 + n], idt[:, :], bapc[:, h, 128:128 + n], False, True, r=["ident", "bapc"], w=[("psB", s, hp)])
        k.op("act", "activation", out=pm[:, :, 0:n], in_=psAv[:, :, 0:n], func=AF.Exp, r=[("psA", s)], w=[("pm", s)])
        for hp in range(2):
            if t >= 2:
                k.op("act", "activation", out=pp[:, hp, :], in_=psB[hp][:, :], func=AF.Exp, r=[("psB", s, hp)], w=[("pp", s, hp)])
            elif t == 1:
                k.op("act", "activation", out=pp[:, hp, :].rearrange("p (h x) -> p h x", h=2)[:, :, 128:256],
                     in_=psB[hp][:, :].rearrange("p (h x) -> p h x", h=2)[:, :, 128:256], func=AF.Exp, r=[("psB", s, hp)], w=[("pp", s, hp)])
        psDv = psD[:, 0:260].rearrange("p (h e) -> p h e", h=4)
        for h in range(4):
            kv = h // 2
            hp, cb = h // 2, (h % 2) * 256
            k.mm(psDv[0:n, h, :], pm[0:16, h, 0:n], va[0:16, 0, kv, :], True, t == 0, r=[("pm", s), ("va", 0), "va_ones"], w=[("psD", s)])
            if t >= 2:
                k.mm(psDv[0:n, h, :], pp[:, hp, cb:cb + n], va[:, t - 1, kv, :], False, False, r=[("pp", s, hp), ("va", t - 1), "va_ones"], w=[("psD", s)])
            if t >= 1:
                k.mm(psDv[0:n, h, :], pp[:, hp, cb + 128:cb + 128 + n], va[:, t, kv, :], False, True, r=[("pp", s, hp), ("va", t), "va_ones"], w=[("psD", s)])
        k.op("dve", "tensor_tensor", out=den[0:n], in0=psDv[0:n, :, 64:65], in1=esk[0:n], op=ALU.add, r=[("psD", s), "esk"], w=[("den", s)])
        k.op("dve", "reciprocal", out=den[0:n], in_=den[0:n], r=[("den", s)], w=[("den", s)])
        k.op("dve", "tensor_tensor", out=yo[0:n], in0=psDv[0:n, :, 0:64], in1=den[0:n].to_broadcast([n, 4, 64]), op=ALU.mult, r=[("psD", s), ("den", s)], w=[("yo", s)])
        k.dma("sp", d["MIX"][p0:p0 + n, 0:256], yo[0:n].rearrange("p h e -> p (h e)"), r=[("yo", s)], w=[("MIXa", t)])
    S.barrier()
    A.release()


def stage_diff(k, l):
    S, A, d = k.S, k.A, k.dr
    A.mark()
    lam_init = 0.8 - 0.6 * math.exp(-0.3 * l)
    idt = A.t([128, 128], BF16, "ident")
    k.dma("sp", idt[:], d["ident_bf"], w=["ident"])
    qT = A.t([32, 8, NPOS], BF16, "qTb")
    kT = A.t([32, 8, NPOS], BF16, "kTb")
    for sl in range(8):
        k.dma("sp", qT[:, sl, :], d["QKB"][32 * sl:32 * sl + 32, :], w=[("qT", sl)])
        k.dma("sp", kT[:, sl, :], d["QKB"][256 + 32 * sl:256 + 32 * sl + 32, :], w=[("kT", sl)])
    vb = A.t([128, NT, 4, 65], BF16, "vb")
    k.op("pool", "memset", ap=vb[:, :, :, 64:65], constant=1.0, w=["vb_ones"])
    for t in range(NT):
        p0, n = tile_rows(t)
        k.dma("act", vb[0:n, t, :, 0:64], d["VB"][p0:p0 + n, :].rearrange("p (h e) -> p h e", h=4), w=[("vb", t)])
    bbf = A.t([128, 4, 256], F32, "bbf")
    k.dma("sp", bbf[:], d["bb_pc"].rearrange("h k q -> k h q"), w=["bbf"])
    b31b = A.t([128, 8], F32, "b31b")
    k.dma("sp", b31b[:], d["b31"].partition_broadcast(128), w=["b31b"])
    bbt = A.t([128, 4, 256], BF16, "bbt")
    for h in range(4):
        k.op("dve", "tensor_scalar", out=bbt[:, h, :], in0=bbf[:, h, :], scalar1=b31b[:, 4 + h:5 + h], scalar2=None, op0=ALU.subtract, r=["bbf", "b31b"], w=["bbt"])
    dl = A.t([128, 4, 32], F32, "dl")
    k.dma("sp", dl[:].rearrange("p a b -> p (a b)"), d["diff_l"][l:l + 1].rearrange("o a b -> o (a b)").partition_broadcast(128), w=["dl"])
    pr = A.t([128, 2, 32], F32, "pr")
    ss = A.t([128, 2], F32, "ss")
    nlam = A.t([128, 1], F32, "nlam")
    k.op("dve", "tensor_tensor", out=pr[:, 0, :], in0=dl[:, 0, :], in1=dl[:, 1, :], op=ALU.mult, r=["dl"], w=["pr0"])
    k.op("dve", "tensor_tensor", out=pr[:, 1, :], in0=dl[:, 2, :], in1=dl[:, 3, :], op=ALU.mult, r=["dl"], w=["pr1"])
    k.op("dve", "tensor_reduce", out=ss[:], in_=pr[:], axis=AX.X, op=ALU.add, r=["pr0", "pr1"], w=["ss"])
    k.op("act", "activation", out=ss[:], in_=ss[:], func=AF.Exp, r=["ss"], w=["ss"])
    k.op("dve", "tensor_tensor", out=nlam[:], in0=ss[:, 1:2], in1=ss[:, 0:1], op=ALU.subtract, r=["ss"], w=["nlam"])
    k.op("dve", "tensor_scalar", out=nlam[:], in0=nlam[:], scalar1=-lam_init, scalar2=None, op0=ALU.add, r=["nlam"], w=["nlam"])
    gvec = A.t([128, 1, 64], F32, "gvec")
    k.dma("sp", gvec[:].rearrange("p o e -> p (o e)"), d["diff_subln_g"][l:l + 1, :].partition_broadcast(128), w=["gvec"])
    k.op("act", "mul", out=gvec[:], in_=gvec[:], mul=(1.0 - lam_init), r=["gvec"], w=["gvec"])
    PTs = [A.t([128, 512], BF16, f"PT{i}") for i in range(3)]
    rr = [A.t([128, 2, 4, 1], F32, f"rr{i}") for i in range(2)]
    t1 = [A.t([128, 4, 64], F32, f"t1{i}") for i in range(2)]
    t2 = [A.t([128, 4, 64], F32, f"t2{i}") for i in range(2)]
    ms = [A.t([128, 4, 1], F32, f"ms{i}") for i in range(2)]
    ybo = [A.t([128, 4, 4, 64], BF16, f"ybo{i}") for i in range(2)]
    sc = 0
    pending = []

    def flush():
        while pending:
            pending.pop(0)()

    for qg, tiles in enumerate(GROUPS):
        ntok = sum(tile_rows(t)[1] for t in tiles)
        gp0 = tile_rows(tiles[0])[0]
        nt = len(tiles)
        nq = tile_rows(tiles[0])[1]
        yb_ = ybo[qg % 2]
        for h in range(4):
            hb = h % 2
            Ob = [k.ps(4 + 2 * hb), k.ps(5 + 2 * hb)]
            Ov = [Ob[c][:, 0:65 * nt].rearrange("p (t e) -> p t e", e=65) for c in range(2)]
            for c in range(2):
                sl = 2 * h + c
                for j in range(0, tiles[-1] + 1):
                    kp0, nk = tile_rows(j)
                    fi = max(0, j - tiles[0])
                    col0 = fi * 128
                    sb = sc % 4
                    pt = PTs[sc % 3]
                    ptk = sc % 3
                    sc += 1
                    psb = k.ps(sb)
                    bl = [i for i in (j, j + 1) if i in tiles]
                    c1 = col0 + 128 * len(bl) if tiles[0] != 0 else (nq if bl else 0)
                    c1 = min(c1, ntok)
                    if bl:
                        k.mm(psb[0:nk, col0:c1], kT[:, sl, kp0:kp0 + nk], qT[:, sl, gp0 + col0:gp0 + c1], True, False, r=[("kT", sl), ("qT", sl)], w=[("psS", sb)])
                        if j == 0:
                            if tiles[0] == 0:
                                k.mm(psb[0:16, 0:16], idt[0:16, 0:16], bbt[0:16, h, 0:16], False, True, r=["ident", "bbt"], w=[("psS", sb)])
                            else:
                                k.mm(psb[0:16, 0:128], idt[:, 112:128], bbt[:, h, 128:256], False, True, r=["ident", "bbt"], w=[("psS", sb)])
                        else:
                            b0 = 0 if bl[0] == j else 128
                            k.mm(psb[:, col0:c1], idt[:, :], bbt[:, h, b0:b0 + (c1 - col0)], False, True, r=["ident", "bbt"], w=[("psS", sb)])
                    else:
                        c1 = col0
                    if c1 < ntok:
                        k.mm(psb[0:nk, c1:ntok], kT[:, sl, kp0:kp0 + nk], qT[:, sl, gp0 + c1:gp0 + ntok], True, True, r=[("kT", sl), ("qT", sl)], w=[("psS", sb)])
                    k.op("act", "activation", out=pt[0:nk, col0:ntok], in_=psb[0:nk, col0:ntok], func=AF.Exp, bias=b31b[0:nk, 4 + h:5 + h], scale=1.0,
                         r=[("psS", sb), "b31b"], w=[("PT", ptk)])

                    def pv(tiles=tiles, j=j, c=c, h=h, hb=hb, nk=nk, pt=pt, ptk=ptk, Ov=Ov):
                        for il, i in enumerate(tiles):
                            if i < j:
                                continue
                            ni = tile_rows(i)[1]
                            k.mm(Ov[c][0:ni, il, :], pt[0:nk, il * 128:il * 128 + ni], vb[0:nk, j, h, :], (j == 0 and il == 0), False, r=[("PT", ptk), ("vb", j), "vb_ones"], w=[("psO", hb, c)], skip=True)
                    flush()
                    pending.append(pv)

            def epilogue(qg=qg, h=h, hb=hb, nt=nt, nq=nq, Ov=Ov, yb_=yb_):
                n = nq
                e = hb
                for c in range(2):
                    k.op("dve", "reciprocal", out=rr[e][0:n, c, 0:nt, :], in_=Ov[c][0:n, :, 64:65], r=[("psO", hb, c)], w=[("rr", e, c)])
                k.op("dve", "tensor_tensor", out=t1[e][0:n, 0:nt, :], in0=Ov[0][0:n, :, 0:64], in1=rr[e][0:n, 0, 0:nt, :].to_broadcast([n, nt, 64]), op=ALU.mult, r=[("psO", hb, 0), ("rr", e, 0)], w=[("t1", e)])
                k.op("dve", "tensor_tensor", out=t2[e][0:n, 0:nt, :], in0=Ov[1][0:n, :, 0:64], in1=rr[e][0:n, 1, 0:nt, :].to_broadcast([n, nt, 64]), op=ALU.mult, r=[("psO", hb, 1), ("rr", e, 1)], w=[("t2", e)])
                k.op("dve", "scalar_tensor_tensor", out=t1[e][0:n, 0:nt, :], in0=t2[e][0:n, 0:nt, :], scalar=nlam[0:n, 0:1], in1=t1[e][0:n, 0:nt, :], op0=ALU.mult, op1=ALU.add, r=[("t1", e), ("t2", e), "nlam"], w=[("t1", e)])
                k.op("act", "activation", out=t2[e][0:n, 0:nt, :], in_=t1[e][0:n, 0:nt, :], func=AF.Square, r=[("t1", e)], w=[("t2", e)])
                k.op("dve", "tensor_reduce", out=ms[e][0:n, 0:nt, :], in_=t2[e][0:n, 0:nt, :], axis=AX.X, op=ALU.add, r=[("t2", e)], w=[("ms", e)])
                k.op("act", "activation", out=ms[e][0:n, 0:nt, :], in_=ms[e][0:n, 0:nt, :], func=AF.Sqrt, bias=1e-5, scale=1.0 / 64, r=[("ms", e)], w=[("ms", e)])
                k.op("dve", "reciprocal", out=ms[e][0:n, 0:nt, :], in_=ms[e][0:n, 0:nt, :], r=[("ms", e)], w=[("ms", e)])
                k.op("dve", "tensor_tensor", out=t1[e][0:n, 0:nt, :], in0=t1[e][0:n, 0:nt, :], in1=ms[e][0:n, 0:nt, :].to_broadcast([n, nt, 64]), op=ALU.mult, r=[("t1", e), ("ms", e)], w=[("t1", e)])
                k.op("dve", "tensor_tensor", out=yb_[0:n, 0:nt, h, :], in0=t1[e][0:n, 0:nt, :], in1=gvec[0:n].to_broadcast([n, nt, 64]), op=ALU.mult, r=[("t1", e), "gvec"], w=[("ybo", qg % 2, h)])
            pending.append(epilogue)

        def store(qg=qg, gp0=gp0, ntok=ntok, nt=nt, nq=nq, yb_=yb_):
            dst = d["MIX"][gp0:gp0 + ntok, 256:512]
            if nt > 1:
                dst = dst.rearrange("(t p) c -> p t c", p=128)
                src = yb_[:, 0:nt].rearrange("p t h e -> p t (h e)")
            else:
                src = yb_[0:nq, 0].rearrange("p h e -> p (h e)")
            k.dma("sp", dst, src, r=[("ybo", qg % 2, h) for h in range(4)], w=[("MIXb", qg)])
        pending.append(store)
    flush()
    S.barrier()
    A.release()


def stage_conv(k, l):
    S, A, d = k.S, k.A, k.dr
    A.mark()
    gmat = A.t([128, 128], F32, "gmat")
    k.dma("sp", gmat[:], d["gmat"], w=["gmat"])
    Ab = [A.t([128, NPOS], F32, f"cva{i}") for i in range(2)]
    Gb = [A.t([128, NPOS], F32, f"cvg{i}") for i in range(2)]
    hgb = [A.t([128, 30 + NPOS], F32, f"hg{i}") for i in range(2)]
    cwb = [A.t([128, 31], F32, f"cw{i}") for i in range(2)]
    cpb = [A.t([128, 3], F32, f"cp{i}") for i in range(2)]
    ctmp = [A.t([128, NPOS], F32, f"ctmp{i}") for i in range(2)]
    sqb = [A.t([128, 512], F32, f"sqb{i}") for i in range(2)]
    ddb = [A.t([128, 512], F32, f"ddb{i}") for i in range(2)]
    m2b = [A.t([128, 512], F32, f"m2b{i}") for i in range(2)]
    ob = [A.t([128, 512], BF16, f"cob{i}") for i in range(2)]
    H2 = NPOS // 2
    st = {"cc": 0}

    def taps(ct):
        r0 = ct * 128
        a, gate, hg, cw, cp = Ab[ct], Gb[ct], hgb[ct], cwb[ct], cpb[ct]
        acc1, acc2 = a, gate
        RA, RG = ("A", ct), [("G", ct, 0), ("G", ct, 1)]
        k.op("pool", "memset", ap=hg[:, 0:30], constant=0.0, w=[("hgz", ct)])
        k.dma("sp", a[:], d["CV"][r0:r0 + 128, :], w=[RA])
        k.dma("sp", gate[:], d["CV"][256 + r0:256 + r0 + 128, :], w=RG)
        k.dma("act", cw[:], d["conv_wT"][l, r0:r0 + 128, :], w=[("cw", ct)])
        k.dma("act", cp[:, 0:1], d["conv_b"][l, r0:r0 + 128, :], w=[("cp0", ct)])
        k.dma("act", cp[:, 1:2], d["conv_gn_g"][l, r0:r0 + 128, :], w=[("cp1", ct)])
        k.dma("act", cp[:, 2:3], d["conv_gn_b"][l, r0:r0 + 128, :], w=[("cp2", ct)])
        for hh in range(2):
            k.op("act", "activation", out=gate[:, hh * H2:(hh + 1) * H2], in_=gate[:, hh * H2:(hh + 1) * H2], func=AF.Sigmoid, r=[RG[hh]], w=[RG[hh]])
            k.op("dve", "tensor_tensor", out=hg[:, 30 + hh * H2:30 + (hh + 1) * H2], in0=a[:, hh * H2:(hh + 1) * H2], in1=gate[:, hh * H2:(hh + 1) * H2], op=ALU.mult,
                 r=[RA, RG[hh]], w=[("hg", ct, hh)])
        yield
        hres = [("hgz", ct), ("hg", ct, 0), ("hg", ct, 1)]
        cwr = ("cw", ct)
        k.op("dve", "tensor_scalar", out=acc1[:], in0=hg[:, 0:NPOS], scalar1=cw[:, 0:1], scalar2=cp[:, 0:1], op0=ALU.mult, op1=ALU.add, r=hres + [cwr, ("cp0", ct)], w=[RA])
        NDVE = 27
        k.op("act", "activation", out=acc2[:], in_=hg[:, NDVE:NDVE + NPOS], func=AF.Identity, scale=cw[:, NDVE:NDVE + 1], r=hres + [cwr], w=RG)
        extra = list(range(NDVE + 1, 31))
        for j in range(1, NDVE):
            k.op("dve", "scalar_tensor_tensor", out=acc1[:], in0=hg[:, j:j + NPOS], scalar=cw[:, j:j + 1], in1=acc1[:], op0=ALU.mult, op1=ALU.add, r=hres + [cwr, RA], w=[RA])
            if j % 8 == 1 and extra:
                jj = extra.pop(0)
                tb = jj % 2
                k.op("act", "activation", out=ctmp[tb][:], in_=hg[:, jj:jj + NPOS], func=AF.Identity, scale=cw[:, jj:jj + 1], r=hres + [cwr], w=[("ctmp", tb)])
                k.op("pool", "tensor_tensor", out=acc2[:], in0=acc2[:], in1=ctmp[tb][:], op=ALU.add, r=[("ctmp", tb)] + RG, w=RG)
            if j % 3 == 0:
                yield
        assert not extra
        k.op("dve", "tensor_tensor", out=acc1[:], in0=acc1[:], in1=acc2[:], op=ALU.add, r=[RA] + RG, w=[RA])

    def gn(ct):
        r0 = ct * 128
        acc1, cp = Ab[ct], cpb[ct]
        RA = ("A", ct)
        for c0 in range(0, NPOS, 512):
            n = min(512, NPOS - c0)
            b = st["cc"] % 2
            st["cc"] += 1
            psM, psE = k.ps(2 * b), k.ps(2 * b + 1)
            k.mm(psM[:, 0:n], gmat[:], acc1[:, c0:c0 + n], True, True, r=["gmat", RA], w=[("psM", b)])
            k.op("dve", "tensor_tensor", out=ddb[b][:, 0:n], in0=acc1[:, c0:c0 + n], in1=psM[:, 0:n], op=ALU.subtract, r=[RA, ("psM", b)], w=[("ddb", b)])
            k.op("act", "activation", out=sqb[b][:, 0:n], in_=ddb[b][:, 0:n], func=AF.Square, r=[("ddb", b)], w=[("sqb", b)])
            k.mm(psE[:, 0:n], gmat[:], sqb[b][:, 0:n], True, True, r=["gmat", ("sqb", b)], w=[("psE", b)])
            k.op("act", "activation", out=m2b[b][:, 0:n], in_=psE[:, 0:n], func=AF.Sqrt, bias=1e-5, scale=1.0, r=[("psE", b)], w=[("m2b", b)])
            k.op("dve", "reciprocal", out=m2b[b][:, 0:n], in_=m2b[b][:, 0:n], r=[("m2b", b)], w=[("m2b", b)])
            k.op("dve", "tensor_tensor", out=ddb[b][:, 0:n], in0=ddb[b][:, 0:n], in1=m2b[b][:, 0:n], op=ALU.mult, r=[("ddb", b), ("m2b", b)], w=[("ddb", b)])
            k.op("act", "activation", out=ob[b][:, 0:n], in_=ddb[b][:, 0:n], func=AF.Silu, bias=cp[:, 2:3], scale=cp[:, 1:2], r=[("ddb", b), ("cp1", ct), ("cp2", ct)], w=[("cob", b)])
            k.dma("sp", d["MIXD"][r0:r0 + 128, c0:c0 + n], ob[b][:, 0:n], r=[("cob", b)], w=[("MIXD", ct, c0)])
            yield

    for _ in taps(0):
        pass
    g0, t1 = gn(0), taps(1)
    alive = [g0, t1]
    while alive:
        for g in list(alive):
            try:
                next(g)
            except StopIteration:
                alive.remove(g)
    for _ in gn(1):
        pass
    S.barrier()
    A.release()


def stage_out(k, l):
    S, A, d = k.S, k.A, k.dr
    A.mark()
    idt = A.t([128, 128], BF16, "ident")
    k.dma("sp", idt[:], d["ident_bf"], w=["ident"])
    wo = A.t([128, 8, 1024], BF16, "wo")
    for kk in range(8):
        k.dma("pool", wo[:, kk, :], d["w_out"][l, kk * 128:(kk + 1) * 128, :], w=[("wo", kk)])
    wres = [("wo", kk) for kk in range(8)]
    gb = A.t([128, 1024], F32, "gb"); bb = A.t([128, 1024], F32, "bb")
    load_bcast(k, gb, d["ln1_g"][l:l + 1, :], "gb"); load_bcast(k, bb, d["ln1_b"][l:l + 1, :], "bb")
    nb = 4
    mxs = [A.t([128, 768], BF16, f"mx{i}") for i in range(nb)]
    mxds = [A.t([128, 2, 128], BF16, f"mxd{i}") for i in range(nb)]
    mTs = [A.t([128, 6, 128], BF16, f"mT{i}") for i in range(nb)]
    hs = [A.t([128, 1024], F32, f"h{i}") for i in range(nb)]
    zs = [A.t([128, 1024], F32, f"z{i}") for i in range(nb)]
    ys = [A.t([128, 1024], F32, f"y{i}") for i in range(nb)]
    tmps = [ln_tmp(k, str(i)) for i in range(nb)]

    def body(t):
        p0, n = tile_rows(t)
        b = t % nb
        mx, mxd, mT, h, z, y = mxs[b], mxds[b], mTs[b], hs[b], zs[b], ys[b]
        k.dma("sp", mx[0:n, :], d["MIX"][p0:p0 + n, :], w=[("mx", b)])
        for c in range(2):
            k.dma("sp", mxd[:, c, 0:n], d["MIXD"][c * 128:(c + 1) * 128, p0:p0 + n], w=[("mxd", b, c)])
        k.dma("act", h[0:n, :], d["H"][p0:p0 + n, :], w=[("h", b)])
        tb = t % 2
        pt = k.ps(tb)[:].bitcast(BF16)
        for kk in range(6):
            k.tr(pt[:, kk * 128:kk * 128 + n], mx[0:n, kk * 128:(kk + 1) * 128], idt[0:n, 0:n], r=[("mx", b), "ident"], w=[("ps", tb)])
        k.op("dve", "tensor_copy", out=mT[:, :, 0:n], in_=pt[:, 0:768].rearrange("p (k t) -> p k t", k=6)[:, :, 0:n], r=[("ps", tb)], w=[("mT", b)])
        yield
        for half in range(2):
            pb = 2 + 2 * (t % 3) + half
            for kk in range(8):
                lhsT = mT[:, kk, 0:n] if kk < 6 else mxd[:, kk - 6, 0:n]
                k.mm(k.ps(pb)[0:n, :], lhsT, wo[:, kk, half * 512:(half + 1) * 512], kk == 0, kk == 7,
                     r=[("mT", b), ("mxd", b, 0), ("mxd", b, 1)] + wres, w=[("ps", pb)])
            k.op("dve", "scalar_tensor_tensor", out=z[0:n, half * 512:(half + 1) * 512], in0=h[0:n, half * 512:(half + 1) * 512], scalar=ALPHA, in1=k.ps(pb)[0:n, :],
                 op0=ALU.mult, op1=ALU.add, r=[("h", b), ("ps", pb)], w=[("z", b)])
        yield
        yield from layernorm_tile_g(k, z, y, n, b, gb, bb, tmp=tmps[b])
        k.dma("act", d["HM"][p0:p0 + n, :], y[0:n, :], r=[("y", b)], w=[("HM", t)])
    run_skewed([body(t) for t in range(NT)])
    S.barrier()
    A.release()


def stage_moe(k, l, last=False):
    S, A, d = k.S, k.A, k.dr
    A.mark()
    comb = A.t([128, NT, 16], F32, "comb")
    A.mark()
    idf = A.t([128, 128], F32, "identf")
    k.dma("sp", idf[:], d["ident_f"], w=["identf"])
    rw = A.t([128, 8, 16], F32, "rw")
    k.dma("sp", rw[:], d["router_w"].rearrange("(k p) e -> p k e", p=128), w=["rw"])
    rb = A.t([128, 16], F32, "rb")
    load_bcast(k, rb, d["router_b"], "rb")
    nb1 = 4
    hms = [A.t([128, 1024], F32, f"hm{i}") for i in range(nb1)]
    hT32 = [A.t([128, 8, 128], F32, f"hT32{i}") for i in range(nb1)]
    hTb = [A.t([128, 8, 128], BF16, f"hTb{i}") for i in range(nb1)]
    rt = [dict((nm, A.t([128, 16], F32, f"{nm}{i}")) for nm in ("sc", "bi", "eq", "b2", "mk", "s1", "mk2", "s2")) for i in range(nb1)]
    rs4 = [dict((nm, A.t([128, 4], F32, f"{nm}{i}")) for nm in ("m1", "m2", "gs", "ing")) for i in range(nb1)]
    rs1 = [dict((nm, A.t([128, 1], F32, f"{nm}{i}")) for nm in ("gm", "t1", "t2", "den")) for i in range(nb1)]

    def body1(t):
        p0, n = tile_rows(t)
        b = t % nb1
        hm = hms[b]
        k.dma("sp", hm[0:n, :], d["HM"][p0:p0 + n, :], r=[("HM", t)], w=[("hm", b)])
        b3 = t % 3
        for kk in range(8):
            pb = 2 * b3 + kk // 4
            k.tr(k.ps(pb)[:, (kk % 4) * 128:(kk % 4) * 128 + n], hm[0:n, kk * 128:(kk + 1) * 128], idf[0:n, 0:n], r=[("hm", b), "identf"], w=[("ps", pb)])
        for hf in range(2):
            pb = 2 * b3 + hf
            src = k.ps(pb)[:].rearrange("p (k t) -> p k t", k=4)[:, :, 0:n]
            k.op("act", "copy", out=hT32[b][:, 4 * hf:4 * hf + 4, 0:n], in_=src, r=[("ps", pb)], w=[("hT32", b, hf)])
            k.op("dve", "tensor_copy", out=hTb[b][:, 4 * hf:4 * hf + 4, 0:n], in_=src, r=[("ps", pb)], w=[("hTb", b, hf)])
        k.dma("act", d["HT"][:, p0:p0 + n].rearrange("(k p) t -> p k t", p=128), hTb[b][:, :, 0:n], r=[("hTb", b, 0), ("hTb", b, 1)], w=[("HT", t)])
        yield
        pr = k.ps(6 + t % 2)
        for kk in range(8):
            k.mm(pr[0:n, 0:16], hT32[b][:, kk, 0:n], rw[:, kk, :], kk == 0, kk == 7, r=[("hT32", b, 0), ("hT32", b, 1), "rw"], w=[("psr", t % 2)])
        R_, R4, R1 = rt[b], rs4[b], rs1[b]
        rk = lambda nm: ("rt", nm, b)
        v4 = lambda ap: ap[0:n, :].rearrange("p (g e) -> p g e", g=4)
        k.op("act", "activation", out=R_["sc"][0:n, :], in_=pr[0:n, 0:16], func=AF.Sigmoid, r=[("psr", t % 2)], w=[rk("sc")])
        yield
        k.op("dve", "tensor_tensor", out=R_["bi"][0:n, :], in0=R_["sc"][0:n, :], in1=rb[0:n, :], op=ALU.add, r=[rk("sc"), "rb"], w=[rk("bi")])
        k.op("dve", "tensor_reduce", out=R4["m1"][0:n, :], in_=v4(R_["bi"]), axis=AX.X, op=ALU.max, r=[rk("bi")], w=[rk("m1")])
        k.op("dve", "tensor_tensor", out=v4(R_["eq"]), in0=v4(R_["bi"]), in1=R4["m1"][0:n, :].unsqueeze(2).to_broadcast([n, 4, 4]), op=ALU.is_equal, r=[rk("bi"), rk("m1")], w=[rk("eq")])
        k.op("dve", "scalar_tensor_tensor", out=R_["b2"][0:n, :], in0=R_["eq"][0:n, :], scalar=NEG, in1=R_["bi"][0:n, :], op0=ALU.mult, op1=ALU.add, r=[rk("eq"), rk("bi")], w=[rk("b2")])
        k.op("dve", "tensor_reduce", out=R4["m2"][0:n, :], in_=v4(R_["b2"]), axis=AX.X, op=ALU.max, r=[rk("b2")], w=[rk("m2")])
        k.op("dve", "tensor_tensor", out=R4["gs"][0:n, :], in0=R4["m1"][0:n, :], in1=R4["m2"][0:n, :], op=ALU.add, r=[rk("m1"), rk("m2")], w=[rk("gs")])
        k.op("dve", "tensor_reduce", out=R1["gm"][0:n, :], in_=R4["gs"][0:n, :], axis=AX.X, op=ALU.max, r=[rk("gs")], w=[rk("gm")])
        k.op("dve", "tensor_scalar", out=R4["ing"][0:n, :], in0=R4["gs"][0:n, :], scalar1=R1["gm"][0:n, 0:1], scalar2=None, op0=ALU.is_equal, r=[rk("gs"), rk("gm")], w=[rk("ing")])
        k.op("dve", "tensor_scalar", out=R4["ing"][0:n, :], in0=R4["ing"][0:n, :], scalar1=1.0, scalar2=-NEG, op0=ALU.subtract, op1=ALU.mult, r=[rk("ing")], w=[rk("ing")])
        k.op("dve", "tensor_tensor", out=v4(R_["mk"]), in0=v4(R_["bi"]), in1=R4["ing"][0:n, :].unsqueeze(2).to_broadcast([n, 4, 4]), op=ALU.add, r=[rk("bi"), rk("ing")], w=[rk("mk")])
        k.op("dve", "tensor_reduce", out=R1["t1"][0:n, :], in_=R_["mk"][0:n, :], axis=AX.X, op=ALU.max, r=[rk("mk")], w=[rk("t1")])
        k.op("dve", "tensor_scalar", out=R_["s1"][0:n, :], in0=R_["mk"][0:n, :], scalar1=R1["t1"][0:n, 0:1], scalar2=None, op0=ALU.is_equal, r=[rk("mk"), rk("t1")], w=[rk("s1")])
        k.op("dve", "scalar_tensor_tensor", out=R_["mk2"][0:n, :], in0=R_["s1"][0:n, :], scalar=NEG, in1=R_["mk"][0:n, :], op0=ALU.mult, op1=ALU.add, r=[rk("s1"), rk("mk")], w=[rk("mk2")])
        k.op("dve", "tensor_reduce", out=R1["t2"][0:n, :], in_=R_["mk2"][0:n, :], axis=AX.X, op=ALU.max, r=[rk("mk2")], w=[rk("t2")])
        k.op("dve", "tensor_scalar", out=R_["s2"][0:n, :], in0=R_["mk2"][0:n, :], scalar1=R1["t2"][0:n, 0:1], scalar2=None, op0=ALU.is_equal, r=[rk("mk2"), rk("t2")], w=[rk("s2")])
        k.op("dve", "tensor_tensor", out=R_["s1"][0:n, :], in0=R_["s1"][0:n, :], in1=R_["s2"][0:n, :], op=ALU.add, r=[rk("s1"), rk("s2")], w=[rk("s1")])
        k.op("dve", "tensor_tensor", out=R_["s1"][0:n, :], in0=R_["s1"][0:n, :], in1=R_["sc"][0:n, :], op=ALU.mult, r=[rk("s1"), rk("sc")], w=[rk("s1")])
        k.op("dve", "tensor_reduce", out=R1["den"][0:n, :], in_=R_["s1"][0:n, :], axis=AX.X, op=ALU.add, r=[rk("s1")], w=[rk("den")])
        k.op("dve", "reciprocal", out=R1["den"][0:n, :], in_=R1["den"][0:n, :], r=[rk("den")], w=[rk("den")])
        k.op("dve", "tensor_scalar", out=comb[0:n, t, :], in0=R_["s1"][0:n, :], scalar1=R1["den"][0:n, 0:1], scalar2=None, op0=ALU.mult, r=[rk("s1"), rk("den")], w=[("comb", t)])
    run_skewed([body1(t) for t in range(NT)], serial=True)
    S.barrier()
    A.release()
    gb = A.t([128, 1024], F32, "gb"); bb = A.t([128, 1024], F32, "bb")
    load_bcast(k, gb, d["ln2_g"][l:l + 1, :], "gb"); load_bcast(k, bb, d["ln2_b"][l:l + 1, :], "bb")
    hTh = A.t([128, 8, 2064], BF16, "hTh")
    acc = A.t([128, 17, 1024], F32, "acc")
    w1s = [A.t([128, 8, 512], BF16, f"w1s{i}") for i in range(2)]
    w3s = [A.t([128, 8, 512], BF16, f"w3s{i}") for i in range(2)]
    w2s = [A.t([128, 4, 1024], BF16, f"w2s{i}") for i in range(2)]
    actT = [A.t([128, 4, 256], BF16, f"actT{i}") for i in range(2)]
    s1b = [A.t([128, 256], F32, f"s1b{i}") for i in range(2)]
    nb2 = 2
    z2s = [A.t([128, 1024], F32, f"z2{i}") for i in range(nb2)]; y2s = [A.t([128, 1024], F32, f"y2{i}") for i in range(nb2)]
    tmp2s = [ln_tmp(k, f"m{i}") for i in range(nb2)]
    ecount = 0
    fcount = 0
    ocount = 0
    gcount = 0
    pending = []
    for half, tiles in enumerate([list(range(0, 17)), list(range(17, 33))]):
        hp0 = tile_rows(tiles[0])[0]
        hn = sum(tile_rows(t)[1] for t in tiles)
        for kk in range(8):
            k.dma("sp", hTh[:, kk, 0:hn], d["HT"][kk * 128:(kk + 1) * 128, hp0:hp0 + hn], r=[("HT", t) for t in tiles], w=[("hTh", kk)])
        hres = [("hTh", kk) for kk in range(8)]
        groups = []
        tl = list(tiles)
        if tl[0] == 0:
            groups.append([0]); tl = tl[1:]
        for i in range(0, len(tl), 2):
            groups.append(tl[i:i + 2])
        for e in range(16):
            eb = ecount % 2
            ecount += 1
            k.dma("pool", w1s[eb][:], d["exp_w1"][l, e].rearrange("(k p) f -> p k f", p=128), w=[("w1s", eb)])
            k.dma("pool", w3s[eb][:], d["exp_w3"][l, e].rearrange("(k p) f -> p k f", p=128), w=[("w3s", eb)])
            k.dma("pool", w2s[eb][:], d["exp_w2"][l, e].rearrange("(k p) f -> p k f", p=128), w=[("w2s", eb)])
            for grp in groups:
                c0 = tile_rows(grp[0])[0] - hp0
                ng = sum(tile_rows(t)[1] for t in grp)
                ab = gcount % 2
                gcount += 1
                for f in range(4):
                    hb_ = fcount % 2
                    fcount += 1
                    ph = k.ps(hb_)
                    for kk in range(8):
                        k.mm(ph[:, 0:ng], w1s[eb][:, kk, f * 128:(f + 1) * 128], hTh[:, kk, c0:c0 + ng], kk == 0, kk == 7, r=hres + [("w1s", eb)], w=[("psh", hb_)])
                    for kk in range(8):
                        k.mm(ph[:, 256:256 + ng], w3s[eb][:, kk, f * 128:(f + 1) * 128], hTh[:, kk, c0:c0 + ng], kk == 0, kk == 7, r=hres + [("w3s", eb)], w=[("psh", hb_)])
                    k.op("act", "activation", out=s1b[hb_][:, 0:ng], in_=ph[:, 0:ng], func=AF.Silu, r=[("psh", hb_)], w=[("s1b", hb_)])
                    k.op("dve", "tensor_tensor", out=actT[ab][:, f, 0:ng], in0=s1b[hb_][:, 0:ng], in1=ph[:, 256:256 + ng], op=ALU.mult, r=[("s1b", hb_), ("psh", hb_)], w=[("actT", ab, f)])
                def phase2(grp=grp, ab=ab, eb=eb, e=e, tiles=tiles):
                    nonlocal ocount
                    for ti, t in enumerate(grp):
                        nt_ = tile_rows(t)[1]
                        tloc = t - tiles[0]
                        for h2 in range(2):
                            ob_ = 2 + ocount % 6
                            ocount += 1
                            po = k.ps(ob_)
                            for f in range(4):
                                k.mm(po[0:nt_, :], actT[ab][:, f, ti * 128:ti * 128 + nt_], w2s[eb][:, f, h2 * 512:(h2 + 1) * 512], f == 0, f == 3, r=[("actT", ab, f), ("w2s", eb)], w=[("pso", ob_)])
                            dst = acc[0:nt_, tloc, h2 * 512:(h2 + 1) * 512]
                            if e == 0:
                                k.op("dve", "tensor_scalar", out=dst, in0=po[0:nt_, :], scalar1=comb[0:nt_, t, e:e + 1], scalar2=None, op0=ALU.mult, r=[("pso", ob_), ("comb", t)], w=[("acc", tloc, h2)])
                            else:
                                k.op("dve", "scalar_tensor_tensor", out=dst, in0=po[0:nt_, :], scalar=comb[0:nt_, t, e:e + 1], in1=dst, op0=ALU.mult, op1=ALU.add, r=[("pso", ob_), ("comb", t), ("acc", tloc, h2)], w=[("acc", tloc, h2)])
                while pending:
                    pending.pop(0)()
                pending.append(phase2)
        while pending:
            pending.pop(0)()
        def body2(t, tiles=tiles):
            p0, n = tile_rows(t)
            tloc = t - tiles[0]
            b = t % nb2
            z, y = z2s[b], y2s[b]
            key = ("m", b)
            k.dma("sp", z[0:n, :], d["HM"][p0:p0 + n, :], r=[("HM", t)], w=[("z", key)])
            k.op("dve", "scalar_tensor_tensor", out=z[0:n, :], in0=z[0:n, :], scalar=ALPHA, in1=acc[0:n, tloc, :], op0=ALU.mult, op1=ALU.add, r=[("z", key), ("acc", tloc, 0), ("acc", tloc, 1)], w=[("z", key)])
            yield
            yield from layernorm_tile_g(k, z, y, n, key, gb, bb, tmp=tmp2s[b])
            if last:
                if t >= 1:
                    k.dma("act", d["out"][128 * (t - 1):128 * t, :], y[0:n, :], r=[("y", key)], w=[("outT", t)])
            else:
                k.dma("act", d["H"][p0:p0 + n, :], y[0:n, :], r=[("y", key)], w=[("H", t)])
        run_skewed([body2(t) for t in tiles])
        S.barrier()
    A.release()


def run_skewed(gens, window=None, serial=False):
    if serial:
        for g in gens:
            for _ in g:
                pass
        return
    active = []
    it = iter(gens)
    while True:
        if window is None or len(active) < window:
            g = next(it, None)
            if g is not None:
                active.append(g)
        if not active:
            break
        for g in list(active):
            try:
                next(g)
            except StopIteration:
                active.remove(g)


def stage_rwkv_a(k, l):
    S, A, d = k.S, k.A, k.dr
    A.mark()
    cnt = [0]

    def T(shape, dt, name):
        cnt[0] += 1
        return TB(A.t(shape, dt, name), (name, cnt[0]))

    P = [TB(k.ps(i), ("ps", i)) for i in range(8)]
    DV = lambda ap: Vw(ap, [])
    idt = T([128, 128], BF16, "ident"); k.do("sp", "dma_start", idt[:], in_=DV(d["ident_bf"]))
    mu_b = T([128, 1024], F32, "mu_b"); k.do("sp", "dma_start", mu_b[:], in_=DV(d["rwkv_mu"][l:l + 1, :].partition_broadcast(128)))
    wa0 = T([128, 512], F32, "wa0")
    k.do("sp", "dma_start", wa0[:, 0:256], in_=DV(d["rwkv_w0"][l:l + 1, :].partition_broadcast(128)))
    k.do("sp", "dma_start", wa0[:, 256:512], in_=DV(d["rwkv_a0"][l:l + 1, :].partition_broadcast(128)))
    kk_b = T([128, 256], F32, "kk_b"); k.do("sp", "dma_start", kk_b[:], in_=DV(d["rwkv_kk"][l:l + 1, :].partition_broadcast(128)))
    ka_b = T([128, 256], F32, "ka_b"); k.do("sp", "dma_start", ka_b[:], in_=DV(d["rwkv_ka"][l:l + 1, :].partition_broadcast(128)))
    rk_b = T([128, 256], F32, "rk_b"); k.do("sp", "dma_start", rk_b[:], in_=DV(d["rwkv_rk"][l:l + 1, :].partition_broadcast(128)))
    WA = T([128, 512], BF16, "WA")
    k.do("pool", "memset", WA[:], constant=0.0)
    k.do("pool", "dma_start", WA[0:64, 0:256], in_=DV(d["rwkv_w2"][l]))
    k.do("pool", "dma_start", WA[64:128, 256:512], in_=DV(d["rwkv_a2"][l]))
    G2 = T([128, 256], BF16, "G2"); k.do("pool", "dma_start", G2[:], in_=DV(d["rwkv_g2"][l]))
    ctri = T([128, 128], F32, "ctri"); k.do("sp", "dma_start", ctri[:], in_=DV(d["ctri"]))
    cblk = T([128, 128], F32, "cblk"); k.do("sp", "dma_start", cblk[:], in_=DV(d["cblk"]))
    cones = T([128, 2, 64], F32, "cones"); k.do("sp", "dma_start", cones[:], in_=DV(d["cones"]))
    idrep = T([64, 1, 256], F32, "idrep"); k.do("sp", "dma_start", idrep[:].re("p o f -> p (o f)"), in_=DV(d["identrep"]))
    zrow = T([1, 1024], F32, "zrow")
    k.do("pool", "memset", zrow[:], constant=0.0)
    uc0 = ("UC0",)
    k.do("sp", "dma_start", Vw(d["UC"][0:1, :], [uc0]), in_=zrow[:])
    nb = 5
    mk_ = lambda shape, dt, nm, n=None: [T(shape, dt, f"{nm}{i}") for i in range(n or nb)]
    cur_, prv_, ucs_ = mk_([128, 1024], F32, "cur", 2), mk_([128, 1024], F32, "prv", 2), mk_([128, 1024], F32, "ucs", 6)
    LI_, LT_ = mk_([128, 256], BF16, "LI"), mk_([128, 2, 128], BF16, "LT")
    wa_, logw_ = mk_([128, 512], F32, "wa"), mk_([128, 256], F32, "logw")
    W_, Wi_, Wp_, Web_ = mk_([128, 256], F32, "W"), mk_([128, 256], F32, "Wi"), mk_([128, 256], F32, "Wp"), mk_([128, 256], F32, "Web")
    DWe_ = mk_([64, 2, 256], F32, "DWe"); DWt_ = mk_([64, 2, 256], BF16, "DWt")
    kq_, sq_, kkn_, kmod_, bb_, t1_ = (mk_([128, 256], F32, nm) for nm in ("kq", "sq", "kkn", "kmod", "bb", "t1"))
    ss_, rs_ = mk_([128, 4, 1], F32, "ss"), mk_([128, 4, 1], F32, "rs")
    kh_, bh_ = mk_([128, 256], F32, "kh"), mk_([128, 256], F32, "bh")
    OT_ = mk_([128, 7, 256], BF16, "OT"); BG_ = mk_([128, 2, 256], F32, "BGt")
    NE5 = -math.exp(-0.5)
    def body(t):
        p0, n = tile_rows(t)
        b = t % nb
        C = 16 if t == 0 else 64
        ncn = 1 if t == 0 else 2
        ch0 = 0 if t == 0 else 2 * (t - 1) + 1
        cur, prv, ucs, LI, LT, wa, logw = cur_[t % 2], prv_[t % 2], ucs_[t % 6], LI_[b], LT_[b], wa_[b], logw_[b]
        W, Wi, Wp, Web, DWe, DWt = W_[b], Wi_[b], Wp_[b], Web_[b], DWe_[b], DWt_[b]
        kq, sq, kkn, kmod, bb, t1, ss, rs, kh, bh, OT, BGt = kq_[b], sq_[b], kkn_[b], kmod_[b], bb_[b], t1_[b], ss_[b], rs_[b], kh_[b], bh_[b], OT_[b], BG_[b]
        k.do("sp", "dma_start", cur[0:n, :], in_=DV(d["UC"][1 + p0:1 + p0 + n, :]))
        k.do("sp", "dma_start", prv[0:n, :], in_=Vw(d["UC"][p0:p0 + n, :], [uc0] if t == 0 else []))
        k.do("dve", "tensor_tensor", prv[0:n, :], in0=prv[0:n, :], in1=cur[0:n, :], op=ALU.subtract)
        k.do("dve", "tensor_tensor", prv[0:n, :], in0=prv[0:n, :], in1=mu_b[0:n, :], op=ALU.mult)
        k.do("dve", "tensor_tensor", ucs[0:n, :], in0=cur[0:n, :], in1=prv[0:n, :], op=ALU.add)
        r_, kraw, v_ = ucs[0:n, 0:256], ucs[0:n, 256:512], ucs[0:n, 512:768]
        yield
        k.do("act", "activation", LI[0:n, 0:64], in_=ucs[0:n, 768:832], func=AF.Tanh)
        k.do("act", "copy", LI[0:n, 64:128], in_=ucs[0:n, 832:896])
        k.do("act", "activation", LI[0:n, 128:256], in_=ucs[0:n, 896:1024], func=AF.Sigmoid)
        ptb = Vw(k.ps(0)[:].bitcast(BF16), P[0].res)
        k.trv(ptb[:, 0:n], LI[0:n, 0:128], idt[0:n, 0:n])
        k.trv(ptb[:, 128:128 + n], LI[0:n, 128:256], idt[0:n, 0:n])
        k.do("dve", "tensor_copy", LT[:, :, 0:n], in_=ptb[:, 0:256].re("p (a t) -> p a t", a=2)[:, :, 0:n])
        yield
        k.mmv(P[1][0:n, :], LT[:, 0, 0:n], WA[:], True, True)
        k.mmv(P[2][0:n, 0:256], LT[:, 1, 0:n], G2[:], True, True)
        k.do("dve", "tensor_tensor", wa[0:n, :], in0=P[1][0:n, :], in1=wa0[0:n, :], op=ALU.add)
        k.do("act", "activation", wa[0:n, :], in_=wa[0:n, :], func=AF.Sigmoid)
        a_ = wa[0:n, 256:512]
        k.do("act", "mul", logw[0:n, :], in_=wa[0:n, 0:256], mul=NE5)
        k.do("act", "copy", BGt[0:n, 1, :], in_=P[2][0:n, 0:256])
        yield
        k.mmv(P[3][0:n, 0:256], ctri[0:n, 0:n], logw[0:n, :], True, True)
        k.mmv(P[4][0:n, 0:256], cblk[0:n, 0:n], logw[0:n, :], True, True)
        for c in range(ncn):
            k.mmv(P[5][0:64, c * 256:(c + 1) * 256], cones[0:n, c, :], logw[0:n, :], True, True)
        k.do("act", "activation", W[0:n, :], in_=P[3][0:n, 0:256], func=AF.Exp)
        k.do("act", "activation", Wi[0:n, :], in_=P[3][0:n, 0:256], func=AF.Exp, scale=-1.0)
        k.do("dve", "tensor_tensor", Wp[0:n, :], in0=P[3][0:n, 0:256], in1=logw[0:n, :], op=ALU.subtract)
        k.do("act", "activation", Wp[0:n, :], in_=Wp[0:n, :], func=AF.Exp)
        k.do("act", "activation", Web[0:n, :], in_=P[4][0:n, 0:256], func=AF.Exp)
        k.do("act", "activation", DWe[:, 0:ncn, :], in_=P[5][0:64, 0:ncn * 256].re("p (c f) -> p c f", c=ncn), func=AF.Exp)
        k.do("dve", "tensor_tensor", DWt[:, 0:ncn, :], in0=DWe[:, 0:ncn, :], in1=idrep[:].bc([64, ncn, 256]), op=ALU.mult)
        k.do("act", "dma_start", DV(d["DW"][ch0:ch0 + ncn].rearrange("c p f -> p c f")), in_=DWt[:, 0:ncn, :])
        yield
        k.do("dve", "tensor_tensor", kq[0:n, :], in0=kraw, in1=kk_b[0:n, :], op=ALU.mult)
        k.do("act", "activation", sq[0:n, :], in_=kq[0:n, :], func=AF.Square)
        k.do("dve", "tensor_reduce", ss[0:n], in_=sq[0:n, :].re("p (h e) -> p h e", h=4), axis=AX.X, op=ALU.add)
        k.do("act", "activation", ss[0:n], in_=ss[0:n], func=AF.Sqrt)
        k.do("dve", "tensor_scalar", ss[0:n], in0=ss[0:n], scalar1=1e-12, scalar2=None, op0=ALU.max)
        k.do("dve", "reciprocal", ss[0:n], in_=ss[0:n])
        k.do("dve", "tensor_tensor", kkn[0:n, :].re("p (h e) -> p h e", h=4), in0=kq[0:n, :].re("p (h e) -> p h e", h=4), in1=ss[0:n].bc([n, 4, 64]), op=ALU.mult)
        k.do("dve", "scalar_tensor_tensor", t1[0:n, :], in0=a_, scalar=-1.0, in1=ka_b[0:n, :], op0=ALU.add, op1=ALU.mult)
        k.do("dve", "scalar_tensor_tensor", kmod[0:n, :], in0=t1[0:n, :], scalar=1.0, in1=kraw, op0=ALU.add, op1=ALU.mult)
        k.do("dve", "tensor_tensor", bb[0:n, :], in0=kkn[0:n, :], in1=a_, op=ALU.mult)
        k.do("pool", "tensor_tensor", t1[0:n, :], in0=r_, in1=kmod[0:n, :], op=ALU.mult)
        k.do("pool", "tensor_tensor", t1[0:n, :], in0=t1[0:n, :], in1=rk_b[0:n, :], op=ALU.mult)
        k.do("dve", "tensor_reduce", rs[0:n], in_=t1[0:n, :].re("p (h e) -> p h e", h=4), axis=AX.X, op=ALU.add)
        k.do("dve", "tensor_tensor", BGt[0:n, 0, :].re("p (h e) -> p h e", h=4), in0=v_.re("p (h e) -> p h e", h=4), in1=rs[0:n].bc([n, 4, 64]), op=ALU.mult)
        yield
        k.do("dve", "tensor_tensor", OT[0:n, 0, :], in0=kkn[0:n, :], in1=Wp[0:n, :], op=ALU.mult)
        k.do("dve", "tensor_tensor", OT[0:n, 1, :], in0=r_, in1=W[0:n, :], op=ALU.mult)
        k.do("dve", "tensor_tensor", kh[0:n, :], in0=kmod[0:n, :], in1=Wi[0:n, :], op=ALU.mult)
        k.do("pool", "tensor_tensor", bh[0:n, :], in0=bb[0:n, :], in1=Wi[0:n, :], op=ALU.mult)
        k.do("act", "copy", OT[0:n, 2, :], in_=kh[0:n, :])
        k.do("act", "copy", OT[0:n, 3, :], in_=bh[0:n, :])
        k.do("dve", "tensor_tensor", OT[0:n, 4, :], in0=kh[0:n, :], in1=Web[0:n, :], op=ALU.mult)
        k.do("dve", "scalar_tensor_tensor", OT[0:n, 5, :], in0=bh[0:n, :], scalar=-1.0, in1=Web[0:n, :], op0=ALU.mult, op1=ALU.mult)
        k.do("act", "copy", OT[0:n, 6, :], in_=v_)
        k.do("sp", "dma_start", DV(d["RWT"][p0:p0 + n]), in_=OT[0:n])
        k.do("sp", "dma_start", DV(d["BG"][p0:p0 + n]), in_=BGt[0:n])
    run_skewed([body(t) for t in range(NT)])
    S.barrier()
    A.release()


def stage_rwkv_b(k, l, tmax=NT):
    S, A, d = k.S, k.A, k.dr
    A.mark()
    cnt = [0]

    def T(shape, dt, name):
        cnt[0] += 1
        return TB(A.t(shape, dt, name), (name, cnt[0]))

    P = [TB(k.ps(i), ("ps", i)) for i in range(8)]
    DV = lambda ap: Vw(ap, [])
    idt = T([128, 128], BF16, "ident"); k.do("sp", "dma_start", idt[:], in_=DV(d["ident_bf"]))
    rmask = T([64, 5, 64], F32, "rmask"); k.do("sp", "dma_start", rmask[:], in_=DV(d["rmask"]))
    lg_b = T([64, 256], F32, "lg_b"); k.do("sp", "dma_start", lg_b[:], in_=DV(d["rwkv_lnx_g"][l:l + 1, :].partition_broadcast(64)))
    lb_b = T([64, 256], F32, "lb_b"); k.do("sp", "dma_start", lb_b[:], in_=DV(d["rwkv_lnx_b"][l:l + 1, :].partition_broadcast(64)))
    Tb = [T([64, 4, 64], BF16, f"Tst{i}") for i in range(2)]
    k.do("pool", "memset", Tb[0][:], constant=0.0)
    nb = 3
    mk_ = lambda shape, dt, nm, n=None: [T(shape, dt, f"{nm}{i}") for i in range(n or nb)]
    tokX_ = mk_([64, 2, 7, 256], BF16, "tokX"); DWc_ = mk_([64, 2, 256], BF16, "DWc"); BGc_ = mk_([64, 2, 2, 256], F32, "BGc")
    XT_ = mk_([64, 2, 4, 4, 64], BF16, "XT")
    AT_ = mk_([64, 8, 2, 64], BF16, "AT"); BT_ = mk_([64, 8, 2, 64], BF16, "BT"); Nn_ = mk_([64, 8, 64], BF16, "Nn")
    NPa_ = [mk_([64, 8, 64], BF16, f"NPa{i}_", 2) for i in range(nb)]; NPTa_ = [mk_([64, 8, 64], BF16, f"NPTa{i}_", 2) for i in range(nb)]
    Zf_ = mk_([64, 8, 128], F32, "Zf"); Zb_ = mk_([64, 8, 128], BF16, "Zb")
    Q1T_ = mk_([64, 8, 64], BF16, "Q1T"); Q2s_ = mk_([64, 8, 64], F32, "Q2s"); GT_ = mk_([64, 8, 64], BF16, "GT"); Hs_ = mk_([64, 8, 64], F32, "Hs")
    ys_ = mk_([64, 4, 64], F32, "ys", 2); dd_ = mk_([64, 4, 64], F32, "dd", 2); sq_ = mk_([64, 4, 64], F32, "sq", 2)
    st_ = mk_([64, 4, 1], F32, "st", 2); yo_ = mk_([64, 256], BF16, "yo", 2)
    state = {"tcur": 0, "ccount": 0}

    def body(t):
        p0, n = tile_rows(t)
        b = t % nb
        Lb = [P[4 * (t % 2) + i] for i in range(4)]
        NPa, NPTa = NPa_[b], NPTa_[b]
        C = 16 if t == 0 else 64
        ncn = 1 if t == 0 else 2
        nq = 4 * ncn
        ch0 = 0 if t == 0 else 2 * (t - 1) + 1
        tokX, DWc, BGc, XT, AT, BT, Nn, Zf, Zb = tokX_[b], DWc_[b], BGc_[b], XT_[b], AT_[b], BT_[b], Nn_[b], Zf_[b], Zb_[b]
        Q1T, Q2s, GT, Hs = Q1T_[b], Q2s_[b], GT_[b], Hs_[b]
        for c in range(ncn):
            r0 = p0 + 64 * c
            k.do("sp", "dma_start", tokX[0:C, c], in_=DV(d["RWT"][r0:r0 + C]))
            k.do("act", "dma_start", BGc[0:C, c], in_=DV(d["BG"][r0:r0 + C]))
        k.do("act", "dma_start", DWc[:, 0:ncn, :], in_=DV(d["DW"][ch0:ch0 + ncn].rearrange("c p f -> p c f")))
        for c in range(ncn):
            ptb = Vw(Lb[2 + c].h[:].bitcast(BF16), Lb[2 + c].res)
            pv = ptb[0:64, :].re("p (h x t) -> p h x t", h=4, x=4)
            for h in range(4):
                for X in range(4):
                    k.trv(pv[:, h, X, 0:C], tokX[0:C, c, X, h * 64:(h + 1) * 64], idt[0:C, 0:C])
            if c == 0:
                k.do("dve", "tensor_copy", XT[:, c, :, :, 0:C], in_=pv[:, :, :, 0:C])
            else:
                k.do("act", "copy", XT[:, c, :, :, 0:C], in_=pv[:, :, :, 0:C])
        yield
        for c in range(ncn):
            for h in range(4):
                q = c * 4 + h
                o1 = Lb[q // 4][0:C, :].re("p (q a t) -> p q a t", q=4, a=2)[:, q % 4, :, 0:C]
                k.mmv(o1, XT[:, c, h, 2, 0:C], XT[:, c, h, 0:2, 0:C], True, True)
                o2 = Lb[2 + q // 4][0:C, :].re("p (q a t) -> p q a t", q=4, a=2)[:, q % 4, :, 0:C]
                k.mmv(o2, XT[:, c, h, 3, 0:C], XT[:, c, h, 0:2, 0:C], True, True)
        for c in range(ncn):
            pv1 = Lb[c][0:C, :].re("p (q a t) -> p q a t", q=4, a=2)[:, :, :, 0:C]
            k.do("dve", "tensor_tensor", AT[0:C, 4 * c:4 * c + 4, :, 0:C], in0=pv1, in1=rmask[0:C, 0:2, 0:C].un(1).bc([C, 4, 2, C]), op=ALU.mult)
            pv2 = Lb[2 + c][0:C, :].re("p (q a t) -> p q a t", q=4, a=2)[:, :, :, 0:C]
            k.do("dve", "tensor_tensor", BT[0:C, 4 * c:4 * c + 4, :, 0:C], in0=pv2, in1=rmask[0:C, 2:4, 0:C].un(1).bc([C, 4, 2, C]), op=ALU.mult)
        for c in range(ncn):
            for h in range(4):
                q = c * 4 + h
                o3 = Lb[0][0:C, :].re("p (q t) -> p q t", q=8)[:, q, 0:C]
                k.mmv(o3, XT[:, c, h, 0, 0:C], XT[:, c, h, 3, 0:C], True, True)
        pv3 = Lb[0][0:C, :].re("p (q t) -> p q t", q=8)[:, 0:nq, 0:C]
        k.do("dve", "tensor_tensor", Nn[0:C, 0:nq, 0:C], in0=pv3, in1=rmask[0:C, 4:5, 0:C].bc([C, nq, C]), op=ALU.mult)
        yield
        pav = Lb[1][0:C, :].re("p (q i) -> p q i", q=8)
        for c in range(ncn):
            for h in range(4):
                q = c * 4 + h
                k.mmv(pav[:, q, :], AT[0:C, q, 0, 0:C], tokX[0:C, c, 6, h * 64:(h + 1) * 64], True, True)
        k.do("act", "copy", Zf[0:C, 0:nq, 0:64].re("p (c h) e -> p c h e", c=ncn), in_=tokX[0:C, 0:ncn, 0, :].re("p c (h e) -> p c h e", h=4))
        k.do("act", "copy", Zf[0:C, 0:nq, 64:128], in_=pav[:, 0:nq, :])
        k.do("dve", "tensor_copy", Zb[0:C, 0:nq, :], in_=Zf[0:C, 0:nq, :])
        yield
        L = 3 if t == 0 else 5
        NPc, NPTc = Nn, None
        for lev in range(L + 1):
            def npt(q):
                return BT[0:C, q, 0, 0:C] if lev == 0 else NPTc[0:C, q, 0:C]
            for q in range(nq):
                oz = Lb[q // 4][0:C, :].re("p (q e) -> p q e", q=4)[:, q % 4, :]
                k.mmv(oz, npt(q), Zb[0:C, q, :], True, True)
            if lev < L:
                nxtP, nxtPT = NPa[lev % 2], NPTa[lev % 2]
                pa = Lb[2][0:C, :].re("p (q i) -> p q i", q=8)
                pbk = Lb[3][0:C, :].re("p (q i) -> p q i", q=8)
                for q in range(nq):
                    if lev + 1 < L:
                        k.mmv(pa[:, q, 0:C], npt(q), NPc[0:C, q, 0:C], True, True)
                    k.mmv(pbk[:, q, 0:C], NPc[0:C, q, 0:C], npt(q), True, True)
            for c in range(ncn):
                zv = Lb[c][0:C, :].re("p (q e) -> p q e", q=4)
                k.do("dve", "tensor_tensor", Zf[0:C, 4 * c:4 * c + 4, :], in0=Zf[0:C, 4 * c:4 * c + 4, :], in1=zv, op=(ALU.subtract if lev == 0 else ALU.add))
            k.do("act", "copy", Zb[0:C, 0:nq, :], in_=Zf[0:C, 0:nq, :])
            if lev < L:
                if lev + 1 < L:
                    k.do("act", "copy", nxtP[0:C, 0:nq, 0:C], in_=pa[:, 0:nq, 0:C])
                k.do("dve", "tensor_copy", nxtPT[0:C, 0:nq, 0:C], in_=pbk[:, 0:nq, 0:C])
                NPc, NPTc = nxtP, nxtPT
            yield
        pq1 = Lb[0][0:64, :].re("p (q t) -> p q t", q=8)
        pq2 = Lb[1][0:C, :].re("p (q i) -> p q i", q=8)
        pg = Lb[2][0:64, :].re("p (q j) -> p q j", q=8)
        ph = Lb[3][0:64, :].re("p (q i) -> p q i", q=8)
        for c in range(ncn):
            for h in range(4):
                q = c * 4 + h
                hs = slice(h * 64, (h + 1) * 64)
                P1b, P2b = Zb[0:C, q, 0:64], Zb[0:C, q, 64:128]
                V_ = tokX[0:C, c, 6, hs]
                k.mmv(pq1[:, q, 0:C], P1b, BT[0:C, q, 1, 0:C], True, False)
                k.mmv(pq1[:, q, 0:C], idt[0:64, 0:64], XT[:, c, h, 1, 0:C], False, True)
                k.mmv(pq2[:, q, :], AT[0:C, q, 1, 0:C], V_, True, False)
                k.mmv(pq2[:, q, :], BT[0:C, q, 1, 0:C], P2b, False, True)
                k.mmv(pg[:, q, :], idt[0:64, 0:64], DWc[:, c, hs], True, False)
                k.mmv(pg[:, q, :], P1b, tokX[0:C, c, 5, hs], False, True)
                k.mmv(ph[:, q, :], tokX[0:C, c, 4, hs], V_, True, False)
                k.mmv(ph[:, q, :], tokX[0:C, c, 5, hs], P2b, False, True)
        k.do("act", "copy", Q1T[:, 0:nq, 0:C], in_=pq1[:, 0:nq, 0:C])
        k.do("dve", "tensor_copy", Q2s[0:C, 0:nq, :], in_=pq2[:, 0:nq, :])
        k.do("act", "copy", GT[:, 0:nq, :], in_=pg[:, 0:nq, :])
        k.do("dve", "tensor_copy", Hs[:, 0:nq, :], in_=ph[:, 0:nq, :])
        yield
        for c in range(ncn):
            cb = state["ccount"] % 2
            state["ccount"] += 1
            tcur = state["tcur"]
            Tc, Tn = Tb[tcur], Tb[1 - tcur]
            state["tcur"] = 1 - tcur
            py = Lb[2 + c][0:C, 0:256].re("p (h i) -> p h i", h=4)
            pt_ = Lb[2 + c][0:64, 256:512].re("p (h i) -> p h i", h=4)
            for h in range(4):
                q = c * 4 + h
                k.mmv(pt_[:, h, :], GT[:, q, :], Tc[:, h, :], True, True)
            for h in range(4):
                q = c * 4 + h
                k.mmv(py[:, h, :], Q1T[:, q, 0:C], Tc[:, h, :], True, True)
            k.do("dve", "tensor_tensor", Tn[:], in0=pt_, in1=Hs[:, 4 * c:4 * c + 4, :], op=ALU.add)
            ys, dd, sq, st, yo = ys_[cb], dd_[cb], sq_[cb], st_[cb], yo_[cb]
            k.do("dve", "tensor_tensor", ys[0:C], in0=py, in1=Q2s[0:C, 4 * c:4 * c + 4, :], op=ALU.add)
            k.do("dve", "tensor_reduce", st[0:C], in_=ys[0:C], axis=AX.X, op=ALU.add)
            k.do("dve", "tensor_scalar", st[0:C], in0=st[0:C], scalar1=-1.0 / 64, scalar2=None, op0=ALU.mult)
            k.do("dve", "tensor_tensor", dd[0:C], in0=ys[0:C], in1=st[0:C].bc([C, 4, 64]), op=ALU.add)
            k.do("act", "activation", sq[0:C], in_=dd[0:C], func=AF.Square)
            k.do("dve", "tensor_reduce", st[0:C], in_=sq[0:C], axis=AX.X, op=ALU.add)
            k.do("act", "activation", st[0:C], in_=st[0:C], func=AF.Sqrt, bias=64e-5, scale=1.0 / 64)
            k.do("dve", "reciprocal", st[0:C], in_=st[0:C])
            k.do("dve", "tensor_tensor", dd[0:C], in0=dd[0:C], in1=st[0:C].bc([C, 4, 64]), op=ALU.mult)
            ddf = dd[0:C].re("p h e -> p (h e)")
            k.do("dve", "tensor_tensor", ddf, in0=ddf, in1=lg_b[0:C, :], op=ALU.mult)
            k.do("dve", "tensor_tensor", ddf, in0=ddf, in1=lb_b[0:C, :], op=ALU.add)
            k.do("pool", "tensor_tensor", ddf, in0=ddf, in1=BGc[0:C, c, 0, :], op=ALU.add)
            k.do("pool", "tensor_tensor", yo[0:C, :], in0=ddf, in1=BGc[0:C, c, 1, :], op=ALU.mult)
            r0 = p0 + 64 * c
            k.do("sp", "dma_start", DV(d["MIX"][r0:r0 + C, 512:768]), in_=yo[0:C, :])
            if c < ncn - 1:
                yield
    run_skewed([body(t) for t in range(tmax)], window=2)
    S.barrier()
    A.release()


def build_full():
    k = K()
    declare_io(k, final_out=True)
    stage_ln0(k)
    for l in range(DEPTH):
        stage_in(k, l)
        stage_swa(k, l)
        stage_diff(k, l)
        stage_rwkv_a(k, l)
        stage_rwkv_b(k, l)
        stage_conv(k, l)
        stage_out(k, l)
        stage_moe(k, l, last=(l == DEPTH - 1))
        k.S.barrier(rotate_dma=True)
    k.S.final_wait("sp")
    k.S.emit()
    k.st.close()
    return k


def kernel(**inputs):
    inp = {kk_: np.asarray(v) for kk_, v in inputs.items()}
    k = build_full()
    consts = host_consts(inp)
    in_maps = []
    for b in range(8):
        m = host_inputs(inp, b)
        m.update(consts)
        in_maps.append(m)
    res = run_bass_kernel_spmd(k.nc, in_maps, core_ids=list(range(8)))
    out = np.stack([np.asarray(res.results[b]["out"], np.float32) for b in range(8)], axis=0)
    return out
```

```python
import numpy as np
import concourse.bass as bass
import concourse.mybir as mybir

F32 = mybir.dt.float32
BF16 = mybir.dt.bfloat16
AF = mybir.ActivationFunctionType
ALU = mybir.AluOpType
AX = mybir.AxisListType

ENGS = ("pe", "act", "dve", "pool", "sp")
DQS = ("sp", "act", "pool")


class Sched:
    def __init__(self, nc, stack, ndma=6):
        self.nc = nc
        self.stack = stack
        self.ndma = ndma
        self.gen = 0
        self.dgen = 0
        self.objs = {}
        self.cnt = {e: 0 for e in ENGS}
        self.dcnt = {q: [0] * ndma for q in DQS}
        self.dnext = {q: 0 for q in DQS}
        self.streams = {e: [] for e in ENGS}
        self.waited = {e: {} for e in ENGS}
        self.lastw = {}
        self.readers = {}
        self.nops = 0
        self._new_csems()
        self._new_dsems()

    def _new_csems(self):
        self.gen += 1
        for e in ENGS:
            self.objs[("c", e, self.gen)] = self.stack.enter_context(self.nc.semaphore(f"c_{e}_{self.gen}"))
            self.cnt[e] = 0

    def _new_dsems(self):
        self.dgen += 1
        for q in DQS:
            for i in range(self.ndma):
                self.objs[("d", q, i, self.dgen)] = self.stack.enter_context(self.nc.semaphore(f"d_{q}{i}_{self.dgen}"))
            self.dcnt[q] = [0] * self.ndma

    def ckey(self, e):
        return ("c", e, self.gen)

    def dkey(self, q, i):
        return ("d", q, i, self.dgen)

    def _semobj(self, key):
        return self.objs[key]

    def _wait(self, eng, tok):
        key, val, src = tok
        if self.waited[eng].get(key, 0) >= val:
            return
        self.waited[eng][key] = val
        self.streams[eng].append(("w", key, val))

    def op(self, eng, fn, kw=None, r=(), w=(), dma=False):
        if isinstance(fn, str):
            name = fn
            kw = dict(kw)
            fn = lambda E, name=name, kw=kw: getattr(E, name)(**kw)
        deps = []
        for res in r:
            t = self.lastw.get(res)
            if t is not None:
                deps.append((t, "raw"))
        for res in w:
            t = self.lastw.get(res)
            if t is not None:
                deps.append((t, "waw"))
            for t in self.readers.get(res, ()):
                deps.append((t, "war"))
        for t, kind in deps:
            src = t[2]
            if src == eng and t[0][0] == "c":
                if eng == "pe":
                    continue
                if kind == "war":
                    continue
            self._wait(eng, t)
        if dma:
            q = eng
            i = self.dnext[q] % self.ndma
            self.dnext[q] += 1
            if self.dcnt[q][i] > 0:
                self._wait(eng, (self.dkey(q, i), self.dcnt[q][i], None))
            self.dcnt[q][i] += 16
            tok = (self.dkey(q, i), self.dcnt[q][i], None)
            self.streams[eng].append(("d", fn, self.dkey(q, i)))
        else:
            self.cnt[eng] += 1
            tok = (self.ckey(eng), self.cnt[eng], eng)
            self.streams[eng].append(("o", fn, self.ckey(eng), self.cnt[eng]))
        for res in w:
            self.lastw[res] = tok
            self.readers[res] = []
        for res in r:
            self.readers.setdefault(res, []).append(tok)
        self.nops += 1
        return tok

    def barrier(self, rotate_dma=False):
        for e in ENGS:
            for s in ENGS:
                if s != e and self.cnt[s] > 0:
                    self._wait(e, (self.ckey(s), self.cnt[s], s))
            for q in DQS:
                for i in range(self.ndma):
                    if self.dcnt[q][i] > 0:
                        self._wait(e, (self.dkey(q, i), self.dcnt[q][i], None))
        self.lastw = {}
        self.readers = {}
        for e in ENGS:
            self.streams[e].append(None)
        if max(self.cnt.values()) > 4000:
            self._new_csems()
        if rotate_dma:
            self._new_dsems()

    def final_wait(self, eng="sp"):
        for q in DQS:
            for i in range(self.ndma):
                if self.dcnt[q][i] > 0:
                    self._wait(eng, (self.dkey(q, i), self.dcnt[q][i], None))
        for s in ENGS:
            if s != eng and self.cnt[s] > 0:
                self._wait(eng, (self.ckey(s), self.cnt[s], s))

    def emit(self):
        nc = self.nc
        targets = {}
        for e in ENGS:
            for it in self.streams[e]:
                if it is not None and it[0] == "w" and it[1][0] == "c":
                    targets.setdefault(it[1], set()).add(it[2])
        rank = {key: {v: i + 1 for i, v in enumerate(sorted(vs))} for key, vs in targets.items()}
        self.n_inc = sum(len(v) for v in rank.values())

        def conv(it):
            if it[0] == "w":
                _, key, val = it
                s = self.objs[key]
                v = rank[key][val] if key[0] == "c" else val
                return lambda E, s=s, v=v: E.wait_ge(s, v)
            if it[0] == "d":
                _, fn, key = it
                s = self.objs[key]
                return lambda E, fn=fn, s=s: fn(E).then_inc(s, 16)
            _, fn, key, idx = it
            if idx in rank.get(key, ()):
                s = self.objs[key]
                return lambda E, fn=fn, s=s: fn(E).then_inc(s, 1)
            return lambda E, fn=fn: fn(E)

        segs = {e: [[]] for e in ENGS}
        for e in ENGS:
            for f in self.streams[e]:
                if f is None:
                    segs[e].append([])
                else:
                    segs[e][-1].append(conv(f))
        nseg = max(len(v) for v in segs.values())
        for i in range(nseg):
            cur = {e: (segs[e][i] if i < len(segs[e]) else []) for e in ENGS}
            if not any(cur.values()):
                continue
            with nc.Block() as block:
                @block.tensor
                def _(E, fs=cur["pe"]):
                    for f in fs:
                        f(E)

                @block.scalar
                def _(E, fs=cur["act"]):
                    for f in fs:
                        f(E)

                @block.vector
                def _(E, fs=cur["dve"]):
                    for f in fs:
                        f(E)

                @block.gpsimd
                def _(E, fs=cur["pool"]):
                    for f in fs:
                        f(E)

                @block.sync
                def _(E, fs=cur["sp"]):
                    for f in fs:
                        f(E)


class SbufAlloc:
    def __init__(self, nc, base=16640, limit=208 * 1024):
        self.nc = nc
        self.off = base
        self.limit = limit
        self.n = 0
        self.marks = []

    def mark(self):
        self.marks.append(self.off)

    def release(self):
        self.off = self.marks.pop()

    def t(self, shape, dtype, name=None):
        esz = 4 if dtype == F32 else 2
        if dtype in (mybir.dt.int32, mybir.dt.uint32):
            esz = 4
        nbytes = int(np.prod(shape[1:])) * esz
        nbytes = (nbytes + 63) // 64 * 64
        assert self.off + nbytes <= self.limit, f"SBUF overflow {self.off}+{nbytes} > {self.limit} ({name})"
        self.n += 1
        h = self.nc.alloc_sbuf_tensor_at(f"{name or 't'}_{self.n}", list(shape), dtype, offset=self.off)
        self.off += nbytes
        return h


import math
import numpy as np
import ml_dtypes
from contextlib import ExitStack
import concourse.bass as bass
import concourse.mybir as mybir
from concourse.bass_utils import run_bass_kernel_spmd

NPOS = 4112
NT = 33
DEPTH = 2
ALPHA = (2 * DEPTH) ** 0.25
NEG = -1e30


def tile_rows(t):
    return (0, 16) if t == 0 else (16 + 128 * (t - 1), 128)


GROUPS = [[0]] + [[4 * g + 1 + i for i in range(4)] for g in range(8)]


def t5_bucket(dist):
    n = np.maximum(dist, 0)
    lr = np.log(np.maximum(n, 1).astype(np.float32) / np.float32(16)) / np.float32(math.log(128 / 16))
    large = np.minimum(16 + (lr * np.float32(16)).astype(np.int32), 31)
    return np.where(n < 16, n, large)


class K:
    def __init__(self, ext_in=(), ext_out=(), layers=(0, 1)):
        self.nc = bass.Bass("TRN2", target_bir_lowering=False)
        self.st = ExitStack()
        self.S = Sched(self.nc, self.st)
        self.A = SbufAlloc(self.nc)
        self.ext_in = set(ext_in)
        self.ext_out = set(ext_out)
        self.dr = {}
        nc = self.nc
        self.psum = [self.st.enter_context(nc.psum_tensor(f"ps{i}", [128, 512], F32)) for i in range(8)]

    def D(self, name, shape, dtype, kind=None):
        if kind is None:
            kind = "ExternalInput" if name in self.ext_in else ("ExternalOutput" if name in self.ext_out else "Internal")
        self.dr[name] = self.nc.dram_tensor(name, list(shape), dtype, kind=kind).ap()
        return self.dr[name]

    def I(self, name, shape, dtype=F32):
        return self.D(name, shape, dtype, kind="ExternalInput")

    def ps(self, i):
        return self.psum[i]

    def op(self, eng, name, r=(), w=(), **kw):
        return self.S.op(eng, name, kw, r=r, w=w)

    def dma(self, q, out, in_, r=(), w=()):
        return self.S.op(q, "dma_start", dict(out=out, in_=in_), r=r, w=w, dma=True)

    def mm(self, out, lhsT, rhs, start, stop, r=(), w=(), skip=False):
        kw = dict(out=out, lhsT=lhsT, rhs=rhs, start=start, stop=stop)
        if skip:
            kw["skip_group_check"] = True
        return self.S.op("pe", "matmul", kw, r=r, w=w)

    def tr(self, out, in_, identity, r=(), w=()):
        return self.S.op("pe", "transpose", dict(out=out, in_=in_, identity=identity), r=r, w=w)

    def ps2(self, i, dtype=F32):
        raise NotImplementedError


class Vw:
    def __init__(self, ap, res):
        self.ap = ap
        self.res = res

    def __getitem__(self, idx):
        return Vw(self.ap[idx], self.res)

    def re(self, pat, **kw):
        return Vw(self.ap.rearrange(pat, **kw), self.res)

    def bc(self, shape):
        return Vw(self.ap.to_broadcast(list(shape)), self.res)

    def un(self, ax):
        return Vw(self.ap.unsqueeze(ax), self.res)


class TB:
    def __init__(self, h, res):
        self.h = h
        self.res = res if isinstance(res, list) else [res]

    def __getitem__(self, idx):
        return Vw(self.h[idx], self.res)


def _do(self, eng, name, out, extra_r=(), extra_w=(), **kw):
    r = list(extra_r)
    w = list(extra_w)
    args = {}
    for kk_, v in kw.items():
        if isinstance(v, Vw):
            r += v.res
            args[kk_] = v.ap
        else:
            args[kk_] = v
    okey = "ap" if name == "memset" else "out"
    if isinstance(out, Vw):
        w += out.res
        args[okey] = out.ap
    else:
        args[okey] = out
    if name == "dma_start":
        return self.S.op(eng, name, args, r=r, w=w, dma=True)
    return self.S.op(eng, name, args, r=r, w=w)


K.do = _do


def _mmv(self, out, lhsT, rhs, start, stop, skip=False):
    kw = dict(out=out.ap, lhsT=lhsT.ap, rhs=rhs.ap, start=start, stop=stop)
    if skip:
        kw["skip_group_check"] = True
    return self.S.op("pe", "matmul", kw, r=lhsT.res + rhs.res, w=out.res)


def _trv(self, out, in_, ident):
    return self.S.op("pe", "transpose", dict(out=out.ap, in_=in_.ap, identity=ident.ap), r=in_.res + ident.res, w=out.res)


K.mmv = _mmv
K.trv = _trv


def declare_io(k, final_out=True):
    L = DEPTH
    k.I("x", [4096, 1024]); k.I("meta", [16, 1024]); k.I("ln0_g", [1, 1024]); k.I("ln0_b", [1, 1024])
    k.I("w_in", [L, 1024, 2816]); k.I("swa_sinks", [L, 4])
    k.I("diff_l", [L, 4, 32]); k.I("diff_subln_g", [L, 64])
    k.I("rwkv_mu", [L, 1024]); k.I("rwkv_w0", [L, 256]); k.I("rwkv_w2", [L, 64, 256]); k.I("rwkv_a0", [L, 256])
    k.I("rwkv_a2", [L, 64, 256]); k.I("rwkv_g2", [L, 128, 256]); k.I("rwkv_kk", [L, 256]); k.I("rwkv_ka", [L, 256])
    k.I("rwkv_rk", [L, 256]); k.I("rwkv_lnx_g", [L, 256]); k.I("rwkv_lnx_b", [L, 256])
    k.I("conv_wT", [L, 256, 31]); k.I("conv_b", [L, 256, 1]); k.I("conv_gn_g", [L, 256, 1]); k.I("conv_gn_b", [L, 256, 1])
    k.I("w_out", [L, 1024, 1024]); k.I("ln1_g", [L, 1024]); k.I("ln1_b", [L, 1024])
    k.I("router_w", [1024, 16]); k.I("router_b", [1, 16])
    k.I("exp_w1", [L, 16, 1024, 512]); k.I("exp_w3", [L, 16, 1024, 512]); k.I("exp_w2", [L, 16, 512, 1024])
    k.I("ln2_g", [L, 1024]); k.I("ln2_b", [L, 1024])
    k.I("ident_bf", [128, 128], BF16); k.I("ident_f", [128, 128], F32)
    k.I("ba_meta", [3, 4, 16, 128], BF16); k.I("ba_pc", [4, 128, 256], BF16)
    k.I("bb_pc", [4, 128, 256], F32); k.I("b31", [1, 8], F32)
    k.I("gmat", [128, 128], F32)
    k.I("tri", [3, 64, 64], F32)
    k.I("ctri", [128, 128], F32); k.I("cblk", [128, 128], F32); k.I("cones", [128, 2, 64], F32); k.I("identrep", [64, 256], F32)
    k.I("rmask", [64, 5, 64], F32)
    k.D("H", [NPOS, 1024], F32); k.D("HM", [NPOS, 1024], F32)
    k.D("QKA", [384, NPOS], BF16); k.D("QKB", [512, NPOS], BF16); k.D("CV", [512, NPOS], F32)
    k.D("VA", [NPOS, 128], BF16); k.D("VB", [NPOS, 256], BF16); k.D("UC", [NPOS + 1, 1024], F32)
    k.D("MIX", [NPOS, 768], BF16); k.D("MIXD", [256, NPOS], BF16)
    k.D("HT", [1024, NPOS], BF16)
    k.D("RWT", [NPOS, 7, 256], BF16); k.D("BG", [NPOS, 2, 256], F32); k.D("DW", [65, 64, 256], BF16)
    if final_out:
        k.D("out", [4096, 1024], F32, kind="ExternalOutput")


def host_consts(inp):
    c = {}
    c["ident_bf"] = np.eye(128, dtype=ml_dtypes.bfloat16)
    c["ident_f"] = np.eye(128, dtype=np.float32)
    rel = np.asarray(inp["rel_bias"], np.float32)
    rel_a, rel_b = rel[:, :4], rel[:, 4:]
    ki = np.arange(128)[:, None]
    qi = np.arange(128)[None, :]
    bam = np.full((3, 4, 16, 128), NEG, np.float32)
    m = np.arange(16)[:, None]
    dq = np.arange(128)[None, :] - m
    vis = (dq >= 0) & (np.arange(128)[None, :] < 16)
    g = rel_a[t5_bucket(dq)]
    bam[0] = np.where(vis[None], np.moveaxis(g, -1, 0), NEG)
    dq = (16 + np.arange(128))[None, :] - m
    bam[1] = np.moveaxis(rel_a[t5_bucket(dq)], -1, 0)
    bam[2] = np.broadcast_to(rel_a[31][:, None, None], (4, 16, 128))
    c["ba_meta"] = bam.astype(ml_dtypes.bfloat16)
    dq_prev = qi - ki + 128
    dq_cur = qi - ki
    bp = np.where((ki > qi)[None], np.moveaxis(rel_a[t5_bucket(dq_prev)], -1, 0), NEG)
    bc = np.where((ki <= qi)[None], np.moveaxis(rel_a[t5_bucket(dq_cur)], -1, 0), NEG)
    c["ba_pc"] = np.concatenate([bp, bc], axis=2).astype(ml_dtypes.bfloat16)
    bcd = np.where((ki <= qi)[None], np.moveaxis(rel_b[t5_bucket(dq_cur)], -1, 0), NEG)
    bpd = np.moveaxis(rel_b[t5_bucket(dq_prev)], -1, 0)
    c["bb_pc"] = np.ascontiguousarray(np.concatenate([bcd, bpd], axis=2).astype(np.float32))
    c["b31"] = np.ascontiguousarray(rel[31][None, :])
    gm = np.zeros((128, 128), np.float32)
    gm[:64, :64] = 1.0 / 64
    gm[64:, 64:] = 1.0 / 64
    c["gmat"] = gm
    s = np.arange(64)[:, None]
    t = np.arange(64)[None, :]
    c["tri"] = np.stack([(s <= t), (s < t), (s >= t)]).astype(np.float32)
    s2 = np.arange(128)[:, None]; t2 = np.arange(128)[None, :]
    same = (s2 // 64) == (t2 // 64)
    c["ctri"] = (same & (s2 <= t2)).astype(np.float32)
    c["cblk"] = same.astype(np.float32)
    c["cones"] = np.ascontiguousarray(np.stack([(np.arange(128) // 64 == cc)[:, None] * np.ones((1, 64)) for cc in range(2)], axis=1).astype(np.float32))
    c["identrep"] = np.tile(np.eye(64, dtype=np.float32), (1, 4))
    rm = np.stack([(s < t), (s <= t), (s < t), -1.0 * (s <= t), (s > t)], axis=1).astype(np.float32)
    c["rmask"] = np.ascontiguousarray(rm)
    return c


def host_inputs(inp, b):
    f = lambda a: np.ascontiguousarray(np.asarray(a, np.float32))
    L = DEPTH
    m = {
        "x": f(inp["x"][b]), "meta": f(inp["meta"]), "ln0_g": f(inp["ln0_g"])[None], "ln0_b": f(inp["ln0_b"])[None],
        "w_in": f(inp["w_in"]), "swa_sinks": f(inp["swa_sinks"]),
        "diff_l": f(np.stack([inp["diff_lq1"], inp["diff_lk1"], inp["diff_lq2"], inp["diff_lk2"]], axis=1)),
        "diff_subln_g": f(inp["diff_subln_g"]),
        "rwkv_mu": f(inp["rwkv_mu"]), "rwkv_w0": f(inp["rwkv_w0"]), "rwkv_w2": f(inp["rwkv_w2"]), "rwkv_a0": f(inp["rwkv_a0"]),
        "rwkv_a2": f(inp["rwkv_a2"]), "rwkv_g2": f(inp["rwkv_g2"]), "rwkv_kk": f(inp["rwkv_kk"]), "rwkv_ka": f(inp["rwkv_ka"]),
        "rwkv_rk": f(np.asarray(inp["rwkv_rk"]).reshape(L, 256)), "rwkv_lnx_g": f(inp["rwkv_lnx_g"]), "rwkv_lnx_b": f(inp["rwkv_lnx_b"]),
        "conv_wT": f(np.transpose(np.asarray(inp["conv_w"]), (0, 2, 1))), "conv_b": f(inp["conv_b"])[..., None],
        "conv_gn_g": f(inp["conv_gn_g"])[..., None], "conv_gn_b": f(inp["conv_gn_b"])[..., None],
        "w_out": f(inp["w_out"]), "ln1_g": f(inp["ln1_g"]), "ln1_b": f(inp["ln1_b"]),
        "router_w": f(inp["router_w"]), "router_b": f(inp["router_b"])[None],
        "exp_w1": f(inp["exp_w1"]), "exp_w3": f(inp["exp_w3"]), "exp_w2": f(inp["exp_w2"]),
        "ln2_g": f(inp["ln2_g"]), "ln2_b": f(inp["ln2_b"]),
    }
    return m


def layernorm_tile(k, z, y, n, key, gb, bb, eps=1e-5, tmp=None, gbres=("gb", "bb")):
    stt, mv, rs = tmp
    for c in range(2):
        k.op("dve", "bn_stats", out=stt[0:n, c, :], in_=z[0:n, c * 512:(c + 1) * 512], r=[("z", key)], w=[("st", key, c)])
    k.op("dve", "bn_aggr", out=mv[0:n, :], in_=stt[0:n].rearrange("p a b -> p (a b)"), r=[("st", key, 0), ("st", key, 1)], w=[("mv", key)])
    k.op("act", "activation", out=rs[0:n, 0:1], in_=mv[0:n, 1:2], func=AF.Sqrt, bias=eps, scale=1.0, r=[("mv", key)], w=[("rs", key, 0)])
    k.op("dve", "scalar_tensor_tensor", out=y[0:n, :], in0=z[0:n, :], scalar=mv[0:n, 0:1], in1=gb[0:n, :], op0=ALU.subtract, op1=ALU.mult, r=[("z", key), ("mv", key), gbres[0]], w=[("y", key)])
    k.op("dve", "reciprocal", out=rs[0:n, 0:1], in_=rs[0:n, 0:1], r=[("rs", key, 0)], w=[("rs", key, 0)])
    k.op("dve", "scalar_tensor_tensor", out=y[0:n, :], in0=y[0:n, :], scalar=rs[0:n, 0:1], in1=bb[0:n, :], op0=ALU.mult, op1=ALU.add, r=[("y", key), ("rs", key, 0), gbres[1]], w=[("y", key)])


def layernorm_tile_g(k, z, y, n, key, gb, bb, eps=1e-5, tmp=None, gbres=("gb", "bb")):
    stt, mv, rs = tmp
    for c in range(2):
        k.op("dve", "bn_stats", out=stt[0:n, c, :], in_=z[0:n, c * 512:(c + 1) * 512], r=[("z", key)], w=[("st", key, c)])
    k.op("dve", "bn_aggr", out=mv[0:n, :], in_=stt[0:n].rearrange("p a b -> p (a b)"), r=[("st", key, 0), ("st", key, 1)], w=[("mv", key)])
    k.op("act", "activation", out=rs[0:n, 0:1], in_=mv[0:n, 1:2], func=AF.Sqrt, bias=eps, scale=1.0, r=[("mv", key)], w=[("rs", key, 0)])
    k.op("dve", "scalar_tensor_tensor", out=y[0:n, :], in0=z[0:n, :], scalar=mv[0:n, 0:1], in1=gb[0:n, :], op0=ALU.subtract, op1=ALU.mult, r=[("z", key), ("mv", key), gbres[0]], w=[("y", key)])
    yield
    k.op("dve", "reciprocal", out=rs[0:n, 0:1], in_=rs[0:n, 0:1], r=[("rs", key, 0)], w=[("rs", key, 0)])
    k.op("dve", "scalar_tensor_tensor", out=y[0:n, :], in0=y[0:n, :], scalar=rs[0:n, 0:1], in1=bb[0:n, :], op0=ALU.mult, op1=ALU.add, r=[("y", key), ("rs", key, 0), gbres[1]], w=[("y", key)])


def ln_tmp(k, nm):
    A = k.A
    return (A.t([128, 2, 6], F32, "st" + nm), A.t([128, 2], F32, "mv" + nm), A.t([128, 2], F32, "rs" + nm))


def load_bcast(k, dst, src_row, res, q="sp"):
    k.dma(q, dst[:], src_row.partition_broadcast(128), w=[res])


def stage_ln0(k):
    S, A, d = k.S, k.A, k.dr
    A.mark()
    gb = A.t([128, 1024], F32, "gb"); bb = A.t([128, 1024], F32, "bb")
    load_bcast(k, gb, d["ln0_g"], "gb"); load_bcast(k, bb, d["ln0_b"], "bb")
    zs = [A.t([128, 1024], F32, f"z{i}") for i in range(3)]
    ys = [A.t([128, 1024], F32, f"y{i}") for i in range(3)]
    tmps = [ln_tmp(k, str(i)) for i in range(3)]
    for t in range(NT):
        p0, n = tile_rows(t)
        b = t % 3
        z, y = zs[b], ys[b]
        src = d["meta"] if t == 0 else d["x"][128 * (t - 1):128 * t, :]
        k.dma("sp", z[0:n, :], src, w=[("z", b)])
        layernorm_tile(k, z, y, n, b, gb, bb, tmp=tmps[b])
        k.dma("act", d["H"][p0:p0 + n, :], y[0:n, :], r=[("y", b)], w=[("H", t)])
    S.barrier()
    A.release()


FM_TILES = [
    ("QKA", 0, 0, 0.125), ("QKA", 128, 128, 0.125), ("QKA", 256, 256, 1.0),
    ("QKB", 0, 512, 32 ** -0.5), ("QKB", 128, 640, 32 ** -0.5), ("QKB", 256, 768, 1.0), ("QKB", 384, 896, 1.0),
    ("CV", 0, 2304, 1.0), ("CV", 128, 2432, 1.0), ("CV", 256, 2560, 1.0), ("CV", 384, 2688, 1.0),
]


def stage_in(k, l):
    S, A, d = k.S, k.A, k.dr
    A.mark()
    idt = A.t([128, 128], BF16, "ident")
    k.dma("sp", idt[:], d["ident_bf"], w=["ident"])
    wsb = A.t([128, 8, 2816], BF16, "w_in")
    for kk in range(8):
        for c in range(2):
            k.dma("pool", wsb[:, kk, c * 1408:(c + 1) * 1408], d["w_in"][l, kk * 128:(kk + 1) * 128, c * 1408:(c + 1) * 1408], w=[("w_in", kk, c)])
    wres = [("w_in", kk, c) for kk in range(8) for c in range(2)]
    zs = [A.t([128, 1024], F32, f"z{i}") for i in range(2)]
    hb = [A.t([128, 1024], BF16, f"hb{i}") for i in range(2)]
    hTg = [A.t([128, 8, 512], BF16, f"hT{i}") for i in range(2)]
    ofm_b = [A.t([128, 512], BF16, f"ofb{i}") for i in range(3)]
    ofm_f = [A.t([128, 512], F32, f"off{i}") for i in range(2)]
    otm_v = [A.t([128, 384], BF16, f"otv{i}") for i in range(2)]
    otm_u = [A.t([128, 1024], F32, f"otu{i}") for i in range(2)]
    tcount = 0
    fmc = 0
    tmc = 0
    for gi, grp in enumerate(GROUPS):
        gb_ = gi % 2
        hT = hTg[gb_]
        ntok = sum(tile_rows(t)[1] for t in grp)
        gp0 = tile_rows(grp[0])[0]
        for ti, t in enumerate(grp):
            p0, n = tile_rows(t)
            b = tcount % 2
            tcount += 1
            z = zs[b]
            k.dma("sp", z[0:n, :], d["H"][p0:p0 + n, :], r=[("H", t)], w=[("z", b)])
            k.op("act", "copy", out=hb[b][0:n, :], in_=z[0:n, :], r=[("z", b)], w=[("hb", b)])
            pt = k.ps(b)[:].bitcast(BF16)
            for kk in range(8):
                k.tr(pt[:, kk * 128:kk * 128 + n], hb[b][0:n, kk * 128:(kk + 1) * 128], idt[0:n, 0:n], r=[("hb", b), "ident"], w=[("ps", b)])
            k.op("dve", "tensor_copy", out=hT[:, :, ti * 128:ti * 128 + n], in_=pt.rearrange("p (k t) -> p k t", k=8)[:, :, 0:n], r=[("ps", b)], w=[("hT", gb_, ti)])
        hres = [("hT", gb_, ti) for ti in range(len(grp))]
        for (dn, r0, c0, sc) in FM_TILES:
            pb = 2 + fmc % 3
            isf = dn == "CV"
            ob = ofm_f[fmc % 2] if isf else ofm_b[fmc % 3]
            ores = ("off", fmc % 2) if isf else ("ofb", fmc % 3)
            for kk in range(8):
                k.mm(k.ps(pb)[:, 0:ntok], wsb[:, kk, c0:c0 + 128], hT[:, kk, 0:ntok], kk == 0, kk == 7, r=hres + wres, w=[("ps", pb)])
            if fmc % 2 == 0:
                k.op("act", "activation", out=ob[:, 0:ntok], in_=k.ps(pb)[:, 0:ntok], func=AF.Copy, scale=sc, r=[("ps", pb)], w=[ores])
            else:
                k.op("dve", "tensor_scalar", out=ob[:, 0:ntok], in0=k.ps(pb)[:, 0:ntok], scalar1=sc, scalar2=None, op0=ALU.mult, r=[("ps", pb)], w=[ores])
            k.dma("sp", d[dn][r0:r0 + 128, gp0:gp0 + ntok], ob[:, 0:ntok], r=[ores], w=[(dn, r0, gi)])
            fmc += 1
        for ti, t in enumerate(grp):
            p0, n = tile_rows(t)
            ov = otm_v[tmc % 2]
            ou = otm_u[tmc % 2]
            tb = tmc % 2
            tmc += 1
            lt = hT[:, :, ti * 128:ti * 128 + n]
            for kk in range(8):
                k.mm(k.ps(5)[0:n, 0:128], lt[:, kk, :], wsb[:, kk, 384:512], kk == 0, kk == 7, r=hres + wres, w=[("ps", 5, 0)])
            for kk in range(8):
                k.mm(k.ps(5)[0:n, 128:384], lt[:, kk, :], wsb[:, kk, 1024:1280], kk == 0, kk == 7, r=hres + wres, w=[("ps", 5, 1)])
            k.op("act", "copy", out=ov[0:n, :], in_=k.ps(5)[0:n, 0:384], r=[("ps", 5, 0), ("ps", 5, 1)], w=[("otv", tb)])
            k.dma("act", d["VA"][p0:p0 + n, :], ov[0:n, 0:128], r=[("otv", tb)], w=[("VA", t)])
            k.dma("act", d["VB"][p0:p0 + n, :], ov[0:n, 128:384], r=[("otv", tb)], w=[("VB", t)])
            for c in range(2):
                pb = 6 + c
                for kk in range(8):
                    k.mm(k.ps(pb)[0:n, :], lt[:, kk, :], wsb[:, kk, 1280 + c * 512:1280 + (c + 1) * 512], kk == 0, kk == 7, r=hres + wres, w=[("ps", pb)])
                k.op("dve", "tensor_copy", out=ou[0:n, c * 512:(c + 1) * 512], in_=k.ps(pb)[0:n, :], r=[("ps", pb)], w=[("otu", tb, c)])
            k.dma("sp", d["UC"][1 + p0:1 + p0 + n, :], ou[0:n, :], r=[("otu", tb, 0), ("otu", tb, 1)], w=[("UC", t)])
    S.barrier()
    A.release()


def stage_swa(k, l):
    S, A, d = k.S, k.A, k.dr
    A.mark()
    idt = A.t([128, 128], BF16, "ident")
    k.dma("sp", idt[:], d["ident_bf"], w=["ident"])
    qT = A.t([64, 4, NPOS], BF16, "qTa")
    kT = A.t([64, 2, NPOS], BF16, "kTa")
    for h in range(4):
        k.dma("sp", qT[:, h, :], d["QKA"][64 * h:64 * h + 64, :], w=[("qT", h)])
    for kv in range(2):
        k.dma("sp", kT[:, kv, :], d["QKA"][256 + 64 * kv:256 + 64 * kv + 64, :], w=[("kT", kv)])
    va = A.t([128, NT, 2, 65], BF16, "va")
    k.op("pool", "memset", ap=va[:, :, :, 64:65], constant=1.0, w=["va_ones"])
    for t in range(NT):
        p0, n = tile_rows(t)
        k.dma("act", va[0:n, t, :, 0:64], d["VA"][p0:p0 + n, :].rearrange("p (h e) -> p h e", h=2), w=[("va", t)])
    bam = A.t([16, 3, 4, 128], BF16, "bam")
    k.dma("sp", bam[:], d["ba_meta"].rearrange("c h m q -> m c h q"), w=["bam"])
    bapc = A.t([128, 4, 256], BF16, "bapc")
    k.dma("sp", bapc[:], d["ba_pc"].rearrange("h k q -> k h q"), w=["bapc"])
    sk = A.t([128, 4, 1], F32, "sk")
    esk = A.t([128, 4, 1], F32, "esk")
    k.dma("sp", sk[:].rearrange("p h o -> p (h o)"), d["swa_sinks"][l:l + 1, :].partition_broadcast(128), w=["sk"])
    k.op("act", "activation", out=esk[:], in_=sk[:], func=AF.Exp, r=["sk"], w=["esk"])
    pms = [A.t([16, 4, 128], BF16, f"pm{i}") for i in range(2)]
    pps = [A.t([128, 2, 512], BF16, f"pp{i}") for i in range(2)]
    dens = [A.t([128, 4, 1], F32, f"den{i}") for i in range(2)]
    yos = [A.t([128, 4, 64], BF16, f"yo{i}") for i in range(2)]
    for t in range(NT):
        p0, n = tile_rows(t)
        s = t % 2
        psA, psB, psD = k.ps(4 * s), [k.ps(4 * s + 1), k.ps(4 * s + 2)], k.ps(4 * s + 3)
        pm, pp, den, yo = pms[s], pps[s], dens[s], yos[s]
        case = min(t, 2)
        psAv = psA[0:16, :].rearrange("p (h q) -> p h q", h=4)
        for h in range(4):
            kv = h // 2
            hp, cb = h // 2, (h % 2) * 256
            k.mm(psAv[:, h, 0:n], kT[:, kv, 0:16], qT[:, h, p0:p0 + n], True, False, r=[("kT", kv), ("qT", h)], w=[("psA", s)])
            k.mm(psAv[:, h, 0:n], idt[0:16, 0:16], bam[:, case, h, 0:n], False, True, r=["ident", "bam"], w=[("psA", s)])
            if t >= 2:
                pp0 = p0 - 128
                k.mm(psB[hp][:, cb:cb + n], kT[:, kv, pp0:pp0 + 128], qT[:, h, p0:p0 + n], True, False, r=[("kT", kv), ("qT", h)], w=[("psB", s, hp)])
                k.mm(psB[hp][:, cb:cb + n], idt[:, :], bapc[:, h, 0:n], False, True, r=["ident", "bapc"], w=[("psB", s, hp)])
            if t >= 1:
                k.mm(psB[hp][:, cb + 128:cb + 128 + n], kT[:, kv, p0:p0 + 128], qT[:, h, p0:p0 + n], True, False, r=[("kT", kv), ("qT", h)], w=[("psB", s, hp)])
                k.mm(psB[hp][:, cb + 128:cb + 128 + n], idt[:, :], bapc[:, h, 128:128 + n], False, True, r=["ident", "bapc"], w=[("psB", s, hp)])
        k.op("act", "activation", out=pm[:, :, 0:n], in_=psAv[:, :, 0:n], func=AF.Exp, r=[("psA", s)], w=[("pm", s)])
        for hp in range(2):
            if t >= 2:
                k.op("act", "activation", out=pp[:, hp, :], in_=psB[hp][:, :], func=AF.Exp, r=[("psB", s, hp)], w=[("pp", s, hp)])
            elif t == 1:
                k.op("act", "activation", out=pp[:, hp, :].rearrange("p (h x) -> p h x", h=2)[:, :, 128:256],
                     in_=psB[hp][:, :].rearrange("p (h x) -> p h x", h=2)[:, :, 128:256], func=AF.Exp, r=[("psB", s, hp)], w=[("pp", s, hp)])
        psDv = psD[:, 0:260].rearrange("p (h e) -> p h e", h=4)
        for h in range(4):
            kv = h // 2
            hp, cb = h // 2, (h % 2) * 256
            k.mm(psDv[0:n, h, :], pm[0:16, h, 0:n], va[0:16, 0, kv, :], True, t == 0, r=[("pm", s), ("va", 0), "va_ones"], w=[("psD", s)])
            if t >= 2:
                k.mm(psDv[0:n, h, :], pp[:, hp, cb:cb + n], va[:, t - 1, kv, :], False, False, r=[("pp", s, hp), ("va", t - 1), "va_ones"], w=[("psD", s)])
            if t >= 1:
                k.mm(psDv[0:n, h, :], pp[:, hp, cb + 128:cb + 128 + n], va[:, t, kv, :], False, True, r=[("pp", s, hp), ("va", t), "va_ones"], w=[("psD", s)])
        k.op("dve", "tensor_tensor", out=den[0:n], in0=psDv[0:n, :, 64:65], in1=esk[0:n], op=ALU.add, r=[("psD", s), "esk"], w=[("den", s)])
        k.op("dve", "reciprocal", out=den[0:n], in_=den[0:n], r=[("den", s)], w=[("den", s)])
        k.op("dve", "tensor_tensor", out=yo[0:n], in0=psDv[0:n, :, 0:64], in1=den[0:n].to_broadcast([n, 4, 64]), op=ALU.mult, r=[("psD", s), ("den", s)], w=[("yo", s)])
        k.dma("sp", d["MIX"][p0:p0 + n, 0:256], yo[0:n].rearrange("p h e -> p (h e)"), r=[("yo", s)], w=[("MIXa", t)])
    S.barrier()
    A.release()


def stage_diff(k, l):
    S, A, d = k.S, k.A, k.dr
    A.mark()
    lam_init = 0.8 - 0.6 * math.exp(-0.3 * l)
    idt = A.t([128, 128], BF16, "ident")
    k.dma("sp", idt[:], d["ident_bf"], w=["ident"])
    qT = A.t([64, 4, NPOS], BF16, "qTb")
    kT = A.t([64, 4, NPOS], BF16, "kTb")
    for hh in range(4):
        k.dma("sp", qT[:, hh, :], d["QKB"][64 * hh:64 * hh + 64, :], w=[("qT", hh)])
        k.dma("sp", kT[:, hh, :], d["QKB"][256 + 64 * hh:256 + 64 * hh + 64, :], w=[("kT", hh)])
    vb = A.t([128, NT, 4, 65], BF16, "vb")
    k.op("pool", "memset", ap=vb[:, :, :, 64:65], constant=1.0, w=["vb_ones"])
    for t in range(NT):
        p0, n = tile_rows(t)
        k.dma("act", vb[0:n, t, :, 0:64], d["VB"][p0:p0 + n, :].rearrange("p (h e) -> p h e", h=4), w=[("vb", t)])
    bbf = A.t([128, 4, 256], F32, "bbf")
    k.dma("sp", bbf[:], d["bb_pc"].rearrange("h k q -> k h q"), w=["bbf"])
    b31b = A.t([128, 8], F32, "b31b")
    k.dma("sp", b31b[:], d["b31"].partition_broadcast(128), w=["b31b"])
    bbt = A.t([128, 4, 256], BF16, "bbt")
    for h in range(4):
        k.op("dve", "tensor_scalar", out=bbt[:, h, :], in0=bbf[:, h, :], scalar1=b31b[:, 4 + h:5 + h], scalar2=None, op0=ALU.subtract, r=["bbf", "b31b"], w=["bbt"])
    dl = A.t([128, 4, 32], F32, "dl")
    k.dma("sp", dl[:].rearrange("p a b -> p (a b)"), d["diff_l"][l:l + 1].rearrange("o a b -> o (a b)").partition_broadcast(128), w=["dl"])
    pr = A.t([128, 2, 32], F32, "pr")
    ss = A.t([128, 2], F32, "ss")
    nlam = A.t([128, 1], F32, "nlam")
    k.op("dve", "tensor_tensor", out=pr[:, 0, :], in0=dl[:, 0, :], in1=dl[:, 1, :], op=ALU.mult, r=["dl"], w=["pr0"])
    k.op("dve", "tensor_tensor", out=pr[:, 1, :], in0=dl[:, 2, :], in1=dl[:, 3, :], op=ALU.mult, r=["dl"], w=["pr1"])
    k.op("dve", "tensor_reduce", out=ss[:], in_=pr[:], axis=AX.X, op=ALU.add, r=["pr0", "pr1"], w=["ss"])
    k.op("act", "activation", out=ss[:], in_=ss[:], func=AF.Exp, r=["ss"], w=["ss"])
    k.op("dve", "tensor_tensor", out=nlam[:], in0=ss[:, 1:2], in1=ss[:, 0:1], op=ALU.subtract, r=["ss"], w=["nlam"])
    k.op("dve", "tensor_scalar", out=nlam[:], in0=nlam[:], scalar1=-lam_init, scalar2=None, op0=ALU.add, r=["nlam"], w=["nlam"])
    gvec = A.t([128, 1, 64], F32, "gvec")
    k.dma("sp", gvec[:].rearrange("p o e -> p (o e)"), d["diff_subln_g"][l:l + 1, :].partition_broadcast(128), w=["gvec"])
    k.op("act", "mul", out=gvec[:], in_=gvec[:], mul=(1.0 - lam_init), r=["gvec"], w=["gvec"])
    PTs = [A.t([128, 512], BF16, f"PT{i}") for i in range(4)]
    rr = [A.t([128, 2, 4, 1], F32, f"rr{i}") for i in range(2)]
    t1 = [A.t([128, 4, 64], F32, f"t1{i}") for i in range(2)]
    t2 = [A.t([128, 4, 64], F32, f"t2{i}") for i in range(2)]
    ms = [A.t([128, 4, 1], F32, f"ms{i}") for i in range(2)]
    ybo = [A.t([128, 4, 4, 64], BF16, f"ybo{i}") for i in range(2)]
    sc = 0
    pending = []

    def flush():
        while pending:
            pending.pop(0)()

    for qg, tiles in enumerate(GROUPS):
        ntok = sum(tile_rows(t)[1] for t in tiles)
        gp0 = tile_rows(tiles[0])[0]
        nt = len(tiles)
        nq = tile_rows(tiles[0])[1]
        yb_ = ybo[qg % 2]
        for h in range(4):
            hb = h % 2
            Ob = [k.ps(4 + 2 * hb), k.ps(5 + 2 * hb)]
            Ov = [Ob[c][:, 0:65 * nt].rearrange("p (t e) -> p t e", e=65) for c in range(2)]
            for j in range(0, tiles[-1] + 1):
                kp0, nk = tile_rows(j)
                fi = max(0, j - tiles[0])
                col0 = fi * 128
                par = sc % 2
                sc += 1
                bl = [i for i in (j, j + 1) if i in tiles]
                c1 = col0 + 128 * len(bl) if tiles[0] != 0 else (nq if bl else 0)
                c1 = min(c1, ntok)
                if not bl:
                    c1 = col0
                kq_r = [("kT", h), ("qT", h)]
                kTc = lambda c: kT[32 * c:32 * c + 32, h, kp0:kp0 + nk]
                qTc = lambda c, a0, a1: qT[32 * c:32 * c + 32, h, gp0 + a0:gp0 + a1]
                sbs = [2 * par + c for c in range(2)]
                if bl:
                    for c in range(2):
                        k.mm(k.ps(sbs[c])[0:nk, col0:c1], kTc(c), qTc(c, col0, c1), True, False, r=kq_r, w=[("psS", sbs[c])])
                    for c in range(2):
                        psb = k.ps(sbs[c])
                        if j == 0:
                            if tiles[0] == 0:
                                k.mm(psb[0:16, 0:16], idt[0:16, 0:16], bbt[0:16, h, 0:16], False, True, r=["ident", "bbt"], w=[("psS", sbs[c])])
                            else:
                                k.mm(psb[0:16, 0:128], idt[:, 112:128], bbt[:, h, 128:256], False, True, r=["ident", "bbt"], w=[("psS", sbs[c])])
                        else:
                            b0 = 0 if bl[0] == j else 128
                            k.mm(psb[:, col0:c1], idt[:, :], bbt[:, h, b0:b0 + (c1 - col0)], False, True, r=["ident", "bbt"], w=[("psS", sbs[c])])
                if c1 < ntok:
                    for c in range(2):
                        k.mm(k.ps(sbs[c])[0:nk, c1:ntok], kTc(c), qTc(c, c1, ntok), True, True, r=kq_r, w=[("psS", sbs[c])])
                pts = []
                for c in range(2):
                    ptk = 2 * par + c
                    pt = PTs[ptk]
                    pts.append((pt, ptk))
                    k.op("act", "activation", out=pt[0:nk, col0:ntok], in_=k.ps(sbs[c])[0:nk, col0:ntok], func=AF.Exp, bias=b31b[0:nk, 4 + h:5 + h], scale=1.0,
                         r=[("psS", sbs[c]), "b31b"], w=[("PT", ptk)])

                def pv(tiles=tiles, j=j, h=h, hb=hb, nk=nk, pts=pts, Ov=Ov):
                    for c in range(2):
                        pt, ptk = pts[c]
                        for il, i in enumerate(tiles):
                            if i < j:
                                continue
                            ni = tile_rows(i)[1]
                            k.mm(Ov[c][0:ni, il, :], pt[0:nk, il * 128:il * 128 + ni], vb[0:nk, j, h, :], (j == 0 and il == 0), False, r=[("PT", ptk), ("vb", j), "vb_ones"], w=[("psO", hb, c)], skip=True)
                flush()
                pending.append(pv)

            def epilogue(qg=qg, h=h, hb=hb, nt=nt, nq=nq, Ov=Ov, yb_=yb_):
                n = nq
                e = hb
                for c in range(2):
                    k.op("dve", "reciprocal", out=rr[e][0:n, c, 0:nt, :], in_=Ov[c][0:n, :, 64:65], r=[("psO", hb, c)], w=[("rr", e, c)])
                k.op("dve", "tensor_tensor", out=t1[e][0:n, 0:nt, :], in0=Ov[0][0:n, :, 0:64], in1=rr[e][0:n, 0, 0:nt, :].to_broadcast([n, nt, 64]), op=ALU.mult, r=[("psO", hb, 0), ("rr", e, 0)], w=[("t1", e)])
                k.op("dve", "tensor_tensor", out=t2[e][0:n, 0:nt, :], in0=Ov[1][0:n, :, 0:64], in1=rr[e][0:n, 1, 0:nt, :].to_broadcast([n, nt, 64]), op=ALU.mult, r=[("psO", hb, 1), ("rr", e, 1)], w=[("t2", e)])
                k.op("dve", "scalar_tensor_tensor", out=t1[e][0:n, 0:nt, :], in0=t2[e][0:n, 0:nt, :], scalar=nlam[0:n, 0:1], in1=t1[e][0:n, 0:nt, :], op0=ALU.mult, op1=ALU.add, r=[("t1", e), ("t2", e), "nlam"], w=[("t1", e)])
                k.op("act", "activation", out=t2[e][0:n, 0:nt, :], in_=t1[e][0:n, 0:nt, :], func=AF.Square, r=[("t1", e)], w=[("t2", e)])
                k.op("dve", "tensor_reduce", out=ms[e][0:n, 0:nt, :], in_=t2[e][0:n, 0:nt, :], axis=AX.X, op=ALU.add, r=[("t2", e)], w=[("ms", e)])
                k.op("act", "activation", out=ms[e][0:n, 0:nt, :], in_=ms[e][0:n, 0:nt, :], func=AF.Sqrt, bias=1e-5, scale=1.0 / 64, r=[("ms", e)], w=[("ms", e)])
                k.op("dve", "reciprocal", out=ms[e][0:n, 0:nt, :], in_=ms[e][0:n, 0:nt, :], r=[("ms", e)], w=[("ms", e)])
                k.op("dve", "tensor_tensor", out=t1[e][0:n, 0:nt, :], in0=t1[e][0:n, 0:nt, :], in1=ms[e][0:n, 0:nt, :].to_broadcast([n, nt, 64]), op=ALU.mult, r=[("t1", e), ("ms", e)], w=[("t1", e)])
                k.op("dve", "tensor_tensor", out=yb_[0:n, 0:nt, h, :], in0=t1[e][0:n, 0:nt, :], in1=gvec[0:n].to_broadcast([n, nt, 64]), op=ALU.mult, r=[("t1", e), "gvec"], w=[("ybo", qg % 2, h)])
            pending.append(epilogue)

        def store(qg=qg, gp0=gp0, ntok=ntok, nt=nt, nq=nq, yb_=yb_):
            dst = d["MIX"][gp0:gp0 + ntok, 256:512]
            if nt > 1:
                dst = dst.rearrange("(t p) c -> p t c", p=128)
                src = yb_[:, 0:nt].rearrange("p t h e -> p t (h e)")
            else:
                src = yb_[0:nq, 0].rearrange("p h e -> p (h e)")
            k.dma("sp", dst, src, r=[("ybo", qg % 2, h) for h in range(4)], w=[("MIXb", qg)])
        pending.append(store)
    flush()
    S.barrier()
    A.release()


def stage_conv(k, l):
    S, A, d = k.S, k.A, k.dr
    A.mark()
    gmat = A.t([128, 128], F32, "gmat")
    k.dma("sp", gmat[:], d["gmat"], w=["gmat"])
    Ab = [A.t([128, NPOS], F32, f"cva{i}") for i in range(2)]
    Gb = [A.t([128, NPOS], F32, f"cvg{i}") for i in range(2)]
    hgb = [A.t([128, 30 + NPOS], F32, f"hg{i}") for i in range(2)]
    cwb = [A.t([128, 31], F32, f"cw{i}") for i in range(2)]
    cpb = [A.t([128, 3], F32, f"cp{i}") for i in range(2)]
    ctmp = [A.t([128, NPOS], F32, f"ctmp{i}") for i in range(2)]
    sqb = [A.t([128, 512], F32, f"sqb{i}") for i in range(2)]
    ddb = [A.t([128, 512], F32, f"ddb{i}") for i in range(2)]
    m2b = [A.t([128, 512], F32, f"m2b{i}") for i in range(2)]
    ob = [A.t([128, 512], BF16, f"cob{i}") for i in range(2)]
    H2 = NPOS // 2
    st = {"cc": 0}

    def taps(ct):
        r0 = ct * 128
        a, gate, hg, cw, cp = Ab[ct], Gb[ct], hgb[ct], cwb[ct], cpb[ct]
        acc1, acc2 = a, gate
        RA, RG = ("A", ct), [("G", ct, 0), ("G", ct, 1)]
        k.op("pool", "memset", ap=hg[:, 0:30], constant=0.0, w=[("hgz", ct)])
        k.dma("sp", a[:], d["CV"][r0:r0 + 128, :], w=[RA])
        k.dma("sp", gate[:], d["CV"][256 + r0:256 + r0 + 128, :], w=RG)
        k.dma("act", cw[:], d["conv_wT"][l, r0:r0 + 128, :], w=[("cw", ct)])
        k.dma("act", cp[:, 0:1], d["conv_b"][l, r0:r0 + 128, :], w=[("cp0", ct)])
        k.dma("act", cp[:, 1:2], d["conv_gn_g"][l, r0:r0 + 128, :], w=[("cp1", ct)])
        k.dma("act", cp[:, 2:3], d["conv_gn_b"][l, r0:r0 + 128, :], w=[("cp2", ct)])
        for hh in range(2):
            k.op("act", "activation", out=gate[:, hh * H2:(hh + 1) * H2], in_=gate[:, hh * H2:(hh + 1) * H2], func=AF.Sigmoid, r=[RG[hh]], w=[RG[hh]])
            k.op("dve", "tensor_tensor", out=hg[:, 30 + hh * H2:30 + (hh + 1) * H2], in0=a[:, hh * H2:(hh + 1) * H2], in1=gate[:, hh * H2:(hh + 1) * H2], op=ALU.mult,
                 r=[RA, RG[hh]], w=[("hg", ct, hh)])
        yield
        hres = [("hgz", ct), ("hg", ct, 0), ("hg", ct, 1)]
        cwr = ("cw", ct)
        k.op("dve", "tensor_scalar", out=acc1[:], in0=hg[:, 0:NPOS], scalar1=cw[:, 0:1], scalar2=cp[:, 0:1], op0=ALU.mult, op1=ALU.add, r=hres + [cwr, ("cp0", ct)], w=[RA])
        NDVE = 27
        k.op("act", "activation", out=acc2[:], in_=hg[:, NDVE:NDVE + NPOS], func=AF.Identity, scale=cw[:, NDVE:NDVE + 1], r=hres + [cwr], w=RG)
        extra = list(range(NDVE + 1, 31))
        for j in range(1, NDVE):
            k.op("dve", "scalar_tensor_tensor", out=acc1[:], in0=hg[:, j:j + NPOS], scalar=cw[:, j:j + 1], in1=acc1[:], op0=ALU.mult, op1=ALU.add, r=hres + [cwr, RA], w=[RA])
            if j % 8 == 1 and extra:
                jj = extra.pop(0)
                tb = jj % 2
                k.op("act", "activation", out=ctmp[tb][:], in_=hg[:, jj:jj + NPOS], func=AF.Identity, scale=cw[:, jj:jj + 1], r=hres + [cwr], w=[("ctmp", tb)])
                k.op("pool", "tensor_tensor", out=acc2[:], in0=acc2[:], in1=ctmp[tb][:], op=ALU.add, r=[("ctmp", tb)] + RG, w=RG)
            if j % 3 == 0:
                yield
        assert not extra
        k.op("dve", "tensor_tensor", out=acc1[:], in0=acc1[:], in1=acc2[:], op=ALU.add, r=[RA] + RG, w=[RA])

    def gn(ct):
        r0 = ct * 128
        acc1, cp = Ab[ct], cpb[ct]
        RA = ("A", ct)
        for c0 in range(0, NPOS, 512):
            n = min(512, NPOS - c0)
            b = st["cc"] % 2
            st["cc"] += 1
            psM, psE = k.ps(2 * b), k.ps(2 * b + 1)
            k.mm(psM[:, 0:n], gmat[:], acc1[:, c0:c0 + n], True, True, r=["gmat", RA], w=[("psM", b)])
            k.op("dve", "tensor_tensor", out=ddb[b][:, 0:n], in0=acc1[:, c0:c0 + n], in1=psM[:, 0:n], op=ALU.subtract, r=[RA, ("psM", b)], w=[("ddb", b)])
            k.op("act", "activation", out=sqb[b][:, 0:n], in_=ddb[b][:, 0:n], func=AF.Square, r=[("ddb", b)], w=[("sqb", b)])
            k.mm(psE[:, 0:n], gmat[:], sqb[b][:, 0:n], True, True, r=["gmat", ("sqb", b)], w=[("psE", b)])
            k.op("act", "activation", out=m2b[b][:, 0:n], in_=psE[:, 0:n], func=AF.Sqrt, bias=1e-5, scale=1.0, r=[("psE", b)], w=[("m2b", b)])
            k.op("dve", "reciprocal", out=m2b[b][:, 0:n], in_=m2b[b][:, 0:n], r=[("m2b", b)], w=[("m2b", b)])
            k.op("dve", "tensor_tensor", out=ddb[b][:, 0:n], in0=ddb[b][:, 0:n], in1=m2b[b][:, 0:n], op=ALU.mult, r=[("ddb", b), ("m2b", b)], w=[("ddb", b)])
            k.op("act", "activation", out=ob[b][:, 0:n], in_=ddb[b][:, 0:n], func=AF.Silu, bias=cp[:, 2:3], scale=cp[:, 1:2], r=[("ddb", b), ("cp1", ct), ("cp2", ct)], w=[("cob", b)])
            k.dma("sp", d["MIXD"][r0:r0 + 128, c0:c0 + n], ob[b][:, 0:n], r=[("cob", b)], w=[("MIXD", ct, c0)])
            yield

    for _ in taps(0):
        pass
    g0, t1 = gn(0), taps(1)
    alive = [g0, t1]
    while alive:
        for g in list(alive):
            try:
                next(g)
            except StopIteration:
                alive.remove(g)
    for _ in gn(1):
        pass
    S.barrier()
    A.release()


def stage_out(k, l):
    S, A, d = k.S, k.A, k.dr
    A.mark()
    idt = A.t([128, 128], BF16, "ident")
    k.dma("sp", idt[:], d["ident_bf"], w=["ident"])
    wo = A.t([128, 8, 1024], BF16, "wo")
    for kk in range(8):
        k.dma("pool", wo[:, kk, :], d["w_out"][l, kk * 128:(kk + 1) * 128, :], w=[("wo", kk)])
    wres = [("wo", kk) for kk in range(8)]
    gb = A.t([128, 1024], F32, "gb"); bb = A.t([128, 1024], F32, "bb")
    load_bcast(k, gb, d["ln1_g"][l:l + 1, :], "gb"); load_bcast(k, bb, d["ln1_b"][l:l + 1, :], "bb")
    nb = 4
    mxs = [A.t([128, 768], BF16, f"mx{i}") for i in range(nb)]
    mxds = [A.t([128, 2, 128], BF16, f"mxd{i}") for i in range(nb)]
    mTs = [A.t([128, 6, 128], BF16, f"mT{i}") for i in range(nb)]
    hs = [A.t([128, 1024], F32, f"h{i}") for i in range(nb)]
    zs = [A.t([128, 1024], F32, f"z{i}") for i in range(nb)]
    ys = [A.t([128, 1024], F32, f"y{i}") for i in range(nb)]
    tmps = [ln_tmp(k, str(i)) for i in range(nb)]

    def body(t):
        p0, n = tile_rows(t)
        b = t % nb
        mx, mxd, mT, h, z, y = mxs[b], mxds[b], mTs[b], hs[b], zs[b], ys[b]
        k.dma("sp", mx[0:n, :], d["MIX"][p0:p0 + n, :], w=[("mx", b)])
        for c in range(2):
            k.dma("sp", mxd[:, c, 0:n], d["MIXD"][c * 128:(c + 1) * 128, p0:p0 + n], w=[("mxd", b, c)])
        k.dma("act", h[0:n, :], d["H"][p0:p0 + n, :], w=[("h", b)])
        tb = t % 2
        pt = k.ps(tb)[:].bitcast(BF16)
        for kk in range(6):
            k.tr(pt[:, kk * 128:kk * 128 + n], mx[0:n, kk * 128:(kk + 1) * 128], idt[0:n, 0:n], r=[("mx", b), "ident"], w=[("ps", tb)])
        k.op("dve", "tensor_copy", out=mT[:, :, 0:n], in_=pt[:, 0:768].rearrange("p (k t) -> p k t", k=6)[:, :, 0:n], r=[("ps", tb)], w=[("mT", b)])
        yield
        for half in range(2):
            pb = 2 + 2 * (t % 3) + half
            for kk in range(8):
                lhsT = mT[:, kk, 0:n] if kk < 6 else mxd[:, kk - 6, 0:n]
                k.mm(k.ps(pb)[0:n, :], lhsT, wo[:, kk, half * 512:(half + 1) * 512], kk == 0, kk == 7,
                     r=[("mT", b), ("mxd", b, 0), ("mxd", b, 1)] + wres, w=[("ps", pb)])
            k.op("dve", "scalar_tensor_tensor", out=z[0:n, half * 512:(half + 1) * 512], in0=h[0:n, half * 512:(half + 1) * 512], scalar=ALPHA, in1=k.ps(pb)[0:n, :],
                 op0=ALU.mult, op1=ALU.add, r=[("h", b), ("ps", pb)], w=[("z", b)])
        yield
        yield from layernorm_tile_g(k, z, y, n, b, gb, bb, tmp=tmps[b])
        k.dma("act", d["HM"][p0:p0 + n, :], y[0:n, :], r=[("y", b)], w=[("HM", t)])
    run_skewed([body(t) for t in range(NT)])
    S.barrier()
    A.release()


def stage_moe(k, l, last=False):
    S, A, d = k.S, k.A, k.dr
    A.mark()
    comb = A.t([128, NT, 16], F32, "comb")
    A.mark()
    idf = A.t([128, 128], F32, "identf")
    k.dma("sp", idf[:], d["ident_f"], w=["identf"])
    rw = A.t([128, 8, 16], F32, "rw")
    k.dma("sp", rw[:], d["router_w"].rearrange("(k p) e -> p k e", p=128), w=["rw"])
    rb = A.t([128, 16], F32, "rb")
    load_bcast(k, rb, d["router_b"], "rb")
    nb1 = 4
    hms = [A.t([128, 1024], F32, f"hm{i}") for i in range(nb1)]
    hT32 = [A.t([128, 8, 128], F32, f"hT32{i}") for i in range(nb1)]
    hTb = [A.t([128, 8, 128], BF16, f"hTb{i}") for i in range(nb1)]
    rt = [dict((nm, A.t([128, 16], F32, f"{nm}{i}")) for nm in ("sc", "bi", "eq", "b2", "mk", "s1", "mk2", "s2")) for i in range(nb1)]
    rs4 = [dict((nm, A.t([128, 4], F32, f"{nm}{i}")) for nm in ("m1", "m2", "gs", "ing")) for i in range(nb1)]
    rs1 = [dict((nm, A.t([128, 1], F32, f"{nm}{i}")) for nm in ("gm", "t1", "t2", "den")) for i in range(nb1)]

    def body1(t):
        p0, n = tile_rows(t)
        b = t % nb1
        hm = hms[b]
        k.dma("sp", hm[0:n, :], d["HM"][p0:p0 + n, :], r=[("HM", t)], w=[("hm", b)])
        b3 = t % 3
        for kk in range(8):
            pb = 2 * b3 + kk // 4
            k.tr(k.ps(pb)[:, (kk % 4) * 128:(kk % 4) * 128 + n], hm[0:n, kk * 128:(kk + 1) * 128], idf[0:n, 0:n], r=[("hm", b), "identf"], w=[("ps", pb)])
        for hf in range(2):
            pb = 2 * b3 + hf
            src = k.ps(pb)[:].rearrange("p (k t) -> p k t", k=4)[:, :, 0:n]
            k.op("act", "copy", out=hT32[b][:, 4 * hf:4 * hf + 4, 0:n], in_=src, r=[("ps", pb)], w=[("hT32", b, hf)])
            k.op("dve", "tensor_copy", out=hTb[b][:, 4 * hf:4 * hf + 4, 0:n], in_=src, r=[("ps", pb)], w=[("hTb", b, hf)])
        k.dma("act", d["HT"][:, p0:p0 + n].rearrange("(k p) t -> p k t", p=128), hTb[b][:, :, 0:n], r=[("hTb", b, 0), ("hTb", b, 1)], w=[("HT", t)])
        yield
        pr = k.ps(6 + t % 2)
        for kk in range(8):
            k.mm(pr[0:n, 0:16], hT32[b][:, kk, 0:n], rw[:, kk, :], kk == 0, kk == 7, r=[("hT32", b, 0), ("hT32", b, 1), "rw"], w=[("psr", t % 2)])
        R_, R4, R1 = rt[b], rs4[b], rs1[b]
        rk = lambda nm: ("rt", nm, b)
        v4 = lambda ap: ap[0:n, :].rearrange("p (g e) -> p g e", g=4)
        k.op("act", "activation", out=R_["sc"][0:n, :], in_=pr[0:n, 0:16], func=AF.Sigmoid, r=[("psr", t % 2)], w=[rk("sc")])
        yield
        k.op("dve", "tensor_tensor", out=R_["bi"][0:n, :], in0=R_["sc"][0:n, :], in1=rb[0:n, :], op=ALU.add, r=[rk("sc"), "rb"], w=[rk("bi")])
        k.op("dve", "tensor_reduce", out=R4["m1"][0:n, :], in_=v4(R_["bi"]), axis=AX.X, op=ALU.max, r=[rk("bi")], w=[rk("m1")])
        k.op("dve", "tensor_tensor", out=v4(R_["eq"]), in0=v4(R_["bi"]), in1=R4["m1"][0:n, :].unsqueeze(2).to_broadcast([n, 4, 4]), op=ALU.is_equal, r=[rk("bi"), rk("m1")], w=[rk("eq")])
        k.op("dve", "scalar_tensor_tensor", out=R_["b2"][0:n, :], in0=R_["eq"][0:n, :], scalar=NEG, in1=R_["bi"][0:n, :], op0=ALU.mult, op1=ALU.add, r=[rk("eq"), rk("bi")], w=[rk("b2")])
        k.op("dve", "tensor_reduce", out=R4["m2"][0:n, :], in_=v4(R_["b2"]), axis=AX.X, op=ALU.max, r=[rk("b2")], w=[rk("m2")])
        k.op("dve", "tensor_tensor", out=R4["gs"][0:n, :], in0=R4["m1"][0:n, :], in1=R4["m2"][0:n, :], op=ALU.add, r=[rk("m1"), rk("m2")], w=[rk("gs")])
        k.op("dve", "tensor_reduce", out=R1["gm"][0:n, :], in_=R4["gs"][0:n, :], axis=AX.X, op=ALU.max, r=[rk("gs")], w=[rk("gm")])
        k.op("dve", "tensor_scalar", out=R4["ing"][0:n, :], in0=R4["gs"][0:n, :], scalar1=R1["gm"][0:n, 0:1], scalar2=None, op0=ALU.is_equal, r=[rk("gs"), rk("gm")], w=[rk("ing")])
        k.op("dve", "tensor_scalar", out=R4["ing"][0:n, :], in0=R4["ing"][0:n, :], scalar1=1.0, scalar2=-NEG, op0=ALU.subtract, op1=ALU.mult, r=[rk("ing")], w=[rk("ing")])
        k.op("dve", "tensor_tensor", out=v4(R_["mk"]), in0=v4(R_["bi"]), in1=R4["ing"][0:n, :].unsqueeze(2).to_broadcast([n, 4, 4]), op=ALU.add, r=[rk("bi"), rk("ing")], w=[rk("mk")])
        k.op("dve", "tensor_reduce", out=R1["t1"][0:n, :], in_=R_["mk"][0:n, :], axis=AX.X, op=ALU.max, r=[rk("mk")], w=[rk("t1")])
        k.op("dve", "tensor_scalar", out=R_["s1"][0:n, :], in0=R_["mk"][0:n, :], scalar1=R1["t1"][0:n, 0:1], scalar2=None, op0=ALU.is_equal, r=[rk("mk"), rk("t1")], w=[rk("s1")])
        k.op("dve", "scalar_tensor_tensor", out=R_["mk2"][0:n, :], in0=R_["s1"][0:n, :], scalar=NEG, in1=R_["mk"][0:n, :], op0=ALU.mult, op1=ALU.add, r=[rk("s1"), rk("mk")], w=[rk("mk2")])
        k.op("dve", "tensor_reduce", out=R1["t2"][0:n, :], in_=R_["mk2"][0:n, :], axis=AX.X, op=ALU.max, r=[rk("mk2")], w=[rk("t2")])
        k.op("dve", "tensor_scalar", out=R_["s2"][0:n, :], in0=R_["mk2"][0:n, :], scalar1=R1["t2"][0:n, 0:1], scalar2=None, op0=ALU.is_equal, r=[rk("mk2"), rk("t2")], w=[rk("s2")])
        k.op("dve", "tensor_tensor", out=R_["s1"][0:n, :], in0=R_["s1"][0:n, :], in1=R_["s2"][0:n, :], op=ALU.add, r=[rk("s1"), rk("s2")], w=[rk("s1")])
        k.op("dve", "tensor_tensor", out=R_["s1"][0:n, :], in0=R_["s1"][0:n, :], in1=R_["sc"][0:n, :], op=ALU.mult, r=[rk("s1"), rk("sc")], w=[rk("s1")])
        k.op("dve", "tensor_reduce", out=R1["den"][0:n, :], in_=R_["s1"][0:n, :], axis=AX.X, op=ALU.add, r=[rk("s1")], w=[rk("den")])
        k.op("dve", "reciprocal", out=R1["den"][0:n, :], in_=R1["den"][0:n, :], r=[rk("den")], w=[rk("den")])
        k.op("dve", "tensor_scalar", out=comb[0:n, t, :], in0=R_["s1"][0:n, :], scalar1=R1["den"][0:n, 0:1], scalar2=None, op0=ALU.mult, r=[rk("s1"), rk("den")], w=[("comb", t)])
    run_skewed([body1(t) for t in range(NT)], serial=True)
    S.barrier()
    A.release()
    gb = A.t([128, 1024], F32, "gb"); bb = A.t([128, 1024], F32, "bb")
    load_bcast(k, gb, d["ln2_g"][l:l + 1, :], "gb"); load_bcast(k, bb, d["ln2_b"][l:l + 1, :], "bb")
    hTh = A.t([128, 8, 2064], BF16, "hTh")
    acc = A.t([128, 17, 1024], F32, "acc")
    w1s = [A.t([128, 8, 512], BF16, f"w1s{i}") for i in range(2)]
    w3s = [A.t([128, 8, 512], BF16, f"w3s{i}") for i in range(2)]
    w2s = [A.t([128, 4, 1024], BF16, f"w2s{i}") for i in range(2)]
    actT = [A.t([128, 4, 256], BF16, f"actT{i}") for i in range(2)]
    s1b = [A.t([128, 256], F32, f"s1b{i}") for i in range(2)]
    nb2 = 2
    z2s = [A.t([128, 1024], F32, f"z2{i}") for i in range(nb2)]; y2s = [A.t([128, 1024], F32, f"y2{i}") for i in range(nb2)]
    tmp2s = [ln_tmp(k, f"m{i}") for i in range(nb2)]
    ecount = 0
    fcount = 0
    ocount = 0
    gcount = 0
    pending = []
    for half, tiles in enumerate([list(range(0, 17)), list(range(17, 33))]):
        hp0 = tile_rows(tiles[0])[0]
        hn = sum(tile_rows(t)[1] for t in tiles)
        for kk in range(8):
            k.dma("sp", hTh[:, kk, 0:hn], d["HT"][kk * 128:(kk + 1) * 128, hp0:hp0 + hn], r=[("HT", t) for t in tiles], w=[("hTh", kk)])
        hres = [("hTh", kk) for kk in range(8)]
        groups = []
        tl = list(tiles)
        if tl[0] == 0:
            groups.append([0]); tl = tl[1:]
        for i in range(0, len(tl), 2):
            groups.append(tl[i:i + 2])
        for e in range(16):
            eb = ecount % 2
            ecount += 1
            k.dma("pool", w1s[eb][:], d["exp_w1"][l, e].rearrange("(k p) f -> p k f", p=128), w=[("w1s", eb)])
            k.dma("pool", w3s[eb][:], d["exp_w3"][l, e].rearrange("(k p) f -> p k f", p=128), w=[("w3s", eb)])
            k.dma("pool", w2s[eb][:], d["exp_w2"][l, e].rearrange("(k p) f -> p k f", p=128), w=[("w2s", eb)])
            for grp in groups:
                c0 = tile_rows(grp[0])[0] - hp0
                ng = sum(tile_rows(t)[1] for t in grp)
                ab = gcount % 2
                gcount += 1
                for f in range(4):
                    hb_ = fcount % 2
                    fcount += 1
                    ph = k.ps(hb_)
                    for kk in range(8):
                        k.mm(ph[:, 0:ng], w1s[eb][:, kk, f * 128:(f + 1) * 128], hTh[:, kk, c0:c0 + ng], kk == 0, kk == 7, r=hres + [("w1s", eb)], w=[("psh", hb_)])
                    for kk in range(8):
                        k.mm(ph[:, 256:256 + ng], w3s[eb][:, kk, f * 128:(f + 1) * 128], hTh[:, kk, c0:c0 + ng], kk == 0, kk == 7, r=hres + [("w3s", eb)], w=[("psh", hb_)])
                    k.op("act", "activation", out=s1b[hb_][:, 0:ng], in_=ph[:, 0:ng], func=AF.Silu, r=[("psh", hb_)], w=[("s1b", hb_)])
                    k.op("dve", "tensor_tensor", out=actT[ab][:, f, 0:ng], in0=s1b[hb_][:, 0:ng], in1=ph[:, 256:256 + ng], op=ALU.mult, r=[("s1b", hb_), ("psh", hb_)], w=[("actT", ab, f)])
                def phase2(grp=grp, ab=ab, eb=eb, e=e, tiles=tiles):
                    nonlocal ocount
                    for ti, t in enumerate(grp):
                        nt_ = tile_rows(t)[1]
                        tloc = t - tiles[0]
                        for h2 in range(2):
                            ob_ = 2 + ocount % 6
                            ocount += 1
                            po = k.ps(ob_)
                            for f in range(4):
                                k.mm(po[0:nt_, :], actT[ab][:, f, ti * 128:ti * 128 + nt_], w2s[eb][:, f, h2 * 512:(h2 + 1) * 512], f == 0, f == 3, r=[("actT", ab, f), ("w2s", eb)], w=[("pso", ob_)])
                            dst = acc[0:nt_, tloc, h2 * 512:(h2 + 1) * 512]
                            if e == 0:
                                k.op("dve", "tensor_scalar", out=dst, in0=po[0:nt_, :], scalar1=comb[0:nt_, t, e:e + 1], scalar2=None, op0=ALU.mult, r=[("pso", ob_), ("comb", t)], w=[("acc", tloc, h2)])
                            else:
                                k.op("dve", "scalar_tensor_tensor", out=dst, in0=po[0:nt_, :], scalar=comb[0:nt_, t, e:e + 1], in1=dst, op0=ALU.mult, op1=ALU.add, r=[("pso", ob_), ("comb", t), ("acc", tloc, h2)], w=[("acc", tloc, h2)])
                while pending:
                    pending.pop(0)()
                pending.append(phase2)
        while pending:
            pending.pop(0)()
        def body2(t, tiles=tiles):
            p0, n = tile_rows(t)
            tloc = t - tiles[0]
            b = t % nb2
            z, y = z2s[b], y2s[b]
            key = ("m", b)
            k.dma("sp", z[0:n, :], d["HM"][p0:p0 + n, :], r=[("HM", t)], w=[("z", key)])
            k.op("dve", "scalar_tensor_tensor", out=z[0:n, :], in0=z[0:n, :], scalar=ALPHA, in1=acc[0:n, tloc, :], op0=ALU.mult, op1=ALU.add, r=[("z", key), ("acc", tloc, 0), ("acc", tloc, 1)], w=[("z", key)])
            yield
            yield from layernorm_tile_g(k, z, y, n, key, gb, bb, tmp=tmp2s[b])
            if last:
                if t >= 1:
                    k.dma("act", d["out"][128 * (t - 1):128 * t, :], y[0:n, :], r=[("y", key)], w=[("outT", t)])
            else:
                k.dma("act", d["H"][p0:p0 + n, :], y[0:n, :], r=[("y", key)], w=[("H", t)])
        run_skewed([body2(t) for t in tiles])
        S.barrier()
    A.release()


def run_skewed(gens, window=None, serial=False):
    if serial:
        for g in gens:
            for _ in g:
                pass
        return
    active = []
    it = iter(gens)
    while True:
        if window is None or len(active) < window:
            g = next(it, None)
            if g is not None:
                active.append(g)
        if not active:
            break
        for g in list(active):
            try:
                next(g)
            except StopIteration:
                active.remove(g)


def stage_rwkv_a(k, l):
    S, A, d = k.S, k.A, k.dr
    A.mark()
    cnt = [0]

    def T(shape, dt, name):
        cnt[0] += 1
        return TB(A.t(shape, dt, name), (name, cnt[0]))

    P = [TB(k.ps(i), ("ps", i)) for i in range(8)]
    DV = lambda ap: Vw(ap, [])
    idt = T([128, 128], BF16, "ident"); k.do("sp", "dma_start", idt[:], in_=DV(d["ident_bf"]))
    mu_b = T([128, 1024], F32, "mu_b"); k.do("sp", "dma_start", mu_b[:], in_=DV(d["rwkv_mu"][l:l + 1, :].partition_broadcast(128)))
    wa0 = T([128, 512], F32, "wa0")
    k.do("sp", "dma_start", wa0[:, 0:256], in_=DV(d["rwkv_w0"][l:l + 1, :].partition_broadcast(128)))
    k.do("sp", "dma_start", wa0[:, 256:512], in_=DV(d["rwkv_a0"][l:l + 1, :].partition_broadcast(128)))
    kk_b = T([128, 256], F32, "kk_b"); k.do("sp", "dma_start", kk_b[:], in_=DV(d["rwkv_kk"][l:l + 1, :].partition_broadcast(128)))
    ka_b = T([128, 256], F32, "ka_b"); k.do("sp", "dma_start", ka_b[:], in_=DV(d["rwkv_ka"][l:l + 1, :].partition_broadcast(128)))
    rk_b = T([128, 256], F32, "rk_b"); k.do("sp", "dma_start", rk_b[:], in_=DV(d["rwkv_rk"][l:l + 1, :].partition_broadcast(128)))
    WA = T([128, 512], BF16, "WA")
    k.do("pool", "memset", WA[:], constant=0.0)
    k.do("pool", "dma_start", WA[0:64, 0:256], in_=DV(d["rwkv_w2"][l]))
    k.do("pool", "dma_start", WA[64:128, 256:512], in_=DV(d["rwkv_a2"][l]))
    G2 = T([128, 256], BF16, "G2"); k.do("pool", "dma_start", G2[:], in_=DV(d["rwkv_g2"][l]))
    ctri = T([128, 128], F32, "ctri"); k.do("sp", "dma_start", ctri[:], in_=DV(d["ctri"]))
    cblk = T([128, 128], F32, "cblk"); k.do("sp", "dma_start", cblk[:], in_=DV(d["cblk"]))
    cones = T([128, 2, 64], F32, "cones"); k.do("sp", "dma_start", cones[:], in_=DV(d["cones"]))
    idrep = T([64, 1, 256], F32, "idrep"); k.do("sp", "dma_start", idrep[:].re("p o f -> p (o f)"), in_=DV(d["identrep"]))
    zrow = T([1, 1024], F32, "zrow")
    k.do("pool", "memset", zrow[:], constant=0.0)
    uc0 = ("UC0",)
    k.do("sp", "dma_start", Vw(d["UC"][0:1, :], [uc0]), in_=zrow[:])
    nb = 5
    mk_ = lambda shape, dt, nm, n=None: [T(shape, dt, f"{nm}{i}") for i in range(n or nb)]
    cur_, prv_, ucs_ = mk_([128, 1024], F32, "cur", 2), mk_([128, 1024], F32, "prv", 2), mk_([128, 1024], F32, "ucs", 6)
    LI_, LT_ = mk_([128, 256], BF16, "LI"), mk_([128, 2, 128], BF16, "LT")
    wa_, logw_ = mk_([128, 512], F32, "wa"), mk_([128, 256], F32, "logw")
    W_, Wi_, Wp_, Web_ = mk_([128, 256], F32, "W"), mk_([128, 256], F32, "Wi"), mk_([128, 256], F32, "Wp"), mk_([128, 256], F32, "Web")
    DWe_ = mk_([64, 2, 256], F32, "DWe"); DWt_ = mk_([64, 2, 256], BF16, "DWt")
    kq_, sq_, kkn_, kmod_, bb_, t1_ = (mk_([128, 256], F32, nm) for nm in ("kq", "sq", "kkn", "kmod", "bb", "t1"))
    ss_, rs_ = mk_([128, 4, 1], F32, "ss"), mk_([128, 4, 1], F32, "rs")
    kh_, bh_ = mk_([128, 256], F32, "kh"), mk_([128, 256], F32, "bh")
    OT_ = mk_([128, 7, 256], BF16, "OT"); BG_ = mk_([128, 2, 256], F32, "BGt")
    NE5 = -math.exp(-0.5)
    def body(t):
        p0, n = tile_rows(t)
        b = t % nb
        C = 16 if t == 0 else 64
        ncn = 1 if t == 0 else 2
        ch0 = 0 if t == 0 else 2 * (t - 1) + 1
        cur, prv, ucs, LI, LT, wa, logw = cur_[t % 2], prv_[t % 2], ucs_[t % 6], LI_[b], LT_[b], wa_[b], logw_[b]
        W, Wi, Wp, Web, DWe, DWt = W_[b], Wi_[b], Wp_[b], Web_[b], DWe_[b], DWt_[b]
        kq, sq, kkn, kmod, bb, t1, ss, rs, kh, bh, OT, BGt = kq_[b], sq_[b], kkn_[b], kmod_[b], bb_[b], t1_[b], ss_[b], rs_[b], kh_[b], bh_[b], OT_[b], BG_[b]
        k.do("sp", "dma_start", cur[0:n, :], in_=DV(d["UC"][1 + p0:1 + p0 + n, :]))
        k.do("sp", "dma_start", prv[0:n, :], in_=Vw(d["UC"][p0:p0 + n, :], [uc0] if t == 0 else []))
        k.do("dve", "tensor_tensor", prv[0:n, :], in0=prv[0:n, :], in1=cur[0:n, :], op=ALU.subtract)
        k.do("dve", "tensor_tensor", prv[0:n, :], in0=prv[0:n, :], in1=mu_b[0:n, :], op=ALU.mult)
        k.do("dve", "tensor_tensor", ucs[0:n, :], in0=cur[0:n, :], in1=prv[0:n, :], op=ALU.add)
        r_, kraw, v_ = ucs[0:n, 0:256], ucs[0:n, 256:512], ucs[0:n, 512:768]
        yield
        k.do("act", "activation", LI[0:n, 0:64], in_=ucs[0:n, 768:832], func=AF.Tanh)
        k.do("act", "copy", LI[0:n, 64:128], in_=ucs[0:n, 832:896])
        k.do("act", "activation", LI[0:n, 128:256], in_=ucs[0:n, 896:1024], func=AF.Sigmoid)
        ptb = Vw(k.ps(0)[:].bitcast(BF16), P[0].res)
        k.trv(ptb[:, 0:n], LI[0:n, 0:128], idt[0:n, 0:n])
        k.trv(ptb[:, 128:128 + n], LI[0:n, 128:256], idt[0:n, 0:n])
        k.do("dve", "tensor_copy", LT[:, :, 0:n], in_=ptb[:, 0:256].re("p (a t) -> p a t", a=2)[:, :, 0:n])
        yield
        k.mmv(P[1][0:n, :], LT[:, 0, 0:n], WA[:], True, True)
        k.mmv(P[2][0:n, 0:256], LT[:, 1, 0:n], G2[:], True, True)
        k.do("dve", "tensor_tensor", wa[0:n, :], in0=P[1][0:n, :], in1=wa0[0:n, :], op=ALU.add)
        k.do("act", "activation", wa[0:n, :], in_=wa[0:n, :], func=AF.Sigmoid)
        a_ = wa[0:n, 256:512]
        k.do("act", "mul", logw[0:n, :], in_=wa[0:n, 0:256], mul=NE5)
        k.do("act", "copy", BGt[0:n, 1, :], in_=P[2][0:n, 0:256])
        yield
        k.mmv(P[3][0:n, 0:256], ctri[0:n, 0:n], logw[0:n, :], True, True)
        k.mmv(P[4][0:n, 0:256], cblk[0:n, 0:n], logw[0:n, :], True, True)
        for c in range(ncn):
            k.mmv(P[5][0:64, c * 256:(c + 1) * 256], cones[0:n, c, :], logw[0:n, :], True, True)
        k.do("act", "activation", W[0:n, :], in_=P[3][0:n, 0:256], func=AF.Exp)
        k.do("act", "activation", Wi[0:n, :], in_=P[3][0:n, 0:256], func=AF.Exp, scale=-1.0)
        k.do("dve", "tensor_tensor", Wp[0:n, :], in0=P[3][0:n, 0:256], in1=logw[0:n, :], op=ALU.subtract)
        k.do("act", "activation", Wp[0:n, :], in_=Wp[0:n, :], func=AF.Exp)
        k.do("act", "activation", Web[0:n, :], in_=P[4][0:n, 0:256], func=AF.Exp)
        k.do("act", "activation", DWe[:, 0:ncn, :], in_=P[5][0:64, 0:ncn * 256].re("p (c f) -> p c f", c=ncn), func=AF.Exp)
        k.do("dve", "tensor_tensor", DWt[:, 0:ncn, :], in0=DWe[:, 0:ncn, :], in1=idrep[:].bc([64, ncn, 256]), op=ALU.mult)
        k.do("act", "dma_start", DV(d["DW"][ch0:ch0 + ncn].rearrange("c p f -> p c f")), in_=DWt[:, 0:ncn, :])
        yield
        k.do("dve", "tensor_tensor", kq[0:n, :], in0=kraw, in1=kk_b[0:n, :], op=ALU.mult)
        k.do("act", "activation", sq[0:n, :], in_=kq[0:n, :], func=AF.Square)
        k.do("dve", "tensor_reduce", ss[0:n], in_=sq[0:n, :].re("p (h e) -> p h e", h=4), axis=AX.X, op=ALU.add)
        k.do("act", "activation", ss[0:n], in_=ss[0:n], func=AF.Sqrt)
        k.do("dve", "tensor_scalar", ss[0:n], in0=ss[0:n], scalar1=1e-12, scalar2=None, op0=ALU.max)
        k.do("dve", "reciprocal", ss[0:n], in_=ss[0:n])
        k.do("dve", "tensor_tensor", kkn[0:n, :].re("p (h e) -> p h e", h=4), in0=kq[0:n, :].re("p (h e) -> p h e", h=4), in1=ss[0:n].bc([n, 4, 64]), op=ALU.mult)
        k.do("dve", "scalar_tensor_tensor", t1[0:n, :], in0=a_, scalar=-1.0, in1=ka_b[0:n, :], op0=ALU.add, op1=ALU.mult)
        k.do("dve", "scalar_tensor_tensor", kmod[0:n, :], in0=t1[0:n, :], scalar=1.0, in1=kraw, op0=ALU.add, op1=ALU.mult)
        k.do("dve", "tensor_tensor", bb[0:n, :], in0=kkn[0:n, :], in1=a_, op=ALU.mult)
        k.do("pool", "tensor_tensor", t1[0:n, :], in0=r_, in1=kmod[0:n, :], op=ALU.mult)
        k.do("pool", "tensor_tensor", t1[0:n, :], in0=t1[0:n, :], in1=rk_b[0:n, :], op=ALU.mult)
        k.do("dve", "tensor_reduce", rs[0:n], in_=t1[0:n, :].re("p (h e) -> p h e", h=4), axis=AX.X, op=ALU.add)
        k.do("dve", "tensor_tensor", BGt[0:n, 0, :].re("p (h e) -> p h e", h=4), in0=v_.re("p (h e) -> p h e", h=4), in1=rs[0:n].bc([n, 4, 64]), op=ALU.mult)
        yield
        k.do("dve", "tensor_tensor", OT[0:n, 0, :], in0=kkn[0:n, :], in1=Wp[0:n, :], op=ALU.mult)
        k.do("dve", "tensor_tensor", OT[0:n, 1, :], in0=r_, in1=W[0:n, :], op=ALU.mult)
        k.do("dve", "tensor_tensor", kh[0:n, :], in0=kmod[0:n, :], in1=Wi[0:n, :], op=ALU.mult)
        k.do("pool", "tensor_tensor", bh[0:n, :], in0=bb[0:n, :], in1=Wi[0:n, :], op=ALU.mult)
        k.do("act", "copy", OT[0:n, 2, :], in_=kh[0:n, :])
        k.do("act", "copy", OT[0:n, 3, :], in_=bh[0:n, :])
        k.do("dve", "tensor_tensor", OT[0:n, 4, :], in0=kh[0:n, :], in1=Web[0:n, :], op=ALU.mult)
        k.do("dve", "scalar_tensor_tensor", OT[0:n, 5, :], in0=bh[0:n, :], scalar=-1.0, in1=Web[0:n, :], op0=ALU.mult, op1=ALU.mult)
        k.do("act", "copy", OT[0:n, 6, :], in_=v_)
        k.do("sp", "dma_start", DV(d["RWT"][p0:p0 + n]), in_=OT[0:n])
        k.do("sp", "dma_start", DV(d["BG"][p0:p0 + n]), in_=BGt[0:n])
    run_skewed([body(t) for t in range(NT)])
    S.barrier()
    A.release()


def stage_rwkv_b(k, l, tmax=NT):
    S, A, d = k.S, k.A, k.dr
    A.mark()
    cnt = [0]

    def T(shape, dt, name):
        cnt[0] += 1
        return TB(A.t(shape, dt, name), (name, cnt[0]))

    P = [TB(k.ps(i), ("ps", i)) for i in range(8)]
    DV = lambda ap: Vw(ap, [])
    idt = T([128, 128], BF16, "ident"); k.do("sp", "dma_start", idt[:], in_=DV(d["ident_bf"]))
    rmask = T([64, 5, 64], F32, "rmask"); k.do("sp", "dma_start", rmask[:], in_=DV(d["rmask"]))
    lg_b = T([64, 256], F32, "lg_b"); k.do("sp", "dma_start", lg_b[:], in_=DV(d["rwkv_lnx_g"][l:l + 1, :].partition_broadcast(64)))
    lb_b = T([64, 256], F32, "lb_b"); k.do("sp", "dma_start", lb_b[:], in_=DV(d["rwkv_lnx_b"][l:l + 1, :].partition_broadcast(64)))
    Tb = [T([64, 4, 64], BF16, f"Tst{i}") for i in range(2)]
    k.do("pool", "memset", Tb[0][:], constant=0.0)
    nb = 3
    mk_ = lambda shape, dt, nm, n=None: [T(shape, dt, f"{nm}{i}") for i in range(n or nb)]
    tokX_ = mk_([64, 2, 7, 256], BF16, "tokX"); DWc_ = mk_([64, 2, 256], BF16, "DWc"); BGc_ = mk_([64, 2, 2, 256], F32, "BGc")
    XT_ = mk_([64, 2, 4, 4, 64], BF16, "XT")
    AT_ = mk_([64, 8, 2, 64], BF16, "AT"); BT_ = mk_([64, 8, 2, 64], BF16, "BT"); Nn_ = mk_([64, 8, 64], BF16, "Nn")
    NPa_ = [mk_([64, 8, 64], BF16, f"NPa{i}_", 2) for i in range(nb)]; NPTa_ = [mk_([64, 8, 64], BF16, f"NPTa{i}_", 2) for i in range(nb)]
    Zf_ = mk_([64, 8, 128], F32, "Zf"); Zb_ = mk_([64, 8, 128], BF16, "Zb")
    Q1T_ = mk_([64, 8, 64], BF16, "Q1T"); Q2s_ = mk_([64, 8, 64], F32, "Q2s"); GT_ = mk_([64, 8, 64], BF16, "GT"); Hs_ = mk_([64, 8, 64], F32, "Hs")
    ys_ = mk_([64, 4, 64], F32, "ys", 2); dd_ = mk_([64, 4, 64], F32, "dd", 2); sq_ = mk_([64, 4, 64], F32, "sq", 2)
    st_ = mk_([64, 4, 1], F32, "st", 2); yo_ = mk_([64, 256], BF16, "yo", 2)
    state = {"tcur": 0, "ccount": 0}

    def body(t):
        p0, n = tile_rows(t)
        b = t % nb
        Lb = [P[4 * (t % 2) + i] for i in range(4)]
        NPa, NPTa = NPa_[b], NPTa_[b]
        C = 16 if t == 0 else 64
        ncn = 1 if t == 0 else 2
        nq = 4 * ncn
        ch0 = 0 if t == 0 else 2 * (t - 1) + 1
        tokX, DWc, BGc, XT, AT, BT, Nn, Zf, Zb = tokX_[b], DWc_[b], BGc_[b], XT_[b], AT_[b], BT_[b], Nn_[b], Zf_[b], Zb_[b]
        Q1T, Q2s, GT, Hs = Q1T_[b], Q2s_[b], GT_[b], Hs_[b]
        for c in range(ncn):
            r0 = p0 + 64 * c
            k.do("sp", "dma_start", tokX[0:C, c], in_=DV(d["RWT"][r0:r0 + C]))
            k.do("act", "dma_start", BGc[0:C, c], in_=DV(d["BG"][r0:r0 + C]))
        k.do("act", "dma_start", DWc[:, 0:ncn, :], in_=DV(d["DW"][ch0:ch0 + ncn].rearrange("c p f -> p c f")))
        for c in range(ncn):
            ptb = Vw(Lb[2 + c].h[:].bitcast(BF16), Lb[2 + c].res)
            pv = ptb[0:64, :].re("p (h x t) -> p h x t", h=4, x=4)
            for h in range(4):
                for X in range(4):
                    k.trv(pv[:, h, X, 0:C], tokX[0:C, c, X, h * 64:(h + 1) * 64], idt[0:C, 0:C])
            if c == 0:
                k.do("dve", "tensor_copy", XT[:, c, :, :, 0:C], in_=pv[:, :, :, 0:C])
            else:
                k.do("act", "copy", XT[:, c, :, :, 0:C], in_=pv[:, :, :, 0:C])
        yield
        for c in range(ncn):
            for h in range(4):
                q = c * 4 + h
                o1 = Lb[q // 4][0:C, :].re("p (q a t) -> p q a t", q=4, a=2)[:, q % 4, :, 0:C]
                k.mmv(o1, XT[:, c, h, 2, 0:C], XT[:, c, h, 0:2, 0:C], True, True)
                o2 = Lb[2 + q // 4][0:C, :].re("p (q a t) -> p q a t", q=4, a=2)[:, q % 4, :, 0:C]
                k.mmv(o2, XT[:, c, h, 3, 0:C], XT[:, c, h, 0:2, 0:C], True, True)
        for c in range(ncn):
            pv1 = Lb[c][0:C, :].re("p (q a t) -> p q a t", q=4, a=2)[:, :, :, 0:C]
            k.do("dve", "tensor_tensor", AT[0:C, 4 * c:4 * c + 4, :, 0:C], in0=pv1, in1=rmask[0:C, 0:2, 0:C].un(1).bc([C, 4, 2, C]), op=ALU.mult)
            pv2 = Lb[2 + c][0:C, :].re("p (q a t) -> p q a t", q=4, a=2)[:, :, :, 0:C]
            k.do("dve", "tensor_tensor", BT[0:C, 4 * c:4 * c + 4, :, 0:C], in0=pv2, in1=rmask[0:C, 2:4, 0:C].un(1).bc([C, 4, 2, C]), op=ALU.mult)
        for c in range(ncn):
            for h in range(4):
                q = c * 4 + h
                o3 = Lb[0][0:C, :].re("p (q t) -> p q t", q=8)[:, q, 0:C]
                k.mmv(o3, XT[:, c, h, 0, 0:C], XT[:, c, h, 3, 0:C], True, True)
        pv3 = Lb[0][0:C, :].re("p (q t) -> p q t", q=8)[:, 0:nq, 0:C]
        k.do("dve", "tensor_tensor", Nn[0:C, 0:nq, 0:C], in0=pv3, in1=rmask[0:C, 4:5, 0:C].bc([C, nq, C]), op=ALU.mult)
        yield
        pav = Lb[1][0:C, :].re("p (q i) -> p q i", q=8)
        for c in range(ncn):
            for h in range(4):
                q = c * 4 + h
                k.mmv(pav[:, q, :], AT[0:C, q, 0, 0:C], tokX[0:C, c, 6, h * 64:(h + 1) * 64], True, True)
        k.do("act", "copy", Zf[0:C, 0:nq, 0:64].re("p (c h) e -> p c h e", c=ncn), in_=tokX[0:C, 0:ncn, 0, :].re("p c (h e) -> p c h e", h=4))
        k.do("act", "copy", Zf[0:C, 0:nq, 64:128], in_=pav[:, 0:nq, :])
        k.do("dve", "tensor_copy", Zb[0:C, 0:nq, :], in_=Zf[0:C, 0:nq, :])
        yield
        L = 3 if t == 0 else 5
        NPc, NPTc = Nn, None
        for lev in range(L + 1):
            def npt(q):
                return BT[0:C, q, 0, 0:C] if lev == 0 else NPTc[0:C, q, 0:C]
            for q in range(nq):
                oz = Lb[q // 4][0:C, :].re("p (q e) -> p q e", q=4)[:, q % 4, :]
                k.mmv(oz, npt(q), Zb[0:C, q, :], True, True)
            if lev < L:
                nxtP, nxtPT = NPa[lev % 2], NPTa[lev % 2]
                pa = Lb[2][0:C, :].re("p (q i) -> p q i", q=8)
                pbk = Lb[3][0:C, :].re("p (q i) -> p q i", q=8)
                for q in range(nq):
                    if lev + 1 < L:
                        k.mmv(pa[:, q, 0:C], npt(q), NPc[0:C, q, 0:C], True, True)
                    k.mmv(pbk[:, q, 0:C], NPc[0:C, q, 0:C], npt(q), True, True)
            for c in range(ncn):
                zv = Lb[c][0:C, :].re("p (q e) -> p q e", q=4)
                k.do("dve", "tensor_tensor", Zf[0:C, 4 * c:4 * c + 4, :], in0=Zf[0:C, 4 * c:4 * c + 4, :], in1=zv, op=(ALU.subtract if lev == 0 else ALU.add))
            k.do("act", "copy", Zb[0:C, 0:nq, :], in_=Zf[0:C, 0:nq, :])
            if lev < L:
                if lev + 1 < L:
                    k.do("act", "copy", nxtP[0:C, 0:nq, 0:C], in_=pa[:, 0:nq, 0:C])
                k.do("dve", "tensor_copy", nxtPT[0:C, 0:nq, 0:C], in_=pbk[:, 0:nq, 0:C])
                NPc, NPTc = nxtP, nxtPT
            yield
        pq1 = Lb[0][0:64, :].re("p (q t) -> p q t", q=8)
        pq2 = Lb[1][0:C, :].re("p (q i) -> p q i", q=8)
        pg = Lb[2][0:64, :].re("p (q j) -> p q j", q=8)
        ph = Lb[3][0:64, :].re("p (q i) -> p q i", q=8)
        for c in range(ncn):
            for h in range(4):
                q = c * 4 + h
                hs = slice(h * 64, (h + 1) * 64)
                P1b, P2b = Zb[0:C, q, 0:64], Zb[0:C, q, 64:128]
                V_ = tokX[0:C, c, 6, hs]
                k.mmv(pq1[:, q, 0:C], P1b, BT[0:C, q, 1, 0:C], True, False)
                k.mmv(pq1[:, q, 0:C], idt[0:64, 0:64], XT[:, c, h, 1, 0:C], False, True)
                k.mmv(pq2[:, q, :], AT[0:C, q, 1, 0:C], V_, True, False)
                k.mmv(pq2[:, q, :], BT[0:C, q, 1, 0:C], P2b, False, True)
                k.mmv(pg[:, q, :], idt[0:64, 0:64], DWc[:, c, hs], True, False)
                k.mmv(pg[:, q, :], P1b, tokX[0:C, c, 5, hs], False, True)
                k.mmv(ph[:, q, :], tokX[0:C, c, 4, hs], V_, True, False)
                k.mmv(ph[:, q, :], tokX[0:C, c, 5, hs], P2b, False, True)
        k.do("act", "copy", Q1T[:, 0:nq, 0:C], in_=pq1[:, 0:nq, 0:C])
        k.do("dve", "tensor_copy", Q2s[0:C, 0:nq, :], in_=pq2[:, 0:nq, :])
        k.do("act", "copy", GT[:, 0:nq, :], in_=pg[:, 0:nq, :])
        k.do("dve", "tensor_copy", Hs[:, 0:nq, :], in_=ph[:, 0:nq, :])
        yield
        for c in range(ncn):
            cb = state["ccount"] % 2
            state["ccount"] += 1
            tcur = state["tcur"]
            Tc, Tn = Tb[tcur], Tb[1 - tcur]
            state["tcur"] = 1 - tcur
            py = Lb[2 + c][0:C, 0:256].re("p (h i) -> p h i", h=4)
            pt_ = Lb[2 + c][0:64, 256:512].re("p (h i) -> p h i", h=4)
            for h in range(4):
                q = c * 4 + h
                k.mmv(pt_[:, h, :], GT[:, q, :], Tc[:, h, :], True, True)
            for h in range(4):
                q = c * 4 + h
                k.mmv(py[:, h, :], Q1T[:, q, 0:C], Tc[:, h, :], True, True)
            k.do("dve", "tensor_tensor", Tn[:], in0=pt_, in1=Hs[:, 4 * c:4 * c + 4, :], op=ALU.add)
            ys, dd, sq, st, yo = ys_[cb], dd_[cb], sq_[cb], st_[cb], yo_[cb]
            k.do("dve", "tensor_tensor", ys[0:C], in0=py, in1=Q2s[0:C, 4 * c:4 * c + 4, :], op=ALU.add)
            k.do("dve", "tensor_reduce", st[0:C], in_=ys[0:C], axis=AX.X, op=ALU.add)
            k.do("dve", "tensor_scalar", st[0:C], in0=st[0:C], scalar1=-1.0 / 64, scalar2=None, op0=ALU.mult)
            k.do("dve", "tensor_tensor", dd[0:C], in0=ys[0:C], in1=st[0:C].bc([C, 4, 64]), op=ALU.add)
            k.do("act", "activation", sq[0:C], in_=dd[0:C], func=AF.Square)
            k.do("dve", "tensor_reduce", st[0:C], in_=sq[0:C], axis=AX.X, op=ALU.add)
            k.do("act", "activation", st[0:C], in_=st[0:C], func=AF.Sqrt, bias=64e-5, scale=1.0 / 64)
            k.do("dve", "reciprocal", st[0:C], in_=st[0:C])
            k.do("dve", "tensor_tensor", dd[0:C], in0=dd[0:C], in1=st[0:C].bc([C, 4, 64]), op=ALU.mult)
            ddf = dd[0:C].re("p h e -> p (h e)")
            k.do("dve", "tensor_tensor", ddf, in0=ddf, in1=lg_b[0:C, :], op=ALU.mult)
            k.do("dve", "tensor_tensor", ddf, in0=ddf, in1=lb_b[0:C, :], op=ALU.add)
            k.do("pool", "tensor_tensor", ddf, in0=ddf, in1=BGc[0:C, c, 0, :], op=ALU.add)
            k.do("pool", "tensor_tensor", yo[0:C, :], in0=ddf, in1=BGc[0:C, c, 1, :], op=ALU.mult)
            r0 = p0 + 64 * c
            k.do("sp", "dma_start", DV(d["MIX"][r0:r0 + C, 512:768]), in_=yo[0:C, :])
            if c < ncn - 1:
                yield
    run_skewed([body(t) for t in range(tmax)], window=2)
    S.barrier()
    A.release()


def build_full():
    k = K()
    declare_io(k, final_out=True)
    stage_ln0(k)
    for l in range(DEPTH):
        stage_in(k, l)
        stage_swa(k, l)
        stage_diff(k, l)
        stage_rwkv_a(k, l)
        stage_rwkv_b(k, l)
        stage_conv(k, l)
        stage_out(k, l)
        stage_moe(k, l, last=(l == DEPTH - 1))
        k.S.barrier(rotate_dma=True)
    k.S.final_wait("sp")
    k.S.emit()
    k.st.close()
    return k


def kernel(**inputs):
    inp = {kk_: np.asarray(v) for kk_, v in inputs.items()}
    k = build_full()
    consts = host_consts(inp)
    in_maps = []
    for b in range(8):
        m = host_inputs(inp, b)
        m.update(consts)
        in_maps.append(m)
    res = run_bass_kernel_spmd(k.nc, in_maps, core_ids=list(range(8)))
    out = np.stack([np.asarray(res.results[b]["out"], np.float32) for b in range(8)], axis=0)
    return out
```

```python
import numpy as np
import concourse.bass as bass
import concourse.mybir as mybir

F32 = mybir.dt.float32
BF16 = mybir.dt.bfloat16
AF = mybir.ActivationFunctionType
ALU = mybir.AluOpType
AX = mybir.AxisListType

ENGS = ("pe", "act", "dve", "pool", "sp")
DQS = ("sp", "act", "pool")


class Sched:
    def __init__(self, nc, stack, ndma=6):
        self.nc = nc
        self.stack = stack
        self.ndma = ndma
        self.gen = 0
        self.dgen = 0
        self.objs = {}
        self.cnt = {e: 0 for e in ENGS}
        self.dcnt = {q: [0] * ndma for q in DQS}
        self.dnext = {q: 0 for q in DQS}
        self.streams = {e: [] for e in ENGS}
        self.waited = {e: {} for e in ENGS}
        self.lastw = {}
        self.readers = {}
        self.nops = 0
        self._new_csems()
        self._new_dsems()

    def _new_csems(self):
        self.gen += 1
        for e in ENGS:
            self.objs[("c", e, self.gen)] = self.stack.enter_context(self.nc.semaphore(f"c_{e}_{self.gen}"))
            self.cnt[e] = 0

    def _new_dsems(self):
        self.dgen += 1
        for q in DQS:
            for i in range(self.ndma):
                self.objs[("d", q, i, self.dgen)] = self.stack.enter_context(self.nc.semaphore(f"d_{q}{i}_{self.dgen}"))
            self.dcnt[q] = [0] * self.ndma

    def ckey(self, e):
        return ("c", e, self.gen)

    def dkey(self, q, i):
        return ("d", q, i, self.dgen)

    def _semobj(self, key):
        return self.objs[key]

    def _wait(self, eng, tok):
        key, val, src = tok
        if self.waited[eng].get(key, 0) >= val:
            return
        self.waited[eng][key] = val
        self.streams[eng].append(("w", key, val))

    def op(self, eng, fn, kw=None, r=(), w=(), dma=False):
        if isinstance(fn, str):
            name = fn
            kw = dict(kw)
            fn = lambda E, name=name, kw=kw: getattr(E, name)(**kw)
        deps = []
        for res in r:
            t = self.lastw.get(res)
            if t is not None:
                deps.append((t, "raw"))
        for res in w:
            t = self.lastw.get(res)
            if t is not None:
                deps.append((t, "waw"))
            for t in self.readers.get(res, ()):
                deps.append((t, "war"))
        for t, kind in deps:
            src = t[2]
            if src == eng and t[0][0] == "c":
                if eng == "pe":
                    continue
                if kind == "war":
                    continue
            self._wait(eng, t)
        if dma:
            q = eng
            i = self.dnext[q] % self.ndma
            self.dnext[q] += 1
            if self.dcnt[q][i] > 0:
                self._wait(eng, (self.dkey(q, i), self.dcnt[q][i], None))
            self.dcnt[q][i] += 16
            tok = (self.dkey(q, i), self.dcnt[q][i], None)
            self.streams[eng].append(("d", fn, self.dkey(q, i)))
        else:
            self.cnt[eng] += 1
            tok = (self.ckey(eng), self.cnt[eng], eng)
            self.streams[eng].append(("o", fn, self.ckey(eng), self.cnt[eng]))
        for res in w:
            self.lastw[res] = tok
            self.readers[res] = []
        for res in r:
            self.readers.setdefault(res, []).append(tok)
        self.nops += 1
        return tok

    def barrier(self, rotate_dma=False):
        for e in ENGS:
            for s in ENGS:
                if s != e and self.cnt[s] > 0:
                    self._wait(e, (self.ckey(s), self.cnt[s], s))
            for q in DQS:
                for i in range(self.ndma):
                    if self.dcnt[q][i] > 0:
                        self._wait(e, (self.dkey(q, i), self.dcnt[q][i], None))
        self.lastw = {}
        self.readers = {}
        for e in ENGS:
            self.streams[e].append(None)
        if max(self.cnt.values()) > 4000:
            self._new_csems()
        if rotate_dma:
            self._new_dsems()

    def final_wait(self, eng="sp"):
        for q in DQS:
            for i in range(self.ndma):
                if self.dcnt[q][i] > 0:
                    self._wait(eng, (self.dkey(q, i), self.dcnt[q][i], None))
        for s in ENGS:
            if s != eng and self.cnt[s] > 0:
                self._wait(eng, (self.ckey(s), self.cnt[s], s))

    def emit(self):
        nc = self.nc
        targets = {}
        for e in ENGS:
            for it in self.streams[e]:
                if it is not None and it[0] == "w" and it[1][0] == "c":
                    targets.setdefault(it[1], set()).add(it[2])
        rank = {key: {v: i + 1 for i, v in enumerate(sorted(vs))} for key, vs in targets.items()}
        self.n_inc = sum(len(v) for v in rank.values())

        def conv(it):
            if it[0] == "w":
                _, key, val = it
                s = self.objs[key]
                v = rank[key][val] if key[0] == "c" else val
                return lambda E, s=s, v=v: E.wait_ge(s, v)
            if it[0] == "d":
                _, fn, key = it
                s = self.objs[key]
                return lambda E, fn=fn, s=s: fn(E).then_inc(s, 16)
            _, fn, key, idx = it
            if idx in rank.get(key, ()):
                s = self.objs[key]
                return lambda E, fn=fn, s=s: fn(E).then_inc(s, 1)
            return lambda E, fn=fn: fn(E)

        segs = {e: [[]] for e in ENGS}
        for e in ENGS:
            for f in self.streams[e]:
                if f is None:
                    segs[e].append([])
                else:
                    segs[e][-1].append(conv(f))
        nseg = max(len(v) for v in segs.values())
        for i in range(nseg):
            cur = {e: (segs[e][i] if i < len(segs[e]) else []) for e in ENGS}
            if not any(cur.values()):
                continue
            with nc.Block() as block:
                @block.tensor
                def _(E, fs=cur["pe"]):
                    for f in fs:
                        f(E)

                @block.scalar
                def _(E, fs=cur["act"]):
                    for f in fs:
                        f(E)

                @block.vector
                def _(E, fs=cur["dve"]):
                    for f in fs:
                        f(E)

                @block.gpsimd
                def _(E, fs=cur["pool"]):
                    for f in fs:
                        f(E)

                @block.sync
                def _(E, fs=cur["sp"]):
                    for f in fs:
                        f(E)


class SbufAlloc:
    def __init__(self, nc, base=16640, limit=208 * 1024):
        self.nc = nc
        self.off = base
        self.limit = limit
        self.n = 0
        self.marks = []

    def mark(self):
        self.marks.append(self.off)

    def release(self):
        self.off = self.marks.pop()

    def t(self, shape, dtype, name=None):
        esz = 4 if dtype == F32 else 2
        if dtype in (mybir.dt.int32, mybir.dt.uint32):
            esz = 4
        nbytes = int(np.prod(shape[1:])) * esz
        nbytes = (nbytes + 63) // 64 * 64
        assert self.off + nbytes <= self.limit, f"SBUF overflow {self.off}+{nbytes} > {self.limit} ({name})"
        self.n += 1
        h = self.nc.alloc_sbuf_tensor_at(f"{name or 't'}_{self.n}", list(shape), dtype, offset=self.off)
        self.off += nbytes
        return h


import math
import numpy as np
import ml_dtypes
from contextlib import ExitStack
import concourse.bass as bass
import concourse.mybir as mybir
from concourse.bass_utils import run_bass_kernel_spmd

NPOS = 4112
NT = 33
DEPTH = 2
ALPHA = (2 * DEPTH) ** 0.25
NEG = -1e30


def tile_rows(t):
    return (0, 16) if t == 0 else (16 + 128 * (t - 1), 128)


GROUPS = [[0]] + [[4 * g + 1 + i for i in range(4)] for g in range(8)]


def t5_bucket(dist):
    n = np.maximum(dist, 0)
    lr = np.log(np.maximum(n, 1).astype(np.float32) / np.float32(16)) / np.float32(math.log(128 / 16))
    large = np.minimum(16 + (lr * np.float32(16)).astype(np.int32), 31)
    return np.where(n < 16, n, large)


class K:
    def __init__(self, ext_in=(), ext_out=(), layers=(0, 1)):
        self.nc = bass.Bass("TRN2", target_bir_lowering=False)
        self.st = ExitStack()
        self.S = Sched(self.nc, self.st)
        self.A = SbufAlloc(self.nc)
        self.ext_in = set(ext_in)
        self.ext_out = set(ext_out)
        self.dr = {}
        nc = self.nc
        self.psum = [self.st.enter_context(nc.psum_tensor(f"ps{i}", [128, 512], F32)) for i in range(8)]

    def D(self, name, shape, dtype, kind=None):
        if kind is None:
            kind = "ExternalInput" if name in self.ext_in else ("ExternalOutput" if name in self.ext_out else "Internal")
        self.dr[name] = self.nc.dram_tensor(name, list(shape), dtype, kind=kind).ap()
        return self.dr[name]

    def I(self, name, shape, dtype=F32):
        return self.D(name, shape, dtype, kind="ExternalInput")

    def ps(self, i):
        return self.psum[i]

    def op(self, eng, name, r=(), w=(), **kw):
        return self.S.op(eng, name, kw, r=r, w=w)

    def dma(self, q, out, in_, r=(), w=()):
        return self.S.op(q, "dma_start", dict(out=out, in_=in_), r=r, w=w, dma=True)

    def mm(self, out, lhsT, rhs, start, stop, r=(), w=(), skip=False):
        kw = dict(out=out, lhsT=lhsT, rhs=rhs, start=start, stop=stop)
        if skip:
            kw["skip_group_check"] = True
        return self.S.op("pe", "matmul", kw, r=r, w=w)

    def tr(self, out, in_, identity, r=(), w=()):
        return self.S.op("pe", "transpose", dict(out=out, in_=in_, identity=identity), r=r, w=w)

    def ps2(self, i, dtype=F32):
        raise NotImplementedError


class Vw:
    def __init__(self, ap, res):
        self.ap = ap
        self.res = res

    def __getitem__(self, idx):
        return Vw(self.ap[idx], self.res)

    def re(self, pat, **kw):
        return Vw(self.ap.rearrange(pat, **kw), self.res)

    def bc(self, shape):
        return Vw(self.ap.to_broadcast(list(shape)), self.res)

    def un(self, ax):
        return Vw(self.ap.unsqueeze(ax), self.res)


class TB:
    def __init__(self, h, res):
        self.h = h
        self.res = res if isinstance(res, list) else [res]

    def __getitem__(self, idx):
        return Vw(self.h[idx], self.res)


def _do(self, eng, name, out, extra_r=(), extra_w=(), **kw):
    r = list(extra_r)
    w = list(extra_w)
    args = {}
    for kk_, v in kw.items():
        if isinstance(v, Vw):
            r += v.res
            args[kk_] = v.ap
        else:
            args[kk_] = v
    okey = "ap" if name == "memset" else "out"
    if isinstance(out, Vw):
        w += out.res
        args[okey] = out.ap
    else:
        args[okey] = out
    if name == "dma_start":
        return self.S.op(eng, name, args, r=r, w=w, dma=True)
    return self.S.op(eng, name, args, r=r, w=w)


K.do = _do


def _mmv(self, out, lhsT, rhs, start, stop, skip=False):
    kw = dict(out=out.ap, lhsT=lhsT.ap, rhs=rhs.ap, start=start, stop=stop)
    if skip:
        kw["skip_group_check"] = True
    return self.S.op("pe", "matmul", kw, r=lhsT.res + rhs.res, w=out.res)


def _trv(self, out, in_, ident):
    return self.S.op("pe", "transpose", dict(out=out.ap, in_=in_.ap, identity=ident.ap), r=in_.res + ident.res, w=out.res)


K.mmv = _mmv
K.trv = _trv


def declare_io(k, final_out=True):
    L = DEPTH
    k.I("x", [4096, 1024]); k.I("meta", [16, 1024]); k.I("ln0_g", [1, 1024]); k.I("ln0_b", [1, 1024])
    k.I("w_in", [L, 1024, 2816]); k.I("swa_sinks", [L, 4])
    k.I("diff_l", [L, 4, 32]); k.I("diff_subln_g", [L, 64])
    k.I("rwkv_mu", [L, 1024]); k.I("rwkv_w0", [L, 256]); k.I("rwkv_w2", [L, 64, 256]); k.I("rwkv_a0", [L, 256])
    k.I("rwkv_a2", [L, 64, 256]); k.I("rwkv_g2", [L, 128, 256]); k.I("rwkv_kk", [L, 256]); k.I("rwkv_ka", [L, 256])
    k.I("rwkv_rk", [L, 256]); k.I("rwkv_lnx_g", [L, 256]); k.I("rwkv_lnx_b", [L, 256])
    k.I("conv_wT", [L, 256, 31]); k.I("conv_b", [L, 256, 1]); k.I("conv_gn_g", [L, 256, 1]); k.I("conv_gn_b", [L, 256, 1])
    k.I("w_out", [L, 1024, 1024]); k.I("ln1_g", [L, 1024]); k.I("ln1_b", [L, 1024])
    k.I("router_w", [1024, 16]); k.I("router_b", [1, 16])
    k.I("exp_w1", [L, 16, 1024, 512]); k.I("exp_w3", [L, 16, 1024, 512]); k.I("exp_w2", [L, 16, 512, 1024])
    k.I("ln2_g", [L, 1024]); k.I("ln2_b", [L, 1024])
    k.I("ident_bf", [128, 128], BF16); k.I("ident_f", [128, 128], F32)
    k.I("ba_meta", [3, 4, 16, 128], BF16); k.I("ba_pc", [4, 128, 256], BF16)
    k.I("bb_pc", [4, 128, 256], F32); k.I("b31", [1, 8], F32)
    k.I("gmat", [128, 128], F32)
    k.I("tri", [3, 64, 64], F32)
    k.I("ctri", [128, 128], F32); k.I("cblk", [128, 128], F32); k.I("cones", [128, 2, 64], F32); k.I("identrep", [64, 256], F32)
    k.I("rmask", [64, 5, 64], F32)
    k.D("H", [NPOS, 1024], F32); k.D("HM", [NPOS, 1024], F32)
    k.D("QKA", [384, NPOS], BF16); k.D("QKB", [512, NPOS], BF16); k.D("CV", [512, NPOS], F32)
    k.D("VA", [NPOS, 128], BF16); k.D("VB", [NPOS, 256], BF16); k.D("UC", [NPOS + 1, 1024], F32)
    k.D("MIX", [NPOS, 768], BF16); k.D("MIXD", [256, NPOS], BF16)
    k.D("HT", [1024, NPOS], BF16)
    k.D("RWT", [NPOS, 7, 256], BF16); k.D("BG", [NPOS, 2, 256], F32); k.D("DW", [65, 64, 256], BF16)
    if final_out:
        k.D("out", [4096, 1024], F32, kind="ExternalOutput")


def host_consts(inp):
    c = {}
    c["ident_bf"] = np.eye(128, dtype=ml_dtypes.bfloat16)
    c["ident_f"] = np.eye(128, dtype=np.float32)
    rel = np.asarray(inp["rel_bias"], np.float32)
    rel_a, rel_b = rel[:, :4], rel[:, 4:]
    ki = np.arange(128)[:, None]
    qi = np.arange(128)[None, :]
    bam = np.full((3, 4, 16, 128), NEG, np.float32)
    m = np.arange(16)[:, None]
    dq = np.arange(128)[None, :] - m
    vis = (dq >= 0) & (np.arange(128)[None, :] < 16)
    g = rel_a[t5_bucket(dq)]
    bam[0] = np.where(vis[None], np.moveaxis(g, -1, 0), NEG)
    dq = (16 + np.arange(128))[None, :] - m
    bam[1] = np.moveaxis(rel_a[t5_bucket(dq)], -1, 0)
    bam[2] = np.broadcast_to(rel_a[31][:, None, None], (4, 16, 128))
    c["ba_meta"] = bam.astype(ml_dtypes.bfloat16)
    dq_prev = qi - ki + 128
    dq_cur = qi - ki
    bp = np.where((ki > qi)[None], np.moveaxis(rel_a[t5_bucket(dq_prev)], -1, 0), NEG)
    bc = np.where((ki <= qi)[None], np.moveaxis(rel_a[t5_bucket(dq_cur)], -1, 0), NEG)
    c["ba_pc"] = np.concatenate([bp, bc], axis=2).astype(ml_dtypes.bfloat16)
    bcd = np.where((ki <= qi)[None], np.moveaxis(rel_b[t5_bucket(dq_cur)], -1, 0), NEG)
    bpd = np.moveaxis(rel_b[t5_bucket(dq_prev)], -1, 0)
    c["bb_pc"] = np.ascontiguousarray(np.concatenate([bcd, bpd], axis=2).astype(np.float32))
    c["b31"] = np.ascontiguousarray(rel[31][None, :])
    gm = np.zeros((128, 128), np.float32)
    gm[:64, :64] = 1.0 / 64
    gm[64:, 64:] = 1.0 / 64
    c["gmat"] = gm
    s = np.arange(64)[:, None]
    t = np.arange(64)[None, :]
    c["tri"] = np.stack([(s <= t), (s < t), (s >= t)]).astype(np.float32)
    s2 = np.arange(128)[:, None]; t2 = np.arange(128)[None, :]
    same = (s2 // 64) == (t2 // 64)
    c["ctri"] = (same & (s2 <= t2)).astype(np.float32)
    c["cblk"] = same.astype(np.float32)
    c["cones"] = np.ascontiguousarray(np.stack([(np.arange(128) // 64 == cc)[:, None] * np.ones((1, 64)) for cc in range(2)], axis=1).astype(np.float32))
    c["identrep"] = np.tile(np.eye(64, dtype=np.float32), (1, 4))
    rm = np.stack([(s < t), (s <= t), (s < t), -1.0 * (s <= t), (s > t)], axis=1).astype(np.float32)
    c["rmask"] = np.ascontiguousarray(rm)
    return c


def host_inputs(inp, b):
    f = lambda a: np.ascontiguousarray(np.asarray(a, np.float32))
    L = DEPTH
    m = {
        "x": f(inp["x"][b]), "meta": f(inp["meta"]), "ln0_g": f(inp["ln0_g"])[None], "ln0_b": f(inp["ln0_b"])[None],
        "w_in": f(inp["w_in"]), "swa_sinks": f(inp["swa_sinks"]),
        "diff_l": f(np.stack([inp["diff_lq1"], inp["diff_lk1"], inp["diff_lq2"], inp["diff_lk2"]], axis=1)),
        "diff_subln_g": f(inp["diff_subln_g"]),
        "rwkv_mu": f(inp["rwkv_mu"]), "rwkv_w0": f(inp["rwkv_w0"]), "rwkv_w2": f(inp["rwkv_w2"]), "rwkv_a0": f(inp["rwkv_a0"]),
        "rwkv_a2": f(inp["rwkv_a2"]), "rwkv_g2": f(inp["rwkv_g2"]), "rwkv_kk": f(inp["rwkv_kk"]), "rwkv_ka": f(inp["rwkv_ka"]),
        "rwkv_rk": f(np.asarray(inp["rwkv_rk"]).reshape(L, 256)), "rwkv_lnx_g": f(inp["rwkv_lnx_g"]), "rwkv_lnx_b": f(inp["rwkv_lnx_b"]),
        "conv_wT": f(np.transpose(np.asarray(inp["conv_w"]), (0, 2, 1))), "conv_b": f(inp["conv_b"])[..., None],
        "conv_gn_g": f(inp["conv_gn_g"])[..., None], "conv_gn_b": f(inp["conv_gn_b"])[..., None],
        "w_out": f(inp["w_out"]), "ln1_g": f(inp["ln1_g"]), "ln1_b": f(inp["ln1_b"]),
        "router_w": f(inp["router_w"]), "router_b": f(inp["router_b"])[None],
        "exp_w1": f(inp["exp_w1"]), "exp_w3": f(inp["exp_w3"]), "exp_w2": f(inp["exp_w2"]),
        "ln2_g": f(inp["ln2_g"]), "ln2_b": f(inp["ln2_b"]),
    }
    return m


def layernorm_tile(k, z, y, n, key, gb, bb, eps=1e-5, tmp=None, gbres=("gb", "bb")):
    stt, mv, rs = tmp
    for c in range(2):
        k.op("dve", "bn_stats", out=stt[0:n, c, :], in_=z[0:n, c * 512:(c + 1) * 512], r=[("z", key)], w=[("st", key, c)])
    k.op("dve", "bn_aggr", out=mv[0:n, :], in_=stt[0:n].rearrange("p a b -> p (a b)"), r=[("st", key, 0), ("st", key, 1)], w=[("mv", key)])
    k.op("act", "activation", out=rs[0:n, 0:1], in_=mv[0:n, 1:2], func=AF.Sqrt, bias=eps, scale=1.0, r=[("mv", key)], w=[("rs", key, 0)])
    k.op("dve", "scalar_tensor_tensor", out=y[0:n, :], in0=z[0:n, :], scalar=mv[0:n, 0:1], in1=gb[0:n, :], op0=ALU.subtract, op1=ALU.mult, r=[("z", key), ("mv", key), gbres[0]], w=[("y", key)])
    k.op("dve", "reciprocal", out=rs[0:n, 0:1], in_=rs[0:n, 0:1], r=[("rs", key, 0)], w=[("rs", key, 0)])
    k.op("dve", "scalar_tensor_tensor", out=y[0:n, :], in0=y[0:n, :], scalar=rs[0:n, 0:1], in1=bb[0:n, :], op0=ALU.mult, op1=ALU.add, r=[("y", key), ("rs", key, 0), gbres[1]], w=[("y", key)])


def layernorm_tile_g(k, z, y, n, key, gb, bb, eps=1e-5, tmp=None, gbres=("gb", "bb")):
    stt, mv, rs = tmp
    for c in range(2):
        k.op("dve", "bn_stats", out=stt[0:n, c, :], in_=z[0:n, c * 512:(c + 1) * 512], r=[("z", key)], w=[("st", key, c)])
    k.op("dve", "bn_aggr", out=mv[0:n, :], in_=stt[0:n].rearrange("p a b -> p (a b)"), r=[("st", key, 0), ("st", key, 1)], w=[("mv", key)])
    k.op("act", "activation", out=rs[0:n, 0:1], in_=mv[0:n, 1:2], func=AF.Sqrt, bias=eps, scale=1.0, r=[("mv", key)], w=[("rs", key, 0)])
    k.op("dve", "scalar_tensor_tensor", out=y[0:n, :], in0=z[0:n, :], scalar=mv[0:n, 0:1], in1=gb[0:n, :], op0=ALU.subtract, op1=ALU.mult, r=[("z", key), ("mv", key), gbres[0]], w=[("y", key)])
    yield
    k.op("dve", "reciprocal", out=rs[0:n, 0:1], in_=rs[0:n, 0:1], r=[("rs", key, 0)], w=[("rs", key, 0)])
    k.op("dve", "scalar_tensor_tensor", out=y[0:n, :], in0=y[0:n, :], scalar=rs[0:n, 0:1], in1=bb[0:n, :], op0=ALU.mult, op1=ALU.add, r=[("y", key), ("rs", key, 0), gbres[1]], w=[("y", key)])


def ln_tmp(k, nm):
    A = k.A
    return (A.t([128, 2, 6], F32, "st" + nm), A.t([128, 2], F32, "mv" + nm), A.t([128, 2], F32, "rs" + nm))


def load_bcast(k, dst, src_row, res, q="sp"):
    k.dma(q, dst[:], src_row.partition_broadcast(128), w=[res])


def stage_ln0(k):
    S, A, d = k.S, k.A, k.dr
    A.mark()
    gb = A.t([128, 1024], F32, "gb"); bb = A.t([128, 1024], F32, "bb")
    load_bcast(k, gb, d["ln0_g"], "gb"); load_bcast(k, bb, d["ln0_b"], "bb")
    zs = [A.t([128, 1024], F32, f"z{i}") for i in range(3)]
    ys = [A.t([128, 1024], F32, f"y{i}") for i in range(3)]
    tmps = [ln_tmp(k, str(i)) for i in range(3)]
    for t in range(NT):
        p0, n = tile_rows(t)
        b = t % 3
        z, y = zs[b], ys[b]
        src = d["meta"] if t == 0 else d["x"][128 * (t - 1):128 * t, :]
        k.dma("sp", z[0:n, :], src, w=[("z", b)])
        layernorm_tile(k, z, y, n, b, gb, bb, tmp=tmps[b])
        k.dma("act", d["H"][p0:p0 + n, :], y[0:n, :], r=[("y", b)], w=[("H", t)])
    S.barrier()
    A.release()


FM_TILES = [
    ("QKA", 0, 0, 0.125), ("QKA", 128, 128, 0.125), ("QKA", 256, 256, 1.0),
    ("QKB", 0, 512, 32 ** -0.5), ("QKB", 128, 640, 32 ** -0.5), ("QKB", 256, 768, 1.0), ("QKB", 384, 896, 1.0),
    ("CV", 0, 2304, 1.0), ("CV", 128, 2432, 1.0), ("CV", 256, 2560, 1.0), ("CV", 384, 2688, 1.0),
]


def stage_in(k, l):
    S, A, d = k.S, k.A, k.dr
    A.mark()
    idt = A.t([128, 128], BF16, "ident")
    k.dma("sp", idt[:], d["ident_bf"], w=["ident"])
    wsb = A.t([128, 8, 2816], BF16, "w_in")
    for kk in range(8):
        for c in range(2):
            k.dma("pool", wsb[:, kk, c * 1408:(c + 1) * 1408], d["w_in"][l, kk * 128:(kk + 1) * 128, c * 1408:(c + 1) * 1408], w=[("w_in", kk, c)])
    wres = [("w_in", kk, c) for kk in range(8) for c in range(2)]
    zs = [A.t([128, 1024], F32, f"z{i}") for i in range(2)]
    hb = [A.t([128, 1024], BF16, f"hb{i}") for i in range(2)]
    hTg = [A.t([128, 8, 512], BF16, f"hT{i}") for i in range(2)]
    ofm_b = [A.t([128, 512], BF16, f"ofb{i}") for i in range(3)]
    ofm_f = [A.t([128, 512], F32, f"off{i}") for i in range(2)]
    otm_v = [A.t([128, 384], BF16, f"otv{i}") for i in range(2)]
    otm_u = [A.t([128, 1024], F32, f"otu{i}") for i in range(2)]
    tcount = 0
    fmc = 0
    tmc = 0
    for gi, grp in enumerate(GROUPS):
        gb_ = gi % 2
        hT = hTg[gb_]
        ntok = sum(tile_rows(t)[1] for t in grp)
        gp0 = tile_rows(grp[0])[0]
        for ti, t in enumerate(grp):
            p0, n = tile_rows(t)
            b = tcount % 2
            tcount += 1
            z = zs[b]
            k.dma("sp", z[0:n, :], d["H"][p0:p0 + n, :], r=[("H", t)], w=[("z", b)])
            k.op("act", "copy", out=hb[b][0:n, :], in_=z[0:n, :], r=[("z", b)], w=[("hb", b)])
            pt = k.ps(b)[:].bitcast(BF16)
            for kk in range(8):
                k.tr(pt[:, kk * 128:kk * 128 + n], hb[b][0:n, kk * 128:(kk + 1) * 128], idt[0:n, 0:n], r=[("hb", b), "ident"], w=[("ps", b)])
            k.op("dve", "tensor_copy", out=hT[:, :, ti * 128:ti * 128 + n], in_=pt.rearrange("p (k t) -> p k t", k=8)[:, :, 0:n], r=[("ps", b)], w=[("hT", gb_, ti)])
        hres = [("hT", gb_, ti) for ti in range(len(grp))]
        for (dn, r0, c0, sc) in FM_TILES:
            pb = 2 + fmc % 3
            isf = dn == "CV"
            ob = ofm_f[fmc % 2] if isf else ofm_b[fmc % 3]
            ores = ("off", fmc % 2) if isf else ("ofb", fmc % 3)
            for kk in range(8):
                k.mm(k.ps(pb)[:, 0:ntok], wsb[:, kk, c0:c0 + 128], hT[:, kk, 0:ntok], kk == 0, kk == 7, r=hres + wres, w=[("ps", pb)])
            if fmc % 2 == 0:
                k.op("act", "activation", out=ob[:, 0:ntok], in_=k.ps(pb)[:, 0:ntok], func=AF.Copy, scale=sc, r=[("ps", pb)], w=[ores])
            else:
                k.op("dve", "tensor_scalar", out=ob[:, 0:ntok], in0=k.ps(pb)[:, 0:ntok], scalar1=sc, scalar2=None, op0=ALU.mult, r=[("ps", pb)], w=[ores])
            k.dma("sp", d[dn][r0:r0 + 128, gp0:gp0 + ntok], ob[:, 0:ntok], r=[ores], w=[(dn, r0, gi)])
            fmc += 1
        for ti, t in enumerate(grp):
            p0, n = tile_rows(t)
            ov = otm_v[tmc % 2]
            ou = otm_u[tmc % 2]
            tb = tmc % 2
            tmc += 1
            lt = hT[:, :, ti * 128:ti * 128 + n]
            for kk in range(8):
                k.mm(k.ps(5)[0:n, 0:128], lt[:, kk, :], wsb[:, kk, 384:512], kk == 0, kk == 7, r=hres + wres, w=[("ps", 5, 0)])
            for kk in range(8):
                k.mm(k.ps(5)[0:n, 128:384], lt[:, kk, :], wsb[:, kk, 1024:1280], kk == 0, kk == 7, r=hres + wres, w=[("ps", 5, 1)])
            k.op("act", "copy", out=ov[0:n, :], in_=k.ps(5)[0:n, 0:384], r=[("ps", 5, 0), ("ps", 5, 1)], w=[("otv", tb)])
            k.dma("act", d["VA"][p0:p0 + n, :], ov[0:n, 0:128], r=[("otv", tb)], w=[("VA", t)])
            k.dma("act", d["VB"][p0:p0 + n, :], ov[0:n, 128:384], r=[("otv", tb)], w=[("VB", t)])
            for c in range(2):
                pb = 6 + c
                for kk in range(8):
                    k.mm(k.ps(pb)[0:n, :], lt[:, kk, :], wsb[:, kk, 1280 + c * 512:1280 + (c + 1) * 512], kk == 0, kk == 7, r=hres + wres, w=[("ps", pb)])
                k.op("dve", "tensor_copy", out=ou[0:n, c * 512:(c + 1) * 512], in_=k.ps(pb)[0:n, :], r=[("ps", pb)], w=[("otu", tb, c)])
            k.dma("sp", d["UC"][1 + p0:1 + p0 + n, :], ou[0:n, :], r=[("otu", tb, 0), ("otu", tb, 1)], w=[("UC", t)])
    S.barrier()
    A.release()


def stage_swa(k, l):
    S, A, d = k.S, k.A, k.dr
    A.mark()
    idt = A.t([128, 128], BF16, "ident")
    k.dma("sp", idt[:], d["ident_bf"], w=["ident"])
    qT = A.t([64, 4, NPOS], BF16, "qTa")
    kT = A.t([64, 2, NPOS], BF16, "kTa")
    for h in range(4):
        k.dma("sp", qT[:, h, :], d["QKA"][64 * h:64 * h + 64, :], w=[("qT", h)])
    for kv in range(2):
        k.dma("sp", kT[:, kv, :], d["QKA"][256 + 64 * kv:256 + 64 * kv + 64, :], w=[("kT", kv)])
    va = A.t([128, NT, 2, 65], BF16, "va")
    k.op("pool", "memset", ap=va[:, :, :, 64:65], constant=1.0, w=["va_ones"])
    for t in range(NT):
        p0, n = tile_rows(t)
        k.dma("act", va[0:n, t, :, 0:64], d["VA"][p0:p0 + n, :].rearrange("p (h e) -> p h e", h=2), w=[("va", t)])
    bam = A.t([16, 3, 4, 128], BF16, "bam")
    k.dma("sp", bam[:], d["ba_meta"].rearrange("c h m q -> m c h q"), w=["bam"])
    bapc = A.t([128, 4, 256], BF16, "bapc")
    k.dma("sp", bapc[:], d["ba_pc"].rearrange("h k q -> k h q"), w=["bapc"])
    sk = A.t([128, 4, 1], F32, "sk")
    esk = A.t([128, 4, 1], F32, "esk")
    k.dma("sp", sk[:].rearrange("p h o -> p (h o)"), d["swa_sinks"][l:l + 1, :].partition_broadcast(128), w=["sk"])
    k.op("act", "activation", out=esk[:], in_=sk[:], func=AF.Exp, r=["sk"], w=["esk"])
    pms = [A.t([16, 4, 128], BF16, f"pm{i}") for i in range(2)]
    pps = [A.t([128, 2, 512], BF16, f"pp{i}") for i in range(2)]
    dens = [A.t([128, 4, 1], F32, f"den{i}") for i in range(2)]
    yos = [A.t([128, 4, 64], BF16, f"yo{i}") for i in range(2)]
    for t in range(NT):
        p0, n = tile_rows(t)
        s = t % 2
        psA, psB, psD = k.ps(4 * s), [k.ps(4 * s + 1), k.ps(4 * s + 2)], k.ps(4 * s + 3)
        pm, pp, den, yo = pms[s], pps[s], dens[s], yos[s]
        case = min(t, 2)
        psAv = psA[0:16, :].rearrange("p (h q) -> p h q", h=4)
        for h in range(4):
            kv = h // 2
            hp, cb = h // 2, (h % 2) * 256
            k.mm(psAv[:, h, 0:n], kT[:, kv, 0:16], qT[:, h, p0:p0 + n], True, False, r=[("kT", kv), ("qT", h)], w=[("psA", s)])
            k.mm(psAv[:, h, 0:n], idt[0:16, 0:16], bam[:, case, h, 0:n], False, True, r=["ident", "bam"], w=[("psA", s)])
            if t >= 2:
                pp0 = p0 - 128
                k.mm(psB[hp][:, cb:cb + n], kT[:, kv, pp0:pp0 + 128], qT[:, h, p0:p0 + n], True, False, r=[("kT", kv), ("qT", h)], w=[("psB", s, hp)])
                k.mm(psB[hp][:, cb:cb + n], idt[:, :], bapc[:, h, 0:n], False, True, r=["ident", "bapc"], w=[("psB", s, hp)])
            if t >= 1:
                k.mm(psB[hp][:, cb + 128:cb + 128 + n], kT[:, kv, p0:p0 + 128], qT[:, h, p0:p0 + n], True, False, r=[("kT", kv), ("qT", h)], w=[("psB", s, hp)])
                k.mm(psB[hp][:, cb + 128:cb + 128 + n], idt[:, :], bapc[:, h, 128:128 + n], False, True, r=["ident", "bapc"], w=[("psB", s, hp)])
        k.op("act", "activation", out=pm[:, :, 0:n], in_=psAv[:, :, 0:n], func=AF.Exp, r=[("psA", s)], w=[("pm", s)])
        for hp in range(2):
            if t >= 2:
                k.op("act", "activation", out=pp[:, hp, :], in_=psB[hp][:, :], func=AF.Exp, r=[("psB", s, hp)], w=[("pp", s, hp)])
            elif t == 1:
                k.op("act", "activation", out=pp[:, hp, :].rearrange("p (h x) -> p h x", h=2)[:, :, 128:256],
                     in_=psB[hp][:, :].rearrange("p (h x) -> p h x", h=2)[:, :, 128:256], func=AF.Exp, r=[("psB", s, hp)], w=[("pp", s, hp)])
        psDv = psD[:, 0:260].rearrange("p (h e) -> p h e", h=4)
        for h in range(4):
            kv = h // 2
            hp, cb = h // 2, (h % 2) * 256
            k.mm(psDv[0:n, h, :], pm[0:16, h, 0:n], va[0:16, 0, kv, :], True, t == 0, r=[("pm", s), ("va", 0), "va_ones"], w=[("psD", s)])
            if t >= 2:
                k.mm(psDv[0:n, h, :], pp[:, hp, cb:cb + n], va[:, t - 1, kv, :], False, False, r=[("pp", s, hp), ("va", t - 1), "va_ones"], w=[("psD", s)])
            if t >= 1:
                k.mm(psDv[0:n, h, :], pp[:, hp, cb + 128:cb + 128 + n], va[:, t, kv, :], False, True, r=[("pp", s, hp), ("va", t), "va_ones"], w=[("psD", s)])
        k.op("dve", "tensor_tensor", out=den[0:n], in0=psDv[0:n, :, 64:65], in1=esk[0:n], op=ALU.add, r=[("psD", s), "esk"], w=[("den", s)])
        k.op("dve", "reciprocal", out=den[0:n], in_=den[0:n], r=[("den", s)], w=[("den", s)])
        k.op("dve", "tensor_tensor", out=yo[0:n], in0=psDv[0:n, :, 0:64], in1=den[0:n].to_broadcast([n, 4, 64]), op=ALU.mult, r=[("psD", s), ("den", s)], w=[("yo", s)])
        k.dma("sp", d["MIX"][p0:p0 + n, 0:256], yo[0:n].rearrange("p h e -> p (h e)"), r=[("yo", s)], w=[("MIXa", t)])
    S.barrier()
    A.release()


def stage_diff(k, l):
    S, A, d = k.S, k.A, k.dr
    A.mark()
    lam_init = 0.8 - 0.6 * math.exp(-0.3 * l)
    idt = A.t([128, 128], BF16, "ident")
    k.dma("sp", idt[:], d["ident_bf"], w=["ident"])
    qT = A.t([64, 4, NPOS], BF16, "qTb")
    kT = A.t([64, 4, NPOS], BF16, "kTb")
    for hh in range(4):
        k.dma("sp", qT[:, hh, :], d["QKB"][64 * hh:64 * hh + 64, :], w=[("qT", hh)])
        k.dma("sp", kT[:, hh, :], d["QKB"][256 + 64 * hh:256 + 64 * hh + 64, :], w=[("kT", hh)])
    vb = A.t([128, NT, 4, 65], BF16, "vb")
    k.op("pool", "memset", ap=vb[:, :, :, 64:65], constant=1.0, w=["vb_ones"])
    for t in range(NT):
        p0, n = tile_rows(t)
        k.dma("act", vb[0:n, t, :, 0:64], d["VB"][p0:p0 + n, :].rearrange("p (h e) -> p h e", h=4), w=[("vb", t)])
    bbf = A.t([128, 4, 256], F32, "bbf")
    k.dma("sp", bbf[:], d["bb_pc"].rearrange("h k q -> k h q"), w=["bbf"])
    b31b = A.t([128, 8], F32, "b31b")
    k.dma("sp", b31b[:], d["b31"].partition_broadcast(128), w=["b31b"])
    bbt = A.t([128, 4, 256], BF16, "bbt")
    for h in range(4):
        k.op("dve", "tensor_scalar", out=bbt[:, h, :], in0=bbf[:, h, :], scalar1=b31b[:, 4 + h:5 + h], scalar2=None, op0=ALU.subtract, r=["bbf", "b31b"], w=["bbt"])
    dl = A.t([128, 4, 32], F32, "dl")
    k.dma("sp", dl[:].rearrange("p a b -> p (a b)"), d["diff_l"][l:l + 1].rearrange("o a b -> o (a b)").partition_broadcast(128), w=["dl"])
    pr = A.t([128, 2, 32], F32, "pr")
    ss = A.t([128, 2], F32, "ss")
    nlam = A.t([128, 1], F32, "nlam")
    k.op("dve", "tensor_tensor", out=pr[:, 0, :], in0=dl[:, 0, :], in1=dl[:, 1, :], op=ALU.mult, r=["dl"], w=["pr0"])
    k.op("dve", "tensor_tensor", out=pr[:, 1, :], in0=dl[:, 2, :], in1=dl[:, 3, :], op=ALU.mult, r=["dl"], w=["pr1"])
    k.op("dve", "tensor_reduce", out=ss[:], in_=pr[:], axis=AX.X, op=ALU.add, r=["pr0", "pr1"], w=["ss"])
    k.op("act", "activation", out=ss[:], in_=ss[:], func=AF.Exp, r=["ss"], w=["ss"])
    k.op("dve", "tensor_tensor", out=nlam[:], in0=ss[:, 1:2], in1=ss[:, 0:1], op=ALU.subtract, r=["ss"], w=["nlam"])
    k.op("dve", "tensor_scalar", out=nlam[:], in0=nlam[:], scalar1=-lam_init, scalar2=None, op0=ALU.add, r=["nlam"], w=["nlam"])
    gvec = A.t([128, 1, 64], F32, "gvec")
    k.dma("sp", gvec[:].rearrange("p o e -> p (o e)"), d["diff_subln_g"][l:l + 1, :].partition_broadcast(128), w=["gvec"])
    k.op("act", "mul", out=gvec[:], in_=gvec[:], mul=(1.0 - lam_init), r=["gvec"], w=["gvec"])
    PTs = [A.t([128, 512], BF16, f"PT{i}") for i in range(4)]
    rr = [A.t([128, 2, 4, 1], F32, f"rr{i}") for i in range(2)]
    t1 = [A.t([128, 4, 64], F32, f"t1{i}") for i in range(2)]
    t2 = [A.t([128, 4, 64], F32, f"t2{i}") for i in range(2)]
    ms = [A.t([128, 4, 1], F32, f"ms{i}") for i in range(2)]
    ybo = [A.t([128, 4, 4, 64], BF16, f"ybo{i}") for i in range(2)]
    sc = 0
    pending = []

    def flush():
        while pending:
            pending.pop(0)()

    for qg, tiles in enumerate(GROUPS):
        ntok = sum(tile_rows(t)[1] for t in tiles)
        gp0 = tile_rows(tiles[0])[0]
        nt = len(tiles)
        nq = tile_rows(tiles[0])[1]
        yb_ = ybo[qg % 2]
        for h in range(4):
            hb = h % 2
            Ob = [k.ps(4 + 2 * hb), k.ps(5 + 2 * hb)]
            Ov = [Ob[c][:, 0:65 * nt].rearrange("p (t e) -> p t e", e=65) for c in range(2)]
            for j in range(0, tiles[-1] + 1):
                kp0, nk = tile_rows(j)
                fi = max(0, j - tiles[0])
                col0 = fi * 128
                par = sc % 2
                sc += 1
                bl = [i for i in (j, j + 1) if i in tiles]
                c1 = col0 + 128 * len(bl) if tiles[0] != 0 else (nq if bl else 0)
                c1 = min(c1, ntok)
                if not bl:
                    c1 = col0
                kq_r = [("kT", h), ("qT", h)]
                kTc = lambda c: kT[32 * c:32 * c + 32, h, kp0:kp0 + nk]
                qTc = lambda c, a0, a1: qT[32 * c:32 * c + 32, h, gp0 + a0:gp0 + a1]
                sbs = [2 * par + c for c in range(2)]
                if bl:
                    for c in range(2):
                        k.mm(k.ps(sbs[c])[0:nk, col0:c1], kTc(c), qTc(c, col0, c1), True, False, r=kq_r, w=[("psS", sbs[c])])
                    for c in range(2):
                        psb = k.ps(sbs[c])
                        if j == 0:
                            if tiles[0] == 0:
                                k.mm(psb[0:16, 0:16], idt[0:16, 0:16], bbt[0:16, h, 0:16], False, True, r=["ident", "bbt"], w=[("psS", sbs[c])])
                            else:
                                k.mm(psb[0:16, 0:128], idt[:, 112:128], bbt[:, h, 128:256], False, True, r=["ident", "bbt"], w=[("psS", sbs[c])])
                        else:
                            b0 = 0 if bl[0] == j else 128
                            k.mm(psb[:, col0:c1], idt[:, :], bbt[:, h, b0:b0 + (c1 - col0)], False, True, r=["ident", "bbt"], w=[("psS", sbs[c])])
                if c1 < ntok:
                    for c in range(2):
                        k.mm(k.ps(sbs[c])[0:nk, c1:ntok], kTc(c), qTc(c, c1, ntok), True, True, r=kq_r, w=[("psS", sbs[c])])
                pts = []
                for c in range(2):
                    ptk = 2 * par + c
                    pt = PTs[ptk]
                    pts.append((pt, ptk))
                    k.op("act", "activation", out=pt[0:nk, col0:ntok], in_=k.ps(sbs[c])[0:nk, col0:ntok], func=AF.Exp, bias=b31b[0:nk, 4 + h:5 + h], scale=1.0,
                         r=[("psS", sbs[c]), "b31b"], w=[("PT", ptk)])

                def pv(tiles=tiles, j=j, h=h, hb=hb, nk=nk, pts=pts, Ov=Ov):
                    for c in range(2):
                        pt, ptk = pts[c]
                        for il, i in enumerate(tiles):
                            if i < j:
                                continue
                            ni = tile_rows(i)[1]
                            k.mm(Ov[c][0:ni, il, :], pt[0:nk, il * 128:il * 128 + ni], vb[0:nk, j, h, :], (j == 0 and il == 0), False, r=[("PT", ptk), ("vb", j), "vb_ones"], w=[("psO", hb, c)], skip=True)
                flush()
                pending.append(pv)

            def epilogue(qg=qg, h=h, hb=hb, nt=nt, nq=nq, Ov=Ov, yb_=yb_):
                n = nq
                e = hb
                for c in range(2):
                    k.op("dve", "reciprocal", out=rr[e][0:n, c, 0:nt, :], in_=Ov[c][0:n, :, 64:65], r=[("psO", hb, c)], w=[("rr", e, c)])
                k.op("dve", "tensor_tensor", out=t1[e][0:n, 0:nt, :], in0=Ov[0][0:n, :, 0:64], in1=rr[e][0:n, 0, 0:nt, :].to_broadcast([n, nt, 64]), op=ALU.mult, r=[("psO", hb, 0), ("rr", e, 0)], w=[("t1", e)])
                k.op("dve", "tensor_tensor", out=t2[e][0:n, 0:nt, :], in0=Ov[1][0:n, :, 0:64], in1=rr[e][0:n, 1, 0:nt, :].to_broadcast([n, nt, 64]), op=ALU.mult, r=[("psO", hb, 1), ("rr", e, 1)], w=[("t2", e)])
                k.op("dve", "scalar_tensor_tensor", out=t1[e][0:n, 0:nt, :], in0=t2[e][0:n, 0:nt, :], scalar=nlam[0:n, 0:1], in1=t1[e][0:n, 0:nt, :], op0=ALU.mult, op1=ALU.add, r=[("t1", e), ("t2", e), "nlam"], w=[("t1", e)])
                k.op("act", "activation", out=t2[e][0:n, 0:nt, :], in_=t1[e][0:n, 0:nt, :], func=AF.Square, r=[("t1", e)], w=[("t2", e)])
                k.op("dve", "tensor_reduce", out=ms[e][0:n, 0:nt, :], in_=t2[e][0:n, 0:nt, :], axis=AX.X, op=ALU.add, r=[("t2", e)], w=[("ms", e)])
                k.op("act", "activation", out=ms[e][0:n, 0:nt, :], in_=ms[e][0:n, 0:nt, :], func=AF.Sqrt, bias=1e-5, scale=1.0 / 64, r=[("ms", e)], w=[("ms", e)])
                k.op("dve", "reciprocal", out=ms[e][0:n, 0:nt, :], in_=ms[e][0:n, 0:nt, :], r=[("ms", e)], w=[("ms", e)])
                k.op("dve", "tensor_tensor", out=t1[e][0:n, 0:nt, :], in0=t1[e][0:n, 0:nt, :], in1=ms[e][0:n, 0:nt, :].to_broadcast([n, nt, 64]), op=ALU.mult, r=[("t1", e), ("ms", e)], w=[("t1", e)])
                k.op("dve", "tensor_tensor", out=yb_[0:n, 0:nt, h, :], in0=t1[e][0:n, 0:nt, :], in1=gvec[0:n].to_broadcast([n, nt, 64]), op=ALU.mult, r=[("t1", e), "gvec"], w=[("ybo", qg % 2, h)])
            pending.append(epilogue)

        def store(qg=qg, gp0=gp0, ntok=ntok, nt=nt, nq=nq, yb_=yb_):
            dst = d["MIX"][gp0:gp0 + ntok, 256:512]
            if nt > 1:
                dst = dst.rearrange("(t p) c -> p t c", p=128)
                src = yb_[:, 0:nt].rearrange("p t h e -> p t (h e)")
            else:
                src = yb_[0:nq, 0].rearrange("p h e -> p (h e)")
            k.dma("sp", dst, src, r=[("ybo", qg % 2, h) for h in range(4)], w=[("MIXb", qg)])
        pending.append(store)
    flush()
    S.barrier()
    A.release()


def stage_conv(k, l):
    S, A, d = k.S, k.A, k.dr
    A.mark()
    gmat = A.t([128, 128], F32, "gmat")
    k.dma("sp", gmat[:], d["gmat"], w=["gmat"])
    Ab = [A.t([128, NPOS], F32, f"cva{i}") for i in range(2)]
    Gb = [A.t([128, NPOS], F32, f"cvg{i}") for i in range(2)]
    hgb = [A.t([128, 30 + NPOS], F32, f"hg{i}") for i in range(2)]
    cwb = [A.t([128, 31], F32, f"cw{i}") for i in range(2)]
    cpb = [A.t([128, 3], F32, f"cp{i}") for i in range(2)]
    ctmp = [A.t([128, NPOS], F32, f"ctmp{i}") for i in range(2)]
    sqb = [A.t([128, 512], F32, f"sqb{i}") for i in range(2)]
    ddb = [A.t([128, 512], F32, f"ddb{i}") for i in range(2)]
    m2b = [A.t([128, 512], F32, f"m2b{i}") for i in range(2)]
    ob = [A.t([128, 512], BF16, f"cob{i}") for i in range(2)]
    H2 = NPOS // 2
    st = {"cc": 0}

    def taps(ct):
        r0 = ct * 128
        a, gate, hg, cw, cp = Ab[ct], Gb[ct], hgb[ct], cwb[ct], cpb[ct]
        acc1, acc2 = a, gate
        RA, RG = ("A", ct), [("G", ct, 0), ("G", ct, 1)]
        k.op("pool", "memset", ap=hg[:, 0:30], constant=0.0, w=[("hgz", ct)])
        k.dma("sp", a[:], d["CV"][r0:r0 + 128, :], w=[RA])
        k.dma("sp", gate[:], d["CV"][256 + r0:256 + r0 + 128, :], w=RG)
        k.dma("act", cw[:], d["conv_wT"][l, r0:r0 + 128, :], w=[("cw", ct)])
        k.dma("act", cp[:, 0:1], d["conv_b"][l, r0:r0 + 128, :], w=[("cp0", ct)])
        k.dma("act", cp[:, 1:2], d["conv_gn_g"][l, r0:r0 + 128, :], w=[("cp1", ct)])
        k.dma("act", cp[:, 2:3], d["conv_gn_b"][l, r0:r0 + 128, :], w=[("cp2", ct)])
        for hh in range(2):
            k.op("act", "activation", out=gate[:, hh * H2:(hh + 1) * H2], in_=gate[:, hh * H2:(hh + 1) * H2], func=AF.Sigmoid, r=[RG[hh]], w=[RG[hh]])
            k.op("dve", "tensor_tensor", out=hg[:, 30 + hh * H2:30 + (hh + 1) * H2], in0=a[:, hh * H2:(hh + 1) * H2], in1=gate[:, hh * H2:(hh + 1) * H2], op=ALU.mult,
                 r=[RA, RG[hh]], w=[("hg", ct, hh)])
        yield
        hres = [("hgz", ct), ("hg", ct, 0), ("hg", ct, 1)]
        cwr = ("cw", ct)
        k.op("dve", "tensor_scalar", out=acc1[:], in0=hg[:, 0:NPOS], scalar1=cw[:, 0:1], scalar2=cp[:, 0:1], op0=ALU.mult, op1=ALU.add, r=hres + [cwr, ("cp0", ct)], w=[RA])
        NDVE = 27
        k.op("act", "activation", out=acc2[:], in_=hg[:, NDVE:NDVE + NPOS], func=AF.Identity, scale=cw[:, NDVE:NDVE + 1], r=hres + [cwr], w=RG)
        extra = list(range(NDVE + 1, 31))
        for j in range(1, NDVE):
            k.op("dve", "scalar_tensor_tensor", out=acc1[:], in0=hg[:, j:j + NPOS], scalar=cw[:, j:j + 1], in1=acc1[:], op0=ALU.mult, op1=ALU.add, r=hres + [cwr, RA], w=[RA])
            if j % 8 == 1 and extra:
                jj = extra.pop(0)
                tb = jj % 2
                k.op("act", "activation", out=ctmp[tb][:], in_=hg[:, jj:jj + NPOS], func=AF.Identity, scale=cw[:, jj:jj + 1], r=hres + [cwr], w=[("ctmp", tb)])
                k.op("pool", "tensor_tensor", out=acc2[:], in0=acc2[:], in1=ctmp[tb][:], op=ALU.add, r=[("ctmp", tb)] + RG, w=RG)
            if j % 3 == 0:
                yield
        assert not extra
        k.op("dve", "tensor_tensor", out=acc1[:], in0=acc1[:], in1=acc2[:], op=ALU.add, r=[RA] + RG, w=[RA])

    def gn(ct):
        r0 = ct * 128
        acc1, cp = Ab[ct], cpb[ct]
        RA = ("A", ct)
        for c0 in range(0, NPOS, 512):
            n = min(512, NPOS - c0)
            b = st["cc"] % 2
            st["cc"] += 1
            psM, psE = k.ps(2 * b), k.ps(2 * b + 1)
            k.mm(psM[:, 0:n], gmat[:], acc1[:, c0:c0 + n], True, True, r=["gmat", RA], w=[("psM", b)])
            k.op("dve", "tensor_tensor", out=ddb[b][:, 0:n], in0=acc1[:, c0:c0 + n], in1=psM[:, 0:n], op=ALU.subtract, r=[RA, ("psM", b)], w=[("ddb", b)])
            k.op("act", "activation", out=sqb[b][:, 0:n], in_=ddb[b][:, 0:n], func=AF.Square, r=[("ddb", b)], w=[("sqb", b)])
            k.mm(psE[:, 0:n], gmat[:], sqb[b][:, 0:n], True, True, r=["gmat", ("sqb", b)], w=[("psE", b)])
            k.op("act", "activation", out=m2b[b][:, 0:n], in_=psE[:, 0:n], func=AF.Sqrt, bias=1e-5, scale=1.0, r=[("psE", b)], w=[("m2b", b)])
            k.op("dve", "reciprocal", out=m2b[b][:, 0:n], in_=m2b[b][:, 0:n], r=[("m2b", b)], w=[("m2b", b)])
            k.op("dve", "tensor_tensor", out=ddb[b][:, 0:n], in0=ddb[b][:, 0:n], in1=m2b[b][:, 0:n], op=ALU.mult, r=[("ddb", b), ("m2b", b)], w=[("ddb", b)])
            k.op("act", "activation", out=ob[b][:, 0:n], in_=ddb[b][:, 0:n], func=AF.Silu, bias=cp[:, 2:3], scale=cp[:, 1:2], r=[("ddb", b), ("cp1", ct), ("cp2", ct)], w=[("cob", b)])
            k.dma("sp", d["MIXD"][r0:r0 + 128, c0:c0 + n], ob[b][:, 0:n], r=[("cob", b)], w=[("MIXD", ct, c0)])
            yield

    for _ in taps(0):
        pass
    g0, t1 = gn(0), taps(1)
    alive = [g0, t1]
    while alive:
        for g in list(alive):
            try:
                next(g)
            except StopIteration:
                alive.remove(g)
    for _ in gn(1):
        pass
    S.barrier()
    A.release()


def stage_out(k, l):
    S, A, d = k.S, k.A, k.dr
    A.mark()
    idt = A.t([128, 128], BF16, "ident")
    k.dma("sp", idt[:], d["ident_bf"], w=["ident"])
    wo = A.t([128, 8, 1024], BF16, "wo")
    for kk in range(8):
        k.dma("pool", wo[:, kk, :], d["w_out"][l, kk * 128:(kk + 1) * 128, :], w=[("wo", kk)])
    wres = [("wo", kk) for kk in range(8)]
    gb = A.t([128, 1024], F32, "gb"); bb = A.t([128, 1024], F32, "bb")
    load_bcast(k, gb, d["ln1_g"][l:l + 1, :], "gb"); load_bcast(k, bb, d["ln1_b"][l:l + 1, :], "bb")
    nb = 4
    mxs = [A.t([128, 768], BF16, f"mx{i}") for i in range(nb)]
    mxds = [A.t([128, 2, 128], BF16, f"mxd{i}") for i in range(nb)]
    mTs = [A.t([128, 6, 128], BF16, f"mT{i}") for i in range(nb)]
    hs = [A.t([128, 1024], F32, f"h{i}") for i in range(nb)]
    zs = [A.t([128, 1024], F32, f"z{i}") for i in range(nb)]
    ys = [A.t([128, 1024], F32, f"y{i}") for i in range(nb)]
    tmps = [ln_tmp(k, str(i)) for i in range(nb)]

    def body(t):
        p0, n = tile_rows(t)
        b = t % nb
        mx, mxd, mT, h, z, y = mxs[b], mxds[b], mTs[b], hs[b], zs[b], ys[b]
        k.dma("sp", mx[0:n, :], d["MIX"][p0:p0 + n, :], w=[("mx", b)])
        for c in range(2):
            k.dma("sp", mxd[:, c, 0:n], d["MIXD"][c * 128:(c + 1) * 128, p0:p0 + n], w=[("mxd", b, c)])
        k.dma("act", h[0:n, :], d["H"][p0:p0 + n, :], w=[("h", b)])
        tb = t % 2
        pt = k.ps(tb)[:].bitcast(BF16)
        for kk in range(6):
            k.tr(pt[:, kk * 128:kk * 128 + n], mx[0:n, kk * 128:(kk + 1) * 128], idt[0:n, 0:n], r=[("mx", b), "ident"], w=[("ps", tb)])
        k.op("dve", "tensor_copy", out=mT[:, :, 0:n], in_=pt[:, 0:768].rearrange("p (k t) -> p k t", k=6)[:, :, 0:n], r=[("ps", tb)], w=[("mT", b)])
        yield
        for half in range(2):
            pb = 2 + 2 * (t % 3) + half
            for kk in range(8):
                lhsT = mT[:, kk, 0:n] if kk < 6 else mxd[:, kk - 6, 0:n]
                k.mm(k.ps(pb)[0:n, :], lhsT, wo[:, kk, half * 512:(half + 1) * 512], kk == 0, kk == 7,
                     r=[("mT", b), ("mxd", b, 0), ("mxd", b, 1)] + wres, w=[("ps", pb)])
            k.op("dve", "scalar_tensor_tensor", out=z[0:n, half * 512:(half + 1) * 512], in0=h[0:n, half * 512:(half + 1) * 512], scalar=ALPHA, in1=k.ps(pb)[0:n, :],
                 op0=ALU.mult, op1=ALU.add, r=[("h", b), ("ps", pb)], w=[("z", b)])
        yield
        yield from layernorm_tile_g(k, z, y, n, b, gb, bb, tmp=tmps[b])
        k.dma("act", d["HM"][p0:p0 + n, :], y[0:n, :], r=[("y", b)], w=[("HM", t)])
    run_skewed([body(t) for t in range(NT)])
    S.barrier()
    A.release()


def stage_moe(k, l, last=False):
    S, A, d = k.S, k.A, k.dr
    A.mark()
    comb = A.t([128, NT, 16], F32, "comb")
    A.mark()
    idf = A.t([128, 128], F32, "identf")
    k.dma("sp", idf[:], d["ident_f"], w=["identf"])
    rw = A.t([128, 8, 16], F32, "rw")
    k.dma("sp", rw[:], d["router_w"].rearrange("(k p) e -> p k e", p=128), w=["rw"])
    rb = A.t([128, 16], F32, "rb")
    load_bcast(k, rb, d["router_b"], "rb")
    nb1 = 4
    hms = [A.t([128, 1024], F32, f"hm{i}") for i in range(nb1)]
    hT32 = [A.t([128, 8, 128], F32, f"hT32{i}") for i in range(nb1)]
    hTb = [A.t([128, 8, 128], BF16, f"hTb{i}") for i in range(nb1)]
    rt = [dict((nm, A.t([128, 16], F32, f"{nm}{i}")) for nm in ("sc", "bi", "eq", "b2", "mk", "s1", "mk2", "s2")) for i in range(nb1)]
    rs4 = [dict((nm, A.t([128, 4], F32, f"{nm}{i}")) for nm in ("m1", "m2", "gs", "ing")) for i in range(nb1)]
    rs1 = [dict((nm, A.t([128, 1], F32, f"{nm}{i}")) for nm in ("gm", "t1", "t2", "den")) for i in range(nb1)]

    def body1(t):
        p0, n = tile_rows(t)
        b = t % nb1
        hm = hms[b]
        k.dma("sp", hm[0:n, :], d["HM"][p0:p0 + n, :], r=[("HM", t)], w=[("hm", b)])
        b3 = t % 3
        for kk in range(8):
            pb = 2 * b3 + kk // 4
            k.tr(k.ps(pb)[:, (kk % 4) * 128:(kk % 4) * 128 + n], hm[0:n, kk * 128:(kk + 1) * 128], idf[0:n, 0:n], r=[("hm", b), "identf"], w=[("ps", pb)])
        for hf in range(2):
            pb = 2 * b3 + hf
            src = k.ps(pb)[:].rearrange("p (k t) -> p k t", k=4)[:, :, 0:n]
            k.op("act", "copy", out=hT32[b][:, 4 * hf:4 * hf + 4, 0:n], in_=src, r=[("ps", pb)], w=[("hT32", b, hf)])
            k.op("dve", "tensor_copy", out=hTb[b][:, 4 * hf:4 * hf + 4, 0:n], in_=src, r=[("ps", pb)], w=[("hTb", b, hf)])
        k.dma("act", d["HT"][:, p0:p0 + n].rearrange("(k p) t -> p k t", p=128), hTb[b][:, :, 0:n], r=[("hTb", b, 0), ("hTb", b, 1)], w=[("HT", t)])
        yield
        pr = k.ps(6 + t % 2)
        for kk in range(8):
            k.mm(pr[0:n, 0:16], hT32[b][:, kk, 0:n], rw[:, kk, :], kk == 0, kk == 7, r=[("hT32", b, 0), ("hT32", b, 1), "rw"], w=[("psr", t % 2)])
        R_, R4, R1 = rt[b], rs4[b], rs1[b]
        rk = lambda nm: ("rt", nm, b)
        v4 = lambda ap: ap[0:n, :].rearrange("p (g e) -> p g e", g=4)
        k.op("act", "activation", out=R_["sc"][0:n, :], in_=pr[0:n, 0:16], func=AF.Sigmoid, r=[("psr", t % 2)], w=[rk("sc")])
        yield
        k.op("dve", "tensor_tensor", out=R_["bi"][0:n, :], in0=R_["sc"][0:n, :], in1=rb[0:n, :], op=ALU.add, r=[rk("sc"), "rb"], w=[rk("bi")])
        k.op("dve", "tensor_reduce", out=R4["m1"][0:n, :], in_=v4(R_["bi"]), axis=AX.X, op=ALU.max, r=[rk("bi")], w=[rk("m1")])
        k.op("dve", "tensor_tensor", out=v4(R_["eq"]), in0=v4(R_["bi"]), in1=R4["m1"][0:n, :].unsqueeze(2).to_broadcast([n, 4, 4]), op=ALU.is_equal, r=[rk("bi"), rk("m1")], w=[rk("eq")])
        k.op("dve", "scalar_tensor_tensor", out=R_["b2"][0:n, :], in0=R_["eq"][0:n, :], scalar=NEG, in1=R_["bi"][0:n, :], op0=ALU.mult, op1=ALU.add, r=[rk("eq"), rk("bi")], w=[rk("b2")])
        k.op("dve", "tensor_reduce", out=R4["m2"][0:n, :], in_=v4(R_["b2"]), axis=AX.X, op=ALU.max, r=[rk("b2")], w=[rk("m2")])
        k.op("dve", "tensor_tensor", out=R4["gs"][0:n, :], in0=R4["m1"][0:n, :], in1=R4["m2"][0:n, :], op=ALU.add, r=[rk("m1"), rk("m2")], w=[rk("gs")])
        k.op("dve", "tensor_reduce", out=R1["gm"][0:n, :], in_=R4["gs"][0:n, :], axis=AX.X, op=ALU.max, r=[rk("gs")], w=[rk("gm")])
        k.op("dve", "tensor_scalar", out=R4["ing"][0:n, :], in0=R4["gs"][0:n, :], scalar1=R1["gm"][0:n, 0:1], scalar2=None, op0=ALU.is_equal, r=[rk("gs"), rk("gm")], w=[rk("ing")])
        k.op("dve", "tensor_scalar", out=R4["ing"][0:n, :], in0=R4["ing"][0:n, :], scalar1=1.0, scalar2=-NEG, op0=ALU.subtract, op1=ALU.mult, r=[rk("ing")], w=[rk("ing")])
        k.op("dve", "tensor_tensor", out=v4(R_["mk"]), in0=v4(R_["bi"]), in1=R4["ing"][0:n, :].unsqueeze(2).to_broadcast([n, 4, 4]), op=ALU.add, r=[rk("bi"), rk("ing")], w=[rk("mk")])
        k.op("dve", "tensor_reduce", out=R1["t1"][0:n, :], in_=R_["mk"][0:n, :], axis=AX.X, op=ALU.max, r=[rk("mk")], w=[rk("t1")])
        k.op("dve", "tensor_scalar", out=R_["s1"][0:n, :], in0=R_["mk"][0:n, :], scalar1=R1["t1"][0:n, 0:1], scalar2=None, op0=ALU.is_equal, r=[rk("mk"), rk("t1")], w=[rk("s1")])
        k.op("dve", "scalar_tensor_tensor", out=R_["mk2"][0:n, :], in0=R_["s1"][0:n, :], scalar=NEG, in1=R_["mk"][0:n, :], op0=ALU.mult, op1=ALU.add, r=[rk("s1"), rk("mk")], w=[rk("mk2")])
        k.op("dve", "tensor_reduce", out=R1["t2"][0:n, :], in_=R_["mk2"][0:n, :], axis=AX.X, op=ALU.max, r=[rk("mk2")], w=[rk("t2")])
        k.op("dve", "tensor_scalar", out=R_["s2"][0:n, :], in0=R_["mk2"][0:n, :], scalar1=R1["t2"][0:n, 0:1], scalar2=None, op0=ALU.is_equal, r=[rk("mk2"), rk("t2")], w=[rk("s2")])
        k.op("dve", "tensor_tensor", out=R_["s1"][0:n, :], in0=R_["s1"][0:n, :], in1=R_["s2"][0:n, :], op=ALU.add, r=[rk("s1"), rk("s2")], w=[rk("s1")])
        k.op("dve", "tensor_tensor", out=R_["s1"][0:n, :], in0=R_["s1"][0:n, :], in1=R_["sc"][0:n, :], op=ALU.mult, r=[rk("s1"), rk("sc")], w=[rk("s1")])
        k.op("dve", "tensor_reduce", out=R1["den"][0:n, :], in_=R_["s1"][0:n, :], axis=AX.X, op=ALU.add, r=[rk("s1")], w=[rk("den")])
        k.op("dve", "reciprocal", out=R1["den"][0:n, :], in_=R1["den"][0:n, :], r=[rk("den")], w=[rk("den")])
        k.op("dve", "tensor_scalar", out=comb[0:n, t, :], in0=R_["s1"][0:n, :], scalar1=R1["den"][0:n, 0:1], scalar2=None, op0=ALU.mult, r=[rk("s1"), rk("den")], w=[("comb", t)])
    run_skewed([body1(t) for t in range(NT)], serial=True)
    S.barrier()
    A.release()
    gb = A.t([128, 1024], F32, "gb"); bb = A.t([128, 1024], F32, "bb")
    load_bcast(k, gb, d["ln2_g"][l:l + 1, :], "gb"); load_bcast(k, bb, d["ln2_b"][l:l + 1, :], "bb")
    hTh = A.t([128, 8, 2064], BF16, "hTh")
    acc = A.t([128, 17, 1024], F32, "acc")
    w1s = [A.t([128, 8, 512], BF16, f"w1s{i}") for i in range(2)]
    w3s = [A.t([128, 8, 512], BF16, f"w3s{i}") for i in range(2)]
    w2s = [A.t([128, 4, 1024], BF16, f"w2s{i}") for i in range(2)]
    actT = [A.t([128, 4, 512], BF16, f"actT{i}") for i in range(2)]
    s1b = [A.t([128, 512], F32, f"s1b{i}") for i in range(2)]
    nb2 = 2
    z2s = [A.t([128, 1024], F32, f"z2{i}") for i in range(nb2)]; y2s = [A.t([128, 1024], F32, f"y2{i}") for i in range(nb2)]
    tmp2s = [ln_tmp(k, f"m{i}") for i in range(nb2)]
    ecount = 0
    fcount = 0
    ocount = 0
    gcount = 0
    pending = []
    for half, tiles in enumerate([list(range(0, 17)), list(range(17, 33))]):
        hp0 = tile_rows(tiles[0])[0]
        hn = sum(tile_rows(t)[1] for t in tiles)
        for kk in range(8):
            k.dma("sp", hTh[:, kk, 0:hn], d["HT"][kk * 128:(kk + 1) * 128, hp0:hp0 + hn], r=[("HT", t) for t in tiles], w=[("hTh", kk)])
        hres = [("hTh", kk) for kk in range(8)]
        groups = []
        tl = list(tiles)
        if tl[0] == 0:
            groups.append([0]); tl = tl[1:]
        for i in range(0, len(tl), 4):
            groups.append(tl[i:i + 4])
        for e in range(16):
            eb = ecount % 2
            ecount += 1
            k.dma("pool", w1s[eb][:], d["exp_w1"][l, e].rearrange("(k p) f -> p k f", p=128), w=[("w1s", eb)])
            k.dma("pool", w3s[eb][:], d["exp_w3"][l, e].rearrange("(k p) f -> p k f", p=128), w=[("w3s", eb)])
            k.dma("pool", w2s[eb][:], d["exp_w2"][l, e].rearrange("(k p) f -> p k f", p=128), w=[("w2s", eb)])
            for grp in groups:
                c0 = tile_rows(grp[0])[0] - hp0
                ng = sum(tile_rows(t)[1] for t in grp)
                ab = gcount % 2
                gcount += 1
                for f in range(4):
                    hb_ = fcount % 2
                    fcount += 1
                    ph1, ph3 = k.ps(2 * hb_), k.ps(2 * hb_ + 1)
                    for kk in range(8):
                        k.mm(ph1[:, 0:ng], w1s[eb][:, kk, f * 128:(f + 1) * 128], hTh[:, kk, c0:c0 + ng], kk == 0, kk == 7, r=hres + [("w1s", eb)], w=[("psh1", hb_)])
                    for kk in range(8):
                        k.mm(ph3[:, 0:ng], w3s[eb][:, kk, f * 128:(f + 1) * 128], hTh[:, kk, c0:c0 + ng], kk == 0, kk == 7, r=hres + [("w3s", eb)], w=[("psh3", hb_)])
                    k.op("act", "activation", out=s1b[hb_][:, 0:ng], in_=ph1[:, 0:ng], func=AF.Silu, r=[("psh1", hb_)], w=[("s1b", hb_)])
                    k.op("dve", "tensor_tensor", out=actT[ab][:, f, 0:ng], in0=s1b[hb_][:, 0:ng], in1=ph3[:, 0:ng], op=ALU.mult, r=[("s1b", hb_), ("psh3", hb_)], w=[("actT", ab, f)])
                def phase2(grp=grp, ab=ab, eb=eb, e=e, tiles=tiles):
                    nonlocal ocount
                    for ti, t in enumerate(grp):
                        nt_ = tile_rows(t)[1]
                        tloc = t - tiles[0]
                        for h2 in range(2):
                            ob_ = 4 + ocount % 4
                            ocount += 1
                            po = k.ps(ob_)
                            for f in range(4):
                                k.mm(po[0:nt_, :], actT[ab][:, f, ti * 128:ti * 128 + nt_], w2s[eb][:, f, h2 * 512:(h2 + 1) * 512], f == 0, f == 3, r=[("actT", ab, f), ("w2s", eb)], w=[("pso", ob_)])
                            dst = acc[0:nt_, tloc, h2 * 512:(h2 + 1) * 512]
                            if e == 0:
                                k.op("dve", "tensor_scalar", out=dst, in0=po[0:nt_, :], scalar1=comb[0:nt_, t, e:e + 1], scalar2=None, op0=ALU.mult, r=[("pso", ob_), ("comb", t)], w=[("acc", tloc, h2)])
                            else:
                                k.op("dve", "scalar_tensor_tensor", out=dst, in0=po[0:nt_, :], scalar=comb[0:nt_, t, e:e + 1], in1=dst, op0=ALU.mult, op1=ALU.add, r=[("pso", ob_), ("comb", t), ("acc", tloc, h2)], w=[("acc", tloc, h2)])
                while pending:
                    pending.pop(0)()
                pending.append(phase2)
        while pending:
            pending.pop(0)()
        def body2(t, tiles=tiles):
            p0, n = tile_rows(t)
            tloc = t - tiles[0]
            b = t % nb2
            z, y = z2s[b], y2s[b]
            key = ("m", b)
            k.dma("sp", z[0:n, :], d["HM"][p0:p0 + n, :], r=[("HM", t)], w=[("z", key)])
            k.op("dve", "scalar_tensor_tensor", out=z[0:n, :], in0=z[0:n, :], scalar=ALPHA, in1=acc[0:n, tloc, :], op0=ALU.mult, op1=ALU.add, r=[("z", key), ("acc", tloc, 0), ("acc", tloc, 1)], w=[("z", key)])
            yield
            yield from layernorm_tile_g(k, z, y, n, key, gb, bb, tmp=tmp2s[b])
            if last:
                if t >= 1:
                    k.dma("act", d["out"][128 * (t - 1):128 * t, :], y[0:n, :], r=[("y", key)], w=[("outT", t)])
            else:
                k.dma("act", d["H"][p0:p0 + n, :], y[0:n, :], r=[("y", key)], w=[("H", t)])
        run_skewed([body2(t) for t in tiles])
        S.barrier()
    A.release()


def run_skewed(gens, window=None, serial=False):
    if serial:
        for g in gens:
            for _ in g:
                pass
        return
    active = []
    it = iter(gens)
    while True:
        if window is None or len(active) < window:
            g = next(it, None)
            if g is not None:
                active.append(g)
        if not active:
            break
        for g in list(active):
            try:
                next(g)
            except StopIteration:
                active.remove(g)


def stage_rwkv_a(k, l):
    S, A, d = k.S, k.A, k.dr
    A.mark()
    cnt = [0]

    def T(shape, dt, name):
        cnt[0] += 1
        return TB(A.t(shape, dt, name), (name, cnt[0]))

    P = [TB(k.ps(i), ("ps", i)) for i in range(8)]
    DV = lambda ap: Vw(ap, [])
    idt = T([128, 128], BF16, "ident"); k.do("sp", "dma_start", idt[:], in_=DV(d["ident_bf"]))
    mu_b = T([128, 1024], F32, "mu_b"); k.do("sp", "dma_start", mu_b[:], in_=DV(d["rwkv_mu"][l:l + 1, :].partition_broadcast(128)))
    wa0 = T([128, 512], F32, "wa0")
    k.do("sp", "dma_start", wa0[:, 0:256], in_=DV(d["rwkv_w0"][l:l + 1, :].partition_broadcast(128)))
    k.do("sp", "dma_start", wa0[:, 256:512], in_=DV(d["rwkv_a0"][l:l + 1, :].partition_broadcast(128)))
    kk_b = T([128, 256], F32, "kk_b"); k.do("sp", "dma_start", kk_b[:], in_=DV(d["rwkv_kk"][l:l + 1, :].partition_broadcast(128)))
    ka_b = T([128, 256], F32, "ka_b"); k.do("sp", "dma_start", ka_b[:], in_=DV(d["rwkv_ka"][l:l + 1, :].partition_broadcast(128)))
    rk_b = T([128, 256], F32, "rk_b"); k.do("sp", "dma_start", rk_b[:], in_=DV(d["rwkv_rk"][l:l + 1, :].partition_broadcast(128)))
    WA = T([128, 512], BF16, "WA")
    k.do("pool", "memset", WA[:], constant=0.0)
    k.do("pool", "dma_start", WA[0:64, 0:256], in_=DV(d["rwkv_w2"][l]))
    k.do("pool", "dma_start", WA[64:128, 256:512], in_=DV(d["rwkv_a2"][l]))
    G2 = T([128, 256], BF16, "G2"); k.do("pool", "dma_start", G2[:], in_=DV(d["rwkv_g2"][l]))
    ctri = T([128, 128], F32, "ctri"); k.do("sp", "dma_start", ctri[:], in_=DV(d["ctri"]))
    cblk = T([128, 128], F32, "cblk"); k.do("sp", "dma_start", cblk[:], in_=DV(d["cblk"]))
    cones = T([128, 2, 64], F32, "cones"); k.do("sp", "dma_start", cones[:], in_=DV(d["cones"]))
    idrep = T([64, 1, 256], F32, "idrep"); k.do("sp", "dma_start", idrep[:].re("p o f -> p (o f)"), in_=DV(d["identrep"]))
    zrow = T([1, 1024], F32, "zrow")
    k.do("pool", "memset", zrow[:], constant=0.0)
    uc0 = ("UC0",)
    k.do("sp", "dma_start", Vw(d["UC"][0:1, :], [uc0]), in_=zrow[:])
    nb = 5
    mk_ = lambda shape, dt, nm, n=None: [T(shape, dt, f"{nm}{i}") for i in range(n or nb)]
    cur_, prv_, ucs_ = mk_([128, 1024], F32, "cur", 2), mk_([128, 1024], F32, "prv", 2), mk_([128, 1024], F32, "ucs", 6)
    LI_, LT_ = mk_([128, 256], BF16, "LI"), mk_([128, 2, 128], BF16, "LT")
    wa_, logw_ = mk_([128, 512], F32, "wa"), mk_([128, 256], F32, "logw")
    W_, Wi_, Wp_, Web_ = mk_([128, 256], F32, "W"), mk_([128, 256], F32, "Wi"), mk_([128, 256], F32, "Wp"), mk_([128, 256], F32, "Web")
    DWe_ = mk_([64, 2, 256], F32, "DWe"); DWt_ = mk_([64, 2, 256], BF16, "DWt")
    kq_, sq_, kkn_, kmod_, bb_, t1_ = (mk_([128, 256], F32, nm) for nm in ("kq", "sq", "kkn", "kmod", "bb", "t1"))
    ss_, rs_ = mk_([128, 4, 1], F32, "ss"), mk_([128, 4, 1], F32, "rs")
    kh_, bh_ = mk_([128, 256], F32, "kh"), mk_([128, 256], F32, "bh")
    OT_ = mk_([128, 7, 256], BF16, "OT"); BG_ = mk_([128, 2, 256], F32, "BGt")
    NE5 = -math.exp(-0.5)
    def body(t):
        p0, n = tile_rows(t)
        b = t % nb
        C = 16 if t == 0 else 64
        ncn = 1 if t == 0 else 2
        ch0 = 0 if t == 0 else 2 * (t - 1) + 1
        cur, prv, ucs, LI, LT, wa, logw = cur_[t % 2], prv_[t % 2], ucs_[t % 6], LI_[b], LT_[b], wa_[b], logw_[b]
        W, Wi, Wp, Web, DWe, DWt = W_[b], Wi_[b], Wp_[b], Web_[b], DWe_[b], DWt_[b]
        kq, sq, kkn, kmod, bb, t1, ss, rs, kh, bh, OT, BGt = kq_[b], sq_[b], kkn_[b], kmod_[b], bb_[b], t1_[b], ss_[b], rs_[b], kh_[b], bh_[b], OT_[b], BG_[b]
        k.do("sp", "dma_start", cur[0:n, :], in_=DV(d["UC"][1 + p0:1 + p0 + n, :]))
        k.do("sp", "dma_start", prv[0:n, :], in_=Vw(d["UC"][p0:p0 + n, :], [uc0] if t == 0 else []))
        k.do("dve", "tensor_tensor", prv[0:n, :], in0=prv[0:n, :], in1=cur[0:n, :], op=ALU.subtract)
        k.do("dve", "tensor_tensor", prv[0:n, :], in0=prv[0:n, :], in1=mu_b[0:n, :], op=ALU.mult)
        k.do("dve", "tensor_tensor", ucs[0:n, :], in0=cur[0:n, :], in1=prv[0:n, :], op=ALU.add)
        r_, kraw, v_ = ucs[0:n, 0:256], ucs[0:n, 256:512], ucs[0:n, 512:768]
        yield
        k.do("act", "activation", LI[0:n, 0:64], in_=ucs[0:n, 768:832], func=AF.Tanh)
        k.do("act", "copy", LI[0:n, 64:128], in_=ucs[0:n, 832:896])
        k.do("act", "activation", LI[0:n, 128:256], in_=ucs[0:n, 896:1024], func=AF.Sigmoid)
        ptb = Vw(k.ps(0)[:].bitcast(BF16), P[0].res)
        k.trv(ptb[:, 0:n], LI[0:n, 0:128], idt[0:n, 0:n])
        k.trv(ptb[:, 128:128 + n], LI[0:n, 128:256], idt[0:n, 0:n])
        k.do("dve", "tensor_copy", LT[:, :, 0:n], in_=ptb[:, 0:256].re("p (a t) -> p a t", a=2)[:, :, 0:n])
        yield
        k.mmv(P[1][0:n, :], LT[:, 0, 0:n], WA[:], True, True)
        k.mmv(P[2][0:n, 0:256], LT[:, 1, 0:n], G2[:], True, True)
        k.do("dve", "tensor_tensor", wa[0:n, :], in0=P[1][0:n, :], in1=wa0[0:n, :], op=ALU.add)
        k.do("act", "activation", wa[0:n, :], in_=wa[0:n, :], func=AF.Sigmoid)
        a_ = wa[0:n, 256:512]
        k.do("act", "mul", logw[0:n, :], in_=wa[0:n, 0:256], mul=NE5)
        k.do("act", "copy", BGt[0:n, 1, :], in_=P[2][0:n, 0:256])
        yield
        k.mmv(P[3][0:n, 0:256], ctri[0:n, 0:n], logw[0:n, :], True, True)
        k.mmv(P[4][0:n, 0:256], cblk[0:n, 0:n], logw[0:n, :], True, True)
        for c in range(ncn):
            k.mmv(P[5][0:64, c * 256:(c + 1) * 256], cones[0:n, c, :], logw[0:n, :], True, True)
        k.do("act", "activation", W[0:n, :], in_=P[3][0:n, 0:256], func=AF.Exp)
        k.do("act", "activation", Wi[0:n, :], in_=P[3][0:n, 0:256], func=AF.Exp, scale=-1.0)
        k.do("dve", "tensor_tensor", Wp[0:n, :], in0=P[3][0:n, 0:256], in1=logw[0:n, :], op=ALU.subtract)
        k.do("act", "activation", Wp[0:n, :], in_=Wp[0:n, :], func=AF.Exp)
        k.do("act", "activation", Web[0:n, :], in_=P[4][0:n, 0:256], func=AF.Exp)
        k.do("act", "activation", DWe[:, 0:ncn, :], in_=P[5][0:64, 0:ncn * 256].re("p (c f) -> p c f", c=ncn), func=AF.Exp)
        k.do("dve", "tensor_tensor", DWt[:, 0:ncn, :], in0=DWe[:, 0:ncn, :], in1=idrep[:].bc([64, ncn, 256]), op=ALU.mult)
        k.do("act", "dma_start", DV(d["DW"][ch0:ch0 + ncn].rearrange("c p f -> p c f")), in_=DWt[:, 0:ncn, :])
        yield
        k.do("dve", "tensor_tensor", kq[0:n, :], in0=kraw, in1=kk_b[0:n, :], op=ALU.mult)
        k.do("act", "activation", sq[0:n, :], in_=kq[0:n, :], func=AF.Square)
        k.do("dve", "tensor_reduce", ss[0:n], in_=sq[0:n, :].re("p (h e) -> p h e", h=4), axis=AX.X, op=ALU.add)
        k.do("act", "activation", ss[0:n], in_=ss[0:n], func=AF.Sqrt)
        k.do("dve", "tensor_scalar", ss[0:n], in0=ss[0:n], scalar1=1e-12, scalar2=None, op0=ALU.max)
        k.do("dve", "reciprocal", ss[0:n], in_=ss[0:n])
        k.do("dve", "tensor_tensor", kkn[0:n, :].re("p (h e) -> p h e", h=4), in0=kq[0:n, :].re("p (h e) -> p h e", h=4), in1=ss[0:n].bc([n, 4, 64]), op=ALU.mult)
        k.do("dve", "scalar_tensor_tensor", t1[0:n, :], in0=a_, scalar=-1.0, in1=ka_b[0:n, :], op0=ALU.add, op1=ALU.mult)
        k.do("dve", "scalar_tensor_tensor", kmod[0:n, :], in0=t1[0:n, :], scalar=1.0, in1=kraw, op0=ALU.add, op1=ALU.mult)
        k.do("dve", "tensor_tensor", bb[0:n, :], in0=kkn[0:n, :], in1=a_, op=ALU.mult)
        k.do("pool", "tensor_tensor", t1[0:n, :], in0=r_, in1=kmod[0:n, :], op=ALU.mult)
        k.do("pool", "tensor_tensor", t1[0:n, :], in0=t1[0:n, :], in1=rk_b[0:n, :], op=ALU.mult)
        k.do("dve", "tensor_reduce", rs[0:n], in_=t1[0:n, :].re("p (h e) -> p h e", h=4), axis=AX.X, op=ALU.add)
        k.do("dve", "tensor_tensor", BGt[0:n, 0, :].re("p (h e) -> p h e", h=4), in0=v_.re("p (h e) -> p h e", h=4), in1=rs[0:n].bc([n, 4, 64]), op=ALU.mult)
        yield
        k.do("dve", "tensor_tensor", OT[0:n, 0, :], in0=kkn[0:n, :], in1=Wp[0:n, :], op=ALU.mult)
        k.do("dve", "tensor_tensor", OT[0:n, 1, :], in0=r_, in1=W[0:n, :], op=ALU.mult)
        k.do("dve", "tensor_tensor", kh[0:n, :], in0=kmod[0:n, :], in1=Wi[0:n, :], op=ALU.mult)
        k.do("pool", "tensor_tensor", bh[0:n, :], in0=bb[0:n, :], in1=Wi[0:n, :], op=ALU.mult)
        k.do("act", "copy", OT[0:n, 2, :], in_=kh[0:n, :])
        k.do("act", "copy", OT[0:n, 3, :], in_=bh[0:n, :])
        k.do("dve", "tensor_tensor", OT[0:n, 4, :], in0=kh[0:n, :], in1=Web[0:n, :], op=ALU.mult)
        k.do("dve", "scalar_tensor_tensor", OT[0:n, 5, :], in0=bh[0:n, :], scalar=-1.0, in1=Web[0:n, :], op0=ALU.mult, op1=ALU.mult)
        k.do("act", "copy", OT[0:n, 6, :], in_=v_)
        k.do("sp", "dma_start", DV(d["RWT"][p0:p0 + n]), in_=OT[0:n])
        k.do("sp", "dma_start", DV(d["BG"][p0:p0 + n]), in_=BGt[0:n])
    run_skewed([body(t) for t in range(NT)])
    S.barrier()
    A.release()


def stage_rwkv_b(k, l, tmax=NT):
    S, A, d = k.S, k.A, k.dr
    A.mark()
    cnt = [0]

    def T(shape, dt, name):
        cnt[0] += 1
        return TB(A.t(shape, dt, name), (name, cnt[0]))

    P = [TB(k.ps(i), ("ps", i)) for i in range(8)]
    DV = lambda ap: Vw(ap, [])
    idt = T([128, 128], BF16, "ident"); k.do("sp", "dma_start", idt[:], in_=DV(d["ident_bf"]))
    rmask = T([64, 5, 64], F32, "rmask"); k.do("sp", "dma_start", rmask[:], in_=DV(d["rmask"]))
    lg_b = T([64, 256], F32, "lg_b"); k.do("sp", "dma_start", lg_b[:], in_=DV(d["rwkv_lnx_g"][l:l + 1, :].partition_broadcast(64)))
    lb_b = T([64, 256], F32, "lb_b"); k.do("sp", "dma_start", lb_b[:], in_=DV(d["rwkv_lnx_b"][l:l + 1, :].partition_broadcast(64)))
    Tb = [T([64, 4, 64], BF16, f"Tst{i}") for i in range(2)]
    k.do("pool", "memset", Tb[0][:], constant=0.0)
    nb = 3
    mk_ = lambda shape, dt, nm, n=None: [T(shape, dt, f"{nm}{i}") for i in range(n or nb)]
    tokX_ = mk_([64, 2, 7, 256], BF16, "tokX"); DWc_ = mk_([64, 2, 256], BF16, "DWc"); BGc_ = mk_([64, 2, 2, 256], F32, "BGc")
    XT_ = mk_([64, 2, 4, 4, 64], BF16, "XT")
    AT_ = mk_([64, 8, 2, 64], BF16, "AT"); BT_ = mk_([64, 8, 2, 64], BF16, "BT"); Nn_ = mk_([64, 8, 64], BF16, "Nn")
    NPa_ = [mk_([64, 8, 64], BF16, f"NPa{i}_", 2) for i in range(nb)]; NPTa_ = [mk_([64, 8, 64], BF16, f"NPTa{i}_", 2) for i in range(nb)]
    Zf_ = mk_([64, 8, 128], F32, "Zf"); Zb_ = mk_([64, 8, 128], BF16, "Zb")
    Q1T_ = mk_([64, 8, 64], BF16, "Q1T"); Q2s_ = mk_([64, 8, 64], F32, "Q2s"); GT_ = mk_([64, 8, 64], BF16, "GT"); Hs_ = mk_([64, 8, 64], F32, "Hs")
    ys_ = mk_([64, 4, 64], F32, "ys", 2); dd_ = mk_([64, 4, 64], F32, "dd", 2); sq_ = mk_([64, 4, 64], F32, "sq", 2)
    st_ = mk_([64, 4, 1], F32, "st", 2); yo_ = mk_([64, 256], BF16, "yo", 2)
    state = {"tcur": 0, "ccount": 0}

    def body(t):
        p0, n = tile_rows(t)
        b = t % nb
        Lb = [P[4 * (t % 2) + i] for i in range(4)]
        NPa, NPTa = NPa_[b], NPTa_[b]
        C = 16 if t == 0 else 64
        ncn = 1 if t == 0 else 2
        nq = 4 * ncn
        ch0 = 0 if t == 0 else 2 * (t - 1) + 1
        tokX, DWc, BGc, XT, AT, BT, Nn, Zf, Zb = tokX_[b], DWc_[b], BGc_[b], XT_[b], AT_[b], BT_[b], Nn_[b], Zf_[b], Zb_[b]
        Q1T, Q2s, GT, Hs = Q1T_[b], Q2s_[b], GT_[b], Hs_[b]
        for c in range(ncn):
            r0 = p0 + 64 * c
            k.do("sp", "dma_start", tokX[0:C, c], in_=DV(d["RWT"][r0:r0 + C]))
            k.do("act", "dma_start", BGc[0:C, c], in_=DV(d["BG"][r0:r0 + C]))
        k.do("act", "dma_start", DWc[:, 0:ncn, :], in_=DV(d["DW"][ch0:ch0 + ncn].rearrange("c p f -> p c f")))
        for c in range(ncn):
            ptb = Vw(Lb[2 + c].h[:].bitcast(BF16), Lb[2 + c].res)
            pv = ptb[0:64, :].re("p (h x t) -> p h x t", h=4, x=4)
            for h in range(4):
                for X in range(4):
                    k.trv(pv[:, h, X, 0:C], tokX[0:C, c, X, h * 64:(h + 1) * 64], idt[0:C, 0:C])
            if c == 0:
                k.do("dve", "tensor_copy", XT[:, c, :, :, 0:C], in_=pv[:, :, :, 0:C])
            else:
                k.do("act", "copy", XT[:, c, :, :, 0:C], in_=pv[:, :, :, 0:C])
        yield
        for c in range(ncn):
            for h in range(4):
                q = c * 4 + h
                o1 = Lb[q // 4][0:C, :].re("p (q a t) -> p q a t", q=4, a=2)[:, q % 4, :, 0:C]
                k.mmv(o1, XT[:, c, h, 2, 0:C], XT[:, c, h, 0:2, 0:C], True, True)
                o2 = Lb[2 + q // 4][0:C, :].re("p (q a t) -> p q a t", q=4, a=2)[:, q % 4, :, 0:C]
                k.mmv(o2, XT[:, c, h, 3, 0:C], XT[:, c, h, 0:2, 0:C], True, True)
        for c in range(ncn):
            pv1 = Lb[c][0:C, :].re("p (q a t) -> p q a t", q=4, a=2)[:, :, :, 0:C]
            k.do("dve", "tensor_tensor", AT[0:C, 4 * c:4 * c + 4, :, 0:C], in0=pv1, in1=rmask[0:C, 0:2, 0:C].un(1).bc([C, 4, 2, C]), op=ALU.mult)
            pv2 = Lb[2 + c][0:C, :].re("p (q a t) -> p q a t", q=4, a=2)[:, :, :, 0:C]
            k.do("dve", "tensor_tensor", BT[0:C, 4 * c:4 * c + 4, :, 0:C], in0=pv2, in1=rmask[0:C, 2:4, 0:C].un(1).bc([C, 4, 2, C]), op=ALU.mult)
        for c in range(ncn):
            for h in range(4):
                q = c * 4 + h
                o3 = Lb[0][0:C, :].re("p (q t) -> p q t", q=8)[:, q, 0:C]
                k.mmv(o3, XT[:, c, h, 0, 0:C], XT[:, c, h, 3, 0:C], True, True)
        pv3 = Lb[0][0:C, :].re("p (q t) -> p q t", q=8)[:, 0:nq, 0:C]
        k.do("dve", "tensor_tensor", Nn[0:C, 0:nq, 0:C], in0=pv3, in1=rmask[0:C, 4:5, 0:C].bc([C, nq, C]), op=ALU.mult)
        yield
        pav = Lb[1][0:C, :].re("p (q i) -> p q i", q=8)
        for c in range(ncn):
            for h in range(4):
                q = c * 4 + h
                k.mmv(pav[:, q, :], AT[0:C, q, 0, 0:C], tokX[0:C, c, 6, h * 64:(h + 1) * 64], True, True)
        k.do("act", "copy", Zf[0:C, 0:nq, 0:64].re("p (c h) e -> p c h e", c=ncn), in_=tokX[0:C, 0:ncn, 0, :].re("p c (h e) -> p c h e", h=4))
        k.do("act", "copy", Zf[0:C, 0:nq, 64:128], in_=pav[:, 0:nq, :])
        k.do("dve", "tensor_copy", Zb[0:C, 0:nq, :], in_=Zf[0:C, 0:nq, :])
        yield
        L = 3 if t == 0 else 5
        NPc, NPTc = Nn, None
        for lev in range(L + 1):
            def npt(q):
                return BT[0:C, q, 0, 0:C] if lev == 0 else NPTc[0:C, q, 0:C]
            for q in range(nq):
                oz = Lb[q // 4][0:C, :].re("p (q e) -> p q e", q=4)[:, q % 4, :]
                k.mmv(oz, npt(q), Zb[0:C, q, :], True, True)
            if lev < L:
                nxtP, nxtPT = NPa[lev % 2], NPTa[lev % 2]
                pa = Lb[2][0:C, :].re("p (q i) -> p q i", q=8)
                pbk = Lb[3][0:C, :].re("p (q i) -> p q i", q=8)
                for q in range(nq):
                    if lev + 1 < L:
                        k.mmv(pa[:, q, 0:C], npt(q), NPc[0:C, q, 0:C], True, True)
                    k.mmv(pbk[:, q, 0:C], NPc[0:C, q, 0:C], npt(q), True, True)
            for c in range(ncn):
                zv = Lb[c][0:C, :].re("p (q e) -> p q e", q=4)
                k.do("dve", "tensor_tensor", Zf[0:C, 4 * c:4 * c + 4, :], in0=Zf[0:C, 4 * c:4 * c + 4, :], in1=zv, op=(ALU.subtract if lev == 0 else ALU.add))
            k.do("act", "copy", Zb[0:C, 0:nq, :], in_=Zf[0:C, 0:nq, :])
            if lev < L:
                if lev + 1 < L:
                    k.do("act", "copy", nxtP[0:C, 0:nq, 0:C], in_=pa[:, 0:nq, 0:C])
                k.do("dve", "tensor_copy", nxtPT[0:C, 0:nq, 0:C], in_=pbk[:, 0:nq, 0:C])
                NPc, NPTc = nxtP, nxtPT
            yield
        pq1 = Lb[0][0:64, :].re("p (q t) -> p q t", q=8)
        pq2 = Lb[1][0:C, :].re("p (q i) -> p q i", q=8)
        pg = Lb[2][0:64, :].re("p (q j) -> p q j", q=8)
        ph = Lb[3][0:64, :].re("p (q i) -> p q i", q=8)
        for c in range(ncn):
            for h in range(4):
                q = c * 4 + h
                hs = slice(h * 64, (h + 1) * 64)
                P1b, P2b = Zb[0:C, q, 0:64], Zb[0:C, q, 64:128]
                V_ = tokX[0:C, c, 6, hs]
                k.mmv(pq1[:, q, 0:C], P1b, BT[0:C, q, 1, 0:C], True, False)
                k.mmv(pq1[:, q, 0:C], idt[0:64, 0:64], XT[:, c, h, 1, 0:C], False, True)
                k.mmv(pq2[:, q, :], AT[0:C, q, 1, 0:C], V_, True, False)
                k.mmv(pq2[:, q, :], BT[0:C, q, 1, 0:C], P2b, False, True)
                k.mmv(pg[:, q, :], idt[0:64, 0:64], DWc[:, c, hs], True, False)
                k.mmv(pg[:, q, :], P1b, tokX[0:C, c, 5, hs], False, True)
                k.mmv(ph[:, q, :], tokX[0:C, c, 4, hs], V_, True, False)
                k.mmv(ph[:, q, :], tokX[0:C, c, 5, hs], P2b, False, True)
        k.do("act", "copy", Q1T[:, 0:nq, 0:C], in_=pq1[:, 0:nq, 0:C])
        k.do("dve", "tensor_copy", Q2s[0:C, 0:nq, :], in_=pq2[:, 0:nq, :])
        k.do("act", "copy", GT[:, 0:nq, :], in_=pg[:, 0:nq, :])
        k.do("dve", "tensor_copy", Hs[:, 0:nq, :], in_=ph[:, 0:nq, :])
        yield
        for c in range(ncn):
            cb = state["ccount"] % 2
            state["ccount"] += 1
            tcur = state["tcur"]
            Tc, Tn = Tb[tcur], Tb[1 - tcur]
            state["tcur"] = 1 - tcur
            py = Lb[2 + c][0:C, 0:256].re("p (h i) -> p h i", h=4)
            pt_ = Lb[2 + c][0:64, 256:512].re("p (h i) -> p h i", h=4)
            for h in range(4):
                q = c * 4 + h
                k.mmv(pt_[:, h, :], GT[:, q, :], Tc[:, h, :], True, True)
            for h in range(4):
                q = c * 4 + h
                k.mmv(py[:, h, :], Q1T[:, q, 0:C], Tc[:, h, :], True, True)
            k.do("dve", "tensor_tensor", Tn[:], in0=pt_, in1=Hs[:, 4 * c:4 * c + 4, :], op=ALU.add)
            ys, dd, sq, st, yo = ys_[cb], dd_[cb], sq_[cb], st_[cb], yo_[cb]
            k.do("dve", "tensor_tensor", ys[0:C], in0=py, in1=Q2s[0:C, 4 * c:4 * c + 4, :], op=ALU.add)
            k.do("dve", "tensor_reduce", st[0:C], in_=ys[0:C], axis=AX.X, op=ALU.add)
            k.do("dve", "tensor_scalar", st[0:C], in0=st[0:C], scalar1=-1.0 / 64, scalar2=None, op0=ALU.mult)
            k.do("dve", "tensor_tensor", dd[0:C], in0=ys[0:C], in1=st[0:C].bc([C, 4, 64]), op=ALU.add)
            k.do("act", "activation", sq[0:C], in_=dd[0:C], func=AF.Square)
            k.do("dve", "tensor_reduce", st[0:C], in_=sq[0:C], axis=AX.X, op=ALU.add)
            k.do("act", "activation", st[0:C], in_=st[0:C], func=AF.Sqrt, bias=64e-5, scale=1.0 / 64)
            k.do("dve", "reciprocal", st[0:C], in_=st[0:C])
            k.do("dve", "tensor_tensor", dd[0:C], in0=dd[0:C], in1=st[0:C].bc([C, 4, 64]), op=ALU.mult)
            ddf = dd[0:C].re("p h e -> p (h e)")
            k.do("dve", "tensor_tensor", ddf, in0=ddf, in1=lg_b[0:C, :], op=ALU.mult)
            k.do("dve", "tensor_tensor", ddf, in0=ddf, in1=lb_b[0:C, :], op=ALU.add)
            k.do("pool", "tensor_tensor", ddf, in0=ddf, in1=BGc[0:C, c, 0, :], op=ALU.add)
            k.do("pool", "tensor_tensor", yo[0:C, :], in0=ddf, in1=BGc[0:C, c, 1, :], op=ALU.mult)
            r0 = p0 + 64 * c
            k.do("sp", "dma_start", DV(d["MIX"][r0:r0 + C, 512:768]), in_=yo[0:C, :])
            if c < ncn - 1:
                yield
    run_skewed([body(t) for t in range(tmax)], window=2)
    S.barrier()
    A.release()


def build_full():
    k = K()
    declare_io(k, final_out=True)
    stage_ln0(k)
    for l in range(DEPTH):
        stage_in(k, l)
        stage_swa(k, l)
        stage_diff(k, l)
        stage_rwkv_a(k, l)
        stage_rwkv_b(k, l)
        stage_conv(k, l)
        stage_out(k, l)
        stage_moe(k, l, last=(l == DEPTH - 1))
        k.S.barrier(rotate_dma=True)
    k.S.final_wait("sp")
    k.S.emit()
    k.st.close()
    return k


def kernel(**inputs):
    inp = {kk_: np.asarray(v) for kk_, v in inputs.items()}
    k = build_full()
    consts = host_consts(inp)
    in_maps = []
    for b in range(8):
        m = host_inputs(inp, b)
        m.update(consts)
        in_maps.append(m)
    res = run_bass_kernel_spmd(k.nc, in_maps, core_ids=list(range(8)))
    out = np.stack([np.asarray(res.results[b]["out"], np.float32) for b in range(8)], axis=0)
    return out
```
